# Optimizing a Trainium2 kernel written in Bass

```python
import jax, jax.numpy as jnp
from jax import lax
import numpy as np

D_MODEL = 1024
BATCH = 8
SEQ = 2048
DEPTH = 1

A_HEADS = 8
A_KV_HEADS = 2
A_HEAD_DIM = 64
IDX_HEADS = 8
IDX_DIM = 64
TOPK_MAX = 256
B_HEADS = 8
B_HEAD_DIM = 64
A_WIDTH = A_HEADS * A_HEAD_DIM
B_WIDTH = B_HEADS * B_HEAD_DIM
MIX_WIDTH = A_WIDTH + B_WIDTH
A_KV_WIDTH = A_KV_HEADS * A_HEAD_DIM
IDX_Q_WIDTH = IDX_HEADS * IDX_DIM
IN_SPLITS = (A_WIDTH, A_KV_WIDTH, A_KV_WIDTH, IDX_Q_WIDTH, IDX_DIM, IDX_HEADS,
             B_WIDTH, B_WIDTH, B_WIDTH, B_HEADS)
IN_COLS = sum(IN_SPLITS)
Q_BLOCK = 128
N_GROUPS = 4
EXPERTS_PER_GROUP = 8
N_EXPERTS = N_GROUPS * EXPERTS_PER_GROUP
TOPK_EXPERT = 2
D_FF_EXPERT = D_MODEL // 4
N_MOD = 6
EPS = 1e-6

kernel_name = 'hymba_dsa_fox_hmoe_adaln_block'


def rms_norm(x, g):
    xf = x.astype(jnp.float32)
    y = xf * lax.rsqrt(jnp.mean(xf * xf, axis=-1, keepdims=True) + EPS)
    return (y * g.astype(jnp.float32)).astype(x.dtype)


def alibi_slopes(n):
    return jnp.exp2(-8.0 * jnp.arange(1, n + 1, dtype=jnp.float32) / n)


def to_blocks(a, nb):
    return jnp.moveaxis(a.reshape(a.shape[0], nb, Q_BLOCK, *a.shape[2:]), 1, 0)


def dsa_attention(q, k, v, iq, ik, iw):
    bsz, L = q.shape[0], q.shape[1]
    n_top = min(TOPK_MAX, L // 4)
    nb = L // Q_BLOCK
    rep = A_HEADS // A_KV_HEADS
    slopes = alibi_slopes(A_HEADS).reshape(A_KV_HEADS, rep)
    scale = A_HEAD_DIM ** -0.5
    kpos = jnp.arange(L)
    ikf = ik.astype(jnp.float32)

    def block(args):
        qb, iqb, iwb, t0 = args
        tpos = t0 + jnp.arange(Q_BLOCK)
        dots = jnp.einsum('bqhd,bsd->bqhs', iqb.astype(jnp.float32), ikf)
        score = jnp.einsum('bqh,bqhs->bqs', iwb.astype(jnp.float32), jax.nn.relu(dots))
        causal = kpos[None, :] <= tpos[:, None]
        score = jnp.where(causal[None], score, -jnp.inf)
        _, idx = lax.top_k(score, n_top)
        valid = idx <= tpos[None, :, None]
        ksel = jax.vmap(lambda kb, ib: kb[ib])(k, idx)
        vsel = jax.vmap(lambda vb, ib: vb[ib])(v, idx)
        qg = qb.reshape(bsz, Q_BLOCK, A_KV_HEADS, rep, A_HEAD_DIM)
        logits = jnp.einsum('bqgrd,bqkgd->bqgrk', qg, ksel).astype(jnp.float32) * scale
        dist = (tpos[None, :, None] - idx).astype(jnp.float32)
        logits = logits - slopes[None, None, :, :, None] * dist[:, :, None, None, :]
        logits = jnp.where(valid[:, :, None, None, :], logits, -jnp.inf)
        p = jax.nn.softmax(logits, axis=-1).astype(v.dtype)
        o = jnp.einsum('bqgrk,bqkgd->bqgrd', p, vsel)
        return o.reshape(bsz, Q_BLOCK, A_WIDTH)

    out = lax.map(block, (to_blocks(q, nb), to_blocks(iq, nb), to_blocks(iw, nb),
                          jnp.arange(nb, dtype=jnp.int32) * Q_BLOCK))
    return jnp.moveaxis(out, 0, 1).reshape(bsz, L, A_WIDTH)


def forgetting_attention(q, k, v, log_f):
    bsz, L = q.shape[0], q.shape[1]
    nb = L // Q_BLOCK
    scale = B_HEAD_DIM ** -0.5
    cum = jnp.cumsum(log_f, axis=1)
    cum_k = jnp.transpose(cum, (0, 2, 1))
    kpos = jnp.arange(L)

    def block(args):
        qb, cb, t0 = args
        tpos = t0 + jnp.arange(Q_BLOCK)
        logits = jnp.einsum('bqhd,bshd->bhqs', qb, k).astype(jnp.float32) * scale
        logits = logits + jnp.transpose(cb, (0, 2, 1))[..., None] - cum_k[:, :, None, :]
        causal = kpos[None, :] <= tpos[:, None]
        logits = jnp.where(causal[None, None], logits, -jnp.inf)
        p = jax.nn.softmax(logits, axis=-1).astype(v.dtype)
        o = jnp.einsum('bhqs,bshd->bqhd', p, v)
        return o.reshape(bsz, Q_BLOCK, B_WIDTH)

    out = lax.map(block, (to_blocks(q, nb), to_blocks(cum, nb),
                          jnp.arange(nb, dtype=jnp.int32) * Q_BLOCK))
    return jnp.moveaxis(out, 0, 1).reshape(bsz, L, B_WIDTH)


def hybrid_mixer(h, w_in, b_forget, g_out_a, g_out_b, w_out):
    bsz, L, _ = h.shape
    proj = h @ w_in
    (aq, ak, av, iq, ik, iw, bq, bk, bv, bf) = jnp.split(
        proj, list(np.cumsum(IN_SPLITS)[:-1]), axis=-1)
    oa = dsa_attention(aq.reshape(bsz, L, A_HEADS, A_HEAD_DIM),
                       ak.reshape(bsz, L, A_KV_HEADS, A_HEAD_DIM),
                       av.reshape(bsz, L, A_KV_HEADS, A_HEAD_DIM),
                       iq.reshape(bsz, L, IDX_HEADS, IDX_DIM), ik, iw)
    log_f = jax.nn.log_sigmoid(bf.astype(jnp.float32) + b_forget.astype(jnp.float32))
    ob = forgetting_attention(bq.reshape(bsz, L, B_HEADS, B_HEAD_DIM),
                              bk.reshape(bsz, L, B_HEADS, B_HEAD_DIM),
                              bv.reshape(bsz, L, B_HEADS, B_HEAD_DIM), log_f)
    o = jnp.concatenate([rms_norm(oa, g_out_a), rms_norm(ob, g_out_b)], axis=-1)
    return o @ w_out


def hierarchical_moe(h, w_group, b_group, w_router, b_router, w_gate, w_up, w_down):
    bsz, L, d = h.shape
    t = h.reshape(-1, d)
    g_logits = (t @ w_group).astype(jnp.float32) + b_group.astype(jnp.float32)
    g_prob = jax.nn.softmax(g_logits, axis=-1)
    g_sel = jnp.argmax(g_logits, axis=-1)
    p_group = jnp.take_along_axis(g_prob, g_sel[:, None], axis=1)[:, 0]
    e_all = jnp.einsum('nd,gde->nge', t, w_router).astype(jnp.float32) + b_router.astype(jnp.float32)
    e_logits = jnp.take_along_axis(e_all, g_sel[:, None, None], axis=1)[:, 0]
    top_v, top_i = lax.top_k(e_logits, TOPK_EXPERT)
    w_top = jax.nn.softmax(top_v, axis=-1) * p_group[:, None]
    eid = g_sel[:, None] * EXPERTS_PER_GROUP + top_i
    comb = jnp.sum(jax.nn.one_hot(eid, N_EXPERTS, dtype=jnp.float32) * w_top[..., None], axis=1)
    hg = jnp.einsum('nd,edf->nef', t, w_gate)
    hu = jnp.einsum('nd,edf->nef', t, w_up)
    a = jax.nn.silu(hg) * hu * comb[:, :, None].astype(h.dtype)
    y = jnp.einsum('nef,efd->nd', a, w_down)
    return y.reshape(bsz, L, d)


def setup_inputs(seed: int = 0) -> dict:
    key = jax.random.key(seed)
    ks = jax.random.split(key, 20)
    f32 = jnp.float32
    nrm = lambda k, shape, s: jax.random.normal(k, shape, f32) * s
    return {
        'x': nrm(ks[0], (BATCH, SEQ, D_MODEL), 1.0),
        'c': nrm(ks[1], (BATCH, D_MODEL), 1.0),
        'w_ada': nrm(ks[2], (DEPTH, D_MODEL, N_MOD * D_MODEL), 0.5 * D_MODEL ** -0.5),
        'b_ada': nrm(ks[3], (DEPTH, N_MOD * D_MODEL), 0.01),
        'g_mix': 1.0 + nrm(ks[4], (DEPTH, D_MODEL), 0.01),
        'w_in': nrm(ks[5], (DEPTH, D_MODEL, IN_COLS), D_MODEL ** -0.5),
        'b_forget': jax.random.uniform(ks[6], (DEPTH, B_HEADS), f32, 1.0, 4.0),
        'g_out_a': 1.0 + nrm(ks[7], (DEPTH, A_WIDTH), 0.01),
        'g_out_b': 1.0 + nrm(ks[8], (DEPTH, B_WIDTH), 0.01),
        'w_out': nrm(ks[9], (DEPTH, MIX_WIDTH, D_MODEL), MIX_WIDTH ** -0.5),
        'g_ffn': 1.0 + nrm(ks[10], (DEPTH, D_MODEL), 0.01),
        'w_group': nrm(ks[11], (DEPTH, D_MODEL, N_GROUPS), D_MODEL ** -0.5),
        'b_group': nrm(ks[12], (DEPTH, N_GROUPS), 0.01),
        'w_router': nrm(ks[13], (DEPTH, N_GROUPS, D_MODEL, EXPERTS_PER_GROUP), D_MODEL ** -0.5),
        'b_router': nrm(ks[14], (DEPTH, N_GROUPS, EXPERTS_PER_GROUP), 0.01),
        'w_gate': nrm(ks[15], (DEPTH, N_EXPERTS, D_MODEL, D_FF_EXPERT), D_MODEL ** -0.5),
        'w_up': nrm(ks[16], (DEPTH, N_EXPERTS, D_MODEL, D_FF_EXPERT), D_MODEL ** -0.5),
        'w_down': nrm(ks[17], (DEPTH, N_EXPERTS, D_FF_EXPERT, D_MODEL), D_FF_EXPERT ** -0.5),
        'g_final': 1.0 + nrm(ks[18], (D_MODEL,), 0.01),
    }


def reference(x, c, w_ada, b_ada, g_mix, w_in, b_forget, g_out_a, g_out_b, w_out,
              g_ffn, w_group, b_group, w_router, b_router, w_gate, w_up, w_down, g_final):
    for l in range(DEPTH):
        mod = (jax.nn.silu(c) @ w_ada[l] + b_ada[l])[:, None, :]
        shift_m, scale_m, gate_m, shift_f, scale_f, gate_f = jnp.split(mod, N_MOD, axis=-1)
        h = rms_norm(x, g_mix[l]) * (1.0 + scale_m) + shift_m
        x = x + gate_m * hybrid_mixer(h, w_in[l], b_forget[l], g_out_a[l], g_out_b[l], w_out[l])
        h = rms_norm(x, g_ffn[l]) * (1.0 + scale_f) + shift_f
        x = x + gate_f * hierarchical_moe(h, w_group[l], b_group[l], w_router[l], b_router[l],
                                          w_gate[l], w_up[l], w_down[l])
    return rms_norm(x, g_final)
```

```python
from contextlib import ExitStack
import numpy as np
import concourse.bass as bass
import concourse.mybir as mybir
from concourse.bass_utils import run_bass_kernel_spmd

F32 = mybir.dt.float32
BF16 = mybir.dt.bfloat16
AF = mybir.ActivationFunctionType
ALU = mybir.AluOpType
AX = mybir.AxisListType

D = 1024
L = 2048
NT = 16
NE = 32
DFF = 256
EPS = 1e-6
NEG = -30000.0
NBIS = 16


class Buf:
    __slots__ = ("name", "lw", "rd")

    def __init__(self, name=""):
        self.name = name
        self.lw = None
        self.rd = []


class Sched:
    ENGS = ("pe", "act", "dve", "pool", "sp")

    def __init__(self, nc, n_dma_sems=32):
        self.nc = nc
        self.prog = {e: [] for e in self.ENGS}
        self.cnt = {e: 0 for e in self.ENGS}
        self.seen = {e: {} for e in self.ENGS}
        self.pend_rd = {e: [] for e in self.ENGS}
        self.pend_wr = {e: [] for e in self.ENGS}
        self.sems = {}
        self.n_dma = n_dma_sems
        self.dma_val = [0] * n_dma_sems
        self.dma_rr = 0
        self.dma_rr2 = [0, 0]
        self._ctx = []

    def open(self):
        nc = self.nc
        for e in self.ENGS:
            cm = nc.semaphore("s_" + e)
            self.sems[e] = cm.__enter__()
            self._ctx.append(cm)
        for i in range(self.n_dma):
            cm = nc.semaphore("s_dma%d" % i)
            self.sems[("dma", i)] = cm.__enter__()
            self._ctx.append(cm)

    def close(self):
        for cm in reversed(self._ctx):
            cm.__exit__(None, None, None)

    def _wait(self, eng, tok):
        if tok is None:
            return
        key, val = tok
        if self.seen[eng].get(key, 0) >= val:
            return
        self.seen[eng][key] = val
        sem = self.sems[key]
        self.prog[eng].append(lambda h, sem=sem, val=val: h.wait_ge(sem, val))

    def _deps(self, eng, reads, writes, pe_acc=False):
        for b in reads:
            self._wait(eng, b.lw)
        for b in writes:
            if not (pe_acc and b.lw is not None and b.lw[0] == "pe"):
                self._wait(eng, b.lw)
            for t in b.rd:
                self._wait(eng, t)

    def op(self, eng, fn, reads=(), writes=(), inc=True, pe_acc=False):
        reads = list(reads)
        writes = list(writes)
        self._deps(eng, reads, writes, pe_acc=pe_acc)
        if not inc:
            self.pend_rd[eng].extend(reads)
            self.pend_wr[eng].extend(writes)
            self.prog[eng].append(lambda h, fn=fn: fn(h))
            return
        self.cnt[eng] += 1
        tok = (eng, self.cnt[eng])
        sem = self.sems[eng]
        self.prog[eng].append(lambda h, fn=fn, sem=sem: fn(h).then_inc(sem, 1))
        for b in reads + self.pend_rd[eng]:
            b.rd.append(tok)
        for b in writes + self.pend_wr[eng]:
            b.lw = tok
            b.rd = []
        self.pend_rd[eng] = []
        self.pend_wr[eng] = []

    def dma(self, eng, fn, reads=(), writes=()):
        reads = list(reads)
        writes = list(writes)
        self._deps(eng, reads, writes)
        half = self.n_dma // 2
        qi = 0 if eng == "sp" else 1
        i = qi * half + self.dma_rr2[qi]
        self.dma_rr2[qi] = (self.dma_rr2[qi] + 1) % half
        key = ("dma", i)
        if self.dma_val[i] > 0:
            self._wait(eng, (key, self.dma_val[i]))
        self.dma_val[i] += 16
        tok = (key, self.dma_val[i])
        sem = self.sems[key]
        self.prog[eng].append(lambda h, fn=fn, sem=sem: fn(h).then_inc(sem, 16))
        for b in reads:
            b.rd.append(tok)
        for b in writes:
            b.lw = tok
            b.rd = []
        return tok

    def wait_all(self, eng, bufs):
        for b in bufs:
            self._wait(eng, b.lw)
            for t in b.rd:
                self._wait(eng, t)

    def emit(self):
        nc = self.nc
        for i in range(self.n_dma):
            if self.dma_val[i] > 0:
                self._wait("sp", (("dma", i), self.dma_val[i]))
        prog = self.prog
        with nc.Block() as block:
            @block.tensor
            def _(h):
                for f in prog["pe"]:
                    f(h)

            @block.scalar
            def _(h):
                for f in prog["act"]:
                    f(h)

            @block.vector
            def _(h):
                for f in prog["dve"]:
                    f(h)

            @block.gpsimd
            def _(h):
                for f in prog["pool"]:
                    f(h)

            @block.sync
            def _(h):
                for f in prog["sp"]:
                    f(h)
        self.prog = {e: [] for e in self.ENGS}


class Tn:
    def __init__(self, t, nbuf=1, name=""):
        self.t = t
        self.b = [Buf("%s%d" % (name, i)) for i in range(nbuf)]

    def __getitem__(self, k):
        return self.t[k]


def build_program(dbg=False, stop_after=99):
    nc = bass.Bass("TRN2", target_bir_lowering=False)
    S = Sched(nc)

    def din(name, shape, dt=F32):
        return nc.dram_tensor(name, list(shape), dt, kind="ExternalInput").ap()

    x_d = din("x", [L, D])
    smalls_d = din("smalls", [64, 128])
    rows_d = din("rows", [3, D])
    brt_d = din("brt", [1, 36])
    wada_d = din("w_ada", [D, 6 * D])
    wfm_d = din("w_fm", [D, 2440])
    wtm_d = din("w_tm", [D, 648])
    nbf_d = din("nbf", [8, 1])
    wout_d = din("w_out", [D, D])
    wrt_d = din("w_rt", [D, 36])
    wg_d = din("w_gate", [NE, D, DFF])
    wu_d = din("w_up", [NE, D, DFF])
    wd_d = din("w_down", [NE, DFF, D])
    identf_d = din("identf", [128, 128])
    tri_d = din("tri", [128, 512])
    esel_d = din("esel", [128, 1024])
    alk_d = din("alibi_k", [128, 128])
    alq_d = din("alibi_q", [2, 8 * 512])
    y_d = nc.dram_tensor("y", [L, D], F32, kind="ExternalOutput").ap()
    iwe_d = nc.dram_tensor("iw_e", [128, 64], F32, kind="Internal").ap()
    iwo_d = nc.dram_tensor("iw_o", [128, 64], F32, kind="Internal").ap()
    combT_d = nc.dram_tensor("combT", [NE, L], BF16, kind="Internal").ap()
    dbg_d = {}

    def dbg_out(name, shape, dt=F32):
        dbg_d[name] = nc.dram_tensor("dbg_" + name, list(shape), dt, kind="ExternalOutput").ap()
        return dbg_d[name]

    S.open()
    es_all = ExitStack()

    def sb(es, name, shape, dt=F32, nbuf=1):
        t = es.enter_context(nc.sbuf_tensor("sb_" + name, list(shape), dt))
        return Tn(t, nbuf, name)

    def ps(es, name, shape, dt=F32, nbuf=1):
        t = es.enter_context(nc.psum_tensor("ps_" + name, list(shape), dt))
        return Tn(t, nbuf, name)

    OUTB = Buf("out")

    def phase_end(n):
        if stop_after == n:
            S.wait_all("sp", [OUTB])
            S.emit()
            return True
        S.emit()
        return False

    P = es_all
    HT = sb(P, "HT", [128, 8, L], BF16, nbuf=8)
    identb = sb(P, "identb", [128, 128], BF16)
    identf = sb(P, "identf", [128, 128], F32)
    smT = sb(P, "smT", [128, 64], F32)
    modp = sb(P, "modp", [128, 64], F32)
    gate_m = sb(P, "gate_m", [128, D], F32)
    gate_f = sb(P, "gate_f", [128, D], F32)
    scb = sb(P, "scb", [128, 8], BF16)
    screp = sb(P, "screp", [128, 8, 128], BF16)
    PSB = [ps(P, "psb%d" % i, [128, 512], F32) for i in range(8)]

    def psv_bf(i):
        return PSB[i].t[:].bitcast(BF16)

    es_dsa = ExitStack()
    QTa = sb(es_dsa, "QTa", [128, 4, L], BF16, nbuf=4)
    KTa = sb(es_dsa, "KTa", [128, 2, L], BF16, nbuf=2)
    IQT = sb(es_dsa, "IQT", [128, L, 4], BF16, nbuf=1)
    IKT = sb(es_dsa, "IKT", [128, L], BF16)
    Va = sb(es_dsa, "Va", [128, NT, 2, 65], BF16)
    wcolT = sb(es_dsa, "wcolT", [128, 128], F32)

    S.dma("sp", lambda h: h.dma_start(out=identf[:], in_=identf_d), writes=identf.b)
    S.dma("pool", lambda h: h.dma_start(out=identb[:], in_=identf_d), writes=identb.b)
    S.op("pool", lambda h: h.memset(Va[:, :, :, 64:65], 1.0), writes=Va.b)

    def norm_to_HT(es, src_tile_fn, a0, b0, tag):
        xn = sb(es, "xn" + tag, [128, 4, D], BF16, nbuf=4)
        junk = sb(es, "junk" + tag, [128, D], BF16)
        ss = sb(es, "ss" + tag, [128, NT], F32)
        rstd = sb(es, "rstd" + tag, [128, NT], F32)
        for g in range(4):
            srcs = [src_tile_fn(g * 4 + il) for il in range(4)]
            for il in range(4):
                i = g * 4 + il
                src, sbufs = srcs[il]
                S.op("act", lambda h, src=src, i=i: h.activation(out=junk[:], in_=src, func=AF.Square, accum_out=ss[:, i:i + 1]), reads=sbufs, writes=junk.b + ss.b)
            S.op("act", lambda h, g=g: h.activation(out=rstd[:, 4 * g:4 * g + 4], in_=ss[:, 4 * g:4 * g + 4], func=AF.Sqrt, bias=EPS, scale=1.0 / D), reads=ss.b, writes=rstd.b)
            S.op("dve", lambda h, g=g: h.reciprocal(rstd[:, 4 * g:4 * g + 4], rstd[:, 4 * g:4 * g + 4]), reads=rstd.b, writes=rstd.b)
            for il in range(4):
                i = g * 4 + il
                src, sbufs = srcs[il]
                S.op("dve", lambda h, src=src, i=i, il=il: h.tensor_scalar(out=xn[:, il, :], in0=src, scalar1=rstd[:, i:i + 1], scalar2=None, op0=ALU.mult), reads=sbufs + rstd.b, writes=[xn.b[il]])
            for kk in range(8):
                bank = PSB[kk // 2]
                off = (kk % 2) * 512
                for il in range(4):
                    S.op("pe", lambda h, bank=bank, off=off, il=il, kk=kk: h.transpose(psv_bf(PSB.index(bank))[:, off + il * 128: off + (il + 1) * 128], xn[:, il, kk::8], identb[:]),
                         reads=[xn.b[il]] + identb.b, writes=bank.b, inc=(il == 3), pe_acc=True)
                dst = HT[:, kk, g * 512:(g + 1) * 512]
                srcp = psv_bf(kk // 2)[:, off:off + 512]
                if kk % 2 == 0:
                    S.op("act", lambda h, dst=dst, srcp=srcp, kk=kk: h.activation(out=dst, in_=srcp, func=AF.Identity, scale=modp[:, a0 + kk:a0 + kk + 1], bias=modp[:, b0 + kk:b0 + kk + 1]),
                         reads=bank.b + modp.b, writes=[HT.b[kk]])
                else:
                    S.op("dve", lambda h, dst=dst, srcp=srcp, kk=kk: h.tensor_scalar(out=dst, in0=srcp, scalar1=modp[:, a0 + kk:a0 + kk + 1], scalar2=modp[:, b0 + kk:b0 + kk + 1], op0=ALU.mult, op1=ALU.add),
                         reads=bank.b + modp.b, writes=[HT.b[kk]])
        return rstd

    wada_v = wada_d.rearrange("(p k) n -> p k n", k=8)

    def mod_load(wa, bi, piece):
        S.dma("pool", lambda h: h.dma_start(out=wa[:, bi, :, :], in_=wada_v[:, :, piece * D:(piece + 1) * D]), writes=[wa.b[bi]])

    def mod_vec_piece(wa, bi, sl, bank):
        for kk in range(8):
            for k2 in range(8):
                S.op("pe", lambda h, kk=kk, k2=k2: h.matmul(bank[:, sl * 8 + kk:sl * 8 + kk + 1], lhsT=wa[:, bi, k2, kk::8], rhs=scb[:, k2:k2 + 1], start=(k2 == 0), stop=(k2 == 7)),
                     reads=[wa.b[bi]] + scb.b, writes=bank.b, inc=(k2 == 7 and kk == 7), pe_acc=True)

    def mod_gate_piece(wa, bi, gt, gi, brow, banks):
        for half in range(2):
            bank = banks[half]
            for k2 in range(8):
                S.op("pe", lambda h, bank=bank, k2=k2, half=half: h.matmul(bank[:, :], lhsT=screp[:, k2, :], rhs=wa[:, bi, k2, half * 512:(half + 1) * 512], start=(k2 == 0), stop=(k2 == 7)),
                     reads=[wa.b[bi]] + screp.b, writes=bank.b, inc=(k2 == 7), pe_acc=True)
            S.op("dve", lambda h, bank=bank, half=half: h.tensor_tensor(out=gt[:, half * 512:(half + 1) * 512], in0=bank[:, :], in1=brow[:, gi, half * 512:(half + 1) * 512], op=ALU.add),
                 reads=bank.b + [brow.b[gi]], writes=gt.b)

    with ExitStack() as es:
        sm_in = sb(es, "sm_in", [64, 128], F32)
        wa = sb(es, "wa", [128, 2, 8, D], BF16, nbuf=2)
        sc32 = sb(es, "sc32", [128, 8], F32)
        S.dma("sp", lambda h: h.dma_start(out=sm_in[:], in_=smalls_d), writes=sm_in.b)
        mod_load(wa, 0, 0)
        mod_load(wa, 1, 1)
        S.op("pe", lambda h: h.transpose(PSB[0][:, 0:64], sm_in[:], identf[0:64, 0:64]), reads=sm_in.b + identf.b, writes=PSB[0].b)
        S.op("dve", lambda h: h.tensor_copy(smT[:], PSB[0][:, 0:64]), reads=PSB[0].b, writes=smT.b)
        S.op("act", lambda h: h.activation(out=sc32[:], in_=smT[:, 0:8], func=AF.Silu), reads=smT.b, writes=sc32.b)
        S.op("dve", lambda h: h.tensor_copy(scb[:], sc32[:]), reads=sc32.b, writes=scb.b)
        S.op("dve", lambda h: h.tensor_copy(screp[:], sc32[:].unsqueeze(2).to_broadcast([128, 8, 128])), reads=sc32.b, writes=screp.b)
        mod_vec_piece(wa, 0, 0, PSB[1])
        mod_vec_piece(wa, 1, 1, PSB[1])
        S.op("dve", lambda h: h.tensor_tensor(out=modp[:, 32:48], in0=PSB[1][:, 0:16], in1=smT[:, 8:24], op=ALU.add), reads=PSB[1].b + smT.b, writes=modp.b)
        S.op("dve", lambda h: h.scalar_tensor_tensor(out=modp[:, 0:8], in0=modp[:, 40:48], scalar=1.0, in1=smT[:, 40:48], op0=ALU.add, op1=ALU.mult), reads=modp.b + smT.b, writes=modp.b)
        S.op("dve", lambda h: h.tensor_copy(modp[:, 8:16], modp[:, 32:40]), reads=modp.b, writes=modp.b)
        xt = sb(es, "xt", [128, 4, D], F32, nbuf=4)
        x_v = x_d.rearrange("(i p) d -> i p d", p=128)

        def src1(i):
            bi = i % 4
            S.dma("sp", lambda h, i=i, bi=bi: h.dma_start(out=xt[:, bi, :], in_=x_v[i]), writes=[xt.b[bi]])
            return xt[:, bi, :], [xt.b[bi]]

        norm_to_HT(es, src1, 0, 8, "1")
        if dbg:
            d3 = dbg_out("hT", [128, 8 * L], BF16)
            S.dma("sp", lambda h: h.dma_start(out=d3, in_=HT[:].rearrange("p k t -> p (k t)")), reads=HT.b, writes=[OUTB])
        S.emit()

    def deferred_mod(es):
        wa2 = sb(es, "wa2", [128, 2, 8, D], BF16, nbuf=2)
        brow = sb(es, "brow", [128, 1, D], F32, nbuf=1)
        S.dma("sp", lambda h: h.dma_start(out=brow[:, 0, :], in_=rows_d[0:1, :].partition_broadcast(128)), writes=[brow.b[0]])
        mod_load(wa2, 0, 3)
        mod_load(wa2, 1, 4)

        def stage1():
            mod_vec_piece(wa2, 0, 0, PSB[5])
            mod_vec_piece(wa2, 1, 1, PSB[5])
            S.op("dve", lambda h: h.tensor_tensor(out=modp[:, 48:64], in0=PSB[5][:, 0:16], in1=smT[:, 24:40], op=ALU.add), reads=PSB[5].b + smT.b, writes=modp.b)
            S.op("dve", lambda h: h.scalar_tensor_tensor(out=modp[:, 16:24], in0=modp[:, 56:64], scalar=1.0, in1=smT[:, 48:56], op0=ALU.add, op1=ALU.mult), reads=modp.b + smT.b, writes=modp.b)
            S.op("dve", lambda h: h.tensor_copy(modp[:, 24:32], modp[:, 48:56]), reads=modp.b, writes=modp.b)
            mod_load(wa2, 0, 2)
            mod_load(wa2, 1, 5)

        def stage2():
            mod_gate_piece(wa2, 0, gate_m, 0, brow, (PSB[4], PSB[5]))
            S.dma("sp", lambda h: h.dma_start(out=brow[:, 0, :], in_=rows_d[1:2, :].partition_broadcast(128)), writes=[brow.b[0]])
            mod_gate_piece(wa2, 1, gate_f, 0, brow, (PSB[4], PSB[5]))
        return stage1, stage2

    es_fox = ExitStack()
    QTb = sb(es_fox, "QTb", [128, 4, L], BF16, nbuf=4)
    KTb = sb(es_fox, "KTb", [128, 4, L], BF16, nbuf=4)
    Vb = sb(es_fox, "Vb", [128, NT, 8, 65], BF16)
    cum3 = sb(es_fox, "cum3", [67, 3, L], BF16)
    csT = sb(es_fox, "csT", [128, NT, 8], F32)
    S.op("pool", lambda h: h.memset(Vb[:, :, :, 64:65], 1.0), writes=Vb.b)

    with ExitStack() as es:
        wf = sb(es, "wf", [128, 2, 8, 640], BF16, nbuf=2)
        wt = sb(es, "wt", [128, 8, 648], BF16, nbuf=8)
        iw_tm = sb(es, "iw_tm", [128, NT, 8], F32)
        e1 = sb(es, "e1", [8, L], F32)
        nbf = sb(es, "nbf", [8, 1], F32)
        bfg = sb(es, "bfg", [8, 1], F32)
        wfm_v = wfm_d.rearrange("(p k) n -> p k n", k=8)
        wtm_v = wtm_d.rearrange("(p k) n -> p k n", k=8)
        for kk in range(8):
            S.dma("pool", lambda h, kk=kk: h.dma_start(out=wt[:, kk, :], in_=wtm_v[:, kk, :]), writes=[wt.b[kk]])
        S.dma("sp", lambda h: h.dma_start(out=bfg[:], in_=nbf_d), writes=bfg.b)
        S.op("dve", lambda h: h.tensor_scalar(out=nbf[:], in0=bfg[:], scalar1=-1.0, scalar2=None, op0=ALU.mult), reads=bfg.b, writes=nbf.b)
        dests = []
        for p_ in range(4):
            dests.append((QTa, p_))
        dests += [(KTa, 0), (KTa, 1)]
        for p_ in range(4):
            dests.append((IQT, p_))
        dests.append((IKT, None))
        for p_ in range(4):
            dests.append((QTb, p_))
        for p_ in range(4):
            dests.append((KTb, p_))
        pieces = [(0, 5), (5, 10), (10, 15), (15, 19)]
        cnt_ = {'ev': 0, 'pb': 0}
        def fm_part():
            ev = 0
            pb = 0
            for pi, (c0, c1) in enumerate(pieces):
                bi = pi % 2
                ncol = (c1 - c0) * 128
                S.dma("pool", lambda h, bi=bi, c0=c0, ncol=ncol: h.dma_start(out=wf[:, bi, :, 0:ncol], in_=wfm_v[:, :, c0 * 128:c0 * 128 + ncol]), writes=[wf.b[bi]])
                for ch in range(c0, c1):
                    T_, idx = dests[ch]
                    for ng in range(4):
                        bank = PSB[4 + (pb % 4)]
                        pb += 1
                        for kk in range(8):
                            S.op("pe", lambda h, bank=bank, bi=bi, kk=kk, ch=ch, c0=c0, ng=ng: h.matmul(bank[:, :], lhsT=wf[:, bi, kk, (ch - c0) * 128:(ch - c0 + 1) * 128], rhs=HT[:, kk, ng * 512:(ng + 1) * 512], start=(kk == 0), stop=(kk == 7)),
                                 reads=[wf.b[bi], HT.b[kk]], writes=bank.b, inc=(kk == 7), pe_acc=True)
                        if idx is None:
                            dst = T_[:, ng * 512:(ng + 1) * 512]
                            wb_ = T_.b
                        elif T_ is IQT:
                            dst = T_[:, ng * 512:(ng + 1) * 512, idx]
                            wb_ = T_.b
                        else:
                            dst = T_[:, idx, ng * 512:(ng + 1) * 512]
                            wb_ = [T_.b[idx]]
                        if ev % 2 == 0:
                            S.op("act", lambda h, dst=dst, bank=bank: h.activation(out=dst, in_=bank[:, :], func=AF.Copy), reads=bank.b, writes=wb_)
                        else:
                            S.op("dve", lambda h, dst=dst, bank=bank: h.tensor_copy(dst, bank[:, :]), reads=bank.b, writes=wb_)
                        ev += 1
        wbf = sb(es, "wbf", [128, 8, 8], BF16)
        S.dma("pool", lambda h: h.dma_start(out=wbf[:], in_=wfm_v[:, :, 2432:2440]), writes=wbf.b)

        def bf_part():
            for ng in range(4):
                bank = PSB[4 + ng]
                for kk in range(8):
                    S.op("pe", lambda h, bank=bank, kk=kk, ng=ng: h.matmul(bank[0:8, :], lhsT=wbf[:, kk, :], rhs=HT[:, kk, ng * 512:(ng + 1) * 512], start=(kk == 0), stop=(kk == 7)),
                         reads=wbf.b + [HT.b[kk]], writes=bank.b, inc=(kk == 7), pe_acc=True)
                S.op("act", lambda h, bank=bank, ng=ng: h.activation(out=e1[:, ng * 512:(ng + 1) * 512], in_=bank[0:8, :], func=AF.Exp, scale=-1.0, bias=nbf[:, 0:1]), reads=bank.b + nbf.b, writes=e1.b)
        def tm_part():
            for i in range(NT):
                bA = PSB[(2 * i) % 4]
                bB = PSB[(2 * i + 1) % 4]
                for kk in range(8):
                    S.op("pe", lambda h, bA=bA, kk=kk, i=i: h.matmul(bA[:, :], lhsT=HT[:, kk, i * 128:(i + 1) * 128], rhs=wt[:, kk, 0:512], start=(kk == 0), stop=(kk == 7)),
                         reads=[HT.b[kk], wt.b[kk]], writes=bA.b, inc=(kk == 7), pe_acc=True)
                for kk in range(8):
                    S.op("pe", lambda h, bB=bB, kk=kk, i=i: h.matmul(bB[:, 0:136], lhsT=HT[:, kk, i * 128:(i + 1) * 128], rhs=wt[:, kk, 512:648], start=(kk == 0), stop=(kk == 7)),
                         reads=[HT.b[kk], wt.b[kk]], writes=bB.b, inc=(kk == 7), pe_acc=True)
                S.op("act", lambda h, bA=bA, i=i: h.activation(out=Vb[:, i, :, 0:64], in_=bA[:, :].rearrange("p (h d) -> p h d", h=8), func=AF.Copy), reads=bA.b, writes=Vb.b)
                S.op("dve", lambda h, bB=bB, i=i: h.tensor_copy(Va[:, i, :, 0:64], bB[:, 0:128].rearrange("p (h d) -> p h d", h=2)), reads=bB.b, writes=Va.b)
                S.op("dve", lambda h, bB=bB, i=i: h.tensor_copy(iw_tm[:, i, :], bB[:, 128:136]), reads=bB.b, writes=iw_tm.b)
        tm_part()
        bf_part()
        S.op("act", lambda h: h.activation(out=e1[:], in_=e1[:], func=AF.Ln, bias=1.0, scale=1.0), reads=e1.b, writes=e1.b)
        cs = sb(es, "cs", [8, L], F32)
        S.op("dve", lambda h: h.tensor_tensor_scan(out=cs[:], data0=e1[:], data1=e1[:], initial=0.0, op0=ALU.add, op1=ALU.max), reads=e1.b, writes=cs.b)
        for j in range(NT):
            S.op("pe", lambda h, j=j: h.transpose(PSB[4][:, j * 8:(j + 1) * 8], cs[:, j * 128:(j + 1) * 128], identf[0:8, 0:8]), reads=cs.b + identf.b, writes=PSB[4].b, inc=(j == NT - 1), pe_acc=True)
        S.op("dve", lambda h: h.tensor_copy(csT[:].rearrange("p j h -> p (j h)"), PSB[4][:, 0:128]), reads=PSB[4].b, writes=csT.b)
        hi3 = Tn(e1.t, 2, "hi3")
        hi3v = e1[:].bitcast(BF16).rearrange("p (a t) -> p a t", a=2)
        S.op("dve", lambda h: h.tensor_scalar(out=cs[:], in0=cs[:], scalar1=-8.0, scalar2=None, op0=ALU.mult), reads=cs.b, writes=cs.b)
        for r in range(3):
            S.op("dve", lambda h, r=r: h.tensor_copy(hi3v[:, r % 2, :], cs[:]), reads=cs.b, writes=[hi3.b[r % 2]] + (e1.b if r < 2 else []))
            if r < 2:
                S.op("dve", lambda h, r=r: h.tensor_tensor(out=cs[:], in0=cs[:], in1=hi3v[:, r % 2, :], op=ALU.subtract), reads=cs.b + [hi3.b[r % 2]], writes=cs.b)
            for hh in range(8):
                pp = 32 * (hh % 3) + r
                S.dma("sp", lambda h, hh=hh, pp=pp, r=r: h.dma_start(out=cum3[pp:pp + 1, hh // 3, :], in_=hi3v[hh:hh + 1, r % 2, :]), reads=[hi3.b[r % 2]], writes=cum3.b)
        iwe_flat = iwe_d.rearrange("g c -> (g c)").rearrange("(i t b) -> t i b", i=16, t=128, b=4)
        iwo_flat = iwo_d.rearrange("g c -> (g c)").rearrange("(i t b) -> t i b", i=16, t=128, b=4)
        SCR = Buf("scr")
        S.dma("sp", lambda h: h.dma_start(out=iwe_flat, in_=iw_tm[:, :, 0:4]), reads=iw_tm.b, writes=[SCR])
        S.dma("sp", lambda h: h.dma_start(out=iwo_flat, in_=iw_tm[:, :, 4:8]), reads=iw_tm.b, writes=[SCR])
        wc_in = sb(es, "wc_in", [128, 128], F32)
        S.dma("sp", lambda h: h.dma_start(out=wc_in[:, 0:64], in_=iwe_d), reads=[SCR], writes=wc_in.b)
        S.dma("sp", lambda h: h.dma_start(out=wc_in[:, 64:128], in_=iwo_d), reads=[SCR], writes=wc_in.b)
        S.op("pe", lambda h: h.transpose(PSB[5][:, 0:128], wc_in[:], identf[:]), reads=wc_in.b + identf.b, writes=PSB[5].b)
        S.op("dve", lambda h: h.tensor_copy(wcolT[:], PSB[5][:, 0:128]), reads=PSB[5].b, writes=wcolT.b)
        fm_part()
        if dbg:
            for nm, T_, shp, dt_ in [("QTa", QTa, [128, 4 * L], BF16), ("KTa", KTa, [128, 2 * L], BF16), ("IKT", IKT, [128, L], BF16),
                                     ("QTb", QTb, [128, 4 * L], BF16), ("KTb", KTb, [128, 4 * L], BF16), ("Vb", Vb, [128, NT * 8 * 65], BF16), ("Va", Va, [128, NT * 2 * 65], BF16),
                                     ("csT", csT, [128, NT * 8], F32), ("cum3", cum3, [67, 3 * L], BF16), ("wcolT", wcolT, [128, 128], F32)]:
                dd = dbg_out(nm, shp, dt_)
                nd = len(T_.t.shape)
                if nd == 2:
                    src_ap = T_[:]
                elif nd == 3:
                    src_ap = T_[:].rearrange("p a b -> p (a b)")
                else:
                    src_ap = T_[:].rearrange("p a b c -> p (a b c)")
                S.dma("sp", lambda h, dd=dd, src_ap=src_ap: h.dma_start(out=dd, in_=src_ap), reads=T_.b, writes=[OUTB])
        if phase_end(1):
            return nc, dbg_d

    def attn_scratch(es, tag):
        sc = {}
        sc["ones67"] = sb(es, "ones67" + tag, [67, 128], BF16)
        sc["PT"] = sb(es, "PT" + tag, [128, 4, 512], BF16, nbuf=4)
        sc["o_sb"] = sb(es, "o_sb" + tag, [128, 4, 512], F32)
        sc["ob"] = sb(es, "ob" + tag, [128, 4, 512], BF16)
        sc["rec"] = sb(es, "rec" + tag, [128, 8, 4], F32)
        sc["oss"] = sb(es, "oss" + tag, [128, 8], F32)
        sc["ojunk"] = sb(es, "ojunk" + tag, [128, 512], BF16)
        S.op("pool", lambda h: h.memset(sc["ones67"][:], 1.0), writes=sc["ones67"].b)
        return sc

    def attention_chunk(sc, c, KT, kt_idx, QT, aug_lhs, aug_rhs, mask_rhs, bias_ap, Vt, v_idx, tick=None):
        PT, o_sb, rec = sc["PT"], sc["o_sb"], sc["rec"]
        T0 = 512 * c
        nj = 4 * c + 4
        steps = [(pr, j) for pr in range(4) for j in range(nj)]

        def s_stage(k):
            pr, j = steps[k]
            col0 = max(0, j - 4 * c) * 128
            ncols = 512 - col0
            heads = (2 * pr, 2 * pr + 1)
            Sbs = (PSB[(2 * k) % 4], PSB[(2 * k + 1) % 4])
            for hh, Sb in zip(heads, Sbs):
                rows = slice(64 * (hh % 2), 64 * (hh % 2) + 64)
                S.op("pe", lambda h, hh=hh, Sb=Sb, rows=rows: h.matmul(Sb[:, 0:ncols], lhsT=KT[rows, kt_idx(hh), j * 128:(j + 1) * 128], rhs=QT[rows, pr, T0 + col0:T0 + 512], start=True, stop=False),
                     reads=[KT.b[kt_idx(hh)], QT.b[pr]], writes=Sb.b, inc=False, pe_acc=True)
            mr, mbufs = mask_rhs(j, col0, ncols)
            if mr is not None:
                for hh, Sb in zip(heads, Sbs):
                    S.op("pe", lambda h, Sb=Sb: h.matmul(Sb[:, 0:ncols], lhsT=identb[:], rhs=mr, start=False, stop=False),
                         reads=identb.b + mbufs, writes=Sb.b, inc=False, pe_acc=True)
            for hh, Sb in zip(heads, Sbs):
                al, albufs = aug_lhs(hh)
                ar, arbufs = aug_rhs(hh, col0)
                S.op("pe", lambda h, Sb=Sb, al=al, ar=ar: h.matmul(Sb[:, 0:ncols], lhsT=al, rhs=ar, start=False, stop=True),
                     reads=albufs + arbufs, writes=Sb.b, inc=True, pe_acc=True)
            for q_, (hh, Sb) in enumerate(zip(heads, Sbs)):
                pt = (2 * k + q_) % 4
                bap, bbufs = bias_ap(hh, j)
                S.op("act", lambda h, Sb=Sb, pt=pt, bap=bap: h.activation(out=PT[:, pt, 0:ncols], in_=Sb[:, 0:ncols], func=AF.Exp, scale=0.125, bias=bap),
                     reads=Sb.b + bbufs, writes=[PT.b[pt]])

        def pv_stage(k):
            pr, j = steps[k]
            col0 = max(0, j - 4 * c) * 128
            il0 = max(0, j - 4 * c)
            for q_ in range(2):
                hh = 2 * pr + q_
                pt = (2 * k + q_) % 4
                Ob = PSB[(6 if pr % 2 == 0 else 4) + q_]
                for il in range(il0, 4):
                    i = 4 * c + il
                    first = (j == 0 and il == il0)
                    S.op("pe", lambda h, il=il, i=i, first=first, Ob=Ob, pt=pt, hh=hh: h.matmul(Ob[:, il * 65:(il + 1) * 65], lhsT=PT[:, pt, il * 128 - col0:il * 128 - col0 + 128], rhs=Vt[:, j, v_idx(hh), :], start=first, stop=(j == i), skip_group_check=True),
                         reads=[PT.b[pt]] + Vt.b, writes=Ob.b, inc=(il == 3), pe_acc=True)
                if j == nj - 1:
                    Ov = Ob[:, 0:260].rearrange("p (a b) -> p a b", b=65)
                    S.op("dve", lambda h, Ov=Ov, hh=hh: h.reciprocal(rec[:, hh, :], Ov[:, :, 64]), reads=Ob.b, writes=rec.b)
                    S.op("dve", lambda h, Ov=Ov, hh=hh: h.tensor_tensor(out=o_sb[:, :, hh * 64:(hh + 1) * 64], in0=Ov[:, :, 0:64], in1=rec[:, hh, :].unsqueeze(2).to_broadcast([128, 4, 64]), op=ALU.mult),
                         reads=Ob.b + rec.b, writes=o_sb.b)

        n = len(steps)
        s_stage(0)
        for k in range(n):
            if k + 1 < n:
                s_stage(k + 1)
            pv_stage(k)
            if tick is not None:
                tick(k, n)

    def out_norm(sc, c, base):
        o_sb, ob, oss, ojunk = sc["o_sb"], sc["ob"], sc["oss"], sc["ojunk"]
        T0 = 512 * c
        for il in range(4):
            S.op("act", lambda h, il=il: h.activation(out=ojunk[:], in_=o_sb[:, il, :], func=AF.Square, accum_out=oss[:, il:il + 1]), reads=o_sb.b, writes=ojunk.b + oss.b)
        S.op("act", lambda h: h.activation(out=oss[:, 4:8], in_=oss[:, 0:4], func=AF.Sqrt, bias=EPS, scale=1.0 / 512), reads=oss.b, writes=oss.b)
        S.op("dve", lambda h: h.reciprocal(oss[:, 4:8], oss[:, 4:8]), reads=oss.b, writes=oss.b)
        S.op("dve", lambda h: h.tensor_tensor(out=ob[:], in0=o_sb[:], in1=oss[:, 4:8].unsqueeze(2).to_broadcast([128, 4, 512]), op=ALU.mult), reads=o_sb.b + oss.b, writes=ob.b)
        for fc in range(4):
            bank = PSB[4 + fc % 2]
            bi = 4 + fc % 2
            for il in range(4):
                S.op("pe", lambda h, bi=bi, il=il, fc=fc: h.transpose(psv_bf(bi)[:, il * 128:(il + 1) * 128], ob[:, il, fc * 128:(fc + 1) * 128], identb[:]),
                     reads=ob.b + identb.b, writes=bank.b, inc=(il == 3), pe_acc=True)
            S.op("act", lambda h, bi=bi, fc=fc: h.activation(out=HT[:, base + fc, T0:T0 + 512], in_=psv_bf(bi)[:, 0:512], func=AF.Identity, scale=smT[:, 56 + base + fc:57 + base + fc]),
                 reads=bank.b + smT.b, writes=[HT.b[base + fc]])

    with ExitStack() as es:
        sc = attn_scratch(es, "f")
        triw = sb(es, "triw", [128, 512], BF16)
        S.dma("pool", lambda h: h.dma_start(out=triw[:], in_=tri_d), writes=triw.b)
        mod_st1, mod_st2 = deferred_mod(es)
        for c in range(4):
            if c == 1:
                mod_st1()
            if c == 2:
                mod_st2()
            attention_chunk(
                sc, c, KTb, lambda hh: hh // 2, QTb,
                aug_lhs=lambda hh: (sc["ones67"][32 * (hh % 3):32 * (hh % 3) + 3, :], sc["ones67"].b),
                aug_rhs=lambda hh, col0, c=c: (cum3[32 * (hh % 3):32 * (hh % 3) + 3, hh // 3, 512 * c + col0:512 * c + 512], cum3.b),
                mask_rhs=lambda j, col0, ncols, c=c: ((triw[:, 0:ncols], triw.b) if j >= 4 * c else (None, [])),
                bias_ap=lambda hh, j: (csT[:, j, hh:hh + 1], csT.b),
                Vt=Vb, v_idx=lambda hh: hh)
            out_norm(sc, c, 4)
        if dbg:
            d4b = dbg_out("oTb", [128, 8 * L], BF16)
            S.dma("sp", lambda h: h.dma_start(out=d4b, in_=HT[:].rearrange("p k t -> p (k t)")), reads=HT.b, writes=[OUTB])
        if phase_end(2):
            return nc, dbg_d
    es_fox.close()

    with ExitStack() as es:
        sc = attn_scratch(es, "a")
        IS = sb(es, "IS", [128, 4, L], F32, nbuf=4)
        NM = sb(es, "NM", [128, L], BF16)
        NMT2 = sb(es, "NMT", [128, 2, NT, 512], BF16, nbuf=2)
        R = sb(es, "R", [128, 4, 512], BF16, nbuf=4)
        Wblk = sb(es, "Wblk", [128, 2, 8, 128], BF16, nbuf=2)
        esel = sb(es, "esel", [128, 8, 128], BF16)
        alk = sb(es, "alk", [128, 8, 16], F32)
        alq = sb(es, "alq", [66, 8, 512], BF16)
        bs = sb(es, "bs", [128, 32], F32)
        cjunk = sb(es, "cjunk", [128, L], BF16)
        S.dma("pool", lambda h: h.dma_start(out=esel[:].rearrange("p a b -> p (a b)"), in_=esel_d), writes=esel.b)
        S.dma("pool", lambda h: h.dma_start(out=alq[0:2].rearrange("p a b -> p (a b)"), in_=alq_d), writes=alq.b)
        S.dma("pool", lambda h: h.dma_start(out=alq[64:66].rearrange("p a b -> p (a b)"), in_=alq_d), writes=alq.b)
        S.dma("sp", lambda h: h.dma_start(out=alk[:].rearrange("p a b -> p (a b)"), in_=alk_d), writes=alk.b)
        evc = [0]

        def indexer(c):
            units = []
            for il in range(4):
                i = 4 * c + il
                ncols = 128 * (i + 1)
                for kc in range((ncols + 511) // 512):
                    for g in range(8):
                        units.append((il, kc, g))

            def dots(u):
                il, kc, g = units[u]
                i = 4 * c + il
                n = min(512, 128 * (i + 1) - 512 * kc)
                tok0 = 128 * i + 16 * g
                Db = PSB[u % 4]
                for hf in range(2):
                    rs = slice(64 * hf, 64 * hf + 64)
                    S.op("pe", lambda h, rs=rs: h.matmul(Db[rs, 0:n], lhsT=IQT[rs, tok0:tok0 + 16, :].rearrange("p t a -> p (t a)"), rhs=IKT[rs, 512 * kc:512 * kc + n], start=True, stop=True),
                         reads=IQT.b + IKT.b, writes=Db.b, inc=(hf == 1), pe_acc=True)
                if evc[0] % 4 != 3:
                    S.op("act", lambda h: h.activation(out=R[:, u % 4, 0:n], in_=Db[:, 0:n], func=AF.Relu), reads=Db.b, writes=[R.b[u % 4]])
                else:
                    S.op("dve", lambda h: h.tensor_scalar(out=R[:, u % 4, 0:n], in0=Db[:, 0:n], scalar1=0.0, scalar2=None, op0=ALU.max), reads=Db.b, writes=[R.b[u % 4]])
                evc[0] += 1

            def headsum(u):
                il, kc, g = units[u]
                i = 4 * c + il
                ncols = 128 * (i + 1)
                n = min(512, ncols - 512 * kc)
                ISb = PSB[4 + kc % 2]
                wb = i % 2
                if kc == 0 and g == 0:
                    S.op("pool", lambda h: h.tensor_tensor(out=Wblk[:, wb], in0=esel[:], in1=wcolT[:, 8 * i:8 * i + 8].unsqueeze(2).to_broadcast([128, 8, 128]), op=ALU.mult), reads=esel.b + wcolT.b, writes=[Wblk.b[wb]])
                S.op("pe", lambda h: h.matmul(ISb[:, 0:n], lhsT=Wblk[:, wb, g, :], rhs=R[:, u % 4, 0:n], start=(g == 0), stop=(g == 7)),
                     reads=[Wblk.b[wb], R.b[u % 4]], writes=ISb.b, inc=(g == 7), pe_acc=True)
                if g == 7:
                    S.op("act", lambda h: h.activation(out=IS[:, il, 512 * kc:512 * kc + n], in_=ISb[:, 0:n], func=AF.Copy), reads=ISb.b, writes=[IS.b[il]])
                    if 512 * kc + n == ncols:
                        S.op("dve", lambda h: h.tensor_reduce(out=bs[:, 20 + il:21 + il], in_=IS[:, il, 0:ncols], axis=AX.X, op=ALU.max, apply_absolute_value=True), reads=[IS.b[il]], writes=bs.b)
                        S.op("pool", lambda h: h.affine_select(out=IS[:, il, 128 * i:128 * i + 128], in_=IS[:, il, 128 * i:128 * i + 128], pattern=[[-1, 128]], compare_op=ALU.is_ge, fill=-1e30, base=0, channel_multiplier=1),
                             reads=[IS.b[il]], writes=[IS.b[il]])

            nu = len(units)
            dots(0)
            if nu > 1:
                dots(1)
            for u in range(nu):
                if u + 2 < nu:
                    dots(u + 2)
                headsum(u)

        def bisect_gen(c):
            S.op("dve", lambda h: h.tensor_scalar(out=bs[:, 0:4], in0=bs[:, 20:24], scalar1=-1.001, scalar2=-1e-3, op0=ALU.mult, op1=ALU.add), reads=bs.b, writes=bs.b)
            S.op("dve", lambda h: h.tensor_scalar(out=bs[:, 4:8], in0=bs[:, 20:24], scalar1=2.002, scalar2=2e-3, op0=ALU.mult, op1=ALU.add), reads=bs.b, writes=bs.b)
            for jb in range(1, NBIS + 1):
                stp = 2.0 ** (-jb)
                S.op("dve", lambda h, stp=stp: h.scalar_tensor_tensor(out=bs[:, 8:12], in0=bs[:, 4:8], scalar=stp, in1=bs[:, 0:4], op0=ALU.mult, op1=ALU.add), reads=bs.b, writes=bs.b)
                for il in range(4):
                    ncols = 128 * (4 * c + il + 1)
                    S.op("dve", lambda h, il=il, ncols=ncols: h.tensor_scalar(out=cjunk[:, 0:ncols], in0=IS[:, il, 0:ncols], scalar1=bs[:, 8 + il:9 + il], scalar2=0.0, op0=ALU.is_ge, op1=ALU.add, accum_out=bs[:, 12 + il:13 + il]),
                         reads=[IS.b[il]] + bs.b, writes=cjunk.b + bs.b)
                S.op("dve", lambda h, stp=stp: h.tensor_scalar(out=bs[:, 16:20], in0=bs[:, 12:16], scalar1=255.5, scalar2=stp, op0=ALU.is_ge, op1=ALU.mult), reads=bs.b, writes=bs.b)
                S.op("dve", lambda h: h.tensor_tensor(out=bs[:, 16:20], in0=bs[:, 16:20], in1=bs[:, 4:8], op=ALU.mult), reads=bs.b, writes=bs.b)
                S.op("dve", lambda h: h.tensor_tensor(out=bs[:, 0:4], in0=bs[:, 0:4], in1=bs[:, 16:20], op=ALU.add), reads=bs.b, writes=bs.b)
                yield

        def mask_epilogue(c):
            tb = 0
            nb_ = c % 2
            for il in range(4):
                i = 4 * c + il
                ncols = 128 * (i + 1)
                S.op("dve", lambda h, il=il, ncols=ncols: h.tensor_scalar(out=NM[:, 0:ncols], in0=IS[:, il, 0:ncols], scalar1=bs[:, il:il + 1], scalar2=NEG, op0=ALU.is_lt, op1=ALU.mult), reads=[IS.b[il]] + bs.b, writes=NM.b)
                for j0 in range(0, i + 1, 8):
                    nb = min(8, i + 1 - j0)
                    bi = 6 + tb % 2
                    tb += 1
                    for jj in range(nb):
                        S.op("pe", lambda h, bi=bi, jj=jj, j0=j0: h.transpose(psv_bf(bi)[:, jj * 128:(jj + 1) * 128], NM[:, (j0 + jj) * 128:(j0 + jj + 1) * 128], identb[:]),
                             reads=NM.b + identb.b, writes=PSB[bi].b, inc=(jj == nb - 1), pe_acc=True)
                    S.op("act", lambda h, bi=bi, j0=j0, nb=nb, il=il: h.activation(out=NMT2[:, nb_, j0:j0 + nb, il * 128:(il + 1) * 128], in_=psv_bf(bi)[:, 0:nb * 128].rearrange("p (a b) -> p a b", b=128), func=AF.Copy),
                         reads=PSB[bi].b, writes=[NMT2.b[nb_]])

        order = [3, 2, 1, 0]
        indexer(order[0])
        for _ in bisect_gen(order[0]):
            pass
        mask_epilogue(order[0])
        for oi, c in enumerate(order):
            gen = None
            cn = order[oi + 1] if oi + 1 < 4 else None
            if cn is not None:
                indexer(cn)
                gen = bisect_gen(cn)
            nsteps = 4 * (4 * c + 4)
            every = max(1, nsteps // (NBIS + 1))

            def tick(k, n, gen=gen, every=every):
                if gen is not None and k % every == 0:
                    next(gen, None)
            nb_ = c % 2
            attention_chunk(
                sc, c, KTa, lambda hh: hh // 4, QTa,
                aug_lhs=lambda hh: (sc["ones67"][64 * (hh % 2):64 * (hh % 2) + 2, :], sc["ones67"].b),
                aug_rhs=lambda hh, col0: (alq[64 * (hh % 2):64 * (hh % 2) + 2, hh, col0:512], alq.b),
                mask_rhs=lambda j, col0, ncols, nb_=nb_: (NMT2[:, nb_, j, col0:512], [NMT2.b[nb_]]),
                bias_ap=lambda hh, j, c=c: (alk[:, hh, 4 * c - j + 3:4 * c - j + 4], alk.b),
                Vt=Va, v_idx=lambda hh: hh // 4, tick=tick)
            if gen is not None:
                for _ in gen:
                    pass
                mask_epilogue(cn)
            out_norm(sc, c, 0)
        if dbg:
            for nm, T_, shp, dt_ in [("IS", IS, [128, 4 * L], F32), ("bs", bs, [128, 32], F32), ("osb", sc["o_sb"], [128, 4 * 512], F32)]:
                dd = dbg_out(nm, shp, dt_)
                src_ap = T_[:] if len(T_.t.shape) == 2 else T_[:].rearrange("p a b -> p (a b)")
                S.dma("sp", lambda h, dd=dd, src_ap=src_ap: h.dma_start(out=dd, in_=src_ap), reads=T_.b, writes=[OUTB])
            d4 = dbg_out("oT", [128, 8 * L], BF16)
            S.dma("sp", lambda h: h.dma_start(out=d4, in_=HT[:].rearrange("p k t -> p (k t)")), reads=HT.b, writes=[OUTB])
        if phase_end(3):
            return nc, dbg_d
    es_dsa.close()

    with ExitStack() as es:
        X1 = sb(es, "X1", [128, NT, D], F32, nbuf=NT)
        x_v = x_d.rearrange("(i p) d -> i p d", p=128)
        with ExitStack() as es2:
            wo = sb(es2, "wo", [128, 8, D], BF16, nbuf=8)
            xt2 = sb(es2, "xt2", [128, 2, D], F32, nbuf=2)
            wout_v = wout_d.rearrange("(c p) d -> p c d", p=128)
            for fc in range(8):
                S.dma("pool", lambda h, fc=fc: h.dma_start(out=wo[:, fc, :], in_=wout_v[:, fc, :]), writes=[wo.b[fc]])
            for fc in range(8):
                S.op("pool", lambda h, fc=fc: h.tensor_tensor(out=wo[:, fc, :], in0=wo[:, fc, :], in1=gate_m[:], op=ALU.mult), reads=[wo.b[fc]] + gate_m.b, writes=[wo.b[fc]])
            for i in range(NT):
                bi = i % 2
                S.dma("sp", lambda h, i=i, bi=bi: h.dma_start(out=xt2[:, bi, :], in_=x_v[i]), writes=[xt2.b[bi]])
                for half in range(2):
                    bank = PSB[(2 * i + half) % 4]
                    for fc in range(8):
                        S.op("pe", lambda h, bank=bank, fc=fc, i=i, half=half: h.matmul(bank[:, :], lhsT=HT[:, fc, i * 128:(i + 1) * 128], rhs=wo[:, fc, half * 512:(half + 1) * 512], start=(fc == 0), stop=(fc == 7)),
                             reads=[HT.b[fc], wo.b[fc]], writes=bank.b, inc=(fc == 7), pe_acc=True)
                    S.op("dve", lambda h, bank=bank, i=i, half=half, bi=bi: h.tensor_tensor(out=X1[:, i, half * 512:(half + 1) * 512], in0=bank[:, :], in1=xt2[:, bi, half * 512:(half + 1) * 512], op=ALU.add),
                         reads=bank.b + [xt2.b[bi]], writes=[X1.b[i]])
            if dbg:
                d5 = dbg_out("X1", [128, NT * D], F32)
                S.dma("sp", lambda h: h.dma_start(out=d5, in_=X1[:].rearrange("p a b -> p (a b)")), reads=X1.b, writes=[OUTB])
            if phase_end(4):
                return nc, dbg_d
        with ExitStack() as es2:
            norm_to_HT(es2, lambda i: (X1[:, i, :], [X1.b[i]]), 16, 24, "2")
            if phase_end(5):
                return nc, dbg_d
        with ExitStack() as es2:
            combT = sb(es2, "combT", [32, L], BF16)
            sel = sb(es2, "sel", [32, NE * 128], BF16)
            es3 = ExitStack()
            wr = sb(es3, "wr", [128, 8, 36], BF16)
            brt = sb(es3, "brt", [128, 36], F32)
            lg = sb(es3, "lg", [128, NT, 36], F32)
            rt = sb(es3, "rt", [128, NT, 64], F32)
            comb = sb(es3, "comb", [128, NT, 32], BF16)
            S.dma("pool", lambda h: h.dma_start(out=wr[:], in_=wrt_d.rearrange("(p k) n -> p k n", k=8)), writes=wr.b)
            S.dma("sp", lambda h: h.dma_start(out=brt[:], in_=brt_d.partition_broadcast(128)), writes=brt.b)
            for i in range(NT):
                bank = PSB[i % 4]
                for kk in range(8):
                    S.op("pe", lambda h, bank=bank, kk=kk, i=i: h.matmul(bank[:, 0:36], lhsT=HT[:, kk, i * 128:(i + 1) * 128], rhs=wr[:, kk, :], start=(kk == 0), stop=(kk == 7)),
                         reads=[HT.b[kk]] + wr.b, writes=bank.b, inc=(kk == 7), pe_acc=True)
                S.op("dve", lambda h, bank=bank, i=i: h.tensor_tensor(out=lg[:, i, :], in0=bank[:, 0:36], in1=brt[:], op=ALU.add), reads=bank.b + brt.b, writes=lg.b)
            RB = rt.b + lg.b

            def dv(fn):
                S.op("dve", fn, reads=RB, writes=RB)

            def bc(ap, n):
                return ap.unsqueeze(2).to_broadcast([128, NT, n])
            gl = lg[:, :, 0:4]
            gmax, gsum, pg, m1, m2, w1, w2 = (rt[:, :, k] for k in range(7))
            ohg, gsh, el, tmp8, oh1, oh2, el2 = rt[:, :, 8:12], rt[:, :, 12:16], rt[:, :, 16:24], rt[:, :, 24:32], rt[:, :, 32:40], rt[:, :, 40:48], rt[:, :, 48:56]
            c8 = rt[:, :, 56:64]
            dv(lambda h: h.tensor_reduce(out=gmax, in_=gl, axis=AX.X, op=ALU.max))
            dv(lambda h: h.tensor_tensor(out=ohg, in0=gl, in1=bc(gmax, 4), op=ALU.is_ge))
            dv(lambda h: h.tensor_tensor(out=gsh, in0=gl, in1=bc(gmax, 4), op=ALU.subtract))
            S.op("act", lambda h: h.activation(out=gsh, in_=gsh, func=AF.Exp), reads=RB, writes=RB)
            dv(lambda h: h.tensor_reduce(out=gsum, in_=gsh, axis=AX.X, op=ALU.add))
            dv(lambda h: h.reciprocal(pg, gsum))
            for g in range(4):
                src_e = lg[:, :, 4 + 8 * g:12 + 8 * g]
                if g == 0:
                    dv(lambda h, src_e=src_e, g=g: h.tensor_tensor(out=el, in0=src_e, in1=bc(ohg[:, :, g], 8), op=ALU.mult))
                else:
                    dv(lambda h, src_e=src_e, g=g: h.tensor_tensor(out=tmp8, in0=src_e, in1=bc(ohg[:, :, g], 8), op=ALU.mult))
                    dv(lambda h: h.tensor_tensor(out=el, in0=el, in1=tmp8, op=ALU.add))
            dv(lambda h: h.tensor_reduce(out=m1, in_=el, axis=AX.X, op=ALU.max))
            dv(lambda h: h.tensor_tensor(out=oh1, in0=el, in1=bc(m1, 8), op=ALU.is_ge))
            dv(lambda h: h.scalar_tensor_tensor(out=el2, in0=oh1, scalar=-1e30, in1=el, op0=ALU.mult, op1=ALU.add))
            dv(lambda h: h.tensor_reduce(out=m2, in_=el2, axis=AX.X, op=ALU.max))
            dv(lambda h: h.tensor_tensor(out=oh2, in0=el2, in1=bc(m2, 8), op=ALU.is_ge))
            dv(lambda h: h.tensor_tensor(out=w2, in0=m2, in1=m1, op=ALU.subtract))
            S.op("act", lambda h: h.activation(out=w2, in_=w2, func=AF.Exp), reads=RB, writes=RB)
            dv(lambda h: h.tensor_scalar(out=w1, in0=w2, scalar1=1.0, scalar2=None, op0=ALU.add))
            dv(lambda h: h.reciprocal(w1, w1))
            dv(lambda h: h.tensor_tensor(out=w2, in0=w2, in1=w1, op=ALU.mult))
            dv(lambda h: h.tensor_tensor(out=w1, in0=w1, in1=pg, op=ALU.mult))
            dv(lambda h: h.tensor_tensor(out=w2, in0=w2, in1=pg, op=ALU.mult))
            dv(lambda h: h.tensor_tensor(out=c8, in0=oh1, in1=bc(w1, 8), op=ALU.mult))
            dv(lambda h: h.tensor_tensor(out=tmp8, in0=oh2, in1=bc(w2, 8), op=ALU.mult))
            dv(lambda h: h.tensor_tensor(out=c8, in0=c8, in1=tmp8, op=ALU.add))
            for g in range(4):
                S.op("dve", lambda h, g=g: h.tensor_tensor(out=comb[:, :, 8 * g:8 * g + 8], in0=c8, in1=bc(ohg[:, :, g], 8), op=ALU.mult), reads=RB, writes=comb.b)
            for i in range(NT):
                bi = 4 + (i // 8)
                S.op("pe", lambda h, bi=bi, i=i: h.transpose(psv_bf(bi)[0:32, (i % 8) * 128:(i % 8 + 1) * 128], comb[:, i, :], identb[:]), reads=comb.b + identb.b, writes=PSB[bi].b, inc=(i % 8 == 7), pe_acc=True)
            for hb in range(2):
                S.op("dve", lambda h, hb=hb: h.tensor_copy(combT[:, hb * 1024:(hb + 1) * 1024], psv_bf(4 + hb)[0:32, :]), reads=PSB[4 + hb].b, writes=combT.b)
            S.op("dve", lambda h: h.tensor_copy(sel[:].rearrange("k (e p) -> k e p", p=128), identb[0:32, 0:32].unsqueeze(2).to_broadcast([32, NE, 128])), reads=identb.b, writes=sel.b)
            S.emit()
            es3.close()
            wgu = sb(es2, "wgu", [128, 3, 2, 8, DFF], BF16, nbuf=3)
            wdn = sb(es2, "wdn", [128, 4, 2, D], BF16, nbuf=4)
            cbb = sb(es2, "cbb", [128, 2, L], BF16, nbuf=2)
            aT = sb(es2, "aT", [128, 4, 2, L], BF16, nbuf=4)
            sl = sb(es2, "sl", [128, 2, 512], BF16, nbuf=2)
            t1 = sb(es2, "t1", [128, 2, 512], BF16, nbuf=2)
            q = 0
            def load_gu(e):
                b = e % 3
                S.dma("pool", lambda h: h.dma_start(out=wgu[:, b, 0], in_=wg_d[e].rearrange("(p k) f -> p k f", k=8)), writes=[wgu.b[b]])
                S.dma("pool", lambda h: h.dma_start(out=wgu[:, b, 1], in_=wu_d[e].rearrange("(p k) f -> p k f", k=8)), writes=[wgu.b[b]])
            load_gu(0)
            load_gu(1)
            for rnd in range(8):
                for er in range(4):
                    e = 4 * rnd + er
                    b = e % 2
                    wb3 = e % 3
                    if e + 2 < NE:
                        load_gu(e + 2)
                    S.dma("pool", lambda h, e=e, er=er: h.dma_start(out=wdn[:, er], in_=wd_d[e].rearrange("(c p) d -> p c d", p=128)), writes=[wdn.b[er]])
                    for tcn in range(4):
                        cbank = PSB[4 + tcn]
                        S.op("pe", lambda h, cbank=cbank, e=e, tcn=tcn: h.matmul(cbank[:, :], lhsT=sel[:, e * 128:(e + 1) * 128], rhs=combT[:, tcn * 512:(tcn + 1) * 512], start=True, stop=True),
                             reads=sel.b + combT.b, writes=cbank.b)
                        if tcn % 2 == 0:
                            S.op("act", lambda h, cbank=cbank, b=b, tcn=tcn: h.activation(out=cbb[:, b, tcn * 512:(tcn + 1) * 512], in_=cbank[:, :], func=AF.Copy), reads=cbank.b, writes=[cbb.b[b]])
                        else:
                            S.op("dve", lambda h, cbank=cbank, b=b, tcn=tcn: h.tensor_copy(cbb[:, b, tcn * 512:(tcn + 1) * 512], cbank[:, :]), reads=cbank.b, writes=[cbb.b[b]])
                    for tcn in range(4):
                        for fc in range(2):
                            Gb = PSB[(2 * q) % 4]
                            Ub = PSB[(2 * q + 1) % 4]
                            qb = q % 2
                            q += 1
                            for gu, bank in ((0, Gb), (1, Ub)):
                                for kk in range(8):
                                    S.op("pe", lambda h, bank=bank, gu=gu, kk=kk, wb3=wb3, fc=fc, tcn=tcn: h.matmul(bank[:, :], lhsT=wgu[:, wb3, gu, kk, fc * 128:(fc + 1) * 128], rhs=HT[:, kk, tcn * 512:(tcn + 1) * 512], start=(kk == 0), stop=(kk == 7)),
                                         reads=[wgu.b[wb3], HT.b[kk]], writes=bank.b, inc=(kk == 7), pe_acc=True)
                            S.op("act", lambda h, Gb=Gb, qb=qb: h.activation(out=sl[:, qb, :], in_=Gb[:, :], func=AF.Silu), reads=Gb.b, writes=[sl.b[qb]])
                            S.op("dve", lambda h, Ub=Ub, qb=qb: h.tensor_tensor(out=t1[:, qb, :], in0=Ub[:, :], in1=sl[:, qb, :], op=ALU.mult), reads=Ub.b + [sl.b[qb]], writes=[t1.b[qb]])
                            S.op("pool", lambda h, qb=qb, er=er, fc=fc, tcn=tcn, b=b: h.tensor_tensor(out=aT[:, er, fc, tcn * 512:(tcn + 1) * 512], in0=t1[:, qb, :], in1=cbb[:, b, tcn * 512:(tcn + 1) * 512], op=ALU.mult),
                                 reads=[t1.b[qb], cbb.b[b]], writes=[aT.b[er]])
                    S.op("pool", lambda h, er=er: h.tensor_tensor(out=wdn[:, er], in0=wdn[:, er], in1=gate_f[:].unsqueeze(1).to_broadcast([128, 2, D]), op=ALU.mult), reads=[wdn.b[er]] + gate_f.b, writes=[wdn.b[er]])
                for i in range(NT):
                    for half in range(2):
                        bank = PSB[4 + (2 * i + half) % 4]
                        for er in range(4):
                            for fc in range(2):
                                S.op("pe", lambda h, bank=bank, er=er, fc=fc, i=i, half=half: h.matmul(bank[:, :], lhsT=aT[:, er, fc, i * 128:(i + 1) * 128], rhs=wdn[:, er, fc, half * 512:(half + 1) * 512], start=(er == 0 and fc == 0), stop=(er == 3 and fc == 1)),
                                     reads=[aT.b[er], wdn.b[er]], writes=bank.b, inc=(er == 3 and fc == 1), pe_acc=True)
                        S.op("dve", lambda h, bank=bank, i=i, half=half: h.tensor_tensor(out=X1[:, i, half * 512:(half + 1) * 512], in0=bank[:, :], in1=X1[:, i, half * 512:(half + 1) * 512], op=ALU.add),
                             reads=bank.b + [X1.b[i]], writes=[X1.b[i]])
            if phase_end(6):
                return nc, dbg_d
        with ExitStack() as es2:
            gfin = sb(es2, "gfin", [128, D], F32)
            yt = sb(es2, "yt", [128, 2, D], F32, nbuf=2)
            fj = sb(es2, "fj", [128, D], BF16)
            fs = sb(es2, "fs", [128, 2 * NT], F32)
            y_v = y_d.rearrange("(i p) d -> i p d", p=128)
            S.dma("sp", lambda h: h.dma_start(out=gfin[:], in_=rows_d[2:3, :].partition_broadcast(128)), writes=gfin.b)
            for g in range(4):
                for il in range(4):
                    i = 4 * g + il
                    S.op("act", lambda h, i=i: h.activation(out=fj[:], in_=X1[:, i, :], func=AF.Square, accum_out=fs[:, i:i + 1]), reads=[X1.b[i]], writes=fj.b + fs.b)
                S.op("act", lambda h, g=g: h.activation(out=fs[:, NT + 4 * g:NT + 4 * g + 4], in_=fs[:, 4 * g:4 * g + 4], func=AF.Sqrt, bias=EPS, scale=1.0 / D), reads=fs.b, writes=fs.b)
                S.op("dve", lambda h, g=g: h.reciprocal(fs[:, NT + 4 * g:NT + 4 * g + 4], fs[:, NT + 4 * g:NT + 4 * g + 4]), reads=fs.b, writes=fs.b)
                for il in range(4):
                    i = 4 * g + il
                    bi = i % 2
                    S.op("dve", lambda h, i=i, bi=bi: h.scalar_tensor_tensor(out=yt[:, bi, :], in0=X1[:, i, :], scalar=fs[:, NT + i:NT + i + 1], in1=gfin[:], op0=ALU.mult, op1=ALU.mult), reads=[X1.b[i]] + fs.b + gfin.b, writes=[yt.b[bi]])
                    S.dma("sp", lambda h, i=i, bi=bi: h.dma_start(out=y_v[i], in_=yt[:, bi, :]), reads=[yt.b[bi]], writes=[OUTB])
            S.wait_all("sp", [OUTB])
            S.emit()
    es_all.close()
    S.close()
    return nc, dbg_d


def _consts():
    identf = np.eye(128, dtype=np.float32)
    tri = np.zeros((128, 512), np.float32)
    s = np.arange(128)[:, None]
    t = np.arange(128)[None, :]
    tri[:, 0:128] = np.where(s > t, NEG, 0.0)
    p = np.arange(128)
    esel = np.zeros((128, 8, 128), np.float32)
    for g in range(8):
        esel[p, g, 16 * g + (p % 64) // 4] = 1.0
    slopes = np.exp2(-8.0 * np.arange(1, 9, dtype=np.float64) / 8).astype(np.float32)
    alk = np.zeros((128, 8, 16), np.float32)
    for di in range(16):
        dj = di - 3
        alk[:, :, di] = slopes[None, :] * (p[:, None] - 128.0 * dj)
    tl = np.arange(512)
    alq = np.zeros((2, 8, 512), np.float32)
    alq[0] = -8.0 * slopes[:, None] * (256.0 * (tl // 256))[None, :]
    alq[1] = -8.0 * slopes[:, None] * (tl % 256)[None, :]
    return dict(identf=identf, tri=tri, esel=esel.reshape(128, 1024), alibi_k=alk.reshape(128, 128), alibi_q=alq.reshape(2, 4096))


def _pk(v):
    return np.ascontiguousarray(np.asarray(v, np.float32).reshape(128, 8).T)


def _host_prep(inp):
    f = lambda a: np.ascontiguousarray(np.asarray(a, dtype=np.float32))
    x = f(inp["x"]); c = f(inp["c"])
    w_ada = f(inp["w_ada"][0]); b_ada = f(inp["b_ada"][0])
    w_in = f(inp["w_in"][0])
    cols = lambda a, b: w_in[:, a:b]
    aq, ak, av, iq, ik, iw, bq, bk, bv, bf = (cols(0, 512), cols(512, 640), cols(640, 768), cols(768, 1280), cols(1280, 1344),
                                              cols(1344, 1352), cols(1352, 1864), cols(1864, 2376), cols(2376, 2888), cols(2888, 2896))
    w_fm = np.concatenate([aq, ak[:, 0:64], ak[:, 0:64], ak[:, 64:128], ak[:, 64:128], iq, ik, ik, bq, bk, bf], axis=1)
    assert w_fm.shape[1] == 2440
    w_tm = np.concatenate([bv, av, iw[:, [0, 2, 4, 6, 1, 3, 5, 7]]], axis=1)
    w_rt = np.concatenate([f(inp["w_group"][0])] + [f(inp["w_router"][0][g]) for g in range(4)], axis=1)
    brt = np.concatenate([f(inp["b_group"][0]), f(inp["b_router"][0]).reshape(-1)])[None, :]
    g_out = np.concatenate([f(inp["g_out_a"][0]), f(inp["g_out_b"][0])])
    rows = np.stack([b_ada[2048:3072], b_ada[5120:6144], f(inp["g_final"])])
    shared = dict(rows=np.ascontiguousarray(rows), brt=np.ascontiguousarray(brt), w_ada=w_ada, w_fm=np.ascontiguousarray(w_fm),
                  w_tm=np.ascontiguousarray(w_tm), nbf=f(inp["b_forget"][0]).reshape(8, 1), w_out=f(inp["w_out"][0]),
                  w_rt=np.ascontiguousarray(w_rt), w_gate=f(inp["w_gate"][0]), w_up=f(inp["w_up"][0]), w_down=f(inp["w_down"][0]))
    shared.update(_consts())
    sm_common = [_pk(b_ada[0:1024]), _pk(b_ada[1024:2048]), _pk(b_ada[3072:4096]), _pk(b_ada[4096:5120]),
                 _pk(inp["g_mix"][0]), _pk(inp["g_ffn"][0]), g_out.reshape(8, 128)]
    in_maps = []
    for b in range(8):
        m = dict(shared)
        m["x"] = x[b]
        m["smalls"] = np.ascontiguousarray(np.concatenate([_pk(c[b])] + sm_common, axis=0))
        in_maps.append(m)
    return in_maps


_CACHE = {}


def kernel(**inputs):
    in_maps = _host_prep(inputs)
    if "nc" not in _CACHE:
        _CACHE["nc"] = build_program(dbg=False)[0]
    res = run_bass_kernel_spmd(_CACHE["nc"], in_maps, core_ids=list(range(8)))
    return np.stack([np.asarray(r["y"], dtype=np.float32) for r in res.results], axis=0)
```

```python
from contextlib import ExitStack
import numpy as np
import concourse.bass as bass
import concourse.mybir as mybir
from concourse.bass_utils import run_bass_kernel_spmd

F32 = mybir.dt.float32
BF16 = mybir.dt.bfloat16
AF = mybir.ActivationFunctionType
ALU = mybir.AluOpType
AX = mybir.AxisListType

D = 1024
L = 2048
NT = 16
NE = 32
DFF = 256
EPS = 1e-6
NEG = -30000.0
NBIS = 16


class Buf:
    __slots__ = ("name", "lw", "rd")

    def __init__(self, name=""):
        self.name = name
        self.lw = None
        self.rd = []


class Sched:
    ENGS = ("pe", "act", "dve", "pool", "sp")

    def __init__(self, nc, n_dma_sems=32):
        self.nc = nc
        self.prog = {e: [] for e in self.ENGS}
        self.cnt = {e: 0 for e in self.ENGS}
        self.seen = {e: {} for e in self.ENGS}
        self.pend_rd = {e: [] for e in self.ENGS}
        self.pend_wr = {e: [] for e in self.ENGS}
        self.sems = {}
        self.n_dma = n_dma_sems
        self.dma_val = [0] * n_dma_sems
        self.dma_rr = 0
        self.dma_rr2 = [0, 0]
        self._ctx = []

    def open(self):
        nc = self.nc
        for e in self.ENGS:
            cm = nc.semaphore("s_" + e)
            self.sems[e] = cm.__enter__()
            self._ctx.append(cm)
        for i in range(self.n_dma):
            cm = nc.semaphore("s_dma%d" % i)
            self.sems[("dma", i)] = cm.__enter__()
            self._ctx.append(cm)

    def close(self):
        for cm in reversed(self._ctx):
            cm.__exit__(None, None, None)

    def _wait(self, eng, tok):
        if tok is None:
            return
        key, val = tok
        if self.seen[eng].get(key, 0) >= val:
            return
        self.seen[eng][key] = val
        sem = self.sems[key]
        self.prog[eng].append(lambda h, sem=sem, val=val: h.wait_ge(sem, val))

    def _deps(self, eng, reads, writes, pe_acc=False):
        for b in reads:
            self._wait(eng, b.lw)
        for b in writes:
            if not (pe_acc and b.lw is not None and b.lw[0] == "pe"):
                self._wait(eng, b.lw)
            for t in b.rd:
                self._wait(eng, t)

    def op(self, eng, fn, reads=(), writes=(), inc=True, pe_acc=False):
        reads = list(reads)
        writes = list(writes)
        self._deps(eng, reads, writes, pe_acc=pe_acc)
        if not inc:
            self.pend_rd[eng].extend(reads)
            self.pend_wr[eng].extend(writes)
            self.prog[eng].append(lambda h, fn=fn: fn(h))
            return
        self.cnt[eng] += 1
        tok = (eng, self.cnt[eng])
        sem = self.sems[eng]
        self.prog[eng].append(lambda h, fn=fn, sem=sem: fn(h).then_inc(sem, 1))
        for b in reads + self.pend_rd[eng]:
            b.rd.append(tok)
        for b in writes + self.pend_wr[eng]:
            b.lw = tok
            b.rd = []
        self.pend_rd[eng] = []
        self.pend_wr[eng] = []

    def dma(self, eng, fn, reads=(), writes=()):
        reads = list(reads)
        writes = list(writes)
        self._deps(eng, reads, writes)
        half = self.n_dma // 2
        qi = 0 if eng == "sp" else 1
        i = qi * half + self.dma_rr2[qi]
        self.dma_rr2[qi] = (self.dma_rr2[qi] + 1) % half
        key = ("dma", i)
        if self.dma_val[i] > 0:
            self._wait(eng, (key, self.dma_val[i]))
        self.dma_val[i] += 16
        tok = (key, self.dma_val[i])
        sem = self.sems[key]
        self.prog[eng].append(lambda h, fn=fn, sem=sem: fn(h).then_inc(sem, 16))
        for b in reads:
            b.rd.append(tok)
        for b in writes:
            b.lw = tok
            b.rd = []
        return tok

    def wait_all(self, eng, bufs):
        for b in bufs:
            self._wait(eng, b.lw)
            for t in b.rd:
                self._wait(eng, t)

    def emit(self):
        nc = self.nc
        for i in range(self.n_dma):
            if self.dma_val[i] > 0:
                self._wait("sp", (("dma", i), self.dma_val[i]))
        prog = self.prog
        with nc.Block() as block:
            @block.tensor
            def _(h):
                for f in prog["pe"]:
                    f(h)

            @block.scalar
            def _(h):
                for f in prog["act"]:
                    f(h)

            @block.vector
            def _(h):
                for f in prog["dve"]:
                    f(h)

            @block.gpsimd
            def _(h):
                for f in prog["pool"]:
                    f(h)

            @block.sync
            def _(h):
                for f in prog["sp"]:
                    f(h)
        self.prog = {e: [] for e in self.ENGS}


class Tn:
    def __init__(self, t, nbuf=1, name=""):
        self.t = t
        self.b = [Buf("%s%d" % (name, i)) for i in range(nbuf)]

    def __getitem__(self, k):
        return self.t[k]


def build_program(dbg=False, stop_after=99):
    nc = bass.Bass("TRN2", target_bir_lowering=False)
    S = Sched(nc)

    def din(name, shape, dt=F32):
        return nc.dram_tensor(name, list(shape), dt, kind="ExternalInput").ap()

    x_d = din("x", [L, D])
    smalls_d = din("smalls", [64, 128])
    rows_d = din("rows", [3, D])
    brt_d = din("brt", [1, 36])
    wada_d = din("w_ada", [D, 6 * D])
    wfm_d = din("w_fm", [4 * 128, 8 * 640])
    wtm_d = din("w_tm", [D, 648])
    nbf_d = din("nbf", [8, 1])
    wout_d = din("w_out", [D, D])
    wrt_d = din("w_rt", [D, 36])
    wg_d = din("w_gate", [NE, D, DFF])
    wu_d = din("w_up", [NE, D, DFF])
    wd_d = din("w_down", [NE, DFF, D])
    identf_d = din("identf", [128, 128])
    tri_d = din("tri", [128, 512])
    esel_d = din("esel", [128, 1024])
    alk_d = din("alibi_k", [128, 128])
    alq_d = din("alibi_q", [2, 8 * 512])
    y_d = nc.dram_tensor("y", [L, D], F32, kind="ExternalOutput").ap()
    iwe_d = nc.dram_tensor("iw_e", [128, 64], F32, kind="Internal").ap()
    iwo_d = nc.dram_tensor("iw_o", [128, 64], F32, kind="Internal").ap()
    combT_d = nc.dram_tensor("combT", [NE, L], BF16, kind="Internal").ap()
    dbg_d = {}

    def dbg_out(name, shape, dt=F32):
        dbg_d[name] = nc.dram_tensor("dbg_" + name, list(shape), dt, kind="ExternalOutput").ap()
        return dbg_d[name]

    S.open()
    es_all = ExitStack()

    def sb(es, name, shape, dt=F32, nbuf=1):
        t = es.enter_context(nc.sbuf_tensor("sb_" + name, list(shape), dt))
        return Tn(t, nbuf, name)

    def ps(es, name, shape, dt=F32, nbuf=1):
        t = es.enter_context(nc.psum_tensor("ps_" + name, list(shape), dt))
        return Tn(t, nbuf, name)

    OUTB = Buf("out")

    def phase_end(n):
        if stop_after == n:
            S.wait_all("sp", [OUTB])
            S.emit()
            return True
        S.emit()
        return False

    P = es_all
    HT = sb(P, "HT", [128, 8, L], BF16, nbuf=8)
    identb = sb(P, "identb", [128, 128], BF16)
    identf = sb(P, "identf", [128, 128], F32)
    smT = sb(P, "smT", [128, 64], F32)
    modp = sb(P, "modp", [128, 64], F32)
    gate_m = sb(P, "gate_m", [128, D], F32)
    gate_f = sb(P, "gate_f", [128, D], F32)
    scb = sb(P, "scb", [128, 8], BF16)
    screp = sb(P, "screp", [128, 8, 128], BF16)
    PSB = [ps(P, "psb%d" % i, [128, 512], F32) for i in range(8)]

    def psv_bf(i):
        return PSB[i].t[:].bitcast(BF16)

    es_dsa = ExitStack()
    QTa = sb(es_dsa, "QTa", [128, 4, L], BF16, nbuf=4)
    KTa = sb(es_dsa, "KTa", [128, 2, L], BF16, nbuf=2)
    IQT = sb(es_dsa, "IQT", [128, L, 4], BF16, nbuf=1)
    IKT = sb(es_dsa, "IKT", [128, L], BF16)
    Va = sb(es_dsa, "Va", [128, NT, 2, 65], BF16)
    wcolT = sb(es_dsa, "wcolT", [128, 128], F32)

    S.dma("sp", lambda h: h.dma_start(out=identf[:], in_=identf_d), writes=identf.b)
    S.dma("pool", lambda h: h.dma_start(out=identb[:], in_=identf_d), writes=identb.b)
    S.op("pool", lambda h: h.memset(Va[:, :, :, 64:65], 1.0), writes=Va.b)

    def norm_to_HT(es, src_tile_fn, a0, b0, tag):
        xn = sb(es, "xn" + tag, [128, 4, D], BF16, nbuf=4)
        junk = sb(es, "junk" + tag, [128, D], BF16)
        ss = sb(es, "ss" + tag, [128, NT], F32)
        rstd = sb(es, "rstd" + tag, [128, NT], F32)
        for g in range(4):
            srcs = [src_tile_fn(g * 4 + il) for il in range(4)]
            for il in range(4):
                i = g * 4 + il
                src, sbufs = srcs[il]
                S.op("act", lambda h, src=src, i=i: h.activation(out=junk[:], in_=src, func=AF.Square, accum_out=ss[:, i:i + 1]), reads=sbufs, writes=junk.b + ss.b)
            S.op("act", lambda h, g=g: h.activation(out=rstd[:, 4 * g:4 * g + 4], in_=ss[:, 4 * g:4 * g + 4], func=AF.Sqrt, bias=EPS, scale=1.0 / D), reads=ss.b, writes=rstd.b)
            S.op("dve", lambda h, g=g: h.reciprocal(rstd[:, 4 * g:4 * g + 4], rstd[:, 4 * g:4 * g + 4]), reads=rstd.b, writes=rstd.b)
            for il in range(4):
                i = g * 4 + il
                src, sbufs = srcs[il]
                S.op("dve", lambda h, src=src, i=i, il=il: h.tensor_scalar(out=xn[:, il, :], in0=src, scalar1=rstd[:, i:i + 1], scalar2=None, op0=ALU.mult), reads=sbufs + rstd.b, writes=[xn.b[il]])
            for kk in range(8):
                bank = PSB[kk // 2]
                off = (kk % 2) * 512
                for il in range(4):
                    S.op("pe", lambda h, bank=bank, off=off, il=il, kk=kk: h.transpose(psv_bf(PSB.index(bank))[:, off + il * 128: off + (il + 1) * 128], xn[:, il, kk::8], identb[:]),
                         reads=[xn.b[il]] + identb.b, writes=bank.b, inc=(il == 3), pe_acc=True)
                dst = HT[:, kk, g * 512:(g + 1) * 512]
                srcp = psv_bf(kk // 2)[:, off:off + 512]
                if kk % 2 == 0:
                    S.op("act", lambda h, dst=dst, srcp=srcp, kk=kk: h.activation(out=dst, in_=srcp, func=AF.Identity, scale=modp[:, a0 + kk:a0 + kk + 1], bias=modp[:, b0 + kk:b0 + kk + 1]),
                         reads=bank.b + modp.b, writes=[HT.b[kk]])
                else:
                    S.op("dve", lambda h, dst=dst, srcp=srcp, kk=kk: h.tensor_scalar(out=dst, in0=srcp, scalar1=modp[:, a0 + kk:a0 + kk + 1], scalar2=modp[:, b0 + kk:b0 + kk + 1], op0=ALU.mult, op1=ALU.add),
                         reads=bank.b + modp.b, writes=[HT.b[kk]])
        return rstd

    wada_v = wada_d.rearrange("(p k) n -> p k n", k=8)

    def mod_load(wa, bi, piece):
        S.dma("pool", lambda h: h.dma_start(out=wa[:, bi, :, :], in_=wada_v[:, :, piece * D:(piece + 1) * D]), writes=[wa.b[bi]])

    def mod_vec_piece(wa, bi, sl, bank):
        for kk in range(8):
            for k2 in range(8):
                S.op("pe", lambda h, kk=kk, k2=k2: h.matmul(bank[:, sl * 8 + kk:sl * 8 + kk + 1], lhsT=wa[:, bi, k2, kk::8], rhs=scb[:, k2:k2 + 1], start=(k2 == 0), stop=(k2 == 7)),
                     reads=[wa.b[bi]] + scb.b, writes=bank.b, inc=(k2 == 7 and kk == 7), pe_acc=True)

    def mod_gate_piece(wa, bi, gt, gi, brow, banks):
        for half in range(2):
            bank = banks[half]
            for k2 in range(8):
                S.op("pe", lambda h, bank=bank, k2=k2, half=half: h.matmul(bank[:, :], lhsT=screp[:, k2, :], rhs=wa[:, bi, k2, half * 512:(half + 1) * 512], start=(k2 == 0), stop=(k2 == 7)),
                     reads=[wa.b[bi]] + screp.b, writes=bank.b, inc=(k2 == 7), pe_acc=True)
            S.op("dve", lambda h, bank=bank, half=half: h.tensor_tensor(out=gt[:, half * 512:(half + 1) * 512], in0=bank[:, :], in1=brow[:, gi, half * 512:(half + 1) * 512], op=ALU.add),
                 reads=bank.b + [brow.b[gi]], writes=gt.b)

    with ExitStack() as es:
        sm_in = sb(es, "sm_in", [64, 128], F32)
        wa = sb(es, "wa", [128, 2, 8, D], BF16, nbuf=2)
        sc32 = sb(es, "sc32", [128, 8], F32)
        S.dma("sp", lambda h: h.dma_start(out=sm_in[:], in_=smalls_d), writes=sm_in.b)
        mod_load(wa, 0, 0)
        mod_load(wa, 1, 1)
        S.op("pe", lambda h: h.transpose(PSB[0][:, 0:64], sm_in[:], identf[0:64, 0:64]), reads=sm_in.b + identf.b, writes=PSB[0].b)
        S.op("dve", lambda h: h.tensor_copy(smT[:], PSB[0][:, 0:64]), reads=PSB[0].b, writes=smT.b)
        S.op("act", lambda h: h.activation(out=sc32[:], in_=smT[:, 0:8], func=AF.Silu), reads=smT.b, writes=sc32.b)
        S.op("dve", lambda h: h.tensor_copy(scb[:], sc32[:]), reads=sc32.b, writes=scb.b)
        S.op("dve", lambda h: h.tensor_copy(screp[:], sc32[:].unsqueeze(2).to_broadcast([128, 8, 128])), reads=sc32.b, writes=screp.b)
        mod_vec_piece(wa, 0, 0, PSB[1])
        mod_vec_piece(wa, 1, 1, PSB[1])
        S.op("dve", lambda h: h.tensor_tensor(out=modp[:, 32:48], in0=PSB[1][:, 0:16], in1=smT[:, 8:24], op=ALU.add), reads=PSB[1].b + smT.b, writes=modp.b)
        S.op("dve", lambda h: h.scalar_tensor_tensor(out=modp[:, 0:8], in0=modp[:, 40:48], scalar=1.0, in1=smT[:, 40:48], op0=ALU.add, op1=ALU.mult), reads=modp.b + smT.b, writes=modp.b)
        S.op("dve", lambda h: h.tensor_copy(modp[:, 8:16], modp[:, 32:40]), reads=modp.b, writes=modp.b)
        xt = sb(es, "xt", [128, 4, D], F32, nbuf=4)
        x_v = x_d.rearrange("(i p) d -> i p d", p=128)

        def src1(i):
            bi = i % 4
            S.dma("sp", lambda h, i=i, bi=bi: h.dma_start(out=xt[:, bi, :], in_=x_v[i]), writes=[xt.b[bi]])
            return xt[:, bi, :], [xt.b[bi]]

        norm_to_HT(es, src1, 0, 8, "1")
        if dbg:
            d3 = dbg_out("hT", [128, 8 * L], BF16)
            S.dma("sp", lambda h: h.dma_start(out=d3, in_=HT[:].rearrange("p k t -> p (k t)")), reads=HT.b, writes=[OUTB])
        S.emit()

    def deferred_mod(es):
        wa2 = sb(es, "wa2", [128, 2, 8, D], BF16, nbuf=2)
        brow = sb(es, "brow", [128, 1, D], F32, nbuf=1)
        S.dma("sp", lambda h: h.dma_start(out=brow[:, 0, :], in_=rows_d[0:1, :].partition_broadcast(128)), writes=[brow.b[0]])
        mod_load(wa2, 0, 3)
        mod_load(wa2, 1, 4)

        def stage1():
            mod_vec_piece(wa2, 0, 0, PSB[5])
            mod_vec_piece(wa2, 1, 1, PSB[5])
            S.op("dve", lambda h: h.tensor_tensor(out=modp[:, 48:64], in0=PSB[5][:, 0:16], in1=smT[:, 24:40], op=ALU.add), reads=PSB[5].b + smT.b, writes=modp.b)
            S.op("dve", lambda h: h.scalar_tensor_tensor(out=modp[:, 16:24], in0=modp[:, 56:64], scalar=1.0, in1=smT[:, 48:56], op0=ALU.add, op1=ALU.mult), reads=modp.b + smT.b, writes=modp.b)
            S.op("dve", lambda h: h.tensor_copy(modp[:, 24:32], modp[:, 48:56]), reads=modp.b, writes=modp.b)
            mod_load(wa2, 0, 2)
            mod_load(wa2, 1, 5)

        def stage2():
            mod_gate_piece(wa2, 0, gate_m, 0, brow, (PSB[4], PSB[5]))
            S.dma("sp", lambda h: h.dma_start(out=brow[:, 0, :], in_=rows_d[1:2, :].partition_broadcast(128)), writes=[brow.b[0]])
            mod_gate_piece(wa2, 1, gate_f, 0, brow, (PSB[4], PSB[5]))
        return stage1, stage2

    es_fox = ExitStack()
    QTb = sb(es_fox, "QTb", [128, 4, L], BF16, nbuf=4)
    KTb = sb(es_fox, "KTb", [128, 4, L], BF16, nbuf=4)
    Vb = sb(es_fox, "Vb", [128, NT, 8, 65], BF16)
    cum3 = sb(es_fox, "cum3", [67, 3, L], BF16)
    csT = sb(es_fox, "csT", [128, NT, 8], F32)
    S.op("pool", lambda h: h.memset(Vb[:, :, :, 64:65], 1.0), writes=Vb.b)

    with ExitStack() as es:
        wf = sb(es, "wf", [128, 2, 8, 640], BF16, nbuf=2)
        wt = sb(es, "wt", [128, 8, 648], BF16, nbuf=8)
        iw_tm = sb(es, "iw_tm", [128, NT, 8], F32)
        e1 = sb(es, "e1", [8, L], F32)
        nbf = sb(es, "nbf", [8, 1], F32)
        bfg = sb(es, "bfg", [8, 1], F32)
        wfm_v = wfm_d.rearrange("(q p) (k c) -> q p k c", p=128, c=640)
        wtm_v = wtm_d.rearrange("(p k) n -> p k n", k=8)
        for kk in range(8):
            S.dma("pool", lambda h, kk=kk: h.dma_start(out=wt[:, kk, :], in_=wtm_v[:, kk, :]), writes=[wt.b[kk]])
        S.dma("sp", lambda h: h.dma_start(out=bfg[:], in_=nbf_d), writes=bfg.b)
        S.op("dve", lambda h: h.tensor_scalar(out=nbf[:], in0=bfg[:], scalar1=-1.0, scalar2=None, op0=ALU.mult), reads=bfg.b, writes=nbf.b)
        dests = []
        for p_ in range(4):
            dests.append((QTa, p_))
        dests += [(KTa, 0), (KTa, 1)]
        for p_ in range(4):
            dests.append((IQT, p_))
        dests.append((IKT, None))
        for p_ in range(4):
            dests.append((QTb, p_))
        for p_ in range(4):
            dests.append((KTb, p_))
        pieces = [(0, 5), (5, 10), (10, 15), (15, 19)]
        cnt_ = {'ev': 0, 'pb': 0}
        def fm_part():
            ev = 0
            pb = 0
            for pi, (c0, c1) in enumerate(pieces):
                bi = pi % 2
                ncol = (c1 - c0) * 128
                S.dma("pool", lambda h, bi=bi, pi=pi: h.dma_start(out=wf[:, bi, :, :], in_=wfm_v[pi]), writes=[wf.b[bi]])
                for ch in range(c0, c1):
                    T_, idx = dests[ch]
                    for ng in range(4):
                        bank = PSB[4 + (pb % 4)]
                        pb += 1
                        for kk in range(8):
                            S.op("pe", lambda h, bank=bank, bi=bi, kk=kk, ch=ch, c0=c0, ng=ng: h.matmul(bank[:, :], lhsT=wf[:, bi, kk, (ch - c0) * 128:(ch - c0 + 1) * 128], rhs=HT[:, kk, ng * 512:(ng + 1) * 512], start=(kk == 0), stop=(kk == 7)),
                                 reads=[wf.b[bi], HT.b[kk]], writes=bank.b, inc=(kk == 7), pe_acc=True)
                        if idx is None:
                            dst = T_[:, ng * 512:(ng + 1) * 512]
                            wb_ = T_.b
                        elif T_ is IQT:
                            dst = T_[:, ng * 512:(ng + 1) * 512, idx]
                            wb_ = T_.b
                        else:
                            dst = T_[:, idx, ng * 512:(ng + 1) * 512]
                            wb_ = [T_.b[idx]]
                        if ev % 2 == 0:
                            S.op("act", lambda h, dst=dst, bank=bank: h.activation(out=dst, in_=bank[:, :], func=AF.Copy), reads=bank.b, writes=wb_)
                        else:
                            S.op("dve", lambda h, dst=dst, bank=bank: h.tensor_copy(dst, bank[:, :]), reads=bank.b, writes=wb_)
                        ev += 1
        wbf = sb(es, "wbf", [128, 8, 8], BF16)
        S.dma("pool", lambda h: h.dma_start(out=wbf[:], in_=wfm_v[3][:, :, 512:520]), writes=wbf.b)

        def bf_part():
            for ng in range(4):
                bank = PSB[4 + ng]
                for kk in range(8):
                    S.op("pe", lambda h, bank=bank, kk=kk, ng=ng: h.matmul(bank[0:8, :], lhsT=wbf[:, kk, :], rhs=HT[:, kk, ng * 512:(ng + 1) * 512], start=(kk == 0), stop=(kk == 7)),
                         reads=wbf.b + [HT.b[kk]], writes=bank.b, inc=(kk == 7), pe_acc=True)
                S.op("act", lambda h, bank=bank, ng=ng: h.activation(out=e1[:, ng * 512:(ng + 1) * 512], in_=bank[0:8, :], func=AF.Exp, scale=-1.0, bias=nbf[:, 0:1]), reads=bank.b + nbf.b, writes=e1.b)
        def tm_part():
            for i in range(NT):
                bA = PSB[(2 * i) % 4]
                bB = PSB[(2 * i + 1) % 4]
                for kk in range(8):
                    S.op("pe", lambda h, bA=bA, kk=kk, i=i: h.matmul(bA[:, :], lhsT=HT[:, kk, i * 128:(i + 1) * 128], rhs=wt[:, kk, 0:512], start=(kk == 0), stop=(kk == 7)),
                         reads=[HT.b[kk], wt.b[kk]], writes=bA.b, inc=(kk == 7), pe_acc=True)
                for kk in range(8):
                    S.op("pe", lambda h, bB=bB, kk=kk, i=i: h.matmul(bB[:, 0:136], lhsT=HT[:, kk, i * 128:(i + 1) * 128], rhs=wt[:, kk, 512:648], start=(kk == 0), stop=(kk == 7)),
                         reads=[HT.b[kk], wt.b[kk]], writes=bB.b, inc=(kk == 7), pe_acc=True)
                S.op("act", lambda h, bA=bA, i=i: h.activation(out=Vb[:, i, :, 0:64], in_=bA[:, :].rearrange("p (h d) -> p h d", h=8), func=AF.Copy), reads=bA.b, writes=Vb.b)
                S.op("dve", lambda h, bB=bB, i=i: h.tensor_copy(Va[:, i, :, 0:64], bB[:, 0:128].rearrange("p (h d) -> p h d", h=2)), reads=bB.b, writes=Va.b)
                S.op("dve", lambda h, bB=bB, i=i: h.tensor_copy(iw_tm[:, i, :], bB[:, 128:136]), reads=bB.b, writes=iw_tm.b)
        tm_part()
        bf_part()
        S.op("act", lambda h: h.activation(out=e1[:], in_=e1[:], func=AF.Ln, bias=1.0, scale=1.0), reads=e1.b, writes=e1.b)
        cs = sb(es, "cs", [8, L], F32)
        S.op("dve", lambda h: h.tensor_tensor_scan(out=cs[:], data0=e1[:], data1=e1[:], initial=0.0, op0=ALU.add, op1=ALU.max), reads=e1.b, writes=cs.b)
        for j in range(NT):
            S.op("pe", lambda h, j=j: h.transpose(PSB[4][:, j * 8:(j + 1) * 8], cs[:, j * 128:(j + 1) * 128], identf[0:8, 0:8]), reads=cs.b + identf.b, writes=PSB[4].b, inc=(j == NT - 1), pe_acc=True)
        S.op("dve", lambda h: h.tensor_copy(csT[:].rearrange("p j h -> p (j h)"), PSB[4][:, 0:128]), reads=PSB[4].b, writes=csT.b)
        hi3 = Tn(e1.t, 2, "hi3")
        hi3v = e1[:].bitcast(BF16).rearrange("p (a t) -> p a t", a=2)
        S.op("dve", lambda h: h.tensor_scalar(out=cs[:], in0=cs[:], scalar1=-8.0, scalar2=None, op0=ALU.mult), reads=cs.b, writes=cs.b)
        for r in range(3):
            S.op("dve", lambda h, r=r: h.tensor_copy(hi3v[:, r % 2, :], cs[:]), reads=cs.b, writes=[hi3.b[r % 2]] + (e1.b if r < 2 else []))
            if r < 2:
                S.op("dve", lambda h, r=r: h.tensor_tensor(out=cs[:], in0=cs[:], in1=hi3v[:, r % 2, :], op=ALU.subtract), reads=cs.b + [hi3.b[r % 2]], writes=cs.b)
            for hh in range(8):
                pp = 32 * (hh % 3) + r
                S.dma("sp", lambda h, hh=hh, pp=pp, r=r: h.dma_start(out=cum3[pp:pp + 1, hh // 3, :], in_=hi3v[hh:hh + 1, r % 2, :]), reads=[hi3.b[r % 2]], writes=cum3.b)
        iwe_flat = iwe_d.rearrange("g c -> (g c)").rearrange("(i t b) -> t i b", i=16, t=128, b=4)
        iwo_flat = iwo_d.rearrange("g c -> (g c)").rearrange("(i t b) -> t i b", i=16, t=128, b=4)
        SCR = Buf("scr")
        S.dma("sp", lambda h: h.dma_start(out=iwe_flat, in_=iw_tm[:, :, 0:4]), reads=iw_tm.b, writes=[SCR])
        S.dma("sp", lambda h: h.dma_start(out=iwo_flat, in_=iw_tm[:, :, 4:8]), reads=iw_tm.b, writes=[SCR])
        wc_in = sb(es, "wc_in", [128, 128], F32)
        S.dma("sp", lambda h: h.dma_start(out=wc_in[:, 0:64], in_=iwe_d), reads=[SCR], writes=wc_in.b)
        S.dma("sp", lambda h: h.dma_start(out=wc_in[:, 64:128], in_=iwo_d), reads=[SCR], writes=wc_in.b)
        S.op("pe", lambda h: h.transpose(PSB[5][:, 0:128], wc_in[:], identf[:]), reads=wc_in.b + identf.b, writes=PSB[5].b)
        S.op("dve", lambda h: h.tensor_copy(wcolT[:], PSB[5][:, 0:128]), reads=PSB[5].b, writes=wcolT.b)
        fm_part()
        if dbg:
            for nm, T_, shp, dt_ in [("QTa", QTa, [128, 4 * L], BF16), ("KTa", KTa, [128, 2 * L], BF16), ("IKT", IKT, [128, L], BF16),
                                     ("QTb", QTb, [128, 4 * L], BF16), ("KTb", KTb, [128, 4 * L], BF16), ("Vb", Vb, [128, NT * 8 * 65], BF16), ("Va", Va, [128, NT * 2 * 65], BF16),
                                     ("csT", csT, [128, NT * 8], F32), ("cum3", cum3, [67, 3 * L], BF16), ("wcolT", wcolT, [128, 128], F32)]:
                dd = dbg_out(nm, shp, dt_)
                nd = len(T_.t.shape)
                if nd == 2:
                    src_ap = T_[:]
                elif nd == 3:
                    src_ap = T_[:].rearrange("p a b -> p (a b)")
                else:
                    src_ap = T_[:].rearrange("p a b c -> p (a b c)")
                S.dma("sp", lambda h, dd=dd, src_ap=src_ap: h.dma_start(out=dd, in_=src_ap), reads=T_.b, writes=[OUTB])
        if phase_end(1):
            return nc, dbg_d

    def attn_scratch(es, tag):
        sc = {}
        sc["ones67"] = sb(es, "ones67" + tag, [67, 128], BF16)
        sc["PT"] = sb(es, "PT" + tag, [128, 4, 512], BF16, nbuf=4)
        sc["o_sb"] = sb(es, "o_sb" + tag, [128, 4, 512], F32)
        sc["ob"] = sb(es, "ob" + tag, [128, 4, 512], BF16)
        sc["rec"] = sb(es, "rec" + tag, [128, 8, 4], F32)
        sc["oss"] = sb(es, "oss" + tag, [128, 8], F32)
        sc["ojunk"] = sb(es, "ojunk" + tag, [128, 512], BF16)
        S.op("pool", lambda h: h.memset(sc["ones67"][:], 1.0), writes=sc["ones67"].b)
        return sc

    def attention_chunk(sc, c, KT, kt_idx, QT, aug_lhs, aug_rhs, mask_rhs, bias_ap, Vt, v_idx, tick=None):
        PT, o_sb, rec = sc["PT"], sc["o_sb"], sc["rec"]
        T0 = 512 * c
        nj = 4 * c + 4
        steps = [(pr, j) for pr in range(4) for j in range(nj)]

        def s_stage(k):
            pr, j = steps[k]
            col0 = max(0, j - 4 * c) * 128
            ncols = 512 - col0
            heads = (2 * pr, 2 * pr + 1)
            Sbs = (PSB[(2 * k) % 4], PSB[(2 * k + 1) % 4])
            for hh, Sb in zip(heads, Sbs):
                rows = slice(64 * (hh % 2), 64 * (hh % 2) + 64)
                S.op("pe", lambda h, hh=hh, Sb=Sb, rows=rows: h.matmul(Sb[:, 0:ncols], lhsT=KT[rows, kt_idx(hh), j * 128:(j + 1) * 128], rhs=QT[rows, pr, T0 + col0:T0 + 512], start=True, stop=False),
                     reads=[KT.b[kt_idx(hh)], QT.b[pr]], writes=Sb.b, inc=False, pe_acc=True)
            mr, mbufs = mask_rhs(j, col0, ncols)
            if mr is not None:
                for hh, Sb in zip(heads, Sbs):
                    S.op("pe", lambda h, Sb=Sb: h.matmul(Sb[:, 0:ncols], lhsT=identb[:], rhs=mr, start=False, stop=False),
                         reads=identb.b + mbufs, writes=Sb.b, inc=False, pe_acc=True)
            for hh, Sb in zip(heads, Sbs):
                al, albufs = aug_lhs(hh)
                ar, arbufs = aug_rhs(hh, col0)
                S.op("pe", lambda h, Sb=Sb, al=al, ar=ar: h.matmul(Sb[:, 0:ncols], lhsT=al, rhs=ar, start=False, stop=True),
                     reads=albufs + arbufs, writes=Sb.b, inc=True, pe_acc=True)
            for q_, (hh, Sb) in enumerate(zip(heads, Sbs)):
                pt = (2 * k + q_) % 4
                bap, bbufs = bias_ap(hh, j)
                S.op("act", lambda h, Sb=Sb, pt=pt, bap=bap: h.activation(out=PT[:, pt, 0:ncols], in_=Sb[:, 0:ncols], func=AF.Exp, scale=0.125, bias=bap),
                     reads=Sb.b + bbufs, writes=[PT.b[pt]])

        def pv_stage(k):
            pr, j = steps[k]
            col0 = max(0, j - 4 * c) * 128
            il0 = max(0, j - 4 * c)
            for q_ in range(2):
                hh = 2 * pr + q_
                pt = (2 * k + q_) % 4
                Ob = PSB[(6 if pr % 2 == 0 else 4) + q_]
                for il in range(il0, 4):
                    i = 4 * c + il
                    first = (j == 0 and il == il0)
                    S.op("pe", lambda h, il=il, i=i, first=first, Ob=Ob, pt=pt, hh=hh: h.matmul(Ob[:, il * 65:(il + 1) * 65], lhsT=PT[:, pt, il * 128 - col0:il * 128 - col0 + 128], rhs=Vt[:, j, v_idx(hh), :], start=first, stop=(j == i), skip_group_check=True),
                         reads=[PT.b[pt]] + Vt.b, writes=Ob.b, inc=(il == 3), pe_acc=True)
                if j == nj - 1:
                    Ov = Ob[:, 0:260].rearrange("p (a b) -> p a b", b=65)
                    S.op("dve", lambda h, Ov=Ov, hh=hh: h.reciprocal(rec[:, hh, :], Ov[:, :, 64]), reads=Ob.b, writes=rec.b)
                    S.op("dve", lambda h, Ov=Ov, hh=hh: h.tensor_tensor(out=o_sb[:, :, hh * 64:(hh + 1) * 64], in0=Ov[:, :, 0:64], in1=rec[:, hh, :].unsqueeze(2).to_broadcast([128, 4, 64]), op=ALU.mult),
                         reads=Ob.b + rec.b, writes=o_sb.b)

        n = len(steps)
        s_stage(0)
        for k in range(n):
            if k + 1 < n:
                s_stage(k + 1)
            pv_stage(k)
            if tick is not None:
                tick(k, n)

    def out_norm(sc, c, base):
        o_sb, ob, oss, ojunk = sc["o_sb"], sc["ob"], sc["oss"], sc["ojunk"]
        T0 = 512 * c
        for il in range(4):
            S.op("act", lambda h, il=il: h.activation(out=ojunk[:], in_=o_sb[:, il, :], func=AF.Square, accum_out=oss[:, il:il + 1]), reads=o_sb.b, writes=ojunk.b + oss.b)
        S.op("act", lambda h: h.activation(out=oss[:, 4:8], in_=oss[:, 0:4], func=AF.Sqrt, bias=EPS, scale=1.0 / 512), reads=oss.b, writes=oss.b)
        S.op("dve", lambda h: h.reciprocal(oss[:, 4:8], oss[:, 4:8]), reads=oss.b, writes=oss.b)
        S.op("dve", lambda h: h.tensor_tensor(out=ob[:], in0=o_sb[:], in1=oss[:, 4:8].unsqueeze(2).to_broadcast([128, 4, 512]), op=ALU.mult), reads=o_sb.b + oss.b, writes=ob.b)
        for fc in range(4):
            bank = PSB[4 + fc % 2]
            bi = 4 + fc % 2
            for il in range(4):
                S.op("pe", lambda h, bi=bi, il=il, fc=fc: h.transpose(psv_bf(bi)[:, il * 128:(il + 1) * 128], ob[:, il, fc * 128:(fc + 1) * 128], identb[:]),
                     reads=ob.b + identb.b, writes=bank.b, inc=(il == 3), pe_acc=True)
            S.op("act", lambda h, bi=bi, fc=fc: h.activation(out=HT[:, base + fc, T0:T0 + 512], in_=psv_bf(bi)[:, 0:512], func=AF.Identity, scale=smT[:, 56 + base + fc:57 + base + fc]),
                 reads=bank.b + smT.b, writes=[HT.b[base + fc]])

    with ExitStack() as es:
        sc = attn_scratch(es, "f")
        triw = sb(es, "triw", [128, 512], BF16)
        S.dma("pool", lambda h: h.dma_start(out=triw[:], in_=tri_d), writes=triw.b)
        mod_st1, mod_st2 = deferred_mod(es)
        for c in range(4):
            if c == 1:
                mod_st1()
            if c == 2:
                mod_st2()
            attention_chunk(
                sc, c, KTb, lambda hh: hh // 2, QTb,
                aug_lhs=lambda hh: (sc["ones67"][32 * (hh % 3):32 * (hh % 3) + 3, :], sc["ones67"].b),
                aug_rhs=lambda hh, col0, c=c: (cum3[32 * (hh % 3):32 * (hh % 3) + 3, hh // 3, 512 * c + col0:512 * c + 512], cum3.b),
                mask_rhs=lambda j, col0, ncols, c=c: ((triw[:, 0:ncols], triw.b) if j >= 4 * c else (None, [])),
                bias_ap=lambda hh, j: (csT[:, j, hh:hh + 1], csT.b),
                Vt=Vb, v_idx=lambda hh: hh)
            out_norm(sc, c, 4)
        if dbg:
            d4b = dbg_out("oTb", [128, 8 * L], BF16)
            S.dma("sp", lambda h: h.dma_start(out=d4b, in_=HT[:].rearrange("p k t -> p (k t)")), reads=HT.b, writes=[OUTB])
        if phase_end(2):
            return nc, dbg_d
    es_fox.close()

    with ExitStack() as es:
        sc = attn_scratch(es, "a")
        IS = sb(es, "IS", [128, 4, L], F32, nbuf=4)
        NM = sb(es, "NM", [128, L], BF16)
        NMT2 = sb(es, "NMT", [128, 2, NT, 512], BF16, nbuf=2)
        R = sb(es, "R", [128, 4, 512], BF16, nbuf=4)
        Wblk = sb(es, "Wblk", [128, 2, 8, 128], BF16, nbuf=2)
        esel = sb(es, "esel", [128, 8, 128], BF16)
        alk = sb(es, "alk", [128, 8, 16], F32)
        alq = sb(es, "alq", [66, 8, 512], BF16)
        bs = sb(es, "bs", [128, 32], F32)
        cjunk = sb(es, "cjunk", [128, L], BF16)
        S.dma("pool", lambda h: h.dma_start(out=esel[:].rearrange("p a b -> p (a b)"), in_=esel_d), writes=esel.b)
        S.dma("pool", lambda h: h.dma_start(out=alq[0:2].rearrange("p a b -> p (a b)"), in_=alq_d), writes=alq.b)
        S.dma("pool", lambda h: h.dma_start(out=alq[64:66].rearrange("p a b -> p (a b)"), in_=alq_d), writes=alq.b)
        S.dma("sp", lambda h: h.dma_start(out=alk[:].rearrange("p a b -> p (a b)"), in_=alk_d), writes=alk.b)
        evc = [0]

        def indexer(c):
            units = []
            for il in range(4):
                i = 4 * c + il
                ncols = 128 * (i + 1)
                for kc in range((ncols + 511) // 512):
                    for g in range(8):
                        units.append((il, kc, g))

            def dots(u):
                il, kc, g = units[u]
                i = 4 * c + il
                n = min(512, 128 * (i + 1) - 512 * kc)
                tok0 = 128 * i + 16 * g
                Db = PSB[u % 4]
                for hf in range(2):
                    rs = slice(64 * hf, 64 * hf + 64)
                    S.op("pe", lambda h, rs=rs: h.matmul(Db[rs, 0:n], lhsT=IQT[rs, tok0:tok0 + 16, :].rearrange("p t a -> p (t a)"), rhs=IKT[rs, 512 * kc:512 * kc + n], start=True, stop=True),
                         reads=IQT.b + IKT.b, writes=Db.b, inc=(hf == 1), pe_acc=True)
                if evc[0] % 4 != 3:
                    S.op("act", lambda h: h.activation(out=R[:, u % 4, 0:n], in_=Db[:, 0:n], func=AF.Relu), reads=Db.b, writes=[R.b[u % 4]])
                else:
                    S.op("dve", lambda h: h.tensor_scalar(out=R[:, u % 4, 0:n], in0=Db[:, 0:n], scalar1=0.0, scalar2=None, op0=ALU.max), reads=Db.b, writes=[R.b[u % 4]])
                evc[0] += 1

            def headsum(u):
                il, kc, g = units[u]
                i = 4 * c + il
                ncols = 128 * (i + 1)
                n = min(512, ncols - 512 * kc)
                ISb = PSB[4 + kc % 2]
                wb = i % 2
                if kc == 0 and g == 0:
                    S.op("pool", lambda h: h.tensor_tensor(out=Wblk[:, wb], in0=esel[:], in1=wcolT[:, 8 * i:8 * i + 8].unsqueeze(2).to_broadcast([128, 8, 128]), op=ALU.mult), reads=esel.b + wcolT.b, writes=[Wblk.b[wb]])
                S.op("pe", lambda h: h.matmul(ISb[:, 0:n], lhsT=Wblk[:, wb, g, :], rhs=R[:, u % 4, 0:n], start=(g == 0), stop=(g == 7)),
                     reads=[Wblk.b[wb], R.b[u % 4]], writes=ISb.b, inc=(g == 7), pe_acc=True)
                if g == 7:
                    S.op("act", lambda h: h.activation(out=IS[:, il, 512 * kc:512 * kc + n], in_=ISb[:, 0:n], func=AF.Copy), reads=ISb.b, writes=[IS.b[il]])
                    if 512 * kc + n == ncols:
                        S.op("dve", lambda h: h.tensor_reduce(out=bs[:, 20 + il:21 + il], in_=IS[:, il, 0:ncols], axis=AX.X, op=ALU.max, apply_absolute_value=True), reads=[IS.b[il]], writes=bs.b)
                        S.op("pool", lambda h: h.affine_select(out=IS[:, il, 128 * i:128 * i + 128], in_=IS[:, il, 128 * i:128 * i + 128], pattern=[[-1, 128]], compare_op=ALU.is_ge, fill=-1e30, base=0, channel_multiplier=1),
                             reads=[IS.b[il]], writes=[IS.b[il]])

            nu = len(units)
            dots(0)
            if nu > 1:
                dots(1)
            for u in range(nu):
                if u + 2 < nu:
                    dots(u + 2)
                headsum(u)

        def bisect_gen(c):
            S.op("dve", lambda h: h.tensor_scalar(out=bs[:, 0:4], in0=bs[:, 20:24], scalar1=-1.001, scalar2=-1e-3, op0=ALU.mult, op1=ALU.add), reads=bs.b, writes=bs.b)
            S.op("dve", lambda h: h.tensor_scalar(out=bs[:, 4:8], in0=bs[:, 20:24], scalar1=2.002, scalar2=2e-3, op0=ALU.mult, op1=ALU.add), reads=bs.b, writes=bs.b)
            for jb in range(1, NBIS + 1):
                stp = 2.0 ** (-jb)
                S.op("dve", lambda h, stp=stp: h.scalar_tensor_tensor(out=bs[:, 8:12], in0=bs[:, 4:8], scalar=stp, in1=bs[:, 0:4], op0=ALU.mult, op1=ALU.add), reads=bs.b, writes=bs.b)
                for il in range(4):
                    ncols = 128 * (4 * c + il + 1)
                    S.op("dve", lambda h, il=il, ncols=ncols: h.tensor_scalar(out=cjunk[:, 0:ncols], in0=IS[:, il, 0:ncols], scalar1=bs[:, 8 + il:9 + il], scalar2=0.0, op0=ALU.is_ge, op1=ALU.add, accum_out=bs[:, 12 + il:13 + il]),
                         reads=[IS.b[il]] + bs.b, writes=cjunk.b + bs.b)
                S.op("dve", lambda h, stp=stp: h.tensor_scalar(out=bs[:, 16:20], in0=bs[:, 12:16], scalar1=255.5, scalar2=stp, op0=ALU.is_ge, op1=ALU.mult), reads=bs.b, writes=bs.b)
                S.op("dve", lambda h: h.tensor_tensor(out=bs[:, 16:20], in0=bs[:, 16:20], in1=bs[:, 4:8], op=ALU.mult), reads=bs.b, writes=bs.b)
                S.op("dve", lambda h: h.tensor_tensor(out=bs[:, 0:4], in0=bs[:, 0:4], in1=bs[:, 16:20], op=ALU.add), reads=bs.b, writes=bs.b)
                yield

        def mask_epilogue(c):
            tb = 0
            nb_ = c % 2
            for il in range(4):
                i = 4 * c + il
                ncols = 128 * (i + 1)
                S.op("dve", lambda h, il=il, ncols=ncols: h.tensor_scalar(out=NM[:, 0:ncols], in0=IS[:, il, 0:ncols], scalar1=bs[:, il:il + 1], scalar2=NEG, op0=ALU.is_lt, op1=ALU.mult), reads=[IS.b[il]] + bs.b, writes=NM.b)
                for j0 in range(0, i + 1, 8):
                    nb = min(8, i + 1 - j0)
                    bi = 6 + tb % 2
                    tb += 1
                    for jj in range(nb):
                        S.op("pe", lambda h, bi=bi, jj=jj, j0=j0: h.transpose(psv_bf(bi)[:, jj * 128:(jj + 1) * 128], NM[:, (j0 + jj) * 128:(j0 + jj + 1) * 128], identb[:]),
                             reads=NM.b + identb.b, writes=PSB[bi].b, inc=(jj == nb - 1), pe_acc=True)
                    S.op("act", lambda h, bi=bi, j0=j0, nb=nb, il=il: h.activation(out=NMT2[:, nb_, j0:j0 + nb, il * 128:(il + 1) * 128], in_=psv_bf(bi)[:, 0:nb * 128].rearrange("p (a b) -> p a b", b=128), func=AF.Copy),
                         reads=PSB[bi].b, writes=[NMT2.b[nb_]])

        order = [3, 2, 1, 0]
        indexer(order[0])
        for _ in bisect_gen(order[0]):
            pass
        mask_epilogue(order[0])
        for oi, c in enumerate(order):
            gen = None
            cn = order[oi + 1] if oi + 1 < 4 else None
            if cn is not None:
                indexer(cn)
                gen = bisect_gen(cn)
            nsteps = 4 * (4 * c + 4)
            every = max(1, nsteps // (NBIS + 1))

            def tick(k, n, gen=gen, every=every):
                if gen is not None and k % every == 0:
                    next(gen, None)
            nb_ = c % 2
            attention_chunk(
                sc, c, KTa, lambda hh: hh // 4, QTa,
                aug_lhs=lambda hh: (sc["ones67"][64 * (hh % 2):64 * (hh % 2) + 2, :], sc["ones67"].b),
                aug_rhs=lambda hh, col0: (alq[64 * (hh % 2):64 * (hh % 2) + 2, hh, col0:512], alq.b),
                mask_rhs=lambda j, col0, ncols, nb_=nb_: (NMT2[:, nb_, j, col0:512], [NMT2.b[nb_]]),
                bias_ap=lambda hh, j, c=c: (alk[:, hh, 4 * c - j + 3:4 * c - j + 4], alk.b),
                Vt=Va, v_idx=lambda hh: hh // 4, tick=tick)
            if gen is not None:
                for _ in gen:
                    pass
                mask_epilogue(cn)
            out_norm(sc, c, 0)
        if dbg:
            for nm, T_, shp, dt_ in [("IS", IS, [128, 4 * L], F32), ("bs", bs, [128, 32], F32), ("osb", sc["o_sb"], [128, 4 * 512], F32)]:
                dd = dbg_out(nm, shp, dt_)
                src_ap = T_[:] if len(T_.t.shape) == 2 else T_[:].rearrange("p a b -> p (a b)")
                S.dma("sp", lambda h, dd=dd, src_ap=src_ap: h.dma_start(out=dd, in_=src_ap), reads=T_.b, writes=[OUTB])
            d4 = dbg_out("oT", [128, 8 * L], BF16)
            S.dma("sp", lambda h: h.dma_start(out=d4, in_=HT[:].rearrange("p k t -> p (k t)")), reads=HT.b, writes=[OUTB])
        if phase_end(3):
            return nc, dbg_d
    es_dsa.close()

    with ExitStack() as es:
        X1 = sb(es, "X1", [128, NT, D], F32, nbuf=NT)
        x_v = x_d.rearrange("(i p) d -> i p d", p=128)
        with ExitStack() as es2:
            wo = sb(es2, "wo", [128, 8, D], BF16, nbuf=8)
            xt2 = sb(es2, "xt2", [128, 2, D], F32, nbuf=2)
            wout_v = wout_d.rearrange("(c p) d -> p c d", p=128)
            for fc in range(8):
                S.dma("pool", lambda h, fc=fc: h.dma_start(out=wo[:, fc, :], in_=wout_v[:, fc, :]), writes=[wo.b[fc]])
            for fc in range(8):
                S.op("pool", lambda h, fc=fc: h.tensor_tensor(out=wo[:, fc, :], in0=wo[:, fc, :], in1=gate_m[:], op=ALU.mult), reads=[wo.b[fc]] + gate_m.b, writes=[wo.b[fc]])
            for i in range(NT):
                bi = i % 2
                S.dma("sp", lambda h, i=i, bi=bi: h.dma_start(out=xt2[:, bi, :], in_=x_v[i]), writes=[xt2.b[bi]])
                for half in range(2):
                    bank = PSB[(2 * i + half) % 4]
                    for fc in range(8):
                        S.op("pe", lambda h, bank=bank, fc=fc, i=i, half=half: h.matmul(bank[:, :], lhsT=HT[:, fc, i * 128:(i + 1) * 128], rhs=wo[:, fc, half * 512:(half + 1) * 512], start=(fc == 0), stop=(fc == 7)),
                             reads=[HT.b[fc], wo.b[fc]], writes=bank.b, inc=(fc == 7), pe_acc=True)
                    S.op("dve", lambda h, bank=bank, i=i, half=half, bi=bi: h.tensor_tensor(out=X1[:, i, half * 512:(half + 1) * 512], in0=bank[:, :], in1=xt2[:, bi, half * 512:(half + 1) * 512], op=ALU.add),
                         reads=bank.b + [xt2.b[bi]], writes=[X1.b[i]])
            if dbg:
                d5 = dbg_out("X1", [128, NT * D], F32)
                S.dma("sp", lambda h: h.dma_start(out=d5, in_=X1[:].rearrange("p a b -> p (a b)")), reads=X1.b, writes=[OUTB])
            if phase_end(4):
                return nc, dbg_d
        with ExitStack() as es2:
            norm_to_HT(es2, lambda i: (X1[:, i, :], [X1.b[i]]), 16, 24, "2")
            if phase_end(5):
                return nc, dbg_d
        with ExitStack() as es2:
            combT = sb(es2, "combT", [32, L], BF16)
            sel = sb(es2, "sel", [32, NE * 128], BF16)
            es3 = ExitStack()
            wr = sb(es3, "wr", [128, 8, 36], BF16)
            brt = sb(es3, "brt", [128, 36], F32)
            lg = sb(es3, "lg", [128, NT, 36], F32)
            rt = sb(es3, "rt", [128, NT, 64], F32)
            comb = sb(es3, "comb", [128, NT, 32], BF16)
            S.dma("pool", lambda h: h.dma_start(out=wr[:], in_=wrt_d.rearrange("(p k) n -> p k n", k=8)), writes=wr.b)
            S.dma("sp", lambda h: h.dma_start(out=brt[:], in_=brt_d.partition_broadcast(128)), writes=brt.b)
            for i in range(NT):
                bank = PSB[i % 4]
                for kk in range(8):
                    S.op("pe", lambda h, bank=bank, kk=kk, i=i: h.matmul(bank[:, 0:36], lhsT=HT[:, kk, i * 128:(i + 1) * 128], rhs=wr[:, kk, :], start=(kk == 0), stop=(kk == 7)),
                         reads=[HT.b[kk]] + wr.b, writes=bank.b, inc=(kk == 7), pe_acc=True)
                S.op("dve", lambda h, bank=bank, i=i: h.tensor_tensor(out=lg[:, i, :], in0=bank[:, 0:36], in1=brt[:], op=ALU.add), reads=bank.b + brt.b, writes=lg.b)
            RB = rt.b + lg.b

            def dv(fn):
                S.op("dve", fn, reads=RB, writes=RB)

            def bc(ap, n):
                return ap.unsqueeze(2).to_broadcast([128, NT, n])
            gl = lg[:, :, 0:4]
            gmax, gsum, pg, m1, m2, w1, w2 = (rt[:, :, k] for k in range(7))
            ohg, gsh, el, tmp8, oh1, oh2, el2 = rt[:, :, 8:12], rt[:, :, 12:16], rt[:, :, 16:24], rt[:, :, 24:32], rt[:, :, 32:40], rt[:, :, 40:48], rt[:, :, 48:56]
            c8 = rt[:, :, 56:64]
            dv(lambda h: h.tensor_reduce(out=gmax, in_=gl, axis=AX.X, op=ALU.max))
            dv(lambda h: h.tensor_tensor(out=ohg, in0=gl, in1=bc(gmax, 4), op=ALU.is_ge))
            dv(lambda h: h.tensor_tensor(out=gsh, in0=gl, in1=bc(gmax, 4), op=ALU.subtract))
            S.op("act", lambda h: h.activation(out=gsh, in_=gsh, func=AF.Exp), reads=RB, writes=RB)
            dv(lambda h: h.tensor_reduce(out=gsum, in_=gsh, axis=AX.X, op=ALU.add))
            dv(lambda h: h.reciprocal(pg, gsum))
            for g in range(4):
                src_e = lg[:, :, 4 + 8 * g:12 + 8 * g]
                if g == 0:
                    dv(lambda h, src_e=src_e, g=g: h.tensor_tensor(out=el, in0=src_e, in1=bc(ohg[:, :, g], 8), op=ALU.mult))
                else:
                    dv(lambda h, src_e=src_e, g=g: h.tensor_tensor(out=tmp8, in0=src_e, in1=bc(ohg[:, :, g], 8), op=ALU.mult))
                    dv(lambda h: h.tensor_tensor(out=el, in0=el, in1=tmp8, op=ALU.add))
            dv(lambda h: h.tensor_reduce(out=m1, in_=el, axis=AX.X, op=ALU.max))
            dv(lambda h: h.tensor_tensor(out=oh1, in0=el, in1=bc(m1, 8), op=ALU.is_ge))
            dv(lambda h: h.scalar_tensor_tensor(out=el2, in0=oh1, scalar=-1e30, in1=el, op0=ALU.mult, op1=ALU.add))
            dv(lambda h: h.tensor_reduce(out=m2, in_=el2, axis=AX.X, op=ALU.max))
            dv(lambda h: h.tensor_tensor(out=oh2, in0=el2, in1=bc(m2, 8), op=ALU.is_ge))
            dv(lambda h: h.tensor_tensor(out=w2, in0=m2, in1=m1, op=ALU.subtract))
            S.op("act", lambda h: h.activation(out=w2, in_=w2, func=AF.Exp), reads=RB, writes=RB)
            dv(lambda h: h.tensor_scalar(out=w1, in0=w2, scalar1=1.0, scalar2=None, op0=ALU.add))
            dv(lambda h: h.reciprocal(w1, w1))
            dv(lambda h: h.tensor_tensor(out=w2, in0=w2, in1=w1, op=ALU.mult))
            dv(lambda h: h.tensor_tensor(out=w1, in0=w1, in1=pg, op=ALU.mult))
            dv(lambda h: h.tensor_tensor(out=w2, in0=w2, in1=pg, op=ALU.mult))
            dv(lambda h: h.tensor_tensor(out=c8, in0=oh1, in1=bc(w1, 8), op=ALU.mult))
            dv(lambda h: h.tensor_tensor(out=tmp8, in0=oh2, in1=bc(w2, 8), op=ALU.mult))
            dv(lambda h: h.tensor_tensor(out=c8, in0=c8, in1=tmp8, op=ALU.add))
            for g in range(4):
                S.op("dve", lambda h, g=g: h.tensor_tensor(out=comb[:, :, 8 * g:8 * g + 8], in0=c8, in1=bc(ohg[:, :, g], 8), op=ALU.mult), reads=RB, writes=comb.b)
            for i in range(NT):
                bi = 4 + (i // 8)
                S.op("pe", lambda h, bi=bi, i=i: h.transpose(psv_bf(bi)[0:32, (i % 8) * 128:(i % 8 + 1) * 128], comb[:, i, :], identb[:]), reads=comb.b + identb.b, writes=PSB[bi].b, inc=(i % 8 == 7), pe_acc=True)
            for hb in range(2):
                S.op("dve", lambda h, hb=hb: h.tensor_copy(combT[:, hb * 1024:(hb + 1) * 1024], psv_bf(4 + hb)[0:32, :]), reads=PSB[4 + hb].b, writes=combT.b)
            S.op("dve", lambda h: h.tensor_copy(sel[:].rearrange("k (e p) -> k e p", p=128), identb[0:32, 0:32].unsqueeze(2).to_broadcast([32, NE, 128])), reads=identb.b, writes=sel.b)
            S.emit()
            es3.close()
            wgu = sb(es2, "wgu", [128, 3, 2, 8, DFF], BF16, nbuf=3)
            wdn = sb(es2, "wdn", [128, 4, 2, D], BF16, nbuf=4)
            cbb = sb(es2, "cbb", [128, 2, L], BF16, nbuf=2)
            aT = sb(es2, "aT", [128, 4, 2, L], BF16, nbuf=4)
            sl = sb(es2, "sl", [128, 2, 512], BF16, nbuf=2)
            t1 = sb(es2, "t1", [128, 2, 512], BF16, nbuf=2)
            q = 0
            def load_gu(e):
                b = e % 3
                S.dma("pool", lambda h: h.dma_start(out=wgu[:, b, 0], in_=wg_d[e].rearrange("(p k) f -> p k f", k=8)), writes=[wgu.b[b]])
                S.dma("pool", lambda h: h.dma_start(out=wgu[:, b, 1], in_=wu_d[e].rearrange("(p k) f -> p k f", k=8)), writes=[wgu.b[b]])
            load_gu(0)
            load_gu(1)
            for rnd in range(8):
                for er in range(4):
                    e = 4 * rnd + er
                    b = e % 2
                    wb3 = e % 3
                    if e + 2 < NE:
                        load_gu(e + 2)
                    S.dma("pool", lambda h, e=e, er=er: h.dma_start(out=wdn[:, er], in_=wd_d[e].rearrange("(c p) d -> p c d", p=128)), writes=[wdn.b[er]])
                    for tcn in range(4):
                        cbank = PSB[4 + tcn]
                        S.op("pe", lambda h, cbank=cbank, e=e, tcn=tcn: h.matmul(cbank[:, :], lhsT=sel[:, e * 128:(e + 1) * 128], rhs=combT[:, tcn * 512:(tcn + 1) * 512], start=True, stop=True),
                             reads=sel.b + combT.b, writes=cbank.b)
                        if tcn % 2 == 0:
                            S.op("act", lambda h, cbank=cbank, b=b, tcn=tcn: h.activation(out=cbb[:, b, tcn * 512:(tcn + 1) * 512], in_=cbank[:, :], func=AF.Copy), reads=cbank.b, writes=[cbb.b[b]])
                        else:
                            S.op("dve", lambda h, cbank=cbank, b=b, tcn=tcn: h.tensor_copy(cbb[:, b, tcn * 512:(tcn + 1) * 512], cbank[:, :]), reads=cbank.b, writes=[cbb.b[b]])
                    for tcn in range(4):
                        for fc in range(2):
                            Gb = PSB[(2 * q) % 4]
                            Ub = PSB[(2 * q + 1) % 4]
                            qb = q % 2
                            q += 1
                            for gu, bank in ((0, Gb), (1, Ub)):
                                for kk in range(8):
                                    S.op("pe", lambda h, bank=bank, gu=gu, kk=kk, wb3=wb3, fc=fc, tcn=tcn: h.matmul(bank[:, :], lhsT=wgu[:, wb3, gu, kk, fc * 128:(fc + 1) * 128], rhs=HT[:, kk, tcn * 512:(tcn + 1) * 512], start=(kk == 0), stop=(kk == 7)),
                                         reads=[wgu.b[wb3], HT.b[kk]], writes=bank.b, inc=(kk == 7), pe_acc=True)
                            S.op("act", lambda h, Gb=Gb, qb=qb: h.activation(out=sl[:, qb, :], in_=Gb[:, :], func=AF.Silu), reads=Gb.b, writes=[sl.b[qb]])
                            S.op("dve", lambda h, Ub=Ub, qb=qb: h.tensor_tensor(out=t1[:, qb, :], in0=Ub[:, :], in1=sl[:, qb, :], op=ALU.mult), reads=Ub.b + [sl.b[qb]], writes=[t1.b[qb]])
                            S.op("pool", lambda h, qb=qb, er=er, fc=fc, tcn=tcn, b=b: h.tensor_tensor(out=aT[:, er, fc, tcn * 512:(tcn + 1) * 512], in0=t1[:, qb, :], in1=cbb[:, b, tcn * 512:(tcn + 1) * 512], op=ALU.mult),
                                 reads=[t1.b[qb], cbb.b[b]], writes=[aT.b[er]])
                    S.op("pool", lambda h, er=er: h.tensor_tensor(out=wdn[:, er], in0=wdn[:, er], in1=gate_f[:].unsqueeze(1).to_broadcast([128, 2, D]), op=ALU.mult), reads=[wdn.b[er]] + gate_f.b, writes=[wdn.b[er]])
                for i in range(NT):
                    for half in range(2):
                        bank = PSB[4 + (2 * i + half) % 4]
                        for er in range(4):
                            for fc in range(2):
                                S.op("pe", lambda h, bank=bank, er=er, fc=fc, i=i, half=half: h.matmul(bank[:, :], lhsT=aT[:, er, fc, i * 128:(i + 1) * 128], rhs=wdn[:, er, fc, half * 512:(half + 1) * 512], start=(er == 0 and fc == 0), stop=(er == 3 and fc == 1)),
                                     reads=[aT.b[er], wdn.b[er]], writes=bank.b, inc=(er == 3 and fc == 1), pe_acc=True)
                        S.op("dve", lambda h, bank=bank, i=i, half=half: h.tensor_tensor(out=X1[:, i, half * 512:(half + 1) * 512], in0=bank[:, :], in1=X1[:, i, half * 512:(half + 1) * 512], op=ALU.add),
                             reads=bank.b + [X1.b[i]], writes=[X1.b[i]])
            if phase_end(6):
                return nc, dbg_d
        with ExitStack() as es2:
            gfin = sb(es2, "gfin", [128, D], F32)
            yt = sb(es2, "yt", [128, 2, D], F32, nbuf=2)
            fj = sb(es2, "fj", [128, D], BF16)
            fs = sb(es2, "fs", [128, 2 * NT], F32)
            y_v = y_d.rearrange("(i p) d -> i p d", p=128)
            S.dma("sp", lambda h: h.dma_start(out=gfin[:], in_=rows_d[2:3, :].partition_broadcast(128)), writes=gfin.b)
            for g in range(4):
                for il in range(4):
                    i = 4 * g + il
                    S.op("act", lambda h, i=i: h.activation(out=fj[:], in_=X1[:, i, :], func=AF.Square, accum_out=fs[:, i:i + 1]), reads=[X1.b[i]], writes=fj.b + fs.b)
                S.op("act", lambda h, g=g: h.activation(out=fs[:, NT + 4 * g:NT + 4 * g + 4], in_=fs[:, 4 * g:4 * g + 4], func=AF.Sqrt, bias=EPS, scale=1.0 / D), reads=fs.b, writes=fs.b)
                S.op("dve", lambda h, g=g: h.reciprocal(fs[:, NT + 4 * g:NT + 4 * g + 4], fs[:, NT + 4 * g:NT + 4 * g + 4]), reads=fs.b, writes=fs.b)
                for il in range(4):
                    i = 4 * g + il
                    bi = i % 2
                    S.op("dve", lambda h, i=i, bi=bi: h.scalar_tensor_tensor(out=yt[:, bi, :], in0=X1[:, i, :], scalar=fs[:, NT + i:NT + i + 1], in1=gfin[:], op0=ALU.mult, op1=ALU.mult), reads=[X1.b[i]] + fs.b + gfin.b, writes=[yt.b[bi]])
                    S.dma("sp", lambda h, i=i, bi=bi: h.dma_start(out=y_v[i], in_=yt[:, bi, :]), reads=[yt.b[bi]], writes=[OUTB])
            S.wait_all("sp", [OUTB])
            S.emit()
    es_all.close()
    S.close()
    return nc, dbg_d


def _consts():
    identf = np.eye(128, dtype=np.float32)
    tri = np.zeros((128, 512), np.float32)
    s = np.arange(128)[:, None]
    t = np.arange(128)[None, :]
    tri[:, 0:128] = np.where(s > t, NEG, 0.0)
    p = np.arange(128)
    esel = np.zeros((128, 8, 128), np.float32)
    for g in range(8):
        esel[p, g, 16 * g + (p % 64) // 4] = 1.0
    slopes = np.exp2(-8.0 * np.arange(1, 9, dtype=np.float64) / 8).astype(np.float32)
    alk = np.zeros((128, 8, 16), np.float32)
    for di in range(16):
        dj = di - 3
        alk[:, :, di] = slopes[None, :] * (p[:, None] - 128.0 * dj)
    tl = np.arange(512)
    alq = np.zeros((2, 8, 512), np.float32)
    alq[0] = -8.0 * slopes[:, None] * (256.0 * (tl // 256))[None, :]
    alq[1] = -8.0 * slopes[:, None] * (tl % 256)[None, :]
    return dict(identf=identf, tri=tri, esel=esel.reshape(128, 1024), alibi_k=alk.reshape(128, 128), alibi_q=alq.reshape(2, 4096))


def _pk(v):
    return np.ascontiguousarray(np.asarray(v, np.float32).reshape(128, 8).T)


def _host_prep(inp):
    f = lambda a: np.ascontiguousarray(np.asarray(a, dtype=np.float32))
    x = f(inp["x"]); c = f(inp["c"])
    w_ada = f(inp["w_ada"][0]); b_ada = f(inp["b_ada"][0])
    w_in = f(inp["w_in"][0])
    cols = lambda a, b: w_in[:, a:b]
    aq, ak, av, iq, ik, iw, bq, bk, bv, bf = (cols(0, 512), cols(512, 640), cols(640, 768), cols(768, 1280), cols(1280, 1344),
                                              cols(1344, 1352), cols(1352, 1864), cols(1864, 2376), cols(2376, 2888), cols(2888, 2896))
    w_fm = np.concatenate([aq, ak[:, 0:64], ak[:, 0:64], ak[:, 64:128], ak[:, 64:128], iq, ik, ik, bq, bk, bf], axis=1)
    assert w_fm.shape[1] == 2440
    w_fm_pad = np.zeros((1024, 4 * 640), np.float32)
    w_fm_pad[:, :2440] = w_fm
    w_fm = np.ascontiguousarray(w_fm_pad.reshape(128, 8, 4, 640).transpose(2, 0, 1, 3).reshape(4 * 128, 8 * 640))
    w_tm = np.concatenate([bv, av, iw[:, [0, 2, 4, 6, 1, 3, 5, 7]]], axis=1)
    w_rt = np.concatenate([f(inp["w_group"][0])] + [f(inp["w_router"][0][g]) for g in range(4)], axis=1)
    brt = np.concatenate([f(inp["b_group"][0]), f(inp["b_router"][0]).reshape(-1)])[None, :]
    g_out = np.concatenate([f(inp["g_out_a"][0]), f(inp["g_out_b"][0])])
    rows = np.stack([b_ada[2048:3072], b_ada[5120:6144], f(inp["g_final"])])
    shared = dict(rows=np.ascontiguousarray(rows), brt=np.ascontiguousarray(brt), w_ada=w_ada, w_fm=np.ascontiguousarray(w_fm),
                  w_tm=np.ascontiguousarray(w_tm), nbf=f(inp["b_forget"][0]).reshape(8, 1), w_out=f(inp["w_out"][0]),
                  w_rt=np.ascontiguousarray(w_rt), w_gate=f(inp["w_gate"][0]), w_up=f(inp["w_up"][0]), w_down=f(inp["w_down"][0]))
    shared.update(_consts())
    sm_common = [_pk(b_ada[0:1024]), _pk(b_ada[1024:2048]), _pk(b_ada[3072:4096]), _pk(b_ada[4096:5120]),
                 _pk(inp["g_mix"][0]), _pk(inp["g_ffn"][0]), g_out.reshape(8, 128)]
    in_maps = []
    for b in range(8):
        m = dict(shared)
        m["x"] = x[b]
        m["smalls"] = np.ascontiguousarray(np.concatenate([_pk(c[b])] + sm_common, axis=0))
        in_maps.append(m)
    return in_maps


_CACHE = {}


def kernel(**inputs):
    in_maps = _host_prep(inputs)
    if "nc" not in _CACHE:
        _CACHE["nc"] = build_program(dbg=False)[0]
    res = run_bass_kernel_spmd(_CACHE["nc"], in_maps, core_ids=list(range(8)))
    return np.stack([np.asarray(r["y"], dtype=np.float32) for r in res.results], axis=0)
```

```python
from contextlib import ExitStack
import numpy as np
import concourse.bass as bass
import concourse.mybir as mybir
from concourse.bass_utils import run_bass_kernel_spmd

F32 = mybir.dt.float32
BF16 = mybir.dt.bfloat16
AF = mybir.ActivationFunctionType
ALU = mybir.AluOpType
AX = mybir.AxisListType

D = 1024
L = 2048
NT = 16
NE = 32
DFF = 256
EPS = 1e-6
NEG = -30000.0
NBIS = 16


class Buf:
    __slots__ = ("name", "lw", "rd")

    def __init__(self, name=""):
        self.name = name
        self.lw = None
        self.rd = []


class Sched:
    ENGS = ("pe", "act", "dve", "pool", "sp")

    def __init__(self, nc, n_dma_sems=32):
        self.nc = nc
        self.prog = {e: [] for e in self.ENGS}
        self.cnt = {e: 0 for e in self.ENGS}
        self.seen = {e: {} for e in self.ENGS}
        self.pend_rd = {e: [] for e in self.ENGS}
        self.pend_wr = {e: [] for e in self.ENGS}
        self.sems = {}
        self.n_dma = n_dma_sems
        self.dma_val = [0] * n_dma_sems
        self.dma_rr = 0
        self.dma_rr2 = [0, 0]
        self._ctx = []

    def open(self):
        nc = self.nc
        for e in self.ENGS:
            cm = nc.semaphore("s_" + e)
            self.sems[e] = cm.__enter__()
            self._ctx.append(cm)
        for i in range(self.n_dma):
            cm = nc.semaphore("s_dma%d" % i)
            self.sems[("dma", i)] = cm.__enter__()
            self._ctx.append(cm)

    def close(self):
        for cm in reversed(self._ctx):
            cm.__exit__(None, None, None)

    def _wait(self, eng, tok):
        if tok is None:
            return
        key, val = tok
        if self.seen[eng].get(key, 0) >= val:
            return
        self.seen[eng][key] = val
        sem = self.sems[key]
        self.prog[eng].append(lambda h, sem=sem, val=val: h.wait_ge(sem, val))

    def _deps(self, eng, reads, writes, pe_acc=False):
        for b in reads:
            self._wait(eng, b.lw)
        for b in writes:
            if not (pe_acc and b.lw is not None and b.lw[0] == "pe"):
                self._wait(eng, b.lw)
            for t in b.rd:
                self._wait(eng, t)

    def op(self, eng, fn, reads=(), writes=(), inc=True, pe_acc=False):
        reads = list(reads)
        writes = list(writes)
        self._deps(eng, reads, writes, pe_acc=pe_acc)
        if not inc:
            self.pend_rd[eng].extend(reads)
            self.pend_wr[eng].extend(writes)
            self.prog[eng].append(lambda h, fn=fn: fn(h))
            return
        self.cnt[eng] += 1
        tok = (eng, self.cnt[eng])
        sem = self.sems[eng]
        self.prog[eng].append(lambda h, fn=fn, sem=sem: fn(h).then_inc(sem, 1))
        for b in reads + self.pend_rd[eng]:
            b.rd.append(tok)
        for b in writes + self.pend_wr[eng]:
            b.lw = tok
            b.rd = []
        self.pend_rd[eng] = []
        self.pend_wr[eng] = []

    def dma(self, eng, fn, reads=(), writes=()):
        reads = list(reads)
        writes = list(writes)
        self._deps(eng, reads, writes)
        half = self.n_dma // 2
        qi = 0 if eng == "sp" else 1
        i = qi * half + self.dma_rr2[qi]
        self.dma_rr2[qi] = (self.dma_rr2[qi] + 1) % half
        key = ("dma", i)
        if self.dma_val[i] > 0:
            self._wait(eng, (key, self.dma_val[i]))
        self.dma_val[i] += 16
        tok = (key, self.dma_val[i])
        sem = self.sems[key]
        self.prog[eng].append(lambda h, fn=fn, sem=sem: fn(h).then_inc(sem, 16))
        for b in reads:
            b.rd.append(tok)
        for b in writes:
            b.lw = tok
            b.rd = []
        return tok

    def wait_all(self, eng, bufs):
        for b in bufs:
            self._wait(eng, b.lw)
            for t in b.rd:
                self._wait(eng, t)

    def emit(self):
        nc = self.nc
        for i in range(self.n_dma):
            if self.dma_val[i] > 0:
                self._wait("sp", (("dma", i), self.dma_val[i]))
        prog = self.prog
        with nc.Block() as block:
            @block.tensor
            def _(h):
                for f in prog["pe"]:
                    f(h)

            @block.scalar
            def _(h):
                for f in prog["act"]:
                    f(h)

            @block.vector
            def _(h):
                for f in prog["dve"]:
                    f(h)

            @block.gpsimd
            def _(h):
                for f in prog["pool"]:
                    f(h)

            @block.sync
            def _(h):
                for f in prog["sp"]:
                    f(h)
        self.prog = {e: [] for e in self.ENGS}


class Tn:
    def __init__(self, t, nbuf=1, name=""):
        self.t = t
        self.b = [Buf("%s%d" % (name, i)) for i in range(nbuf)]

    def __getitem__(self, k):
        return self.t[k]


def build_program(dbg=False, stop_after=99):
    nc = bass.Bass("TRN2", target_bir_lowering=False)
    S = Sched(nc)

    def din(name, shape, dt=F32):
        return nc.dram_tensor(name, list(shape), dt, kind="ExternalInput").ap()

    x_d = din("x", [L, D])
    smalls_d = din("smalls", [64, 128])
    rows_d = din("rows", [3, D])
    brt_d = din("brt", [1, 36])
    wada_d = din("w_ada", [D, 6 * D])
    wfm_d = din("w_fm", [4 * 128, 8 * 640])
    wtm_d = din("w_tm", [D, 648])
    nbf_d = din("nbf", [8, 1])
    wout_d = din("w_out", [D, D])
    wrt_d = din("w_rt", [D, 36])
    wg_d = din("w_gate", [NE, D, DFF])
    wu_d = din("w_up", [NE, D, DFF])
    wd_d = din("w_down", [NE, DFF, D])
    identf_d = din("identf", [128, 128])
    tri_d = din("tri", [128, 512])
    esel_d = din("esel", [128, 1024])
    alk_d = din("alibi_k", [128, 128])
    alq_d = din("alibi_q", [2, 8 * 512])
    y_d = nc.dram_tensor("y", [L, D], F32, kind="ExternalOutput").ap()
    iwe_d = nc.dram_tensor("iw_e", [128, 64], F32, kind="Internal").ap()
    iwo_d = nc.dram_tensor("iw_o", [128, 64], F32, kind="Internal").ap()
    combT_d = nc.dram_tensor("combT", [NE, L], BF16, kind="Internal").ap()
    dbg_d = {}

    def dbg_out(name, shape, dt=F32):
        dbg_d[name] = nc.dram_tensor("dbg_" + name, list(shape), dt, kind="ExternalOutput").ap()
        return dbg_d[name]

    S.open()
    es_all = ExitStack()

    def sb(es, name, shape, dt=F32, nbuf=1):
        t = es.enter_context(nc.sbuf_tensor("sb_" + name, list(shape), dt))
        return Tn(t, nbuf, name)

    def ps(es, name, shape, dt=F32, nbuf=1):
        t = es.enter_context(nc.psum_tensor("ps_" + name, list(shape), dt))
        return Tn(t, nbuf, name)

    OUTB = Buf("out")

    def phase_end(n):
        if stop_after == n:
            S.wait_all("sp", [OUTB])
            S.emit()
            return True
        S.emit()
        return False

    P = es_all
    HT = sb(P, "HT", [128, 8, L], BF16, nbuf=8)
    identb = sb(P, "identb", [128, 128], BF16)
    identf = sb(P, "identf", [128, 128], F32)
    smT = sb(P, "smT", [128, 64], F32)
    modp = sb(P, "modp", [128, 64], F32)
    gate_m = sb(P, "gate_m", [128, D], F32)
    gate_f = sb(P, "gate_f", [128, D], F32)
    scb = sb(P, "scb", [128, 8], BF16)
    screp = sb(P, "screp", [128, 8, 128], BF16)
    PSB = [ps(P, "psb%d" % i, [128, 512], F32) for i in range(8)]

    def psv_bf(i):
        return PSB[i].t[:].bitcast(BF16)

    es_dsa = ExitStack()
    QTa = sb(es_dsa, "QTa", [128, 4, L], BF16, nbuf=4)
    KTa = sb(es_dsa, "KTa", [128, 2, L], BF16, nbuf=2)
    IQT = sb(es_dsa, "IQT", [128, L, 4], BF16, nbuf=1)
    IKT = sb(es_dsa, "IKT", [128, L], BF16)
    Va = sb(es_dsa, "Va", [128, NT, 2, 65], BF16)
    wcolT = sb(es_dsa, "wcolT", [128, 128], F32)

    S.dma("sp", lambda h: h.dma_start(out=identf[:], in_=identf_d), writes=identf.b)
    S.dma("pool", lambda h: h.dma_start(out=identb[:], in_=identf_d), writes=identb.b)
    S.op("pool", lambda h: h.memset(Va[:, :, :, 64:65], 1.0), writes=Va.b)

    def norm_to_HT(es, src_tile_fn, a0, b0, tag):
        xn = sb(es, "xn" + tag, [128, 4, D], BF16, nbuf=4)
        junk = sb(es, "junk" + tag, [128, D], BF16)
        ss = sb(es, "ss" + tag, [128, NT], F32)
        rstd = sb(es, "rstd" + tag, [128, NT], F32)
        for g in range(4):
            srcs = [src_tile_fn(g * 4 + il) for il in range(4)]
            for il in range(4):
                i = g * 4 + il
                src, sbufs = srcs[il]
                S.op("act", lambda h, src=src, i=i: h.activation(out=junk[:], in_=src, func=AF.Square, accum_out=ss[:, i:i + 1]), reads=sbufs, writes=junk.b + ss.b)
            S.op("act", lambda h, g=g: h.activation(out=rstd[:, 4 * g:4 * g + 4], in_=ss[:, 4 * g:4 * g + 4], func=AF.Sqrt, bias=EPS, scale=1.0 / D), reads=ss.b, writes=rstd.b)
            S.op("dve", lambda h, g=g: h.reciprocal(rstd[:, 4 * g:4 * g + 4], rstd[:, 4 * g:4 * g + 4]), reads=rstd.b, writes=rstd.b)
            for il in range(4):
                i = g * 4 + il
                src, sbufs = srcs[il]
                S.op("dve", lambda h, src=src, i=i, il=il: h.tensor_scalar(out=xn[:, il, :], in0=src, scalar1=rstd[:, i:i + 1], scalar2=None, op0=ALU.mult), reads=sbufs + rstd.b, writes=[xn.b[il]])
            for kk in range(8):
                bank = PSB[kk // 2]
                off = (kk % 2) * 512
                for il in range(4):
                    S.op("pe", lambda h, bank=bank, off=off, il=il, kk=kk: h.transpose(psv_bf(PSB.index(bank))[:, off + il * 128: off + (il + 1) * 128], xn[:, il, kk::8], identb[:]),
                         reads=[xn.b[il]] + identb.b, writes=bank.b, inc=(il == 3), pe_acc=True)
                dst = HT[:, kk, g * 512:(g + 1) * 512]
                srcp = psv_bf(kk // 2)[:, off:off + 512]
                if kk % 2 == 0:
                    S.op("act", lambda h, dst=dst, srcp=srcp, kk=kk: h.activation(out=dst, in_=srcp, func=AF.Identity, scale=modp[:, a0 + kk:a0 + kk + 1], bias=modp[:, b0 + kk:b0 + kk + 1]),
                         reads=bank.b + modp.b, writes=[HT.b[kk]])
                else:
                    S.op("dve", lambda h, dst=dst, srcp=srcp, kk=kk: h.tensor_scalar(out=dst, in0=srcp, scalar1=modp[:, a0 + kk:a0 + kk + 1], scalar2=modp[:, b0 + kk:b0 + kk + 1], op0=ALU.mult, op1=ALU.add),
                         reads=bank.b + modp.b, writes=[HT.b[kk]])
        return rstd

    wada_v = wada_d.rearrange("(p k) n -> p k n", k=8)

    def mod_load(wa, bi, piece):
        S.dma("pool", lambda h: h.dma_start(out=wa[:, bi, :, :], in_=wada_v[:, :, piece * D:(piece + 1) * D]), writes=[wa.b[bi]])

    def mod_vec_piece(wa, bi, sl, bank):
        for kk in range(8):
            for k2 in range(8):
                S.op("pe", lambda h, kk=kk, k2=k2: h.matmul(bank[:, sl * 8 + kk:sl * 8 + kk + 1], lhsT=wa[:, bi, k2, kk::8], rhs=scb[:, k2:k2 + 1], start=(k2 == 0), stop=(k2 == 7)),
                     reads=[wa.b[bi]] + scb.b, writes=bank.b, inc=(k2 == 7 and kk == 7), pe_acc=True)

    def mod_gate_piece(wa, bi, gt, gi, brow, banks):
        for half in range(2):
            bank = banks[half]
            for k2 in range(8):
                S.op("pe", lambda h, bank=bank, k2=k2, half=half: h.matmul(bank[:, :], lhsT=screp[:, k2, :], rhs=wa[:, bi, k2, half * 512:(half + 1) * 512], start=(k2 == 0), stop=(k2 == 7)),
                     reads=[wa.b[bi]] + screp.b, writes=bank.b, inc=(k2 == 7), pe_acc=True)
            S.op("dve", lambda h, bank=bank, half=half: h.tensor_tensor(out=gt[:, half * 512:(half + 1) * 512], in0=bank[:, :], in1=brow[:, gi, half * 512:(half + 1) * 512], op=ALU.add),
                 reads=bank.b + [brow.b[gi]], writes=gt.b)

    with ExitStack() as es:
        sm_in = sb(es, "sm_in", [64, 128], F32)
        wa = sb(es, "wa", [128, 2, 8, D], BF16, nbuf=2)
        sc32 = sb(es, "sc32", [128, 8], F32)
        S.dma("sp", lambda h: h.dma_start(out=sm_in[:], in_=smalls_d), writes=sm_in.b)
        mod_load(wa, 0, 0)
        mod_load(wa, 1, 1)
        S.op("pe", lambda h: h.transpose(PSB[0][:, 0:64], sm_in[:], identf[0:64, 0:64]), reads=sm_in.b + identf.b, writes=PSB[0].b)
        S.op("dve", lambda h: h.tensor_copy(smT[:], PSB[0][:, 0:64]), reads=PSB[0].b, writes=smT.b)
        S.op("act", lambda h: h.activation(out=sc32[:], in_=smT[:, 0:8], func=AF.Silu), reads=smT.b, writes=sc32.b)
        S.op("dve", lambda h: h.tensor_copy(scb[:], sc32[:]), reads=sc32.b, writes=scb.b)
        S.op("dve", lambda h: h.tensor_copy(screp[:], sc32[:].unsqueeze(2).to_broadcast([128, 8, 128])), reads=sc32.b, writes=screp.b)
        mod_vec_piece(wa, 0, 0, PSB[1])
        mod_vec_piece(wa, 1, 1, PSB[1])
        S.op("dve", lambda h: h.tensor_tensor(out=modp[:, 32:48], in0=PSB[1][:, 0:16], in1=smT[:, 8:24], op=ALU.add), reads=PSB[1].b + smT.b, writes=modp.b)
        S.op("dve", lambda h: h.scalar_tensor_tensor(out=modp[:, 0:8], in0=modp[:, 40:48], scalar=1.0, in1=smT[:, 40:48], op0=ALU.add, op1=ALU.mult), reads=modp.b + smT.b, writes=modp.b)
        S.op("dve", lambda h: h.tensor_copy(modp[:, 8:16], modp[:, 32:40]), reads=modp.b, writes=modp.b)
        xt = sb(es, "xt", [128, 4, D], F32, nbuf=4)
        x_v = x_d.rearrange("(i p) d -> i p d", p=128)

        def src1(i):
            bi = i % 4
            S.dma("sp", lambda h, i=i, bi=bi: h.dma_start(out=xt[:, bi, :], in_=x_v[i]), writes=[xt.b[bi]])
            return xt[:, bi, :], [xt.b[bi]]

        norm_to_HT(es, src1, 0, 8, "1")
        if dbg:
            d3 = dbg_out("hT", [128, 8 * L], BF16)
            S.dma("sp", lambda h: h.dma_start(out=d3, in_=HT[:].rearrange("p k t -> p (k t)")), reads=HT.b, writes=[OUTB])
        S.emit()

    def deferred_mod(es):
        wa2 = sb(es, "wa2", [128, 2, 8, D], BF16, nbuf=2)
        brow = sb(es, "brow", [128, 1, D], F32, nbuf=1)
        S.dma("sp", lambda h: h.dma_start(out=brow[:, 0, :], in_=rows_d[0:1, :].partition_broadcast(128)), writes=[brow.b[0]])
        mod_load(wa2, 0, 3)
        mod_load(wa2, 1, 4)

        def stage1():
            mod_vec_piece(wa2, 0, 0, PSB[5])
            mod_vec_piece(wa2, 1, 1, PSB[5])
            S.op("dve", lambda h: h.tensor_tensor(out=modp[:, 48:64], in0=PSB[5][:, 0:16], in1=smT[:, 24:40], op=ALU.add), reads=PSB[5].b + smT.b, writes=modp.b)
            S.op("dve", lambda h: h.scalar_tensor_tensor(out=modp[:, 16:24], in0=modp[:, 56:64], scalar=1.0, in1=smT[:, 48:56], op0=ALU.add, op1=ALU.mult), reads=modp.b + smT.b, writes=modp.b)
            S.op("dve", lambda h: h.tensor_copy(modp[:, 24:32], modp[:, 48:56]), reads=modp.b, writes=modp.b)
            mod_load(wa2, 0, 2)
            mod_load(wa2, 1, 5)

        def stage2():
            mod_gate_piece(wa2, 0, gate_m, 0, brow, (PSB[4], PSB[5]))
            S.dma("sp", lambda h: h.dma_start(out=brow[:, 0, :], in_=rows_d[1:2, :].partition_broadcast(128)), writes=[brow.b[0]])
            mod_gate_piece(wa2, 1, gate_f, 0, brow, (PSB[4], PSB[5]))
        return stage1, stage2

    es_fox = ExitStack()
    QTb = sb(es_fox, "QTb", [128, 4, L], BF16, nbuf=4)
    KTb = sb(es_fox, "KTb", [128, 4, L], BF16, nbuf=4)
    Vb = sb(es_fox, "Vb", [128, NT, 8, 65], BF16)
    cum3 = sb(es_fox, "cum3", [67, 3, L], BF16)
    csT = sb(es_fox, "csT", [128, NT, 8], F32)
    S.op("pool", lambda h: h.memset(Vb[:, :, :, 64:65], 1.0), writes=Vb.b)

    with ExitStack() as es:
        wf = sb(es, "wf", [128, 2, 8, 640], BF16, nbuf=2)
        wt = sb(es, "wt", [128, 8, 648], BF16, nbuf=8)
        iw_tm = sb(es, "iw_tm", [128, NT, 8], F32)
        e1 = sb(es, "e1", [8, L], F32)
        nbf = sb(es, "nbf", [8, 1], F32)
        bfg = sb(es, "bfg", [8, 1], F32)
        wfm_v = wfm_d.rearrange("(q p) (k c) -> q p k c", p=128, c=640)
        wtm_v = wtm_d.rearrange("(p k) n -> p k n", k=8)
        for kk in range(8):
            S.dma("pool", lambda h, kk=kk: h.dma_start(out=wt[:, kk, :], in_=wtm_v[:, kk, :]), writes=[wt.b[kk]])
        S.dma("sp", lambda h: h.dma_start(out=bfg[:], in_=nbf_d), writes=bfg.b)
        S.op("dve", lambda h: h.tensor_scalar(out=nbf[:], in0=bfg[:], scalar1=-1.0, scalar2=None, op0=ALU.mult), reads=bfg.b, writes=nbf.b)
        dests = []
        for p_ in range(4):
            dests.append((QTa, p_))
        dests += [(KTa, 0), (KTa, 1)]
        for p_ in range(4):
            dests.append((IQT, p_))
        dests.append((IKT, None))
        for p_ in range(4):
            dests.append((QTb, p_))
        for p_ in range(4):
            dests.append((KTb, p_))
        pieces = [(0, 5), (5, 10), (10, 15), (15, 19)]
        cnt_ = {'ev': 0, 'pb': 0}
        def fm_part():
            ev = 0
            pb = 0
            for pi, (c0, c1) in enumerate(pieces):
                bi = pi % 2
                ncol = (c1 - c0) * 128
                S.dma("pool", lambda h, bi=bi, pi=pi: h.dma_start(out=wf[:, bi, :, :], in_=wfm_v[pi]), writes=[wf.b[bi]])
                for ch in range(c0, c1):
                    T_, idx = dests[ch]
                    for ng in range(4):
                        bank = PSB[4 + (pb % 4)]
                        pb += 1
                        for kk in range(8):
                            S.op("pe", lambda h, bank=bank, bi=bi, kk=kk, ch=ch, c0=c0, ng=ng: h.matmul(bank[:, :], lhsT=wf[:, bi, kk, (ch - c0) * 128:(ch - c0 + 1) * 128], rhs=HT[:, kk, ng * 512:(ng + 1) * 512], start=(kk == 0), stop=(kk == 7)),
                                 reads=[wf.b[bi], HT.b[kk]], writes=bank.b, inc=(kk == 7), pe_acc=True)
                        if idx is None:
                            dst = T_[:, ng * 512:(ng + 1) * 512]
                            wb_ = T_.b
                        elif T_ is IQT:
                            dst = T_[:, ng * 512:(ng + 1) * 512, idx]
                            wb_ = T_.b
                        else:
                            dst = T_[:, idx, ng * 512:(ng + 1) * 512]
                            wb_ = [T_.b[idx]]
                        if ev % 2 == 0:
                            S.op("act", lambda h, dst=dst, bank=bank: h.activation(out=dst, in_=bank[:, :], func=AF.Copy), reads=bank.b, writes=wb_)
                        else:
                            S.op("dve", lambda h, dst=dst, bank=bank: h.tensor_copy(dst, bank[:, :]), reads=bank.b, writes=wb_)
                        ev += 1
        wbf = sb(es, "wbf", [128, 8, 8], BF16)
        S.dma("pool", lambda h: h.dma_start(out=wbf[:], in_=wfm_v[3][:, :, 512:520]), writes=wbf.b)

        def bf_part():
            for ng in range(4):
                bank = PSB[4 + ng]
                for kk in range(8):
                    S.op("pe", lambda h, bank=bank, kk=kk, ng=ng: h.matmul(bank[0:8, :], lhsT=wbf[:, kk, :], rhs=HT[:, kk, ng * 512:(ng + 1) * 512], start=(kk == 0), stop=(kk == 7)),
                         reads=wbf.b + [HT.b[kk]], writes=bank.b, inc=(kk == 7), pe_acc=True)
                S.op("act", lambda h, bank=bank, ng=ng: h.activation(out=e1[:, ng * 512:(ng + 1) * 512], in_=bank[0:8, :], func=AF.Exp, scale=-1.0, bias=nbf[:, 0:1]), reads=bank.b + nbf.b, writes=e1.b)
        def tm_part():
            for i in range(NT):
                bA = PSB[(2 * i) % 4]
                bB = PSB[(2 * i + 1) % 4]
                for kk in range(8):
                    S.op("pe", lambda h, bA=bA, kk=kk, i=i: h.matmul(bA[:, :], lhsT=HT[:, kk, i * 128:(i + 1) * 128], rhs=wt[:, kk, 0:512], start=(kk == 0), stop=(kk == 7)),
                         reads=[HT.b[kk], wt.b[kk]], writes=bA.b, inc=(kk == 7), pe_acc=True)
                for kk in range(8):
                    S.op("pe", lambda h, bB=bB, kk=kk, i=i: h.matmul(bB[:, 0:136], lhsT=HT[:, kk, i * 128:(i + 1) * 128], rhs=wt[:, kk, 512:648], start=(kk == 0), stop=(kk == 7)),
                         reads=[HT.b[kk], wt.b[kk]], writes=bB.b, inc=(kk == 7), pe_acc=True)
                S.op("act", lambda h, bA=bA, i=i: h.activation(out=Vb[:, i, :, 0:64], in_=bA[:, :].rearrange("p (h d) -> p h d", h=8), func=AF.Copy), reads=bA.b, writes=Vb.b)
                S.op("dve", lambda h, bB=bB, i=i: h.tensor_copy(Va[:, i, :, 0:64], bB[:, 0:128].rearrange("p (h d) -> p h d", h=2)), reads=bB.b, writes=Va.b)
                S.op("dve", lambda h, bB=bB, i=i: h.tensor_copy(iw_tm[:, i, :], bB[:, 128:136]), reads=bB.b, writes=iw_tm.b)
        tm_part()
        bf_part()
        S.op("act", lambda h: h.activation(out=e1[:], in_=e1[:], func=AF.Ln, bias=1.0, scale=1.0), reads=e1.b, writes=e1.b)
        cs = sb(es, "cs", [8, L], F32)
        S.op("dve", lambda h: h.tensor_tensor_scan(out=cs[:], data0=e1[:], data1=e1[:], initial=0.0, op0=ALU.add, op1=ALU.max), reads=e1.b, writes=cs.b)
        for j in range(NT):
            S.op("pe", lambda h, j=j: h.transpose(PSB[4][:, j * 8:(j + 1) * 8], cs[:, j * 128:(j + 1) * 128], identf[0:8, 0:8]), reads=cs.b + identf.b, writes=PSB[4].b, inc=(j == NT - 1), pe_acc=True)
        S.op("dve", lambda h: h.tensor_copy(csT[:].rearrange("p j h -> p (j h)"), PSB[4][:, 0:128]), reads=PSB[4].b, writes=csT.b)
        hi3 = Tn(e1.t, 2, "hi3")
        hi3v = e1[:].bitcast(BF16).rearrange("p (a t) -> p a t", a=2)
        S.op("dve", lambda h: h.tensor_scalar(out=cs[:], in0=cs[:], scalar1=-8.0, scalar2=None, op0=ALU.mult), reads=cs.b, writes=cs.b)
        for r in range(3):
            S.op("dve", lambda h, r=r: h.tensor_copy(hi3v[:, r % 2, :], cs[:]), reads=cs.b, writes=[hi3.b[r % 2]] + (e1.b if r < 2 else []))
            if r < 2:
                S.op("dve", lambda h, r=r: h.tensor_tensor(out=cs[:], in0=cs[:], in1=hi3v[:, r % 2, :], op=ALU.subtract), reads=cs.b + [hi3.b[r % 2]], writes=cs.b)
            for hh in range(8):
                pp = 32 * (hh % 3) + r
                S.dma("sp", lambda h, hh=hh, pp=pp, r=r: h.dma_start(out=cum3[pp:pp + 1, hh // 3, :], in_=hi3v[hh:hh + 1, r % 2, :]), reads=[hi3.b[r % 2]], writes=cum3.b)
        iwe_flat = iwe_d.rearrange("g c -> (g c)").rearrange("(i t b) -> t i b", i=16, t=128, b=4)
        iwo_flat = iwo_d.rearrange("g c -> (g c)").rearrange("(i t b) -> t i b", i=16, t=128, b=4)
        SCR = Buf("scr")
        S.dma("sp", lambda h: h.dma_start(out=iwe_flat, in_=iw_tm[:, :, 0:4]), reads=iw_tm.b, writes=[SCR])
        S.dma("sp", lambda h: h.dma_start(out=iwo_flat, in_=iw_tm[:, :, 4:8]), reads=iw_tm.b, writes=[SCR])
        wc_in = sb(es, "wc_in", [128, 128], F32)
        S.dma("sp", lambda h: h.dma_start(out=wc_in[:, 0:64], in_=iwe_d), reads=[SCR], writes=wc_in.b)
        S.dma("sp", lambda h: h.dma_start(out=wc_in[:, 64:128], in_=iwo_d), reads=[SCR], writes=wc_in.b)
        fm_part()
        S.op("pe", lambda h: h.transpose(PSB[5][:, 0:128], wc_in[:], identf[:]), reads=wc_in.b + identf.b, writes=PSB[5].b)
        S.op("dve", lambda h: h.tensor_copy(wcolT[:], PSB[5][:, 0:128]), reads=PSB[5].b, writes=wcolT.b)
        if dbg:
            for nm, T_, shp, dt_ in [("QTa", QTa, [128, 4 * L], BF16), ("KTa", KTa, [128, 2 * L], BF16), ("IKT", IKT, [128, L], BF16),
                                     ("QTb", QTb, [128, 4 * L], BF16), ("KTb", KTb, [128, 4 * L], BF16), ("Vb", Vb, [128, NT * 8 * 65], BF16), ("Va", Va, [128, NT * 2 * 65], BF16),
                                     ("csT", csT, [128, NT * 8], F32), ("cum3", cum3, [67, 3 * L], BF16), ("wcolT", wcolT, [128, 128], F32)]:
                dd = dbg_out(nm, shp, dt_)
                nd = len(T_.t.shape)
                if nd == 2:
                    src_ap = T_[:]
                elif nd == 3:
                    src_ap = T_[:].rearrange("p a b -> p (a b)")
                else:
                    src_ap = T_[:].rearrange("p a b c -> p (a b c)")
                S.dma("sp", lambda h, dd=dd, src_ap=src_ap: h.dma_start(out=dd, in_=src_ap), reads=T_.b, writes=[OUTB])
        if phase_end(1):
            return nc, dbg_d

    def attn_scratch(es, tag):
        sc = {}
        sc["ones67"] = sb(es, "ones67" + tag, [67, 128], BF16)
        sc["PT"] = sb(es, "PT" + tag, [128, 4, 512], BF16, nbuf=4)
        sc["o_sb"] = sb(es, "o_sb" + tag, [128, 4, 512], F32)
        sc["ob"] = sb(es, "ob" + tag, [128, 4, 512], BF16)
        sc["rec"] = sb(es, "rec" + tag, [128, 8, 4], F32)
        sc["oss"] = sb(es, "oss" + tag, [128, 8], F32)
        sc["ojunk"] = sb(es, "ojunk" + tag, [128, 512], BF16)
        S.op("pool", lambda h: h.memset(sc["ones67"][:], 1.0), writes=sc["ones67"].b)
        return sc

    def attention_chunk(sc, c, KT, kt_idx, QT, aug_lhs, aug_rhs, mask_rhs, bias_ap, Vt, v_idx, tick=None):
        PT, o_sb, rec = sc["PT"], sc["o_sb"], sc["rec"]
        T0 = 512 * c
        nj = 4 * c + 4
        steps = [(pr, j) for pr in range(4) for j in range(nj)]

        def s_stage(k):
            pr, j = steps[k]
            col0 = max(0, j - 4 * c) * 128
            ncols = 512 - col0
            heads = (2 * pr, 2 * pr + 1)
            Sbs = (PSB[(2 * k) % 4], PSB[(2 * k + 1) % 4])
            for hh, Sb in zip(heads, Sbs):
                rows = slice(64 * (hh % 2), 64 * (hh % 2) + 64)
                S.op("pe", lambda h, hh=hh, Sb=Sb, rows=rows: h.matmul(Sb[:, 0:ncols], lhsT=KT[rows, kt_idx(hh), j * 128:(j + 1) * 128], rhs=QT[rows, pr, T0 + col0:T0 + 512], start=True, stop=False),
                     reads=[KT.b[kt_idx(hh)], QT.b[pr]], writes=Sb.b, inc=False, pe_acc=True)
            mr, mbufs = mask_rhs(j, col0, ncols)
            if mr is not None:
                for hh, Sb in zip(heads, Sbs):
                    S.op("pe", lambda h, Sb=Sb: h.matmul(Sb[:, 0:ncols], lhsT=identb[:], rhs=mr, start=False, stop=False),
                         reads=identb.b + mbufs, writes=Sb.b, inc=False, pe_acc=True)
            for hh, Sb in zip(heads, Sbs):
                al, albufs = aug_lhs(hh)
                ar, arbufs = aug_rhs(hh, col0)
                S.op("pe", lambda h, Sb=Sb, al=al, ar=ar: h.matmul(Sb[:, 0:ncols], lhsT=al, rhs=ar, start=False, stop=True),
                     reads=albufs + arbufs, writes=Sb.b, inc=True, pe_acc=True)
            for q_, (hh, Sb) in enumerate(zip(heads, Sbs)):
                pt = (2 * k + q_) % 4
                bap, bbufs = bias_ap(hh, j)
                S.op("act", lambda h, Sb=Sb, pt=pt, bap=bap: h.activation(out=PT[:, pt, 0:ncols], in_=Sb[:, 0:ncols], func=AF.Exp, scale=0.125, bias=bap),
                     reads=Sb.b + bbufs, writes=[PT.b[pt]])

        def pv_stage(k):
            pr, j = steps[k]
            col0 = max(0, j - 4 * c) * 128
            il0 = max(0, j - 4 * c)
            for q_ in range(2):
                hh = 2 * pr + q_
                pt = (2 * k + q_) % 4
                Ob = PSB[(6 if pr % 2 == 0 else 4) + q_]
                for il in range(il0, 4):
                    i = 4 * c + il
                    first = (j == 0 and il == il0)
                    S.op("pe", lambda h, il=il, i=i, first=first, Ob=Ob, pt=pt, hh=hh: h.matmul(Ob[:, il * 65:(il + 1) * 65], lhsT=PT[:, pt, il * 128 - col0:il * 128 - col0 + 128], rhs=Vt[:, j, v_idx(hh), :], start=first, stop=(j == i), skip_group_check=True),
                         reads=[PT.b[pt]] + Vt.b, writes=Ob.b, inc=(il == 3), pe_acc=True)
                if j == nj - 1:
                    Ov = Ob[:, 0:260].rearrange("p (a b) -> p a b", b=65)
                    S.op("dve", lambda h, Ov=Ov, hh=hh: h.reciprocal(rec[:, hh, :], Ov[:, :, 64]), reads=Ob.b, writes=rec.b)
                    S.op("dve", lambda h, Ov=Ov, hh=hh: h.tensor_tensor(out=o_sb[:, :, hh * 64:(hh + 1) * 64], in0=Ov[:, :, 0:64], in1=rec[:, hh, :].unsqueeze(2).to_broadcast([128, 4, 64]), op=ALU.mult),
                         reads=Ob.b + rec.b, writes=o_sb.b)

        n = len(steps)
        s_stage(0)
        for k in range(n):
            if k + 1 < n:
                s_stage(k + 1)
            pv_stage(k)
            if tick is not None:
                tick(k, n)

    def out_norm(sc, c, base):
        o_sb, ob, oss, ojunk = sc["o_sb"], sc["ob"], sc["oss"], sc["ojunk"]
        T0 = 512 * c
        for il in range(4):
            S.op("act", lambda h, il=il: h.activation(out=ojunk[:], in_=o_sb[:, il, :], func=AF.Square, accum_out=oss[:, il:il + 1]), reads=o_sb.b, writes=ojunk.b + oss.b)
        S.op("act", lambda h: h.activation(out=oss[:, 4:8], in_=oss[:, 0:4], func=AF.Sqrt, bias=EPS, scale=1.0 / 512), reads=oss.b, writes=oss.b)
        S.op("dve", lambda h: h.reciprocal(oss[:, 4:8], oss[:, 4:8]), reads=oss.b, writes=oss.b)
        S.op("dve", lambda h: h.tensor_tensor(out=ob[:], in0=o_sb[:], in1=oss[:, 4:8].unsqueeze(2).to_broadcast([128, 4, 512]), op=ALU.mult), reads=o_sb.b + oss.b, writes=ob.b)
        for fc in range(4):
            bank = PSB[4 + fc % 2]
            bi = 4 + fc % 2
            for il in range(4):
                S.op("pe", lambda h, bi=bi, il=il, fc=fc: h.transpose(psv_bf(bi)[:, il * 128:(il + 1) * 128], ob[:, il, fc * 128:(fc + 1) * 128], identb[:]),
                     reads=ob.b + identb.b, writes=bank.b, inc=(il == 3), pe_acc=True)
            S.op("act", lambda h, bi=bi, fc=fc: h.activation(out=HT[:, base + fc, T0:T0 + 512], in_=psv_bf(bi)[:, 0:512], func=AF.Identity, scale=smT[:, 56 + base + fc:57 + base + fc]),
                 reads=bank.b + smT.b, writes=[HT.b[base + fc]])

    with ExitStack() as es:
        sc = attn_scratch(es, "f")
        triw = sb(es, "triw", [128, 512], BF16)
        S.dma("pool", lambda h: h.dma_start(out=triw[:], in_=tri_d), writes=triw.b)
        mod_st1, mod_st2 = deferred_mod(es)
        for c in range(4):
            if c == 1:
                mod_st1()
            if c == 2:
                mod_st2()
            attention_chunk(
                sc, c, KTb, lambda hh: hh // 2, QTb,
                aug_lhs=lambda hh: (sc["ones67"][32 * (hh % 3):32 * (hh % 3) + 3, :], sc["ones67"].b),
                aug_rhs=lambda hh, col0, c=c: (cum3[32 * (hh % 3):32 * (hh % 3) + 3, hh // 3, 512 * c + col0:512 * c + 512], cum3.b),
                mask_rhs=lambda j, col0, ncols, c=c: ((triw[:, 0:ncols], triw.b) if j >= 4 * c else (None, [])),
                bias_ap=lambda hh, j: (csT[:, j, hh:hh + 1], csT.b),
                Vt=Vb, v_idx=lambda hh: hh)
            out_norm(sc, c, 4)
        if dbg:
            d4b = dbg_out("oTb", [128, 8 * L], BF16)
            S.dma("sp", lambda h: h.dma_start(out=d4b, in_=HT[:].rearrange("p k t -> p (k t)")), reads=HT.b, writes=[OUTB])
        if phase_end(2):
            return nc, dbg_d
    es_fox.close()

    with ExitStack() as es:
        sc = attn_scratch(es, "a")
        IS = sb(es, "IS", [128, 4, L], F32, nbuf=4)
        NM = sb(es, "NM", [128, L], BF16)
        NMT2 = sb(es, "NMT", [128, 2, NT, 512], BF16, nbuf=2)
        R = sb(es, "R", [128, 4, 512], BF16, nbuf=4)
        Wblk = sb(es, "Wblk", [128, 2, 8, 128], BF16, nbuf=2)
        esel = sb(es, "esel", [128, 8, 128], BF16)
        alk = sb(es, "alk", [128, 8, 16], F32)
        alq = sb(es, "alq", [66, 8, 512], BF16)
        bs = sb(es, "bs", [128, 32], F32)
        cjunk = sb(es, "cjunk", [128, L], BF16)
        S.dma("pool", lambda h: h.dma_start(out=esel[:].rearrange("p a b -> p (a b)"), in_=esel_d), writes=esel.b)
        S.dma("pool", lambda h: h.dma_start(out=alq[0:2].rearrange("p a b -> p (a b)"), in_=alq_d), writes=alq.b)
        S.dma("pool", lambda h: h.dma_start(out=alq[64:66].rearrange("p a b -> p (a b)"), in_=alq_d), writes=alq.b)
        S.dma("sp", lambda h: h.dma_start(out=alk[:].rearrange("p a b -> p (a b)"), in_=alk_d), writes=alk.b)
        evc = [0]

        def indexer(c):
            units = []
            for il in range(4):
                i = 4 * c + il
                ncols = 128 * (i + 1)
                for kc in range((ncols + 511) // 512):
                    for g in range(8):
                        units.append((il, kc, g))

            def dots(u):
                il, kc, g = units[u]
                i = 4 * c + il
                n = min(512, 128 * (i + 1) - 512 * kc)
                tok0 = 128 * i + 16 * g
                Db = PSB[u % 4]
                for hf in range(2):
                    rs = slice(64 * hf, 64 * hf + 64)
                    S.op("pe", lambda h, rs=rs: h.matmul(Db[rs, 0:n], lhsT=IQT[rs, tok0:tok0 + 16, :].rearrange("p t a -> p (t a)"), rhs=IKT[rs, 512 * kc:512 * kc + n], start=True, stop=True),
                         reads=IQT.b + IKT.b, writes=Db.b, inc=(hf == 1), pe_acc=True)
                if evc[0] % 4 != 3:
                    S.op("act", lambda h: h.activation(out=R[:, u % 4, 0:n], in_=Db[:, 0:n], func=AF.Relu), reads=Db.b, writes=[R.b[u % 4]])
                else:
                    S.op("dve", lambda h: h.tensor_scalar(out=R[:, u % 4, 0:n], in0=Db[:, 0:n], scalar1=0.0, scalar2=None, op0=ALU.max), reads=Db.b, writes=[R.b[u % 4]])
                evc[0] += 1

            def headsum(u):
                il, kc, g = units[u]
                i = 4 * c + il
                ncols = 128 * (i + 1)
                n = min(512, ncols - 512 * kc)
                ISb = PSB[4 + kc % 2]
                wb = i % 2
                if kc == 0 and g == 0:
                    S.op("pool", lambda h: h.tensor_tensor(out=Wblk[:, wb], in0=esel[:], in1=wcolT[:, 8 * i:8 * i + 8].unsqueeze(2).to_broadcast([128, 8, 128]), op=ALU.mult), reads=esel.b + wcolT.b, writes=[Wblk.b[wb]])
                S.op("pe", lambda h: h.matmul(ISb[:, 0:n], lhsT=Wblk[:, wb, g, :], rhs=R[:, u % 4, 0:n], start=(g == 0), stop=(g == 7)),
                     reads=[Wblk.b[wb], R.b[u % 4]], writes=ISb.b, inc=(g == 7), pe_acc=True)
                if g == 7:
                    S.op("act", lambda h: h.activation(out=IS[:, il, 512 * kc:512 * kc + n], in_=ISb[:, 0:n], func=AF.Copy), reads=ISb.b, writes=[IS.b[il]])
                    if 512 * kc + n == ncols:
                        S.op("dve", lambda h: h.tensor_reduce(out=bs[:, 20 + il:21 + il], in_=IS[:, il, 0:ncols], axis=AX.X, op=ALU.max, apply_absolute_value=True), reads=[IS.b[il]], writes=bs.b)
                        S.op("pool", lambda h: h.affine_select(out=IS[:, il, 128 * i:128 * i + 128], in_=IS[:, il, 128 * i:128 * i + 128], pattern=[[-1, 128]], compare_op=ALU.is_ge, fill=-1e30, base=0, channel_multiplier=1),
                             reads=[IS.b[il]], writes=[IS.b[il]])

            nu = len(units)
            dots(0)
            if nu > 1:
                dots(1)
            for u in range(nu):
                if u + 2 < nu:
                    dots(u + 2)
                headsum(u)

        def bisect_gen(c):
            S.op("dve", lambda h: h.tensor_scalar(out=bs[:, 0:4], in0=bs[:, 20:24], scalar1=-1.001, scalar2=-1e-3, op0=ALU.mult, op1=ALU.add), reads=bs.b, writes=bs.b)
            S.op("dve", lambda h: h.tensor_scalar(out=bs[:, 4:8], in0=bs[:, 20:24], scalar1=2.002, scalar2=2e-3, op0=ALU.mult, op1=ALU.add), reads=bs.b, writes=bs.b)
            for jb in range(1, NBIS + 1):
                stp = 2.0 ** (-jb)
                S.op("dve", lambda h, stp=stp: h.scalar_tensor_tensor(out=bs[:, 8:12], in0=bs[:, 4:8], scalar=stp, in1=bs[:, 0:4], op0=ALU.mult, op1=ALU.add), reads=bs.b, writes=bs.b)
                for il in range(4):
                    ncols = 128 * (4 * c + il + 1)
                    S.op("dve", lambda h, il=il, ncols=ncols: h.tensor_scalar(out=cjunk[:, 0:ncols], in0=IS[:, il, 0:ncols], scalar1=bs[:, 8 + il:9 + il], scalar2=0.0, op0=ALU.is_ge, op1=ALU.add, accum_out=bs[:, 12 + il:13 + il]),
                         reads=[IS.b[il]] + bs.b, writes=cjunk.b + bs.b)
                S.op("dve", lambda h, stp=stp: h.tensor_scalar(out=bs[:, 16:20], in0=bs[:, 12:16], scalar1=255.5, scalar2=stp, op0=ALU.is_ge, op1=ALU.mult), reads=bs.b, writes=bs.b)
                S.op("dve", lambda h: h.tensor_tensor(out=bs[:, 16:20], in0=bs[:, 16:20], in1=bs[:, 4:8], op=ALU.mult), reads=bs.b, writes=bs.b)
                S.op("dve", lambda h: h.tensor_tensor(out=bs[:, 0:4], in0=bs[:, 0:4], in1=bs[:, 16:20], op=ALU.add), reads=bs.b, writes=bs.b)
                yield

        def mask_epilogue(c):
            tb = 0
            nb_ = c % 2
            for il in range(4):
                i = 4 * c + il
                ncols = 128 * (i + 1)
                S.op("dve", lambda h, il=il, ncols=ncols: h.tensor_scalar(out=NM[:, 0:ncols], in0=IS[:, il, 0:ncols], scalar1=bs[:, il:il + 1], scalar2=NEG, op0=ALU.is_lt, op1=ALU.mult), reads=[IS.b[il]] + bs.b, writes=NM.b)
                for j0 in range(0, i + 1, 8):
                    nb = min(8, i + 1 - j0)
                    bi = 6 + tb % 2
                    tb += 1
                    for jj in range(nb):
                        S.op("pe", lambda h, bi=bi, jj=jj, j0=j0: h.transpose(psv_bf(bi)[:, jj * 128:(jj + 1) * 128], NM[:, (j0 + jj) * 128:(j0 + jj + 1) * 128], identb[:]),
                             reads=NM.b + identb.b, writes=PSB[bi].b, inc=(jj == nb - 1), pe_acc=True)
                    S.op("act", lambda h, bi=bi, j0=j0, nb=nb, il=il: h.activation(out=NMT2[:, nb_, j0:j0 + nb, il * 128:(il + 1) * 128], in_=psv_bf(bi)[:, 0:nb * 128].rearrange("p (a b) -> p a b", b=128), func=AF.Copy),
                         reads=PSB[bi].b, writes=[NMT2.b[nb_]])

        order = [3, 2, 1, 0]
        indexer(order[0])
        for _ in bisect_gen(order[0]):
            pass
        mask_epilogue(order[0])
        for oi, c in enumerate(order):
            gen = None
            cn = order[oi + 1] if oi + 1 < 4 else None
            if cn is not None:
                indexer(cn)
                gen = bisect_gen(cn)
            nsteps = 4 * (4 * c + 4)
            every = max(1, nsteps // (NBIS + 1))

            def tick(k, n, gen=gen, every=every):
                if gen is not None and k % every == 0:
                    next(gen, None)
            nb_ = c % 2
            attention_chunk(
                sc, c, KTa, lambda hh: hh // 4, QTa,
                aug_lhs=lambda hh: (sc["ones67"][64 * (hh % 2):64 * (hh % 2) + 2, :], sc["ones67"].b),
                aug_rhs=lambda hh, col0: (alq[64 * (hh % 2):64 * (hh % 2) + 2, hh, col0:512], alq.b),
                mask_rhs=lambda j, col0, ncols, nb_=nb_: (NMT2[:, nb_, j, col0:512], [NMT2.b[nb_]]),
                bias_ap=lambda hh, j, c=c: (alk[:, hh, 4 * c - j + 3:4 * c - j + 4], alk.b),
                Vt=Va, v_idx=lambda hh: hh // 4, tick=tick)
            if gen is not None:
                for _ in gen:
                    pass
                mask_epilogue(cn)
            out_norm(sc, c, 0)
        if dbg:
            for nm, T_, shp, dt_ in [("IS", IS, [128, 4 * L], F32), ("bs", bs, [128, 32], F32), ("osb", sc["o_sb"], [128, 4 * 512], F32)]:
                dd = dbg_out(nm, shp, dt_)
                src_ap = T_[:] if len(T_.t.shape) == 2 else T_[:].rearrange("p a b -> p (a b)")
                S.dma("sp", lambda h, dd=dd, src_ap=src_ap: h.dma_start(out=dd, in_=src_ap), reads=T_.b, writes=[OUTB])
            d4 = dbg_out("oT", [128, 8 * L], BF16)
            S.dma("sp", lambda h: h.dma_start(out=d4, in_=HT[:].rearrange("p k t -> p (k t)")), reads=HT.b, writes=[OUTB])
        if phase_end(3):
            return nc, dbg_d
    es_dsa.close()

    with ExitStack() as es:
        X1 = sb(es, "X1", [128, NT, D], F32, nbuf=NT)
        x_v = x_d.rearrange("(i p) d -> i p d", p=128)
        with ExitStack() as es2:
            wo = sb(es2, "wo", [128, 8, D], BF16, nbuf=8)
            xt2 = sb(es2, "xt2", [128, 2, D], F32, nbuf=2)
            wout_v = wout_d.rearrange("(c p) d -> p c d", p=128)
            for fc in range(8):
                S.dma("pool", lambda h, fc=fc: h.dma_start(out=wo[:, fc, :], in_=wout_v[:, fc, :]), writes=[wo.b[fc]])
            for fc in range(8):
                S.op("pool", lambda h, fc=fc: h.tensor_tensor(out=wo[:, fc, :], in0=wo[:, fc, :], in1=gate_m[:], op=ALU.mult), reads=[wo.b[fc]] + gate_m.b, writes=[wo.b[fc]])
            for i in range(NT):
                bi = i % 2
                S.dma("sp", lambda h, i=i, bi=bi: h.dma_start(out=xt2[:, bi, :], in_=x_v[i]), writes=[xt2.b[bi]])
                for half in range(2):
                    bank = PSB[(2 * i + half) % 4]
                    for fc in range(8):
                        S.op("pe", lambda h, bank=bank, fc=fc, i=i, half=half: h.matmul(bank[:, :], lhsT=HT[:, fc, i * 128:(i + 1) * 128], rhs=wo[:, fc, half * 512:(half + 1) * 512], start=(fc == 0), stop=(fc == 7)),
                             reads=[HT.b[fc], wo.b[fc]], writes=bank.b, inc=(fc == 7), pe_acc=True)
                    S.op("dve", lambda h, bank=bank, i=i, half=half, bi=bi: h.tensor_tensor(out=X1[:, i, half * 512:(half + 1) * 512], in0=bank[:, :], in1=xt2[:, bi, half * 512:(half + 1) * 512], op=ALU.add),
                         reads=bank.b + [xt2.b[bi]], writes=[X1.b[i]])
            if dbg:
                d5 = dbg_out("X1", [128, NT * D], F32)
                S.dma("sp", lambda h: h.dma_start(out=d5, in_=X1[:].rearrange("p a b -> p (a b)")), reads=X1.b, writes=[OUTB])
            if phase_end(4):
                return nc, dbg_d
        with ExitStack() as es2:
            norm_to_HT(es2, lambda i: (X1[:, i, :], [X1.b[i]]), 16, 24, "2")
            if phase_end(5):
                return nc, dbg_d
        with ExitStack() as es2:
            combT = sb(es2, "combT", [32, L], BF16)
            sel = sb(es2, "sel", [32, NE * 128], BF16)
            es3 = ExitStack()
            wr = sb(es3, "wr", [128, 8, 36], BF16)
            brt = sb(es3, "brt", [128, 36], F32)
            lg = sb(es3, "lg", [128, NT, 36], F32)
            rt = sb(es3, "rt", [128, NT, 64], F32)
            comb = sb(es3, "comb", [128, NT, 32], BF16)
            S.dma("pool", lambda h: h.dma_start(out=wr[:], in_=wrt_d.rearrange("(p k) n -> p k n", k=8)), writes=wr.b)
            S.dma("sp", lambda h: h.dma_start(out=brt[:], in_=brt_d.partition_broadcast(128)), writes=brt.b)
            for i in range(NT):
                bank = PSB[i % 4]
                for kk in range(8):
                    S.op("pe", lambda h, bank=bank, kk=kk, i=i: h.matmul(bank[:, 0:36], lhsT=HT[:, kk, i * 128:(i + 1) * 128], rhs=wr[:, kk, :], start=(kk == 0), stop=(kk == 7)),
                         reads=[HT.b[kk]] + wr.b, writes=bank.b, inc=(kk == 7), pe_acc=True)
                S.op("dve", lambda h, bank=bank, i=i: h.tensor_tensor(out=lg[:, i, :], in0=bank[:, 0:36], in1=brt[:], op=ALU.add), reads=bank.b + brt.b, writes=lg.b)
            RB = rt.b + lg.b

            def dv(fn):
                S.op("dve", fn, reads=RB, writes=RB)

            def bc(ap, n):
                return ap.unsqueeze(2).to_broadcast([128, NT, n])
            gl = lg[:, :, 0:4]
            gmax, gsum, pg, m1, m2, w1, w2 = (rt[:, :, k] for k in range(7))
            ohg, gsh, el, tmp8, oh1, oh2, el2 = rt[:, :, 8:12], rt[:, :, 12:16], rt[:, :, 16:24], rt[:, :, 24:32], rt[:, :, 32:40], rt[:, :, 40:48], rt[:, :, 48:56]
            c8 = rt[:, :, 56:64]
            dv(lambda h: h.tensor_reduce(out=gmax, in_=gl, axis=AX.X, op=ALU.max))
            dv(lambda h: h.tensor_tensor(out=ohg, in0=gl, in1=bc(gmax, 4), op=ALU.is_ge))
            dv(lambda h: h.tensor_tensor(out=gsh, in0=gl, in1=bc(gmax, 4), op=ALU.subtract))
            S.op("act", lambda h: h.activation(out=gsh, in_=gsh, func=AF.Exp), reads=RB, writes=RB)
            dv(lambda h: h.tensor_reduce(out=gsum, in_=gsh, axis=AX.X, op=ALU.add))
            dv(lambda h: h.reciprocal(pg, gsum))
            for g in range(4):
                src_e = lg[:, :, 4 + 8 * g:12 + 8 * g]
                if g == 0:
                    dv(lambda h, src_e=src_e, g=g: h.tensor_tensor(out=el, in0=src_e, in1=bc(ohg[:, :, g], 8), op=ALU.mult))
                else:
                    dv(lambda h, src_e=src_e, g=g: h.tensor_tensor(out=tmp8, in0=src_e, in1=bc(ohg[:, :, g], 8), op=ALU.mult))
                    dv(lambda h: h.tensor_tensor(out=el, in0=el, in1=tmp8, op=ALU.add))
            dv(lambda h: h.tensor_reduce(out=m1, in_=el, axis=AX.X, op=ALU.max))
            dv(lambda h: h.tensor_tensor(out=oh1, in0=el, in1=bc(m1, 8), op=ALU.is_ge))
            dv(lambda h: h.scalar_tensor_tensor(out=el2, in0=oh1, scalar=-1e30, in1=el, op0=ALU.mult, op1=ALU.add))
            dv(lambda h: h.tensor_reduce(out=m2, in_=el2, axis=AX.X, op=ALU.max))
            dv(lambda h: h.tensor_tensor(out=oh2, in0=el2, in1=bc(m2, 8), op=ALU.is_ge))
            dv(lambda h: h.tensor_tensor(out=w2, in0=m2, in1=m1, op=ALU.subtract))
            S.op("act", lambda h: h.activation(out=w2, in_=w2, func=AF.Exp), reads=RB, writes=RB)
            dv(lambda h: h.tensor_scalar(out=w1, in0=w2, scalar1=1.0, scalar2=None, op0=ALU.add))
            dv(lambda h: h.reciprocal(w1, w1))
            dv(lambda h: h.tensor_tensor(out=w2, in0=w2, in1=w1, op=ALU.mult))
            dv(lambda h: h.tensor_tensor(out=w1, in0=w1, in1=pg, op=ALU.mult))
            dv(lambda h: h.tensor_tensor(out=w2, in0=w2, in1=pg, op=ALU.mult))
            dv(lambda h: h.tensor_tensor(out=c8, in0=oh1, in1=bc(w1, 8), op=ALU.mult))
            dv(lambda h: h.tensor_tensor(out=tmp8, in0=oh2, in1=bc(w2, 8), op=ALU.mult))
            dv(lambda h: h.tensor_tensor(out=c8, in0=c8, in1=tmp8, op=ALU.add))
            for g in range(4):
                S.op("dve", lambda h, g=g: h.tensor_tensor(out=comb[:, :, 8 * g:8 * g + 8], in0=c8, in1=bc(ohg[:, :, g], 8), op=ALU.mult), reads=RB, writes=comb.b)
            for i in range(NT):
                bi = 4 + (i // 8)
                S.op("pe", lambda h, bi=bi, i=i: h.transpose(psv_bf(bi)[0:32, (i % 8) * 128:(i % 8 + 1) * 128], comb[:, i, :], identb[:]), reads=comb.b + identb.b, writes=PSB[bi].b, inc=(i % 8 == 7), pe_acc=True)
            for hb in range(2):
                S.op("dve", lambda h, hb=hb: h.tensor_copy(combT[:, hb * 1024:(hb + 1) * 1024], psv_bf(4 + hb)[0:32, :]), reads=PSB[4 + hb].b, writes=combT.b)
            S.op("dve", lambda h: h.tensor_copy(sel[:].rearrange("k (e p) -> k e p", p=128), identb[0:32, 0:32].unsqueeze(2).to_broadcast([32, NE, 128])), reads=identb.b, writes=sel.b)
            S.emit()
            es3.close()
            wgu = sb(es2, "wgu", [128, 3, 2, 8, DFF], BF16, nbuf=3)
            wdn = sb(es2, "wdn", [128, 4, 2, D], BF16, nbuf=4)
            cbb = sb(es2, "cbb", [128, 2, L], BF16, nbuf=2)
            aT = sb(es2, "aT", [128, 4, 2, L], BF16, nbuf=4)
            sl = sb(es2, "sl", [128, 2, 512], BF16, nbuf=2)
            t1 = sb(es2, "t1", [128, 2, 512], BF16, nbuf=2)
            q = 0
            def load_gu(e):
                b = e % 3
                S.dma("pool", lambda h: h.dma_start(out=wgu[:, b, 0], in_=wg_d[e].rearrange("(p k) f -> p k f", k=8)), writes=[wgu.b[b]])
                S.dma("pool", lambda h: h.dma_start(out=wgu[:, b, 1], in_=wu_d[e].rearrange("(p k) f -> p k f", k=8)), writes=[wgu.b[b]])
            load_gu(0)
            load_gu(1)
            for rnd in range(8):
                for er in range(4):
                    e = 4 * rnd + er
                    b = e % 2
                    wb3 = e % 3
                    if e + 2 < NE:
                        load_gu(e + 2)
                    S.dma("pool", lambda h, e=e, er=er: h.dma_start(out=wdn[:, er], in_=wd_d[e].rearrange("(c p) d -> p c d", p=128)), writes=[wdn.b[er]])
                    for tcn in range(4):
                        cbank = PSB[4 + tcn]
                        S.op("pe", lambda h, cbank=cbank, e=e, tcn=tcn: h.matmul(cbank[:, :], lhsT=sel[:, e * 128:(e + 1) * 128], rhs=combT[:, tcn * 512:(tcn + 1) * 512], start=True, stop=True),
                             reads=sel.b + combT.b, writes=cbank.b)
                        if tcn % 2 == 0:
                            S.op("act", lambda h, cbank=cbank, b=b, tcn=tcn: h.activation(out=cbb[:, b, tcn * 512:(tcn + 1) * 512], in_=cbank[:, :], func=AF.Copy), reads=cbank.b, writes=[cbb.b[b]])
                        else:
                            S.op("dve", lambda h, cbank=cbank, b=b, tcn=tcn: h.tensor_copy(cbb[:, b, tcn * 512:(tcn + 1) * 512], cbank[:, :]), reads=cbank.b, writes=[cbb.b[b]])
                    for tcn in range(4):
                        for fc in range(2):
                            Gb = PSB[(2 * q) % 4]
                            Ub = PSB[(2 * q + 1) % 4]
                            qb = q % 2
                            q += 1
                            for gu, bank in ((0, Gb), (1, Ub)):
                                for kk in range(8):
                                    S.op("pe", lambda h, bank=bank, gu=gu, kk=kk, wb3=wb3, fc=fc, tcn=tcn: h.matmul(bank[:, :], lhsT=wgu[:, wb3, gu, kk, fc * 128:(fc + 1) * 128], rhs=HT[:, kk, tcn * 512:(tcn + 1) * 512], start=(kk == 0), stop=(kk == 7)),
                                         reads=[wgu.b[wb3], HT.b[kk]], writes=bank.b, inc=(kk == 7), pe_acc=True)
                            S.op("act", lambda h, Gb=Gb, qb=qb: h.activation(out=sl[:, qb, :], in_=Gb[:, :], func=AF.Silu), reads=Gb.b, writes=[sl.b[qb]])
                            S.op("dve", lambda h, Ub=Ub, qb=qb: h.tensor_tensor(out=t1[:, qb, :], in0=Ub[:, :], in1=sl[:, qb, :], op=ALU.mult), reads=Ub.b + [sl.b[qb]], writes=[t1.b[qb]])
                            S.op("pool", lambda h, qb=qb, er=er, fc=fc, tcn=tcn, b=b: h.tensor_tensor(out=aT[:, er, fc, tcn * 512:(tcn + 1) * 512], in0=t1[:, qb, :], in1=cbb[:, b, tcn * 512:(tcn + 1) * 512], op=ALU.mult),
                                 reads=[t1.b[qb], cbb.b[b]], writes=[aT.b[er]])
                    S.op("pool", lambda h, er=er: h.tensor_tensor(out=wdn[:, er], in0=wdn[:, er], in1=gate_f[:].unsqueeze(1).to_broadcast([128, 2, D]), op=ALU.mult), reads=[wdn.b[er]] + gate_f.b, writes=[wdn.b[er]])
                for i in range(NT):
                    for half in range(2):
                        bank = PSB[4 + (2 * i + half) % 4]
                        for er in range(4):
                            for fc in range(2):
                                S.op("pe", lambda h, bank=bank, er=er, fc=fc, i=i, half=half: h.matmul(bank[:, :], lhsT=aT[:, er, fc, i * 128:(i + 1) * 128], rhs=wdn[:, er, fc, half * 512:(half + 1) * 512], start=(er == 0 and fc == 0), stop=(er == 3 and fc == 1)),
                                     reads=[aT.b[er], wdn.b[er]], writes=bank.b, inc=(er == 3 and fc == 1), pe_acc=True)
                        S.op("dve", lambda h, bank=bank, i=i, half=half: h.tensor_tensor(out=X1[:, i, half * 512:(half + 1) * 512], in0=bank[:, :], in1=X1[:, i, half * 512:(half + 1) * 512], op=ALU.add),
                             reads=bank.b + [X1.b[i]], writes=[X1.b[i]])
            if phase_end(6):
                return nc, dbg_d
        with ExitStack() as es2:
            gfin = sb(es2, "gfin", [128, D], F32)
            yt = sb(es2, "yt", [128, 2, D], F32, nbuf=2)
            fj = sb(es2, "fj", [128, D], BF16)
            fs = sb(es2, "fs", [128, 2 * NT], F32)
            y_v = y_d.rearrange("(i p) d -> i p d", p=128)
            S.dma("sp", lambda h: h.dma_start(out=gfin[:], in_=rows_d[2:3, :].partition_broadcast(128)), writes=gfin.b)
            for g in range(4):
                for il in range(4):
                    i = 4 * g + il
                    S.op("act", lambda h, i=i: h.activation(out=fj[:], in_=X1[:, i, :], func=AF.Square, accum_out=fs[:, i:i + 1]), reads=[X1.b[i]], writes=fj.b + fs.b)
                S.op("act", lambda h, g=g: h.activation(out=fs[:, NT + 4 * g:NT + 4 * g + 4], in_=fs[:, 4 * g:4 * g + 4], func=AF.Sqrt, bias=EPS, scale=1.0 / D), reads=fs.b, writes=fs.b)
                S.op("dve", lambda h, g=g: h.reciprocal(fs[:, NT + 4 * g:NT + 4 * g + 4], fs[:, NT + 4 * g:NT + 4 * g + 4]), reads=fs.b, writes=fs.b)
                for il in range(4):
                    i = 4 * g + il
                    bi = i % 2
                    S.op("dve", lambda h, i=i, bi=bi: h.scalar_tensor_tensor(out=yt[:, bi, :], in0=X1[:, i, :], scalar=fs[:, NT + i:NT + i + 1], in1=gfin[:], op0=ALU.mult, op1=ALU.mult), reads=[X1.b[i]] + fs.b + gfin.b, writes=[yt.b[bi]])
                    S.dma("sp", lambda h, i=i, bi=bi: h.dma_start(out=y_v[i], in_=yt[:, bi, :]), reads=[yt.b[bi]], writes=[OUTB])
            S.wait_all("sp", [OUTB])
            S.emit()
    es_all.close()
    S.close()
    return nc, dbg_d


def _consts():
    identf = np.eye(128, dtype=np.float32)
    tri = np.zeros((128, 512), np.float32)
    s = np.arange(128)[:, None]
    t = np.arange(128)[None, :]
    tri[:, 0:128] = np.where(s > t, NEG, 0.0)
    p = np.arange(128)
    esel = np.zeros((128, 8, 128), np.float32)
    for g in range(8):
        esel[p, g, 16 * g + (p % 64) // 4] = 1.0
    slopes = np.exp2(-8.0 * np.arange(1, 9, dtype=np.float64) / 8).astype(np.float32)
    alk = np.zeros((128, 8, 16), np.float32)
    for di in range(16):
        dj = di - 3
        alk[:, :, di] = slopes[None, :] * (p[:, None] - 128.0 * dj)
    tl = np.arange(512)
    alq = np.zeros((2, 8, 512), np.float32)
    alq[0] = -8.0 * slopes[:, None] * (256.0 * (tl // 256))[None, :]
    alq[1] = -8.0 * slopes[:, None] * (tl % 256)[None, :]
    return dict(identf=identf, tri=tri, esel=esel.reshape(128, 1024), alibi_k=alk.reshape(128, 128), alibi_q=alq.reshape(2, 4096))


def _pk(v):
    return np.ascontiguousarray(np.asarray(v, np.float32).reshape(128, 8).T)


def _host_prep(inp):
    f = lambda a: np.ascontiguousarray(np.asarray(a, dtype=np.float32))
    x = f(inp["x"]); c = f(inp["c"])
    w_ada = f(inp["w_ada"][0]); b_ada = f(inp["b_ada"][0])
    w_in = f(inp["w_in"][0])
    cols = lambda a, b: w_in[:, a:b]
    aq, ak, av, iq, ik, iw, bq, bk, bv, bf = (cols(0, 512), cols(512, 640), cols(640, 768), cols(768, 1280), cols(1280, 1344),
                                              cols(1344, 1352), cols(1352, 1864), cols(1864, 2376), cols(2376, 2888), cols(2888, 2896))
    w_fm = np.concatenate([aq, ak[:, 0:64], ak[:, 0:64], ak[:, 64:128], ak[:, 64:128], iq, ik, ik, bq, bk, bf], axis=1)
    assert w_fm.shape[1] == 2440
    w_fm_pad = np.zeros((1024, 4 * 640), np.float32)
    w_fm_pad[:, :2440] = w_fm
    w_fm = np.ascontiguousarray(w_fm_pad.reshape(128, 8, 4, 640).transpose(2, 0, 1, 3).reshape(4 * 128, 8 * 640))
    w_tm = np.concatenate([bv, av, iw[:, [0, 2, 4, 6, 1, 3, 5, 7]]], axis=1)
    w_rt = np.concatenate([f(inp["w_group"][0])] + [f(inp["w_router"][0][g]) for g in range(4)], axis=1)
    brt = np.concatenate([f(inp["b_group"][0]), f(inp["b_router"][0]).reshape(-1)])[None, :]
    g_out = np.concatenate([f(inp["g_out_a"][0]), f(inp["g_out_b"][0])])
    rows = np.stack([b_ada[2048:3072], b_ada[5120:6144], f(inp["g_final"])])
    shared = dict(rows=np.ascontiguousarray(rows), brt=np.ascontiguousarray(brt), w_ada=w_ada, w_fm=np.ascontiguousarray(w_fm),
                  w_tm=np.ascontiguousarray(w_tm), nbf=f(inp["b_forget"][0]).reshape(8, 1), w_out=f(inp["w_out"][0]),
                  w_rt=np.ascontiguousarray(w_rt), w_gate=f(inp["w_gate"][0]), w_up=f(inp["w_up"][0]), w_down=f(inp["w_down"][0]))
    shared.update(_consts())
    sm_common = [_pk(b_ada[0:1024]), _pk(b_ada[1024:2048]), _pk(b_ada[3072:4096]), _pk(b_ada[4096:5120]),
                 _pk(inp["g_mix"][0]), _pk(inp["g_ffn"][0]), g_out.reshape(8, 128)]
    in_maps = []
    for b in range(8):
        m = dict(shared)
        m["x"] = x[b]
        m["smalls"] = np.ascontiguousarray(np.concatenate([_pk(c[b])] + sm_common, axis=0))
        in_maps.append(m)
    return in_maps


_CACHE = {}


def kernel(**inputs):
    in_maps = _host_prep(inputs)
    if "nc" not in _CACHE:
        _CACHE["nc"] = build_program(dbg=False)[0]
    res = run_bass_kernel_spmd(_CACHE["nc"], in_maps, core_ids=list(range(8)))
    return np.stack([np.asarray(r["y"], dtype=np.float32) for r in res.results], axis=0)
```

```python
from contextlib import ExitStack
import numpy as np
import concourse.bass as bass
import concourse.mybir as mybir
from concourse.bass_utils import run_bass_kernel_spmd

F32 = mybir.dt.float32
BF16 = mybir.dt.bfloat16
AF = mybir.ActivationFunctionType
ALU = mybir.AluOpType
AX = mybir.AxisListType

D = 1024
L = 2048
NT = 16
NE = 32
DFF = 256
EPS = 1e-6
NEG = -30000.0
NBIS = 16


class Buf:
    __slots__ = ("name", "lw", "rd")

    def __init__(self, name=""):
        self.name = name
        self.lw = None
        self.rd = []


class Sched:
    ENGS = ("pe", "act", "dve", "pool", "sp")

    def __init__(self, nc, n_dma_sems=32):
        self.nc = nc
        self.prog = {e: [] for e in self.ENGS}
        self.cnt = {e: 0 for e in self.ENGS}
        self.seen = {e: {} for e in self.ENGS}
        self.pend_rd = {e: [] for e in self.ENGS}
        self.pend_wr = {e: [] for e in self.ENGS}
        self.sems = {}
        self.n_dma = n_dma_sems
        self.dma_val = [0] * n_dma_sems
        self.dma_rr = 0
        self.dma_rr2 = [0, 0]
        self._ctx = []

    def open(self):
        nc = self.nc
        for e in self.ENGS:
            cm = nc.semaphore("s_" + e)
            self.sems[e] = cm.__enter__()
            self._ctx.append(cm)
        for i in range(self.n_dma):
            cm = nc.semaphore("s_dma%d" % i)
            self.sems[("dma", i)] = cm.__enter__()
            self._ctx.append(cm)

    def close(self):
        for cm in reversed(self._ctx):
            cm.__exit__(None, None, None)

    def _wait(self, eng, tok):
        if tok is None:
            return
        key, val = tok
        if self.seen[eng].get(key, 0) >= val:
            return
        self.seen[eng][key] = val
        sem = self.sems[key]
        self.prog[eng].append(lambda h, sem=sem, val=val: h.wait_ge(sem, val))

    def _deps(self, eng, reads, writes, pe_acc=False):
        for b in reads:
            self._wait(eng, b.lw)
        for b in writes:
            if not (pe_acc and b.lw is not None and b.lw[0] == "pe"):
                self._wait(eng, b.lw)
            for t in b.rd:
                self._wait(eng, t)

    def op(self, eng, fn, reads=(), writes=(), inc=True, pe_acc=False):
        reads = list(reads)
        writes = list(writes)
        self._deps(eng, reads, writes, pe_acc=pe_acc)
        if not inc:
            self.pend_rd[eng].extend(reads)
            self.pend_wr[eng].extend(writes)
            self.prog[eng].append(lambda h, fn=fn: fn(h))
            return
        self.cnt[eng] += 1
        tok = (eng, self.cnt[eng])
        sem = self.sems[eng]
        self.prog[eng].append(lambda h, fn=fn, sem=sem: fn(h).then_inc(sem, 1))
        for b in reads + self.pend_rd[eng]:
            b.rd.append(tok)
        for b in writes + self.pend_wr[eng]:
            b.lw = tok
            b.rd = []
        self.pend_rd[eng] = []
        self.pend_wr[eng] = []

    def dma(self, eng, fn, reads=(), writes=()):
        reads = list(reads)
        writes = list(writes)
        self._deps(eng, reads, writes)
        half = self.n_dma // 2
        qi = 0 if eng == "sp" else 1
        i = qi * half + self.dma_rr2[qi]
        self.dma_rr2[qi] = (self.dma_rr2[qi] + 1) % half
        key = ("dma", i)
        if self.dma_val[i] > 0:
            self._wait(eng, (key, self.dma_val[i]))
        self.dma_val[i] += 16
        tok = (key, self.dma_val[i])
        sem = self.sems[key]
        self.prog[eng].append(lambda h, fn=fn, sem=sem: fn(h).then_inc(sem, 16))
        for b in reads:
            b.rd.append(tok)
        for b in writes:
            b.lw = tok
            b.rd = []
        return tok

    def wait_all(self, eng, bufs):
        for b in bufs:
            self._wait(eng, b.lw)
            for t in b.rd:
                self._wait(eng, t)

    def emit(self):
        nc = self.nc
        for i in range(self.n_dma):
            if self.dma_val[i] > 0:
                self._wait("sp", (("dma", i), self.dma_val[i]))
        prog = self.prog
        with nc.Block() as block:
            @block.tensor
            def _(h):
                for f in prog["pe"]:
                    f(h)

            @block.scalar
            def _(h):
                for f in prog["act"]:
                    f(h)

            @block.vector
            def _(h):
                for f in prog["dve"]:
                    f(h)

            @block.gpsimd
            def _(h):
                for f in prog["pool"]:
                    f(h)

            @block.sync
            def _(h):
                for f in prog["sp"]:
                    f(h)
        self.prog = {e: [] for e in self.ENGS}


class Tn:
    def __init__(self, t, nbuf=1, name=""):
        self.t = t
        self.b = [Buf("%s%d" % (name, i)) for i in range(nbuf)]

    def __getitem__(self, k):
        return self.t[k]


def build_program(dbg=False, stop_after=99):
    nc = bass.Bass("TRN2", target_bir_lowering=False)
    S = Sched(nc)

    def din(name, shape, dt=F32):
        return nc.dram_tensor(name, list(shape), dt, kind="ExternalInput").ap()

    x_d = din("x", [L, D])
    smalls_d = din("smalls", [64, 128])
    rows_d = din("rows", [3, D])
    brt_d = din("brt", [1, 36])
    wada_d = din("w_ada", [D, 6 * D])
    wfm_d = din("w_fm", [4 * 128, 8 * 640])
    wtm_d = din("w_tm", [D, 648])
    nbf_d = din("nbf", [8, 1])
    wout_d = din("w_out", [D, D])
    wrt_d = din("w_rt", [D, 36])
    wg_d = din("w_gate", [NE, D, DFF])
    wu_d = din("w_up", [NE, D, DFF])
    wd_d = din("w_down", [NE, DFF, D])
    identf_d = din("identf", [128, 128])
    tri_d = din("tri", [128, 512])
    esel_d = din("esel", [128, 1024])
    alk_d = din("alibi_k", [128, 128])
    alq_d = din("alibi_q", [2, 8 * 512])
    y_d = nc.dram_tensor("y", [L, D], F32, kind="ExternalOutput").ap()
    iwe_d = nc.dram_tensor("iw_e", [128, 64], F32, kind="Internal").ap()
    iwo_d = nc.dram_tensor("iw_o", [128, 64], F32, kind="Internal").ap()
    combT_d = nc.dram_tensor("combT", [NE, L], BF16, kind="Internal").ap()
    dbg_d = {}

    def dbg_out(name, shape, dt=F32):
        dbg_d[name] = nc.dram_tensor("dbg_" + name, list(shape), dt, kind="ExternalOutput").ap()
        return dbg_d[name]

    S.open()
    es_all = ExitStack()

    def sb(es, name, shape, dt=F32, nbuf=1):
        t = es.enter_context(nc.sbuf_tensor("sb_" + name, list(shape), dt))
        return Tn(t, nbuf, name)

    def ps(es, name, shape, dt=F32, nbuf=1):
        t = es.enter_context(nc.psum_tensor("ps_" + name, list(shape), dt))
        return Tn(t, nbuf, name)

    OUTB = Buf("out")

    def phase_end(n):
        if stop_after == n:
            S.wait_all("sp", [OUTB])
            S.emit()
            return True
        S.emit()
        return False

    P = es_all
    HT = sb(P, "HT", [128, 8, L], BF16, nbuf=8)
    identb = sb(P, "identb", [128, 128], BF16)
    identf = sb(P, "identf", [128, 128], F32)
    smT = sb(P, "smT", [128, 64], F32)
    modp = sb(P, "modp", [128, 64], F32)
    gate_m = sb(P, "gate_m", [128, D], F32)
    gate_f = sb(P, "gate_f", [128, D], F32)
    scb = sb(P, "scb", [128, 8], BF16)
    screp = sb(P, "screp", [128, 8, 128], BF16)
    PSB = [ps(P, "psb%d" % i, [128, 512], F32) for i in range(8)]

    def psv_bf(i):
        return PSB[i].t[:].bitcast(BF16)

    es_dsa = ExitStack()
    QTa = sb(es_dsa, "QTa", [128, 4, L], BF16, nbuf=4)
    KTa = sb(es_dsa, "KTa", [128, 2, L], BF16, nbuf=2)
    IQT = sb(es_dsa, "IQT", [128, L, 4], BF16, nbuf=1)
    IKT = sb(es_dsa, "IKT", [128, L], BF16)
    Va = sb(es_dsa, "Va", [128, NT, 2, 65], BF16)
    wcolT = sb(es_dsa, "wcolT", [128, 128], F32)

    S.dma("sp", lambda h: h.dma_start(out=identf[:], in_=identf_d), writes=identf.b)
    S.dma("pool", lambda h: h.dma_start(out=identb[:], in_=identf_d), writes=identb.b)
    S.op("pool", lambda h: h.memset(Va[:, :, :, 64:65], 1.0), writes=Va.b)

    def norm_to_HT(es, src_tile_fn, a0, b0, tag):
        xn = sb(es, "xn" + tag, [128, 4, D], BF16, nbuf=4)
        junk = sb(es, "junk" + tag, [128, D], BF16)
        ss = sb(es, "ss" + tag, [128, NT], F32)
        rstd = sb(es, "rstd" + tag, [128, NT], F32)
        for g in range(4):
            srcs = [src_tile_fn(g * 4 + il) for il in range(4)]
            for il in range(4):
                i = g * 4 + il
                src, sbufs = srcs[il]
                S.op("act", lambda h, src=src, i=i: h.activation(out=junk[:], in_=src, func=AF.Square, accum_out=ss[:, i:i + 1]), reads=sbufs, writes=junk.b + ss.b)
            S.op("act", lambda h, g=g: h.activation(out=rstd[:, 4 * g:4 * g + 4], in_=ss[:, 4 * g:4 * g + 4], func=AF.Sqrt, bias=EPS, scale=1.0 / D), reads=ss.b, writes=rstd.b)
            S.op("dve", lambda h, g=g: h.reciprocal(rstd[:, 4 * g:4 * g + 4], rstd[:, 4 * g:4 * g + 4]), reads=rstd.b, writes=rstd.b)
            for il in range(4):
                i = g * 4 + il
                src, sbufs = srcs[il]
                S.op("dve", lambda h, src=src, i=i, il=il: h.tensor_scalar(out=xn[:, il, :], in0=src, scalar1=rstd[:, i:i + 1], scalar2=None, op0=ALU.mult), reads=sbufs + rstd.b, writes=[xn.b[il]])
            for kk in range(8):
                bank = PSB[kk // 2]
                off = (kk % 2) * 512
                for il in range(4):
                    S.op("pe", lambda h, bank=bank, off=off, il=il, kk=kk: h.transpose(psv_bf(PSB.index(bank))[:, off + il * 128: off + (il + 1) * 128], xn[:, il, kk::8], identb[:]),
                         reads=[xn.b[il]] + identb.b, writes=bank.b, inc=(il == 3), pe_acc=True)
                dst = HT[:, kk, g * 512:(g + 1) * 512]
                srcp = psv_bf(kk // 2)[:, off:off + 512]
                if kk % 2 == 0:
                    S.op("act", lambda h, dst=dst, srcp=srcp, kk=kk: h.activation(out=dst, in_=srcp, func=AF.Identity, scale=modp[:, a0 + kk:a0 + kk + 1], bias=modp[:, b0 + kk:b0 + kk + 1]),
                         reads=bank.b + modp.b, writes=[HT.b[kk]])
                else:
                    S.op("dve", lambda h, dst=dst, srcp=srcp, kk=kk: h.tensor_scalar(out=dst, in0=srcp, scalar1=modp[:, a0 + kk:a0 + kk + 1], scalar2=modp[:, b0 + kk:b0 + kk + 1], op0=ALU.mult, op1=ALU.add),
                         reads=bank.b + modp.b, writes=[HT.b[kk]])
        return rstd

    wada_v = wada_d.rearrange("(p k) n -> p k n", k=8)

    def mod_load(wa, bi, piece):
        S.dma("pool", lambda h: h.dma_start(out=wa[:, bi, :, :], in_=wada_v[:, :, piece * D:(piece + 1) * D]), writes=[wa.b[bi]])

    def mod_vec_piece(wa, bi, sl, bank):
        for kk in range(8):
            for k2 in range(8):
                S.op("pe", lambda h, kk=kk, k2=k2: h.matmul(bank[:, sl * 8 + kk:sl * 8 + kk + 1], lhsT=wa[:, bi, k2, kk::8], rhs=scb[:, k2:k2 + 1], start=(k2 == 0), stop=(k2 == 7)),
                     reads=[wa.b[bi]] + scb.b, writes=bank.b, inc=(k2 == 7 and kk == 7), pe_acc=True)

    def mod_gate_piece(wa, bi, gt, gi, brow, banks):
        for half in range(2):
            bank = banks[half]
            for k2 in range(8):
                S.op("pe", lambda h, bank=bank, k2=k2, half=half: h.matmul(bank[:, :], lhsT=screp[:, k2, :], rhs=wa[:, bi, k2, half * 512:(half + 1) * 512], start=(k2 == 0), stop=(k2 == 7)),
                     reads=[wa.b[bi]] + screp.b, writes=bank.b, inc=(k2 == 7), pe_acc=True)
            S.op("dve", lambda h, bank=bank, half=half: h.tensor_tensor(out=gt[:, half * 512:(half + 1) * 512], in0=bank[:, :], in1=brow[:, gi, half * 512:(half + 1) * 512], op=ALU.add),
                 reads=bank.b + [brow.b[gi]], writes=gt.b)

    with ExitStack() as es:
        sm_in = sb(es, "sm_in", [64, 128], F32)
        wa = sb(es, "wa", [128, 2, 8, D], BF16, nbuf=2)
        sc32 = sb(es, "sc32", [128, 8], F32)
        S.dma("sp", lambda h: h.dma_start(out=sm_in[:], in_=smalls_d), writes=sm_in.b)
        mod_load(wa, 0, 0)
        mod_load(wa, 1, 1)
        S.op("pe", lambda h: h.transpose(PSB[0][:, 0:64], sm_in[:], identf[0:64, 0:64]), reads=sm_in.b + identf.b, writes=PSB[0].b)
        S.op("dve", lambda h: h.tensor_copy(smT[:], PSB[0][:, 0:64]), reads=PSB[0].b, writes=smT.b)
        S.op("act", lambda h: h.activation(out=sc32[:], in_=smT[:, 0:8], func=AF.Silu), reads=smT.b, writes=sc32.b)
        S.op("dve", lambda h: h.tensor_copy(scb[:], sc32[:]), reads=sc32.b, writes=scb.b)
        S.op("dve", lambda h: h.tensor_copy(screp[:], sc32[:].unsqueeze(2).to_broadcast([128, 8, 128])), reads=sc32.b, writes=screp.b)
        mod_vec_piece(wa, 0, 0, PSB[1])
        mod_vec_piece(wa, 1, 1, PSB[1])
        S.op("dve", lambda h: h.tensor_tensor(out=modp[:, 32:48], in0=PSB[1][:, 0:16], in1=smT[:, 8:24], op=ALU.add), reads=PSB[1].b + smT.b, writes=modp.b)
        S.op("dve", lambda h: h.scalar_tensor_tensor(out=modp[:, 0:8], in0=modp[:, 40:48], scalar=1.0, in1=smT[:, 40:48], op0=ALU.add, op1=ALU.mult), reads=modp.b + smT.b, writes=modp.b)
        S.op("dve", lambda h: h.tensor_copy(modp[:, 8:16], modp[:, 32:40]), reads=modp.b, writes=modp.b)
        xt = sb(es, "xt", [128, 4, D], F32, nbuf=4)
        x_v = x_d.rearrange("(i p) d -> i p d", p=128)

        def src1(i):
            bi = i % 4
            S.dma("sp", lambda h, i=i, bi=bi: h.dma_start(out=xt[:, bi, :], in_=x_v[i]), writes=[xt.b[bi]])
            return xt[:, bi, :], [xt.b[bi]]

        norm_to_HT(es, src1, 0, 8, "1")
        if dbg:
            d3 = dbg_out("hT", [128, 8 * L], BF16)
            S.dma("sp", lambda h: h.dma_start(out=d3, in_=HT[:].rearrange("p k t -> p (k t)")), reads=HT.b, writes=[OUTB])
        S.emit()

    def deferred_mod(es):
        wa2 = sb(es, "wa2", [128, 2, 8, D], BF16, nbuf=2)
        brow = sb(es, "brow", [128, 1, D], F32, nbuf=1)
        S.dma("sp", lambda h: h.dma_start(out=brow[:, 0, :], in_=rows_d[0:1, :].partition_broadcast(128)), writes=[brow.b[0]])
        mod_load(wa2, 0, 3)
        mod_load(wa2, 1, 4)

        def stage1():
            mod_vec_piece(wa2, 0, 0, PSB[5])
            mod_vec_piece(wa2, 1, 1, PSB[5])
            S.op("dve", lambda h: h.tensor_tensor(out=modp[:, 48:64], in0=PSB[5][:, 0:16], in1=smT[:, 24:40], op=ALU.add), reads=PSB[5].b + smT.b, writes=modp.b)
            S.op("dve", lambda h: h.scalar_tensor_tensor(out=modp[:, 16:24], in0=modp[:, 56:64], scalar=1.0, in1=smT[:, 48:56], op0=ALU.add, op1=ALU.mult), reads=modp.b + smT.b, writes=modp.b)
            S.op("dve", lambda h: h.tensor_copy(modp[:, 24:32], modp[:, 48:56]), reads=modp.b, writes=modp.b)
            mod_load(wa2, 0, 2)
            mod_load(wa2, 1, 5)

        def stage2():
            mod_gate_piece(wa2, 0, gate_m, 0, brow, (PSB[4], PSB[5]))
            S.dma("sp", lambda h: h.dma_start(out=brow[:, 0, :], in_=rows_d[1:2, :].partition_broadcast(128)), writes=[brow.b[0]])
            mod_gate_piece(wa2, 1, gate_f, 0, brow, (PSB[4], PSB[5]))
        return stage1, stage2

    es_fox = ExitStack()
    QTb = sb(es_fox, "QTb", [128, 4, L], BF16, nbuf=4)
    KTb = sb(es_fox, "KTb", [128, 4, L], BF16, nbuf=4)
    Vb = sb(es_fox, "Vb", [128, NT, 8, 65], BF16)
    cum3 = sb(es_fox, "cum3", [67, 3, L], BF16)
    csT = sb(es_fox, "csT", [128, NT, 8], F32)
    S.op("pool", lambda h: h.memset(Vb[:, :, :, 64:65], 1.0), writes=Vb.b)

    with ExitStack() as es:
        wf = sb(es, "wf", [128, 2, 8, 640], BF16, nbuf=2)
        wt = sb(es, "wt", [128, 8, 648], BF16, nbuf=8)
        iw_tm = sb(es, "iw_tm", [128, NT, 8], F32)
        e1 = sb(es, "e1", [8, L], F32)
        nbf = sb(es, "nbf", [8, 1], F32)
        bfg = sb(es, "bfg", [8, 1], F32)
        wfm_v = wfm_d.rearrange("(q p) (k c) -> q p k c", p=128, c=640)
        wtm_v = wtm_d.rearrange("(p k) n -> p k n", k=8)
        for kk in range(8):
            S.dma("pool", lambda h, kk=kk: h.dma_start(out=wt[:, kk, :], in_=wtm_v[:, kk, :]), writes=[wt.b[kk]])
        S.dma("sp", lambda h: h.dma_start(out=bfg[:], in_=nbf_d), writes=bfg.b)
        S.op("dve", lambda h: h.tensor_scalar(out=nbf[:], in0=bfg[:], scalar1=-1.0, scalar2=None, op0=ALU.mult), reads=bfg.b, writes=nbf.b)
        dests = []
        for p_ in range(4):
            dests.append((QTa, p_))
        dests += [(KTa, 0), (KTa, 1)]
        for p_ in range(4):
            dests.append((IQT, p_))
        dests.append((IKT, None))
        for p_ in range(4):
            dests.append((QTb, p_))
        for p_ in range(4):
            dests.append((KTb, p_))
        pieces = [(0, 5), (5, 10), (10, 15), (15, 19)]
        cnt_ = {'ev': 0, 'pb': 0}
        def fm_part():
            ev = 0
            pb = 0
            for pi, (c0, c1) in enumerate(pieces):
                bi = pi % 2
                ncol = (c1 - c0) * 128
                S.dma("pool", lambda h, bi=bi, pi=pi: h.dma_start(out=wf[:, bi, :, :], in_=wfm_v[pi]), writes=[wf.b[bi]])
                for ch in range(c0, c1):
                    T_, idx = dests[ch]
                    for ng in range(4):
                        bank = PSB[4 + (pb % 4)]
                        pb += 1
                        for kk in range(8):
                            S.op("pe", lambda h, bank=bank, bi=bi, kk=kk, ch=ch, c0=c0, ng=ng: h.matmul(bank[:, :], lhsT=wf[:, bi, kk, (ch - c0) * 128:(ch - c0 + 1) * 128], rhs=HT[:, kk, ng * 512:(ng + 1) * 512], start=(kk == 0), stop=(kk == 7)),
                                 reads=[wf.b[bi], HT.b[kk]], writes=bank.b, inc=(kk == 7), pe_acc=True)
                        if idx is None:
                            dst = T_[:, ng * 512:(ng + 1) * 512]
                            wb_ = T_.b
                        elif T_ is IQT:
                            dst = T_[:, ng * 512:(ng + 1) * 512, idx]
                            wb_ = T_.b
                        else:
                            dst = T_[:, idx, ng * 512:(ng + 1) * 512]
                            wb_ = [T_.b[idx]]
                        if ev % 2 == 0:
                            S.op("act", lambda h, dst=dst, bank=bank: h.activation(out=dst, in_=bank[:, :], func=AF.Copy), reads=bank.b, writes=wb_)
                        else:
                            S.op("dve", lambda h, dst=dst, bank=bank: h.tensor_copy(dst, bank[:, :]), reads=bank.b, writes=wb_)
                        ev += 1
        wbf = sb(es, "wbf", [128, 8, 8], BF16)
        S.dma("pool", lambda h: h.dma_start(out=wbf[:], in_=wfm_v[3][:, :, 512:520]), writes=wbf.b)

        def bf_part():
            for ng in range(4):
                bank = PSB[4 + ng]
                for kk in range(8):
                    S.op("pe", lambda h, bank=bank, kk=kk, ng=ng: h.matmul(bank[0:8, :], lhsT=wbf[:, kk, :], rhs=HT[:, kk, ng * 512:(ng + 1) * 512], start=(kk == 0), stop=(kk == 7)),
                         reads=wbf.b + [HT.b[kk]], writes=bank.b, inc=(kk == 7), pe_acc=True)
                S.op("act", lambda h, bank=bank, ng=ng: h.activation(out=e1[:, ng * 512:(ng + 1) * 512], in_=bank[0:8, :], func=AF.Exp, scale=-1.0, bias=nbf[:, 0:1]), reads=bank.b + nbf.b, writes=e1.b)
        def tm_part():
            for i in range(NT):
                bA = PSB[(2 * i) % 4]
                bB = PSB[(2 * i + 1) % 4]
                for kk in range(8):
                    S.op("pe", lambda h, bA=bA, kk=kk, i=i: h.matmul(bA[:, :], lhsT=HT[:, kk, i * 128:(i + 1) * 128], rhs=wt[:, kk, 0:512], start=(kk == 0), stop=(kk == 7)),
                         reads=[HT.b[kk], wt.b[kk]], writes=bA.b, inc=(kk == 7), pe_acc=True)
                for kk in range(8):
                    S.op("pe", lambda h, bB=bB, kk=kk, i=i: h.matmul(bB[:, 0:136], lhsT=HT[:, kk, i * 128:(i + 1) * 128], rhs=wt[:, kk, 512:648], start=(kk == 0), stop=(kk == 7)),
                         reads=[HT.b[kk], wt.b[kk]], writes=bB.b, inc=(kk == 7), pe_acc=True)
                S.op("act", lambda h, bA=bA, i=i: h.activation(out=Vb[:, i, :, 0:64], in_=bA[:, :].rearrange("p (h d) -> p h d", h=8), func=AF.Copy), reads=bA.b, writes=Vb.b)
                S.op("dve", lambda h, bB=bB, i=i: h.tensor_copy(Va[:, i, :, 0:64], bB[:, 0:128].rearrange("p (h d) -> p h d", h=2)), reads=bB.b, writes=Va.b)
                S.op("dve", lambda h, bB=bB, i=i: h.tensor_copy(iw_tm[:, i, :], bB[:, 128:136]), reads=bB.b, writes=iw_tm.b)
        tm_part()
        bf_part()
        S.op("act", lambda h: h.activation(out=e1[:], in_=e1[:], func=AF.Ln, bias=1.0, scale=1.0), reads=e1.b, writes=e1.b)
        cs = sb(es, "cs", [8, L], F32)
        S.op("dve", lambda h: h.tensor_tensor_scan(out=cs[:], data0=e1[:], data1=e1[:], initial=0.0, op0=ALU.add, op1=ALU.max), reads=e1.b, writes=cs.b)
        for j in range(NT):
            S.op("pe", lambda h, j=j: h.transpose(PSB[4][:, j * 8:(j + 1) * 8], cs[:, j * 128:(j + 1) * 128], identf[0:8, 0:8]), reads=cs.b + identf.b, writes=PSB[4].b, inc=(j == NT - 1), pe_acc=True)
        S.op("dve", lambda h: h.tensor_copy(csT[:].rearrange("p j h -> p (j h)"), PSB[4][:, 0:128]), reads=PSB[4].b, writes=csT.b)
        hi3 = Tn(e1.t, 2, "hi3")
        hi3v = e1[:].bitcast(BF16).rearrange("p (a t) -> p a t", a=2)
        S.op("dve", lambda h: h.tensor_scalar(out=cs[:], in0=cs[:], scalar1=-8.0, scalar2=None, op0=ALU.mult), reads=cs.b, writes=cs.b)
        for r in range(3):
            S.op("dve", lambda h, r=r: h.tensor_copy(hi3v[:, r % 2, :], cs[:]), reads=cs.b, writes=[hi3.b[r % 2]] + (e1.b if r < 2 else []))
            if r < 2:
                S.op("dve", lambda h, r=r: h.tensor_tensor(out=cs[:], in0=cs[:], in1=hi3v[:, r % 2, :], op=ALU.subtract), reads=cs.b + [hi3.b[r % 2]], writes=cs.b)
            for hh in range(8):
                pp = 32 * (hh % 3) + r
                S.dma("sp", lambda h, hh=hh, pp=pp, r=r: h.dma_start(out=cum3[pp:pp + 1, hh // 3, :], in_=hi3v[hh:hh + 1, r % 2, :]), reads=[hi3.b[r % 2]], writes=cum3.b)
        iwe_flat = iwe_d.rearrange("g c -> (g c)").rearrange("(i t b) -> t i b", i=16, t=128, b=4)
        iwo_flat = iwo_d.rearrange("g c -> (g c)").rearrange("(i t b) -> t i b", i=16, t=128, b=4)
        SCR = Buf("scr")
        S.dma("sp", lambda h: h.dma_start(out=iwe_flat, in_=iw_tm[:, :, 0:4]), reads=iw_tm.b, writes=[SCR])
        S.dma("sp", lambda h: h.dma_start(out=iwo_flat, in_=iw_tm[:, :, 4:8]), reads=iw_tm.b, writes=[SCR])
        wc_in = sb(es, "wc_in", [128, 128], F32)
        S.dma("sp", lambda h: h.dma_start(out=wc_in[:, 0:64], in_=iwe_d), reads=[SCR], writes=wc_in.b)
        S.dma("sp", lambda h: h.dma_start(out=wc_in[:, 64:128], in_=iwo_d), reads=[SCR], writes=wc_in.b)
        fm_part()
        S.op("pe", lambda h: h.transpose(PSB[5][:, 0:128], wc_in[:], identf[:]), reads=wc_in.b + identf.b, writes=PSB[5].b)
        S.op("dve", lambda h: h.tensor_copy(wcolT[:], PSB[5][:, 0:128]), reads=PSB[5].b, writes=wcolT.b)
        if dbg:
            for nm, T_, shp, dt_ in [("QTa", QTa, [128, 4 * L], BF16), ("KTa", KTa, [128, 2 * L], BF16), ("IKT", IKT, [128, L], BF16),
                                     ("QTb", QTb, [128, 4 * L], BF16), ("KTb", KTb, [128, 4 * L], BF16), ("Vb", Vb, [128, NT * 8 * 65], BF16), ("Va", Va, [128, NT * 2 * 65], BF16),
                                     ("csT", csT, [128, NT * 8], F32), ("cum3", cum3, [67, 3 * L], BF16), ("wcolT", wcolT, [128, 128], F32)]:
                dd = dbg_out(nm, shp, dt_)
                nd = len(T_.t.shape)
                if nd == 2:
                    src_ap = T_[:]
                elif nd == 3:
                    src_ap = T_[:].rearrange("p a b -> p (a b)")
                else:
                    src_ap = T_[:].rearrange("p a b c -> p (a b c)")
                S.dma("sp", lambda h, dd=dd, src_ap=src_ap: h.dma_start(out=dd, in_=src_ap), reads=T_.b, writes=[OUTB])
        if phase_end(1):
            return nc, dbg_d

    def attn_scratch(es, tag):
        sc = {}
        sc["ones67"] = sb(es, "ones67" + tag, [67, 128], BF16)
        sc["PT"] = sb(es, "PT" + tag, [128, 4, 512], BF16, nbuf=4)
        sc["o_sb"] = sb(es, "o_sb" + tag, [128, 4, 512], F32)
        sc["ob"] = sb(es, "ob" + tag, [128, 4, 512], BF16)
        sc["rec"] = sb(es, "rec" + tag, [128, 8, 4], F32)
        sc["oss"] = sb(es, "oss" + tag, [128, 8], F32)
        sc["ojunk"] = sb(es, "ojunk" + tag, [128, 512], BF16)
        S.op("pool", lambda h: h.memset(sc["ones67"][:], 1.0), writes=sc["ones67"].b)
        return sc

    def attention_chunk(sc, c, KT, kt_idx, QT, aug_lhs, aug_rhs, mask_rhs, bias_ap, Vt, v_idx, tick=None):
        PT, o_sb, rec = sc["PT"], sc["o_sb"], sc["rec"]
        T0 = 512 * c
        nj = 4 * c + 4
        steps = [(pr, j) for pr in range(4) for j in range(nj)]

        def s_stage(k):
            pr, j = steps[k]
            col0 = max(0, j - 4 * c) * 128
            ncols = 512 - col0
            heads = (2 * pr, 2 * pr + 1)
            Sbs = (PSB[(2 * k) % 4], PSB[(2 * k + 1) % 4])
            for hh, Sb in zip(heads, Sbs):
                rows = slice(64 * (hh % 2), 64 * (hh % 2) + 64)
                S.op("pe", lambda h, hh=hh, Sb=Sb, rows=rows: h.matmul(Sb[:, 0:ncols], lhsT=KT[rows, kt_idx(hh), j * 128:(j + 1) * 128], rhs=QT[rows, pr, T0 + col0:T0 + 512], start=True, stop=False),
                     reads=[KT.b[kt_idx(hh)], QT.b[pr]], writes=Sb.b, inc=False, pe_acc=True)
            mr, mbufs = mask_rhs(j, col0, ncols)
            if mr is not None:
                for hh, Sb in zip(heads, Sbs):
                    S.op("pe", lambda h, Sb=Sb: h.matmul(Sb[:, 0:ncols], lhsT=identb[:], rhs=mr, start=False, stop=False),
                         reads=identb.b + mbufs, writes=Sb.b, inc=False, pe_acc=True)
            for hh, Sb in zip(heads, Sbs):
                al, albufs = aug_lhs(hh)
                ar, arbufs = aug_rhs(hh, col0)
                S.op("pe", lambda h, Sb=Sb, al=al, ar=ar: h.matmul(Sb[:, 0:ncols], lhsT=al, rhs=ar, start=False, stop=True),
                     reads=albufs + arbufs, writes=Sb.b, inc=True, pe_acc=True)
            for q_, (hh, Sb) in enumerate(zip(heads, Sbs)):
                pt = (2 * k + q_) % 4
                bap, bbufs = bias_ap(hh, j)
                S.op("act", lambda h, Sb=Sb, pt=pt, bap=bap: h.activation(out=PT[:, pt, 0:ncols], in_=Sb[:, 0:ncols], func=AF.Exp, scale=0.125, bias=bap),
                     reads=Sb.b + bbufs, writes=[PT.b[pt]])

        def pv_stage(k):
            pr, j = steps[k]
            col0 = max(0, j - 4 * c) * 128
            il0 = max(0, j - 4 * c)
            for q_ in range(2):
                hh = 2 * pr + q_
                pt = (2 * k + q_) % 4
                Ob = PSB[(6 if pr % 2 == 0 else 4) + q_]
                for il in range(il0, 4):
                    i = 4 * c + il
                    first = (j == 0 and il == il0)
                    S.op("pe", lambda h, il=il, i=i, first=first, Ob=Ob, pt=pt, hh=hh: h.matmul(Ob[:, il * 65:(il + 1) * 65], lhsT=PT[:, pt, il * 128 - col0:il * 128 - col0 + 128], rhs=Vt[:, j, v_idx(hh), :], start=first, stop=(j == i), skip_group_check=True),
                         reads=[PT.b[pt]] + Vt.b, writes=Ob.b, inc=(il == 3), pe_acc=True)
                if j == nj - 1:
                    Ov = Ob[:, 0:260].rearrange("p (a b) -> p a b", b=65)
                    S.op("dve", lambda h, Ov=Ov, hh=hh: h.reciprocal(rec[:, hh, :], Ov[:, :, 64]), reads=Ob.b, writes=rec.b)
                    S.op("dve", lambda h, Ov=Ov, hh=hh: h.tensor_tensor(out=o_sb[:, :, hh * 64:(hh + 1) * 64], in0=Ov[:, :, 0:64], in1=rec[:, hh, :].unsqueeze(2).to_broadcast([128, 4, 64]), op=ALU.mult),
                         reads=Ob.b + rec.b, writes=o_sb.b)

        n = len(steps)
        s_stage(0)
        for k in range(n):
            if k + 1 < n:
                s_stage(k + 1)
            pv_stage(k)
            if tick is not None:
                tick(k, n)

    def out_norm(sc, c, base):
        o_sb, ob, oss, ojunk = sc["o_sb"], sc["ob"], sc["oss"], sc["ojunk"]
        T0 = 512 * c
        for il in range(4):
            S.op("act", lambda h, il=il: h.activation(out=ojunk[:], in_=o_sb[:, il, :], func=AF.Square, accum_out=oss[:, il:il + 1]), reads=o_sb.b, writes=ojunk.b + oss.b)
        S.op("act", lambda h: h.activation(out=oss[:, 4:8], in_=oss[:, 0:4], func=AF.Sqrt, bias=EPS, scale=1.0 / 512), reads=oss.b, writes=oss.b)
        S.op("dve", lambda h: h.reciprocal(oss[:, 4:8], oss[:, 4:8]), reads=oss.b, writes=oss.b)
        S.op("dve", lambda h: h.tensor_tensor(out=ob[:], in0=o_sb[:], in1=oss[:, 4:8].unsqueeze(2).to_broadcast([128, 4, 512]), op=ALU.mult), reads=o_sb.b + oss.b, writes=ob.b)
        for fc in range(4):
            bank = PSB[4 + fc % 2]
            bi = 4 + fc % 2
            for il in range(4):
                S.op("pe", lambda h, bi=bi, il=il, fc=fc: h.transpose(psv_bf(bi)[:, il * 128:(il + 1) * 128], ob[:, il, fc * 128:(fc + 1) * 128], identb[:]),
                     reads=ob.b + identb.b, writes=bank.b, inc=(il == 3), pe_acc=True)
            S.op("act", lambda h, bi=bi, fc=fc: h.activation(out=HT[:, base + fc, T0:T0 + 512], in_=psv_bf(bi)[:, 0:512], func=AF.Identity, scale=smT[:, 56 + base + fc:57 + base + fc]),
                 reads=bank.b + smT.b, writes=[HT.b[base + fc]])

    with ExitStack() as es:
        sc = attn_scratch(es, "f")
        triw = sb(es, "triw", [128, 512], BF16)
        S.dma("pool", lambda h: h.dma_start(out=triw[:], in_=tri_d), writes=triw.b)
        mod_st1, mod_st2 = deferred_mod(es)
        for c in range(4):
            if c == 1:
                mod_st1()
            if c == 2:
                mod_st2()
            attention_chunk(
                sc, c, KTb, lambda hh: hh // 2, QTb,
                aug_lhs=lambda hh: (sc["ones67"][32 * (hh % 3):32 * (hh % 3) + 3, :], sc["ones67"].b),
                aug_rhs=lambda hh, col0, c=c: (cum3[32 * (hh % 3):32 * (hh % 3) + 3, hh // 3, 512 * c + col0:512 * c + 512], cum3.b),
                mask_rhs=lambda j, col0, ncols, c=c: ((triw[:, 0:ncols], triw.b) if j >= 4 * c else (None, [])),
                bias_ap=lambda hh, j: (csT[:, j, hh:hh + 1], csT.b),
                Vt=Vb, v_idx=lambda hh: hh)
            out_norm(sc, c, 4)
        if dbg:
            d4b = dbg_out("oTb", [128, 8 * L], BF16)
            S.dma("sp", lambda h: h.dma_start(out=d4b, in_=HT[:].rearrange("p k t -> p (k t)")), reads=HT.b, writes=[OUTB])
        if phase_end(2):
            return nc, dbg_d
    es_fox.close()

    with ExitStack() as es:
        sc = attn_scratch(es, "a")
        IS = sb(es, "IS", [128, 4, L], F32, nbuf=4)
        NM = sb(es, "NM", [128, L], BF16)
        NMT2 = sb(es, "NMT", [128, 2, NT, 512], BF16, nbuf=2)
        R = sb(es, "R", [128, 4, 512], BF16, nbuf=4)
        Wblk = sb(es, "Wblk", [128, 2, 8, 128], BF16, nbuf=2)
        esel = sb(es, "esel", [128, 8, 128], BF16)
        alk = sb(es, "alk", [128, 8, 16], F32)
        alq = sb(es, "alq", [66, 8, 512], BF16)
        bs = sb(es, "bs", [128, 32], F32)
        cjunk = sb(es, "cjunk", [128, L], BF16)
        S.dma("pool", lambda h: h.dma_start(out=esel[:].rearrange("p a b -> p (a b)"), in_=esel_d), writes=esel.b)
        S.dma("pool", lambda h: h.dma_start(out=alq[0:2].rearrange("p a b -> p (a b)"), in_=alq_d), writes=alq.b)
        S.dma("pool", lambda h: h.dma_start(out=alq[64:66].rearrange("p a b -> p (a b)"), in_=alq_d), writes=alq.b)
        S.dma("sp", lambda h: h.dma_start(out=alk[:].rearrange("p a b -> p (a b)"), in_=alk_d), writes=alk.b)
        evc = [0]

        def indexer(c):
            units = []
            for il in range(4):
                i = 4 * c + il
                ncols = 128 * (i + 1)
                for kc in range((ncols + 511) // 512):
                    for g in range(8):
                        units.append((il, kc, g))

            def dots(u):
                il, kc, g = units[u]
                i = 4 * c + il
                n = min(512, 128 * (i + 1) - 512 * kc)
                tok0 = 128 * i + 16 * g
                Db = PSB[u % 4]
                for hf in range(2):
                    rs = slice(64 * hf, 64 * hf + 64)
                    S.op("pe", lambda h, rs=rs: h.matmul(Db[rs, 0:n], lhsT=IQT[rs, tok0:tok0 + 16, :].rearrange("p t a -> p (t a)"), rhs=IKT[rs, 512 * kc:512 * kc + n], start=True, stop=True),
                         reads=IQT.b + IKT.b, writes=Db.b, inc=(hf == 1), pe_acc=True)
                if evc[0] % 4 != 3:
                    S.op("act", lambda h: h.activation(out=R[:, u % 4, 0:n], in_=Db[:, 0:n], func=AF.Relu), reads=Db.b, writes=[R.b[u % 4]])
                else:
                    S.op("dve", lambda h: h.tensor_scalar(out=R[:, u % 4, 0:n], in0=Db[:, 0:n], scalar1=0.0, scalar2=None, op0=ALU.max), reads=Db.b, writes=[R.b[u % 4]])
                evc[0] += 1

            def headsum(u):
                il, kc, g = units[u]
                i = 4 * c + il
                ncols = 128 * (i + 1)
                n = min(512, ncols - 512 * kc)
                ISb = PSB[4 + kc % 2]
                wb = i % 2
                if kc == 0 and g == 0:
                    S.op("pool", lambda h: h.tensor_tensor(out=Wblk[:, wb], in0=esel[:], in1=wcolT[:, 8 * i:8 * i + 8].unsqueeze(2).to_broadcast([128, 8, 128]), op=ALU.mult), reads=esel.b + wcolT.b, writes=[Wblk.b[wb]])
                S.op("pe", lambda h: h.matmul(ISb[:, 0:n], lhsT=Wblk[:, wb, g, :], rhs=R[:, u % 4, 0:n], start=(g == 0), stop=(g == 7)),
                     reads=[Wblk.b[wb], R.b[u % 4]], writes=ISb.b, inc=(g == 7), pe_acc=True)
                if g == 7:
                    S.op("act", lambda h: h.activation(out=IS[:, il, 512 * kc:512 * kc + n], in_=ISb[:, 0:n], func=AF.Copy), reads=ISb.b, writes=[IS.b[il]])
                    if 512 * kc + n == ncols:
                        S.op("dve", lambda h: h.tensor_reduce(out=bs[:, 20 + il:21 + il], in_=IS[:, il, 0:ncols], axis=AX.X, op=ALU.max, apply_absolute_value=True), reads=[IS.b[il]], writes=bs.b)
                        S.op("pool", lambda h: h.affine_select(out=IS[:, il, 128 * i:128 * i + 128], in_=IS[:, il, 128 * i:128 * i + 128], pattern=[[-1, 128]], compare_op=ALU.is_ge, fill=-1e30, base=0, channel_multiplier=1),
                             reads=[IS.b[il]], writes=[IS.b[il]])

            nu = len(units)
            dots(0)
            if nu > 1:
                dots(1)
            for u in range(nu):
                if u + 2 < nu:
                    dots(u + 2)
                headsum(u)

        def bisect_gen(c):
            S.op("dve", lambda h: h.tensor_scalar(out=bs[:, 0:4], in0=bs[:, 20:24], scalar1=-1.001, scalar2=-1e-3, op0=ALU.mult, op1=ALU.add), reads=bs.b, writes=bs.b)
            S.op("dve", lambda h: h.tensor_scalar(out=bs[:, 4:8], in0=bs[:, 20:24], scalar1=2.002, scalar2=2e-3, op0=ALU.mult, op1=ALU.add), reads=bs.b, writes=bs.b)
            for jb in range(1, NBIS + 1):
                stp = 2.0 ** (-jb)
                S.op("dve", lambda h, stp=stp: h.scalar_tensor_tensor(out=bs[:, 8:12], in0=bs[:, 4:8], scalar=stp, in1=bs[:, 0:4], op0=ALU.mult, op1=ALU.add), reads=bs.b, writes=bs.b)
                for il in range(4):
                    ncols = 128 * (4 * c + il + 1)
                    S.op("dve", lambda h, il=il, ncols=ncols: h.tensor_scalar(out=cjunk[:, 0:ncols], in0=IS[:, il, 0:ncols], scalar1=bs[:, 8 + il:9 + il], scalar2=0.0, op0=ALU.is_ge, op1=ALU.add, accum_out=bs[:, 12 + il:13 + il]),
                         reads=[IS.b[il]] + bs.b, writes=cjunk.b + bs.b)
                S.op("dve", lambda h, stp=stp: h.tensor_scalar(out=bs[:, 16:20], in0=bs[:, 12:16], scalar1=255.5, scalar2=stp, op0=ALU.is_ge, op1=ALU.mult), reads=bs.b, writes=bs.b)
                S.op("dve", lambda h: h.tensor_tensor(out=bs[:, 16:20], in0=bs[:, 16:20], in1=bs[:, 4:8], op=ALU.mult), reads=bs.b, writes=bs.b)
                S.op("dve", lambda h: h.tensor_tensor(out=bs[:, 0:4], in0=bs[:, 0:4], in1=bs[:, 16:20], op=ALU.add), reads=bs.b, writes=bs.b)
                yield

        def mask_epilogue(c):
            tb = 0
            nb_ = c % 2
            for il in range(4):
                i = 4 * c + il
                ncols = 128 * (i + 1)
                S.op("dve", lambda h, il=il, ncols=ncols: h.tensor_scalar(out=NM[:, 0:ncols], in0=IS[:, il, 0:ncols], scalar1=bs[:, il:il + 1], scalar2=NEG, op0=ALU.is_lt, op1=ALU.mult), reads=[IS.b[il]] + bs.b, writes=NM.b)
                for j0 in range(0, i + 1, 8):
                    nb = min(8, i + 1 - j0)
                    bi = 6 + tb % 2
                    tb += 1
                    for jj in range(nb):
                        S.op("pe", lambda h, bi=bi, jj=jj, j0=j0: h.transpose(psv_bf(bi)[:, jj * 128:(jj + 1) * 128], NM[:, (j0 + jj) * 128:(j0 + jj + 1) * 128], identb[:]),
                             reads=NM.b + identb.b, writes=PSB[bi].b, inc=(jj == nb - 1), pe_acc=True)
                    S.op("act", lambda h, bi=bi, j0=j0, nb=nb, il=il: h.activation(out=NMT2[:, nb_, j0:j0 + nb, il * 128:(il + 1) * 128], in_=psv_bf(bi)[:, 0:nb * 128].rearrange("p (a b) -> p a b", b=128), func=AF.Copy),
                         reads=PSB[bi].b, writes=[NMT2.b[nb_]])

        order = [3, 2, 1, 0]
        indexer(order[0])
        for _ in bisect_gen(order[0]):
            pass
        mask_epilogue(order[0])
        for oi, c in enumerate(order):
            gen = None
            cn = order[oi + 1] if oi + 1 < 4 else None
            if cn is not None:
                indexer(cn)
                gen = bisect_gen(cn)
            nsteps = 4 * (4 * c + 4)
            every = max(1, nsteps // (NBIS + 1))

            def tick(k, n, gen=gen, every=every):
                if gen is not None and k % every == 0:
                    next(gen, None)
            nb_ = c % 2
            attention_chunk(
                sc, c, KTa, lambda hh: hh // 4, QTa,
                aug_lhs=lambda hh: (sc["ones67"][64 * (hh % 2):64 * (hh % 2) + 2, :], sc["ones67"].b),
                aug_rhs=lambda hh, col0: (alq[64 * (hh % 2):64 * (hh % 2) + 2, hh, col0:512], alq.b),
                mask_rhs=lambda j, col0, ncols, nb_=nb_: (NMT2[:, nb_, j, col0:512], [NMT2.b[nb_]]),
                bias_ap=lambda hh, j, c=c: (alk[:, hh, 4 * c - j + 3:4 * c - j + 4], alk.b),
                Vt=Va, v_idx=lambda hh: hh // 4, tick=tick)
            if gen is not None:
                for _ in gen:
                    pass
                mask_epilogue(cn)
            out_norm(sc, c, 0)
        if dbg:
            for nm, T_, shp, dt_ in [("IS", IS, [128, 4 * L], F32), ("bs", bs, [128, 32], F32), ("osb", sc["o_sb"], [128, 4 * 512], F32)]:
                dd = dbg_out(nm, shp, dt_)
                src_ap = T_[:] if len(T_.t.shape) == 2 else T_[:].rearrange("p a b -> p (a b)")
                S.dma("sp", lambda h, dd=dd, src_ap=src_ap: h.dma_start(out=dd, in_=src_ap), reads=T_.b, writes=[OUTB])
            d4 = dbg_out("oT", [128, 8 * L], BF16)
            S.dma("sp", lambda h: h.dma_start(out=d4, in_=HT[:].rearrange("p k t -> p (k t)")), reads=HT.b, writes=[OUTB])
        if phase_end(3):
            return nc, dbg_d
    es_dsa.close()

    with ExitStack() as es:
        X1 = sb(es, "X1", [128, NT, D], F32, nbuf=NT)
        x_v = x_d.rearrange("(i p) d -> i p d", p=128)
        with ExitStack() as es2:
            wo = sb(es2, "wo", [128, 8, D], BF16, nbuf=8)
            xt2 = sb(es2, "xt2", [128, 2, D], F32, nbuf=2)
            wout_v = wout_d.rearrange("(c p) d -> p c d", p=128)
            for fc in range(8):
                S.dma("pool", lambda h, fc=fc: h.dma_start(out=wo[:, fc, :], in_=wout_v[:, fc, :]), writes=[wo.b[fc]])
            for fc in range(8):
                S.op("dve" if fc % 2 == 0 else "pool", lambda h, fc=fc: h.tensor_tensor(out=wo[:, fc, :], in0=wo[:, fc, :], in1=gate_m[:], op=ALU.mult), reads=[wo.b[fc]] + gate_m.b, writes=[wo.b[fc]])
            for i in range(NT):
                bi = i % 2
                S.dma("sp", lambda h, i=i, bi=bi: h.dma_start(out=xt2[:, bi, :], in_=x_v[i]), writes=[xt2.b[bi]])
                for half in range(2):
                    bank = PSB[(2 * i + half) % 4]
                    for fc in range(8):
                        S.op("pe", lambda h, bank=bank, fc=fc, i=i, half=half: h.matmul(bank[:, :], lhsT=HT[:, fc, i * 128:(i + 1) * 128], rhs=wo[:, fc, half * 512:(half + 1) * 512], start=(fc == 0), stop=(fc == 7)),
                             reads=[HT.b[fc], wo.b[fc]], writes=bank.b, inc=(fc == 7), pe_acc=True)
                    S.op("dve", lambda h, bank=bank, i=i, half=half, bi=bi: h.tensor_tensor(out=X1[:, i, half * 512:(half + 1) * 512], in0=bank[:, :], in1=xt2[:, bi, half * 512:(half + 1) * 512], op=ALU.add),
                         reads=bank.b + [xt2.b[bi]], writes=[X1.b[i]])
            if dbg:
                d5 = dbg_out("X1", [128, NT * D], F32)
                S.dma("sp", lambda h: h.dma_start(out=d5, in_=X1[:].rearrange("p a b -> p (a b)")), reads=X1.b, writes=[OUTB])
            if phase_end(4):
                return nc, dbg_d
        with ExitStack() as es2:
            norm_to_HT(es2, lambda i: (X1[:, i, :], [X1.b[i]]), 16, 24, "2")
            if phase_end(5):
                return nc, dbg_d
        with ExitStack() as es2:
            combT = sb(es2, "combT", [32, L], BF16)
            sel = sb(es2, "sel", [32, NE * 128], BF16)
            es3 = ExitStack()
            wr = sb(es3, "wr", [128, 8, 36], BF16)
            brt = sb(es3, "brt", [128, 36], F32)
            lg = sb(es3, "lg", [128, NT, 36], F32)
            rt = sb(es3, "rt", [128, NT, 64], F32)
            comb = sb(es3, "comb", [128, NT, 32], BF16)
            S.dma("pool", lambda h: h.dma_start(out=wr[:], in_=wrt_d.rearrange("(p k) n -> p k n", k=8)), writes=wr.b)
            S.dma("sp", lambda h: h.dma_start(out=brt[:], in_=brt_d.partition_broadcast(128)), writes=brt.b)
            for i in range(NT):
                bank = PSB[i % 4]
                for kk in range(8):
                    S.op("pe", lambda h, bank=bank, kk=kk, i=i: h.matmul(bank[:, 0:36], lhsT=HT[:, kk, i * 128:(i + 1) * 128], rhs=wr[:, kk, :], start=(kk == 0), stop=(kk == 7)),
                         reads=[HT.b[kk]] + wr.b, writes=bank.b, inc=(kk == 7), pe_acc=True)
                S.op("dve", lambda h, bank=bank, i=i: h.tensor_tensor(out=lg[:, i, :], in0=bank[:, 0:36], in1=brt[:], op=ALU.add), reads=bank.b + brt.b, writes=lg.b)
            RB = rt.b + lg.b

            def dv(fn):
                S.op("dve", fn, reads=RB, writes=RB)

            def bc(ap, n):
                return ap.unsqueeze(2).to_broadcast([128, NT, n])
            gl = lg[:, :, 0:4]
            gmax, gsum, pg, m1, m2, w1, w2 = (rt[:, :, k] for k in range(7))
            ohg, gsh, el, tmp8, oh1, oh2, el2 = rt[:, :, 8:12], rt[:, :, 12:16], rt[:, :, 16:24], rt[:, :, 24:32], rt[:, :, 32:40], rt[:, :, 40:48], rt[:, :, 48:56]
            c8 = rt[:, :, 56:64]
            dv(lambda h: h.tensor_reduce(out=gmax, in_=gl, axis=AX.X, op=ALU.max))
            dv(lambda h: h.tensor_tensor(out=ohg, in0=gl, in1=bc(gmax, 4), op=ALU.is_ge))
            dv(lambda h: h.tensor_tensor(out=gsh, in0=gl, in1=bc(gmax, 4), op=ALU.subtract))
            S.op("act", lambda h: h.activation(out=gsh, in_=gsh, func=AF.Exp), reads=RB, writes=RB)
            dv(lambda h: h.tensor_reduce(out=gsum, in_=gsh, axis=AX.X, op=ALU.add))
            dv(lambda h: h.reciprocal(pg, gsum))
            for g in range(4):
                src_e = lg[:, :, 4 + 8 * g:12 + 8 * g]
                if g == 0:
                    dv(lambda h, src_e=src_e, g=g: h.tensor_tensor(out=el, in0=src_e, in1=bc(ohg[:, :, g], 8), op=ALU.mult))
                else:
                    dv(lambda h, src_e=src_e, g=g: h.tensor_tensor(out=tmp8, in0=src_e, in1=bc(ohg[:, :, g], 8), op=ALU.mult))
                    dv(lambda h: h.tensor_tensor(out=el, in0=el, in1=tmp8, op=ALU.add))
            dv(lambda h: h.tensor_reduce(out=m1, in_=el, axis=AX.X, op=ALU.max))
            dv(lambda h: h.tensor_tensor(out=oh1, in0=el, in1=bc(m1, 8), op=ALU.is_ge))
            dv(lambda h: h.scalar_tensor_tensor(out=el2, in0=oh1, scalar=-1e30, in1=el, op0=ALU.mult, op1=ALU.add))
            dv(lambda h: h.tensor_reduce(out=m2, in_=el2, axis=AX.X, op=ALU.max))
            dv(lambda h: h.tensor_tensor(out=oh2, in0=el2, in1=bc(m2, 8), op=ALU.is_ge))
            dv(lambda h: h.tensor_tensor(out=w2, in0=m2, in1=m1, op=ALU.subtract))
            S.op("act", lambda h: h.activation(out=w2, in_=w2, func=AF.Exp), reads=RB, writes=RB)
            dv(lambda h: h.tensor_scalar(out=w1, in0=w2, scalar1=1.0, scalar2=None, op0=ALU.add))
            dv(lambda h: h.reciprocal(w1, w1))
            dv(lambda h: h.tensor_tensor(out=w2, in0=w2, in1=w1, op=ALU.mult))
            dv(lambda h: h.tensor_tensor(out=w1, in0=w1, in1=pg, op=ALU.mult))
            dv(lambda h: h.tensor_tensor(out=w2, in0=w2, in1=pg, op=ALU.mult))
            dv(lambda h: h.tensor_tensor(out=c8, in0=oh1, in1=bc(w1, 8), op=ALU.mult))
            dv(lambda h: h.tensor_tensor(out=tmp8, in0=oh2, in1=bc(w2, 8), op=ALU.mult))
            dv(lambda h: h.tensor_tensor(out=c8, in0=c8, in1=tmp8, op=ALU.add))
            for g in range(4):
                S.op("dve", lambda h, g=g: h.tensor_tensor(out=comb[:, :, 8 * g:8 * g + 8], in0=c8, in1=bc(ohg[:, :, g], 8), op=ALU.mult), reads=RB, writes=comb.b)
            for i in range(NT):
                bi = 4 + (i // 8)
                S.op("pe", lambda h, bi=bi, i=i: h.transpose(psv_bf(bi)[0:32, (i % 8) * 128:(i % 8 + 1) * 128], comb[:, i, :], identb[:]), reads=comb.b + identb.b, writes=PSB[bi].b, inc=(i % 8 == 7), pe_acc=True)
            for hb in range(2):
                S.op("dve", lambda h, hb=hb: h.tensor_copy(combT[:, hb * 1024:(hb + 1) * 1024], psv_bf(4 + hb)[0:32, :]), reads=PSB[4 + hb].b, writes=combT.b)
            S.op("dve", lambda h: h.tensor_copy(sel[:].rearrange("k (e p) -> k e p", p=128), identb[0:32, 0:32].unsqueeze(2).to_broadcast([32, NE, 128])), reads=identb.b, writes=sel.b)
            S.emit()
            es3.close()
            wgu = sb(es2, "wgu", [128, 3, 2, 8, DFF], BF16, nbuf=3)
            wdn = sb(es2, "wdn", [128, 4, 2, D], BF16, nbuf=4)
            cbb = sb(es2, "cbb", [128, 2, L], BF16, nbuf=2)
            aT = sb(es2, "aT", [128, 4, 2, L], BF16, nbuf=4)
            sl = sb(es2, "sl", [128, 2, 512], BF16, nbuf=2)
            t1 = sb(es2, "t1", [128, 2, 512], BF16, nbuf=2)
            gfin = sb(es2, "gfin", [128, D], F32)
            yt_ap = cbb[:, 0, :].bitcast(F32)
            fs = sb(es2, "fs", [128, 2 * NT], F32)
            y_v = y_d.rearrange("(i p) d -> i p d", p=128)
            S.dma("sp", lambda h: h.dma_start(out=gfin[:], in_=rows_d[2:3, :].partition_broadcast(128)), writes=gfin.b)
            fj_ap = sl[:].rearrange("p a b -> p (a b)")
            q = 0
            def load_gu(e):
                b = e % 3
                S.dma("pool", lambda h: h.dma_start(out=wgu[:, b, 0], in_=wg_d[e].rearrange("(p k) f -> p k f", k=8)), writes=[wgu.b[b]])
                S.dma("pool", lambda h: h.dma_start(out=wgu[:, b, 1], in_=wu_d[e].rearrange("(p k) f -> p k f", k=8)), writes=[wgu.b[b]])
            load_gu(0)
            load_gu(1)
            for rnd in range(8):
                for er in range(4):
                    e = 4 * rnd + er
                    b = e % 2
                    wb3 = e % 3
                    if e + 2 < NE:
                        load_gu(e + 2)
                    S.dma("pool", lambda h, e=e, er=er: h.dma_start(out=wdn[:, er], in_=wd_d[e].rearrange("(c p) d -> p c d", p=128)), writes=[wdn.b[er]])
                    for tcn in range(4):
                        cbank = PSB[4 + tcn]
                        S.op("pe", lambda h, cbank=cbank, e=e, tcn=tcn: h.matmul(cbank[:, :], lhsT=sel[:, e * 128:(e + 1) * 128], rhs=combT[:, tcn * 512:(tcn + 1) * 512], start=True, stop=True),
                             reads=sel.b + combT.b, writes=cbank.b)
                        if tcn % 2 == 0:
                            S.op("act", lambda h, cbank=cbank, b=b, tcn=tcn: h.activation(out=cbb[:, b, tcn * 512:(tcn + 1) * 512], in_=cbank[:, :], func=AF.Copy), reads=cbank.b, writes=[cbb.b[b]])
                        else:
                            S.op("dve", lambda h, cbank=cbank, b=b, tcn=tcn: h.tensor_copy(cbb[:, b, tcn * 512:(tcn + 1) * 512], cbank[:, :]), reads=cbank.b, writes=[cbb.b[b]])
                    for tcn in range(4):
                        for fc in range(2):
                            Gb = PSB[(2 * q) % 4]
                            Ub = PSB[(2 * q + 1) % 4]
                            qb = q % 2
                            q += 1
                            for gu, bank in ((0, Gb), (1, Ub)):
                                for kk in range(8):
                                    S.op("pe", lambda h, bank=bank, gu=gu, kk=kk, wb3=wb3, fc=fc, tcn=tcn: h.matmul(bank[:, :], lhsT=wgu[:, wb3, gu, kk, fc * 128:(fc + 1) * 128], rhs=HT[:, kk, tcn * 512:(tcn + 1) * 512], start=(kk == 0), stop=(kk == 7)),
                                         reads=[wgu.b[wb3], HT.b[kk]], writes=bank.b, inc=(kk == 7), pe_acc=True)
                            S.op("act", lambda h, Gb=Gb, qb=qb: h.activation(out=sl[:, qb, :], in_=Gb[:, :], func=AF.Silu), reads=Gb.b, writes=[sl.b[qb]])
                            S.op("dve", lambda h, Ub=Ub, qb=qb: h.tensor_tensor(out=t1[:, qb, :], in0=Ub[:, :], in1=sl[:, qb, :], op=ALU.mult), reads=Ub.b + [sl.b[qb]], writes=[t1.b[qb]])
                            S.op("pool", lambda h, qb=qb, er=er, fc=fc, tcn=tcn, b=b: h.tensor_tensor(out=aT[:, er, fc, tcn * 512:(tcn + 1) * 512], in0=t1[:, qb, :], in1=cbb[:, b, tcn * 512:(tcn + 1) * 512], op=ALU.mult),
                                 reads=[t1.b[qb], cbb.b[b]], writes=[aT.b[er]])
                    S.op("pool", lambda h, er=er: h.tensor_tensor(out=wdn[:, er], in0=wdn[:, er], in1=gate_f[:].unsqueeze(1).to_broadcast([128, 2, D]), op=ALU.mult), reads=[wdn.b[er]] + gate_f.b, writes=[wdn.b[er]])
                for i in range(NT):
                    for half in range(2):
                        bank = PSB[4 + (2 * i + half) % 4]
                        for er in range(4):
                            for fc in range(2):
                                S.op("pe", lambda h, bank=bank, er=er, fc=fc, i=i, half=half: h.matmul(bank[:, :], lhsT=aT[:, er, fc, i * 128:(i + 1) * 128], rhs=wdn[:, er, fc, half * 512:(half + 1) * 512], start=(er == 0 and fc == 0), stop=(er == 3 and fc == 1)),
                                     reads=[aT.b[er], wdn.b[er]], writes=bank.b, inc=(er == 3 and fc == 1), pe_acc=True)
                        S.op("dve", lambda h, bank=bank, i=i, half=half: h.tensor_tensor(out=X1[:, i, half * 512:(half + 1) * 512], in0=bank[:, :], in1=X1[:, i, half * 512:(half + 1) * 512], op=ALU.add),
                             reads=bank.b + [X1.b[i]], writes=[X1.b[i]])
                    if rnd == 7:
                        S.op("act", lambda h, i=i: h.activation(out=fj_ap, in_=X1[:, i, :], func=AF.Square, accum_out=fs[:, i:i + 1]), reads=[X1.b[i]], writes=sl.b + fs.b)
                        if i % 4 == 3:
                            g = i // 4
                            S.op("act", lambda h, g=g: h.activation(out=fs[:, NT + 4 * g:NT + 4 * g + 4], in_=fs[:, 4 * g:4 * g + 4], func=AF.Sqrt, bias=EPS, scale=1.0 / D), reads=fs.b, writes=fs.b)
                            S.op("dve", lambda h, g=g: h.reciprocal(fs[:, NT + 4 * g:NT + 4 * g + 4], fs[:, NT + 4 * g:NT + 4 * g + 4]), reads=fs.b, writes=fs.b)
                            for i2 in range(4 * g, 4 * g + 4):
                                S.op("dve", lambda h, i2=i2: h.scalar_tensor_tensor(out=yt_ap, in0=X1[:, i2, :], scalar=fs[:, NT + i2:NT + i2 + 1], in1=gfin[:], op0=ALU.mult, op1=ALU.mult), reads=[X1.b[i2]] + fs.b + gfin.b, writes=[cbb.b[0]])
                                S.dma("sp", lambda h, i2=i2: h.dma_start(out=y_v[i2], in_=yt_ap), reads=[cbb.b[0]], writes=[OUTB])
            S.wait_all("sp", [OUTB])
            S.emit()
    es_all.close()
    S.close()
    return nc, dbg_d


def _consts():
    identf = np.eye(128, dtype=np.float32)
    tri = np.zeros((128, 512), np.float32)
    s = np.arange(128)[:, None]
    t = np.arange(128)[None, :]
    tri[:, 0:128] = np.where(s > t, NEG, 0.0)
    p = np.arange(128)
    esel = np.zeros((128, 8, 128), np.float32)
    for g in range(8):
        esel[p, g, 16 * g + (p % 64) // 4] = 1.0
    slopes = np.exp2(-8.0 * np.arange(1, 9, dtype=np.float64) / 8).astype(np.float32)
    alk = np.zeros((128, 8, 16), np.float32)
    for di in range(16):
        dj = di - 3
        alk[:, :, di] = slopes[None, :] * (p[:, None] - 128.0 * dj)
    tl = np.arange(512)
    alq = np.zeros((2, 8, 512), np.float32)
    alq[0] = -8.0 * slopes[:, None] * (256.0 * (tl // 256))[None, :]
    alq[1] = -8.0 * slopes[:, None] * (tl % 256)[None, :]
    return dict(identf=identf, tri=tri, esel=esel.reshape(128, 1024), alibi_k=alk.reshape(128, 128), alibi_q=alq.reshape(2, 4096))


def _pk(v):
    return np.ascontiguousarray(np.asarray(v, np.float32).reshape(128, 8).T)


def _host_prep(inp):
    f = lambda a: np.ascontiguousarray(np.asarray(a, dtype=np.float32))
    x = f(inp["x"]); c = f(inp["c"])
    w_ada = f(inp["w_ada"][0]); b_ada = f(inp["b_ada"][0])
    w_in = f(inp["w_in"][0])
    cols = lambda a, b: w_in[:, a:b]
    aq, ak, av, iq, ik, iw, bq, bk, bv, bf = (cols(0, 512), cols(512, 640), cols(640, 768), cols(768, 1280), cols(1280, 1344),
                                              cols(1344, 1352), cols(1352, 1864), cols(1864, 2376), cols(2376, 2888), cols(2888, 2896))
    w_fm = np.concatenate([aq, ak[:, 0:64], ak[:, 0:64], ak[:, 64:128], ak[:, 64:128], iq, ik, ik, bq, bk, bf], axis=1)
    assert w_fm.shape[1] == 2440
    w_fm_pad = np.zeros((1024, 4 * 640), np.float32)
    w_fm_pad[:, :2440] = w_fm
    w_fm = np.ascontiguousarray(w_fm_pad.reshape(128, 8, 4, 640).transpose(2, 0, 1, 3).reshape(4 * 128, 8 * 640))
    w_tm = np.concatenate([bv, av, iw[:, [0, 2, 4, 6, 1, 3, 5, 7]]], axis=1)
    w_rt = np.concatenate([f(inp["w_group"][0])] + [f(inp["w_router"][0][g]) for g in range(4)], axis=1)
    brt = np.concatenate([f(inp["b_group"][0]), f(inp["b_router"][0]).reshape(-1)])[None, :]
    g_out = np.concatenate([f(inp["g_out_a"][0]), f(inp["g_out_b"][0])])
    rows = np.stack([b_ada[2048:3072], b_ada[5120:6144], f(inp["g_final"])])
    shared = dict(rows=np.ascontiguousarray(rows), brt=np.ascontiguousarray(brt), w_ada=w_ada, w_fm=np.ascontiguousarray(w_fm),
                  w_tm=np.ascontiguousarray(w_tm), nbf=f(inp["b_forget"][0]).reshape(8, 1), w_out=f(inp["w_out"][0]),
                  w_rt=np.ascontiguousarray(w_rt), w_gate=f(inp["w_gate"][0]), w_up=f(inp["w_up"][0]), w_down=f(inp["w_down"][0]))
    shared.update(_consts())
    sm_common = [_pk(b_ada[0:1024]), _pk(b_ada[1024:2048]), _pk(b_ada[3072:4096]), _pk(b_ada[4096:5120]),
                 _pk(inp["g_mix"][0]), _pk(inp["g_ffn"][0]), g_out.reshape(8, 128)]
    in_maps = []
    for b in range(8):
        m = dict(shared)
        m["x"] = x[b]
        m["smalls"] = np.ascontiguousarray(np.concatenate([_pk(c[b])] + sm_common, axis=0))
        in_maps.append(m)
    return in_maps


_CACHE = {}


def kernel(**inputs):
    in_maps = _host_prep(inputs)
    if "nc" not in _CACHE:
        _CACHE["nc"] = build_program(dbg=False)[0]
    res = run_bass_kernel_spmd(_CACHE["nc"], in_maps, core_ids=list(range(8)))
    return np.stack([np.asarray(r["y"], dtype=np.float32) for r in res.results], axis=0)
```

```python
from contextlib import ExitStack
import numpy as np
import concourse.bass as bass
import concourse.mybir as mybir
from concourse.bass_utils import run_bass_kernel_spmd

F32 = mybir.dt.float32
BF16 = mybir.dt.bfloat16
AF = mybir.ActivationFunctionType
ALU = mybir.AluOpType
AX = mybir.AxisListType

D = 1024
L = 2048
NT = 16
NE = 32
DFF = 256
EPS = 1e-6
NEG = -30000.0
NBIS = 16


class Buf:
    __slots__ = ("name", "lw", "rd")

    def __init__(self, name=""):
        self.name = name
        self.lw = None
        self.rd = []


class Sched:
    ENGS = ("pe", "act", "dve", "pool", "sp")

    def __init__(self, nc, n_dma_sems=32):
        self.nc = nc
        self.prog = {e: [] for e in self.ENGS}
        self.cnt = {e: 0 for e in self.ENGS}
        self.seen = {e: {} for e in self.ENGS}
        self.pend_rd = {e: [] for e in self.ENGS}
        self.pend_wr = {e: [] for e in self.ENGS}
        self.sems = {}
        self.n_dma = n_dma_sems
        self.dma_val = [0] * n_dma_sems
        self.dma_rr = 0
        self.dma_rr2 = [0, 0]
        self._ctx = []

    def open(self):
        nc = self.nc
        for e in self.ENGS:
            cm = nc.semaphore("s_" + e)
            self.sems[e] = cm.__enter__()
            self._ctx.append(cm)
        for i in range(self.n_dma):
            cm = nc.semaphore("s_dma%d" % i)
            self.sems[("dma", i)] = cm.__enter__()
            self._ctx.append(cm)

    def close(self):
        for cm in reversed(self._ctx):
            cm.__exit__(None, None, None)

    def _wait(self, eng, tok):
        if tok is None:
            return
        key, val = tok
        if self.seen[eng].get(key, 0) >= val:
            return
        self.seen[eng][key] = val
        sem = self.sems[key]
        self.prog[eng].append(lambda h, sem=sem, val=val: h.wait_ge(sem, val))

    def _deps(self, eng, reads, writes, pe_acc=False):
        for b in reads:
            self._wait(eng, b.lw)
        for b in writes:
            if not (pe_acc and b.lw is not None and b.lw[0] == "pe"):
                self._wait(eng, b.lw)
            for t in b.rd:
                self._wait(eng, t)

    def op(self, eng, fn, reads=(), writes=(), inc=True, pe_acc=False):
        reads = list(reads)
        writes = list(writes)
        self._deps(eng, reads, writes, pe_acc=pe_acc)
        if not inc:
            self.pend_rd[eng].extend(reads)
            self.pend_wr[eng].extend(writes)
            self.prog[eng].append(lambda h, fn=fn: fn(h))
            return
        self.cnt[eng] += 1
        tok = (eng, self.cnt[eng])
        sem = self.sems[eng]
        self.prog[eng].append(lambda h, fn=fn, sem=sem: fn(h).then_inc(sem, 1))
        for b in reads + self.pend_rd[eng]:
            b.rd.append(tok)
        for b in writes + self.pend_wr[eng]:
            b.lw = tok
            b.rd = []
        self.pend_rd[eng] = []
        self.pend_wr[eng] = []

    def dma(self, eng, fn, reads=(), writes=()):
        reads = list(reads)
        writes = list(writes)
        self._deps(eng, reads, writes)
        half = self.n_dma // 2
        qi = 0 if eng == "sp" else 1
        i = qi * half + self.dma_rr2[qi]
        self.dma_rr2[qi] = (self.dma_rr2[qi] + 1) % half
        key = ("dma", i)
        if self.dma_val[i] > 0:
            self._wait(eng, (key, self.dma_val[i]))
        self.dma_val[i] += 16
        tok = (key, self.dma_val[i])
        sem = self.sems[key]
        self.prog[eng].append(lambda h, fn=fn, sem=sem: fn(h).then_inc(sem, 16))
        for b in reads:
            b.rd.append(tok)
        for b in writes:
            b.lw = tok
            b.rd = []
        return tok

    def wait_all(self, eng, bufs):
        for b in bufs:
            self._wait(eng, b.lw)
            for t in b.rd:
                self._wait(eng, t)

    def emit(self):
        nc = self.nc
        for i in range(self.n_dma):
            if self.dma_val[i] > 0:
                self._wait("sp", (("dma", i), self.dma_val[i]))
        prog = self.prog
        with nc.Block() as block:
            @block.tensor
            def _(h):
                for f in prog["pe"]:
                    f(h)

            @block.scalar
            def _(h):
                for f in prog["act"]:
                    f(h)

            @block.vector
            def _(h):
                for f in prog["dve"]:
                    f(h)

            @block.gpsimd
            def _(h):
                for f in prog["pool"]:
                    f(h)

            @block.sync
            def _(h):
                for f in prog["sp"]:
                    f(h)
        self.prog = {e: [] for e in self.ENGS}


class Tn:
    def __init__(self, t, nbuf=1, name=""):
        self.t = t
        self.b = [Buf("%s%d" % (name, i)) for i in range(nbuf)]

    def __getitem__(self, k):
        return self.t[k]


def build_program(dbg=False, stop_after=99):
    nc = bass.Bass("TRN2", target_bir_lowering=False)
    S = Sched(nc)

    def din(name, shape, dt=F32):
        return nc.dram_tensor(name, list(shape), dt, kind="ExternalInput").ap()

    x_d = din("x", [L, D])
    smalls_d = din("smalls", [64, 128])
    rows_d = din("rows", [3, D])
    brt_d = din("brt", [1, 36])
    wada_d = din("w_ada", [D, 6 * D])
    wfm_d = din("w_fm", [4 * 128, 8 * 640])
    wtm_d = din("w_tm", [D, 648])
    nbf_d = din("nbf", [8, 1])
    wout_d = din("w_out", [D, D])
    wrt_d = din("w_rt", [D, 36])
    wg_d = din("w_gate", [NE, D, DFF])
    wu_d = din("w_up", [NE, D, DFF])
    wd_d = din("w_down", [NE, DFF, D])
    identf_d = din("identf", [128, 128])
    tri_d = din("tri", [128, 512])
    esel_d = din("esel", [128, 1024])
    alk_d = din("alibi_k", [128, 128])
    alq_d = din("alibi_q", [2, 8 * 512])
    y_d = nc.dram_tensor("y", [L, D], F32, kind="ExternalOutput").ap()
    iwe_d = nc.dram_tensor("iw_e", [128, 64], F32, kind="Internal").ap()
    iwo_d = nc.dram_tensor("iw_o", [128, 64], F32, kind="Internal").ap()
    combT_d = nc.dram_tensor("combT", [NE, L], BF16, kind="Internal").ap()
    dbg_d = {}

    def dbg_out(name, shape, dt=F32):
        dbg_d[name] = nc.dram_tensor("dbg_" + name, list(shape), dt, kind="ExternalOutput").ap()
        return dbg_d[name]

    S.open()
    es_all = ExitStack()

    def sb(es, name, shape, dt=F32, nbuf=1):
        t = es.enter_context(nc.sbuf_tensor("sb_" + name, list(shape), dt))
        return Tn(t, nbuf, name)

    def ps(es, name, shape, dt=F32, nbuf=1):
        t = es.enter_context(nc.psum_tensor("ps_" + name, list(shape), dt))
        return Tn(t, nbuf, name)

    OUTB = Buf("out")

    def phase_end(n):
        if stop_after == n:
            S.wait_all("sp", [OUTB])
            S.emit()
            return True
        S.emit()
        return False

    P = es_all
    HT = sb(P, "HT", [128, 8, L], BF16, nbuf=8)
    identb = sb(P, "identb", [128, 128], BF16)
    identf = sb(P, "identf", [128, 128], F32)
    smT = sb(P, "smT", [128, 64], F32)
    modp = sb(P, "modp", [128, 64], F32)
    gate_m = sb(P, "gate_m", [128, D], F32)
    gate_f = sb(P, "gate_f", [128, D], F32)
    scb = sb(P, "scb", [128, 8], BF16)
    screp = sb(P, "screp", [128, 8, 128], BF16)
    PSB = [ps(P, "psb%d" % i, [128, 512], F32) for i in range(8)]

    def psv_bf(i):
        return PSB[i].t[:].bitcast(BF16)

    es_dsa = ExitStack()
    QTa = sb(es_dsa, "QTa", [128, 4, L], BF16, nbuf=4)
    KTa = sb(es_dsa, "KTa", [128, 2, L], BF16, nbuf=2)
    IQT = sb(es_dsa, "IQT", [128, L, 4], BF16, nbuf=1)
    IKT = sb(es_dsa, "IKT", [128, L], BF16)
    Va = sb(es_dsa, "Va", [128, NT, 2, 65], BF16)
    wcolT = sb(es_dsa, "wcolT", [128, 128], F32)

    S.dma("sp", lambda h: h.dma_start(out=identf[:], in_=identf_d), writes=identf.b)
    S.dma("pool", lambda h: h.dma_start(out=identb[:], in_=identf_d), writes=identb.b)
    S.op("pool", lambda h: h.memset(Va[:, :, :, 64:65], 1.0), writes=Va.b)

    def norm_to_HT(es, src_tile_fn, a0, b0, tag):
        xn = sb(es, "xn" + tag, [128, 4, D], BF16, nbuf=4)
        junk = sb(es, "junk" + tag, [128, D], BF16)
        ss = sb(es, "ss" + tag, [128, NT], F32)
        rstd = sb(es, "rstd" + tag, [128, NT], F32)
        for g in range(4):
            srcs = [src_tile_fn(g * 4 + il) for il in range(4)]
            for il in range(4):
                i = g * 4 + il
                src, sbufs = srcs[il]
                S.op("act", lambda h, src=src, i=i: h.activation(out=junk[:], in_=src, func=AF.Square, accum_out=ss[:, i:i + 1]), reads=sbufs, writes=junk.b + ss.b)
            S.op("act", lambda h, g=g: h.activation(out=rstd[:, 4 * g:4 * g + 4], in_=ss[:, 4 * g:4 * g + 4], func=AF.Sqrt, bias=EPS, scale=1.0 / D), reads=ss.b, writes=rstd.b)
            S.op("dve", lambda h, g=g: h.reciprocal(rstd[:, 4 * g:4 * g + 4], rstd[:, 4 * g:4 * g + 4]), reads=rstd.b, writes=rstd.b)
            for il in range(4):
                i = g * 4 + il
                src, sbufs = srcs[il]
                S.op("dve", lambda h, src=src, i=i, il=il: h.tensor_scalar(out=xn[:, il, :], in0=src, scalar1=rstd[:, i:i + 1], scalar2=None, op0=ALU.mult), reads=sbufs + rstd.b, writes=[xn.b[il]])
            for kk in range(8):
                bank = PSB[kk // 2]
                off = (kk % 2) * 512
                for il in range(4):
                    S.op("pe", lambda h, bank=bank, off=off, il=il, kk=kk: h.transpose(psv_bf(PSB.index(bank))[:, off + il * 128: off + (il + 1) * 128], xn[:, il, kk::8], identb[:]),
                         reads=[xn.b[il]] + identb.b, writes=bank.b, inc=(il == 3), pe_acc=True)
                dst = HT[:, kk, g * 512:(g + 1) * 512]
                srcp = psv_bf(kk // 2)[:, off:off + 512]
                if kk % 2 == 0:
                    S.op("act", lambda h, dst=dst, srcp=srcp, kk=kk: h.activation(out=dst, in_=srcp, func=AF.Identity, scale=modp[:, a0 + kk:a0 + kk + 1], bias=modp[:, b0 + kk:b0 + kk + 1]),
                         reads=bank.b + modp.b, writes=[HT.b[kk]])
                else:
                    S.op("dve", lambda h, dst=dst, srcp=srcp, kk=kk: h.tensor_scalar(out=dst, in0=srcp, scalar1=modp[:, a0 + kk:a0 + kk + 1], scalar2=modp[:, b0 + kk:b0 + kk + 1], op0=ALU.mult, op1=ALU.add),
                         reads=bank.b + modp.b, writes=[HT.b[kk]])
        return rstd

    wada_v = wada_d.rearrange("(p k) n -> p k n", k=8)

    def mod_load(wa, bi, piece):
        S.dma("pool", lambda h: h.dma_start(out=wa[:, bi, :, :], in_=wada_v[:, :, piece * D:(piece + 1) * D]), writes=[wa.b[bi]])

    def mod_vec_piece(wa, bi, sl, bank):
        for kk in range(8):
            for k2 in range(8):
                S.op("pe", lambda h, kk=kk, k2=k2: h.matmul(bank[:, sl * 8 + kk:sl * 8 + kk + 1], lhsT=wa[:, bi, k2, kk::8], rhs=scb[:, k2:k2 + 1], start=(k2 == 0), stop=(k2 == 7)),
                     reads=[wa.b[bi]] + scb.b, writes=bank.b, inc=(k2 == 7 and kk == 7), pe_acc=True)

    def mod_gate_piece(wa, bi, gt, gi, brow, banks):
        for half in range(2):
            bank = banks[half]
            for k2 in range(8):
                S.op("pe", lambda h, bank=bank, k2=k2, half=half: h.matmul(bank[:, :], lhsT=screp[:, k2, :], rhs=wa[:, bi, k2, half * 512:(half + 1) * 512], start=(k2 == 0), stop=(k2 == 7)),
                     reads=[wa.b[bi]] + screp.b, writes=bank.b, inc=(k2 == 7), pe_acc=True)
            S.op("dve", lambda h, bank=bank, half=half: h.tensor_tensor(out=gt[:, half * 512:(half + 1) * 512], in0=bank[:, :], in1=brow[:, gi, half * 512:(half + 1) * 512], op=ALU.add),
                 reads=bank.b + [brow.b[gi]], writes=gt.b)

    with ExitStack() as es:
        sm_in = sb(es, "sm_in", [64, 128], F32)
        wa = sb(es, "wa", [128, 2, 8, D], BF16, nbuf=2)
        sc32 = sb(es, "sc32", [128, 8], F32)
        S.dma("sp", lambda h: h.dma_start(out=sm_in[:], in_=smalls_d), writes=sm_in.b)
        mod_load(wa, 0, 0)
        mod_load(wa, 1, 1)
        S.op("pe", lambda h: h.transpose(PSB[0][:, 0:64], sm_in[:], identf[0:64, 0:64]), reads=sm_in.b + identf.b, writes=PSB[0].b)
        S.op("dve", lambda h: h.tensor_copy(smT[:], PSB[0][:, 0:64]), reads=PSB[0].b, writes=smT.b)
        S.op("act", lambda h: h.activation(out=sc32[:], in_=smT[:, 0:8], func=AF.Silu), reads=smT.b, writes=sc32.b)
        S.op("dve", lambda h: h.tensor_copy(scb[:], sc32[:]), reads=sc32.b, writes=scb.b)
        S.op("dve", lambda h: h.tensor_copy(screp[:], sc32[:].unsqueeze(2).to_broadcast([128, 8, 128])), reads=sc32.b, writes=screp.b)
        mod_vec_piece(wa, 0, 0, PSB[1])
        mod_vec_piece(wa, 1, 1, PSB[1])
        S.op("dve", lambda h: h.tensor_tensor(out=modp[:, 32:48], in0=PSB[1][:, 0:16], in1=smT[:, 8:24], op=ALU.add), reads=PSB[1].b + smT.b, writes=modp.b)
        S.op("dve", lambda h: h.scalar_tensor_tensor(out=modp[:, 0:8], in0=modp[:, 40:48], scalar=1.0, in1=smT[:, 40:48], op0=ALU.add, op1=ALU.mult), reads=modp.b + smT.b, writes=modp.b)
        S.op("dve", lambda h: h.tensor_copy(modp[:, 8:16], modp[:, 32:40]), reads=modp.b, writes=modp.b)
        xt = sb(es, "xt", [128, 4, D], F32, nbuf=4)
        x_v = x_d.rearrange("(i p) d -> i p d", p=128)

        def src1(i):
            bi = i % 4
            S.dma("sp", lambda h, i=i, bi=bi: h.dma_start(out=xt[:, bi, :], in_=x_v[i]), writes=[xt.b[bi]])
            return xt[:, bi, :], [xt.b[bi]]

        norm_to_HT(es, src1, 0, 8, "1")
        if dbg:
            d3 = dbg_out("hT", [128, 8 * L], BF16)
            S.dma("sp", lambda h: h.dma_start(out=d3, in_=HT[:].rearrange("p k t -> p (k t)")), reads=HT.b, writes=[OUTB])
        S.emit()

    def deferred_mod(es):
        wa2 = sb(es, "wa2", [128, 2, 8, D], BF16, nbuf=2)
        brow = sb(es, "brow", [128, 1, D], F32, nbuf=1)
        S.dma("sp", lambda h: h.dma_start(out=brow[:, 0, :], in_=rows_d[0:1, :].partition_broadcast(128)), writes=[brow.b[0]])
        mod_load(wa2, 0, 3)
        mod_load(wa2, 1, 4)

        def stage1():
            mod_vec_piece(wa2, 0, 0, PSB[5])
            mod_vec_piece(wa2, 1, 1, PSB[5])
            S.op("dve", lambda h: h.tensor_tensor(out=modp[:, 48:64], in0=PSB[5][:, 0:16], in1=smT[:, 24:40], op=ALU.add), reads=PSB[5].b + smT.b, writes=modp.b)
            S.op("dve", lambda h: h.scalar_tensor_tensor(out=modp[:, 16:24], in0=modp[:, 56:64], scalar=1.0, in1=smT[:, 48:56], op0=ALU.add, op1=ALU.mult), reads=modp.b + smT.b, writes=modp.b)
            S.op("dve", lambda h: h.tensor_copy(modp[:, 24:32], modp[:, 48:56]), reads=modp.b, writes=modp.b)
            mod_load(wa2, 0, 2)
            mod_load(wa2, 1, 5)

        def stage2():
            mod_gate_piece(wa2, 0, gate_m, 0, brow, (PSB[4], PSB[5]))
            S.dma("sp", lambda h: h.dma_start(out=brow[:, 0, :], in_=rows_d[1:2, :].partition_broadcast(128)), writes=[brow.b[0]])
            mod_gate_piece(wa2, 1, gate_f, 0, brow, (PSB[4], PSB[5]))
        return stage1, stage2

    es_fox = ExitStack()
    QTb = sb(es_fox, "QTb", [128, 4, L], BF16, nbuf=4)
    KTb = sb(es_fox, "KTb", [128, 4, L], BF16, nbuf=4)
    Vb = sb(es_fox, "Vb", [128, NT, 8, 65], BF16)
    cum3 = sb(es_fox, "cum3", [67, 3, L], BF16)
    csT = sb(es_fox, "csT", [128, NT, 8], F32)
    S.op("pool", lambda h: h.memset(Vb[:, :, :, 64:65], 1.0), writes=Vb.b)

    with ExitStack() as es:
        wf = sb(es, "wf", [128, 2, 8, 640], BF16, nbuf=2)
        wt = sb(es, "wt", [128, 8, 648], BF16, nbuf=8)
        iw_tm = sb(es, "iw_tm", [128, NT, 8], F32)
        e1 = sb(es, "e1", [8, L], F32)
        nbf = sb(es, "nbf", [8, 1], F32)
        bfg = sb(es, "bfg", [8, 1], F32)
        wfm_v = wfm_d.rearrange("(q p) (k c) -> q p k c", p=128, c=640)
        wtm_v = wtm_d.rearrange("(p k) n -> p k n", k=8)
        for kk in range(8):
            S.dma("pool", lambda h, kk=kk: h.dma_start(out=wt[:, kk, :], in_=wtm_v[:, kk, :]), writes=[wt.b[kk]])
        S.dma("sp", lambda h: h.dma_start(out=bfg[:], in_=nbf_d), writes=bfg.b)
        S.op("dve", lambda h: h.tensor_scalar(out=nbf[:], in0=bfg[:], scalar1=-1.0, scalar2=None, op0=ALU.mult), reads=bfg.b, writes=nbf.b)
        dests = []
        for p_ in range(4):
            dests.append((QTa, p_))
        dests += [(KTa, 0), (KTa, 1)]
        for p_ in range(4):
            dests.append((IQT, p_))
        dests.append((IKT, None))
        for p_ in range(4):
            dests.append((QTb, p_))
        for p_ in range(4):
            dests.append((KTb, p_))
        pieces = [(0, 5), (5, 10), (10, 15), (15, 19)]
        cnt_ = {'ev': 0, 'pb': 0}
        def fm_part():
            ev = 0
            pb = 0
            for pi, (c0, c1) in enumerate(pieces):
                bi = pi % 2
                ncol = (c1 - c0) * 128
                S.dma("pool", lambda h, bi=bi, pi=pi: h.dma_start(out=wf[:, bi, :, :], in_=wfm_v[pi]), writes=[wf.b[bi]])
                for ch in range(c0, c1):
                    T_, idx = dests[ch]
                    for ng in range(4):
                        bank = PSB[4 + (pb % 4)]
                        pb += 1
                        for kk in range(8):
                            S.op("pe", lambda h, bank=bank, bi=bi, kk=kk, ch=ch, c0=c0, ng=ng: h.matmul(bank[:, :], lhsT=wf[:, bi, kk, (ch - c0) * 128:(ch - c0 + 1) * 128], rhs=HT[:, kk, ng * 512:(ng + 1) * 512], start=(kk == 0), stop=(kk == 7)),
                                 reads=[wf.b[bi], HT.b[kk]], writes=bank.b, inc=(kk == 7), pe_acc=True)
                        if idx is None:
                            dst = T_[:, ng * 512:(ng + 1) * 512]
                            wb_ = T_.b
                        elif T_ is IQT:
                            dst = T_[:, ng * 512:(ng + 1) * 512, idx]
                            wb_ = T_.b
                        else:
                            dst = T_[:, idx, ng * 512:(ng + 1) * 512]
                            wb_ = [T_.b[idx]]
                        if ev % 2 == 0:
                            S.op("act", lambda h, dst=dst, bank=bank: h.activation(out=dst, in_=bank[:, :], func=AF.Copy), reads=bank.b, writes=wb_)
                        else:
                            S.op("dve", lambda h, dst=dst, bank=bank: h.tensor_copy(dst, bank[:, :]), reads=bank.b, writes=wb_)
                        ev += 1
        wbf = sb(es, "wbf", [128, 8, 8], BF16)
        S.dma("pool", lambda h: h.dma_start(out=wbf[:], in_=wfm_v[3][:, :, 512:520]), writes=wbf.b)

        def bf_part():
            for ng in range(4):
                bank = PSB[4 + ng]
                for kk in range(8):
                    S.op("pe", lambda h, bank=bank, kk=kk, ng=ng: h.matmul(bank[0:8, :], lhsT=wbf[:, kk, :], rhs=HT[:, kk, ng * 512:(ng + 1) * 512], start=(kk == 0), stop=(kk == 7)),
                         reads=wbf.b + [HT.b[kk]], writes=bank.b, inc=(kk == 7), pe_acc=True)
                S.op("act", lambda h, bank=bank, ng=ng: h.activation(out=e1[:, ng * 512:(ng + 1) * 512], in_=bank[0:8, :], func=AF.Exp, scale=-1.0, bias=nbf[:, 0:1]), reads=bank.b + nbf.b, writes=e1.b)
        def tm_part():
            for i in range(NT):
                bA = PSB[(2 * i) % 4]
                bB = PSB[(2 * i + 1) % 4]
                for kk in range(8):
                    S.op("pe", lambda h, bA=bA, kk=kk, i=i: h.matmul(bA[:, :], lhsT=HT[:, kk, i * 128:(i + 1) * 128], rhs=wt[:, kk, 0:512], start=(kk == 0), stop=(kk == 7)),
                         reads=[HT.b[kk], wt.b[kk]], writes=bA.b, inc=(kk == 7), pe_acc=True)
                for kk in range(8):
                    S.op("pe", lambda h, bB=bB, kk=kk, i=i: h.matmul(bB[:, 0:136], lhsT=HT[:, kk, i * 128:(i + 1) * 128], rhs=wt[:, kk, 512:648], start=(kk == 0), stop=(kk == 7)),
                         reads=[HT.b[kk], wt.b[kk]], writes=bB.b, inc=(kk == 7), pe_acc=True)
                S.op("act", lambda h, bA=bA, i=i: h.activation(out=Vb[:, i, :, 0:64], in_=bA[:, :].rearrange("p (h d) -> p h d", h=8), func=AF.Copy), reads=bA.b, writes=Vb.b)
                S.op("dve", lambda h, bB=bB, i=i: h.tensor_copy(Va[:, i, :, 0:64], bB[:, 0:128].rearrange("p (h d) -> p h d", h=2)), reads=bB.b, writes=Va.b)
                S.op("dve", lambda h, bB=bB, i=i: h.tensor_copy(iw_tm[:, i, :], bB[:, 128:136]), reads=bB.b, writes=iw_tm.b)
        tm_part()
        bf_part()
        S.op("act", lambda h: h.activation(out=e1[:], in_=e1[:], func=AF.Ln, bias=1.0, scale=1.0), reads=e1.b, writes=e1.b)
        cs = sb(es, "cs", [8, L], F32)
        S.op("dve", lambda h: h.tensor_tensor_scan(out=cs[:], data0=e1[:], data1=e1[:], initial=0.0, op0=ALU.add, op1=ALU.max), reads=e1.b, writes=cs.b)
        for j in range(NT):
            S.op("pe", lambda h, j=j: h.transpose(PSB[4][:, j * 8:(j + 1) * 8], cs[:, j * 128:(j + 1) * 128], identf[0:8, 0:8]), reads=cs.b + identf.b, writes=PSB[4].b, inc=(j == NT - 1), pe_acc=True)
        S.op("dve", lambda h: h.tensor_copy(csT[:].rearrange("p j h -> p (j h)"), PSB[4][:, 0:128]), reads=PSB[4].b, writes=csT.b)
        hi3 = Tn(e1.t, 2, "hi3")
        hi3v = e1[:].bitcast(BF16).rearrange("p (a t) -> p a t", a=2)
        S.op("dve", lambda h: h.tensor_scalar(out=cs[:], in0=cs[:], scalar1=-8.0, scalar2=None, op0=ALU.mult), reads=cs.b, writes=cs.b)
        for r in range(3):
            S.op("dve", lambda h, r=r: h.tensor_copy(hi3v[:, r % 2, :], cs[:]), reads=cs.b, writes=[hi3.b[r % 2]] + (e1.b if r < 2 else []))
            if r < 2:
                S.op("dve", lambda h, r=r: h.tensor_tensor(out=cs[:], in0=cs[:], in1=hi3v[:, r % 2, :], op=ALU.subtract), reads=cs.b + [hi3.b[r % 2]], writes=cs.b)
            for hh in range(8):
                pp = 32 * (hh % 3) + r
                S.dma("sp", lambda h, hh=hh, pp=pp, r=r: h.dma_start(out=cum3[pp:pp + 1, hh // 3, :], in_=hi3v[hh:hh + 1, r % 2, :]), reads=[hi3.b[r % 2]], writes=cum3.b)
        iwe_flat = iwe_d.rearrange("g c -> (g c)").rearrange("(i t b) -> t i b", i=16, t=128, b=4)
        iwo_flat = iwo_d.rearrange("g c -> (g c)").rearrange("(i t b) -> t i b", i=16, t=128, b=4)
        SCR = Buf("scr")
        S.dma("sp", lambda h: h.dma_start(out=iwe_flat, in_=iw_tm[:, :, 0:4]), reads=iw_tm.b, writes=[SCR])
        S.dma("sp", lambda h: h.dma_start(out=iwo_flat, in_=iw_tm[:, :, 4:8]), reads=iw_tm.b, writes=[SCR])
        wc_in = sb(es, "wc_in", [128, 128], F32)
        S.dma("sp", lambda h: h.dma_start(out=wc_in[:, 0:64], in_=iwe_d), reads=[SCR], writes=wc_in.b)
        S.dma("sp", lambda h: h.dma_start(out=wc_in[:, 64:128], in_=iwo_d), reads=[SCR], writes=wc_in.b)
        fm_part()
        S.op("pe", lambda h: h.transpose(PSB[5][:, 0:128], wc_in[:], identf[:]), reads=wc_in.b + identf.b, writes=PSB[5].b)
        S.op("dve", lambda h: h.tensor_copy(wcolT[:], PSB[5][:, 0:128]), reads=PSB[5].b, writes=wcolT.b)
        if dbg:
            for nm, T_, shp, dt_ in [("QTa", QTa, [128, 4 * L], BF16), ("KTa", KTa, [128, 2 * L], BF16), ("IKT", IKT, [128, L], BF16),
                                     ("QTb", QTb, [128, 4 * L], BF16), ("KTb", KTb, [128, 4 * L], BF16), ("Vb", Vb, [128, NT * 8 * 65], BF16), ("Va", Va, [128, NT * 2 * 65], BF16),
                                     ("csT", csT, [128, NT * 8], F32), ("cum3", cum3, [67, 3 * L], BF16), ("wcolT", wcolT, [128, 128], F32)]:
                dd = dbg_out(nm, shp, dt_)
                nd = len(T_.t.shape)
                if nd == 2:
                    src_ap = T_[:]
                elif nd == 3:
                    src_ap = T_[:].rearrange("p a b -> p (a b)")
                else:
                    src_ap = T_[:].rearrange("p a b c -> p (a b c)")
                S.dma("sp", lambda h, dd=dd, src_ap=src_ap: h.dma_start(out=dd, in_=src_ap), reads=T_.b, writes=[OUTB])
        if phase_end(1):
            return nc, dbg_d

    def attn_scratch(es, tag):
        sc = {}
        sc["ones67"] = sb(es, "ones67" + tag, [67, 128], BF16)
        sc["PT"] = sb(es, "PT" + tag, [128, 4, 512], BF16, nbuf=4)
        sc["o_sb"] = sb(es, "o_sb" + tag, [128, 4, 512], F32)
        sc["ob"] = sb(es, "ob" + tag, [128, 4, 512], BF16)
        sc["rec"] = sb(es, "rec" + tag, [128, 8, 4], F32)
        sc["oss"] = sb(es, "oss" + tag, [128, 8], F32)
        sc["ojunk"] = sb(es, "ojunk" + tag, [128, 512], BF16)
        S.op("pool", lambda h: h.memset(sc["ones67"][:], 1.0), writes=sc["ones67"].b)
        return sc

    def attention_chunk(sc, c, KT, kt_idx, QT, aug_lhs, aug_rhs, mask_rhs, bias_ap, Vt, v_idx, tick=None):
        PT, o_sb, rec = sc["PT"], sc["o_sb"], sc["rec"]
        T0 = 512 * c
        nj = 4 * c + 4
        steps = [(pr, j) for pr in range(4) for j in range(nj)]

        def s_stage(k):
            pr, j = steps[k]
            col0 = max(0, j - 4 * c) * 128
            ncols = 512 - col0
            heads = (2 * pr, 2 * pr + 1)
            Sbs = (PSB[(2 * k) % 4], PSB[(2 * k + 1) % 4])
            for hh, Sb in zip(heads, Sbs):
                rows = slice(64 * (hh % 2), 64 * (hh % 2) + 64)
                S.op("pe", lambda h, hh=hh, Sb=Sb, rows=rows: h.matmul(Sb[:, 0:ncols], lhsT=KT[rows, kt_idx(hh), j * 128:(j + 1) * 128], rhs=QT[rows, pr, T0 + col0:T0 + 512], start=True, stop=False),
                     reads=[KT.b[kt_idx(hh)], QT.b[pr]], writes=Sb.b, inc=False, pe_acc=True)
            mr, mbufs = mask_rhs(j, col0, ncols)
            if mr is not None:
                for hh, Sb in zip(heads, Sbs):
                    S.op("pe", lambda h, Sb=Sb: h.matmul(Sb[:, 0:ncols], lhsT=identb[:], rhs=mr, start=False, stop=False),
                         reads=identb.b + mbufs, writes=Sb.b, inc=False, pe_acc=True)
            for hh, Sb in zip(heads, Sbs):
                al, albufs = aug_lhs(hh)
                ar, arbufs = aug_rhs(hh, col0)
                S.op("pe", lambda h, Sb=Sb, al=al, ar=ar: h.matmul(Sb[:, 0:ncols], lhsT=al, rhs=ar, start=False, stop=True),
                     reads=albufs + arbufs, writes=Sb.b, inc=True, pe_acc=True)
            for q_, (hh, Sb) in enumerate(zip(heads, Sbs)):
                pt = (2 * k + q_) % 4
                bap, bbufs = bias_ap(hh, j)
                S.op("act", lambda h, Sb=Sb, pt=pt, bap=bap: h.activation(out=PT[:, pt, 0:ncols], in_=Sb[:, 0:ncols], func=AF.Exp, scale=0.125, bias=bap),
                     reads=Sb.b + bbufs, writes=[PT.b[pt]])

        def pv_stage(k):
            pr, j = steps[k]
            col0 = max(0, j - 4 * c) * 128
            il0 = max(0, j - 4 * c)
            for q_ in range(2):
                hh = 2 * pr + q_
                pt = (2 * k + q_) % 4
                Ob = PSB[(6 if pr % 2 == 0 else 4) + q_]
                for il in range(il0, 4):
                    i = 4 * c + il
                    first = (j == 0 and il == il0)
                    S.op("pe", lambda h, il=il, i=i, first=first, Ob=Ob, pt=pt, hh=hh: h.matmul(Ob[:, il * 65:(il + 1) * 65], lhsT=PT[:, pt, il * 128 - col0:il * 128 - col0 + 128], rhs=Vt[:, j, v_idx(hh), :], start=first, stop=(j == i), skip_group_check=True),
                         reads=[PT.b[pt]] + Vt.b, writes=Ob.b, inc=(il == 3), pe_acc=True)
                if j == nj - 1:
                    Ov = Ob[:, 0:260].rearrange("p (a b) -> p a b", b=65)
                    S.op("dve", lambda h, Ov=Ov, hh=hh: h.reciprocal(rec[:, hh, :], Ov[:, :, 64]), reads=Ob.b, writes=rec.b)
                    S.op("dve", lambda h, Ov=Ov, hh=hh: h.tensor_tensor(out=o_sb[:, :, hh * 64:(hh + 1) * 64], in0=Ov[:, :, 0:64], in1=rec[:, hh, :].unsqueeze(2).to_broadcast([128, 4, 64]), op=ALU.mult),
                         reads=Ob.b + rec.b, writes=o_sb.b)

        n = len(steps)
        s_stage(0)
        for k in range(n):
            if k + 1 < n:
                s_stage(k + 1)
            pv_stage(k)
            if tick is not None:
                tick(k, n)

    def out_norm(sc, c, base):
        o_sb, ob, oss, ojunk = sc["o_sb"], sc["ob"], sc["oss"], sc["ojunk"]
        T0 = 512 * c
        for il in range(4):
            S.op("act", lambda h, il=il: h.activation(out=ojunk[:], in_=o_sb[:, il, :], func=AF.Square, accum_out=oss[:, il:il + 1]), reads=o_sb.b, writes=ojunk.b + oss.b)
        S.op("act", lambda h: h.activation(out=oss[:, 4:8], in_=oss[:, 0:4], func=AF.Sqrt, bias=EPS, scale=1.0 / 512), reads=oss.b, writes=oss.b)
        S.op("dve", lambda h: h.reciprocal(oss[:, 4:8], oss[:, 4:8]), reads=oss.b, writes=oss.b)
        S.op("dve", lambda h: h.tensor_tensor(out=ob[:], in0=o_sb[:], in1=oss[:, 4:8].unsqueeze(2).to_broadcast([128, 4, 512]), op=ALU.mult), reads=o_sb.b + oss.b, writes=ob.b)
        for fc in range(4):
            bank = PSB[4 + fc % 2]
            bi = 4 + fc % 2
            for il in range(4):
                S.op("pe", lambda h, bi=bi, il=il, fc=fc: h.transpose(psv_bf(bi)[:, il * 128:(il + 1) * 128], ob[:, il, fc * 128:(fc + 1) * 128], identb[:]),
                     reads=ob.b + identb.b, writes=bank.b, inc=(il == 3), pe_acc=True)
            S.op("act", lambda h, bi=bi, fc=fc: h.activation(out=HT[:, base + fc, T0:T0 + 512], in_=psv_bf(bi)[:, 0:512], func=AF.Identity, scale=smT[:, 56 + base + fc:57 + base + fc]),
                 reads=bank.b + smT.b, writes=[HT.b[base + fc]])

    with ExitStack() as es:
        sc = attn_scratch(es, "f")
        triw = sb(es, "triw", [128, 512], BF16)
        S.dma("pool", lambda h: h.dma_start(out=triw[:], in_=tri_d), writes=triw.b)
        mod_st1, mod_st2 = deferred_mod(es)
        for c in range(4):
            if c == 1:
                mod_st1()
            if c == 2:
                mod_st2()
            attention_chunk(
                sc, c, KTb, lambda hh: hh // 2, QTb,
                aug_lhs=lambda hh: (sc["ones67"][32 * (hh % 3):32 * (hh % 3) + 3, :], sc["ones67"].b),
                aug_rhs=lambda hh, col0, c=c: (cum3[32 * (hh % 3):32 * (hh % 3) + 3, hh // 3, 512 * c + col0:512 * c + 512], cum3.b),
                mask_rhs=lambda j, col0, ncols, c=c: ((triw[:, 0:ncols], triw.b) if j >= 4 * c else (None, [])),
                bias_ap=lambda hh, j: (csT[:, j, hh:hh + 1], csT.b),
                Vt=Vb, v_idx=lambda hh: hh)
            out_norm(sc, c, 4)
        if dbg:
            d4b = dbg_out("oTb", [128, 8 * L], BF16)
            S.dma("sp", lambda h: h.dma_start(out=d4b, in_=HT[:].rearrange("p k t -> p (k t)")), reads=HT.b, writes=[OUTB])
        if phase_end(2):
            return nc, dbg_d
    es_fox.close()

    with ExitStack() as es:
        sc = attn_scratch(es, "a")
        IS = sb(es, "IS", [128, 4, L], F32, nbuf=4)
        NM = sb(es, "NM", [128, L], BF16)
        NMT2 = sb(es, "NMT", [128, 2, NT, 512], BF16, nbuf=2)
        R = sb(es, "R", [128, 4, 512], BF16, nbuf=4)
        Wblk = sb(es, "Wblk", [128, 2, 8, 128], BF16, nbuf=2)
        esel = sb(es, "esel", [128, 8, 128], BF16)
        alk = sb(es, "alk", [128, 8, 16], F32)
        alq = sb(es, "alq", [66, 8, 512], BF16)
        bs = sb(es, "bs", [128, 32], F32)
        cjunk = sb(es, "cjunk", [128, L], BF16)
        S.dma("pool", lambda h: h.dma_start(out=esel[:].rearrange("p a b -> p (a b)"), in_=esel_d), writes=esel.b)
        S.dma("pool", lambda h: h.dma_start(out=alq[0:2].rearrange("p a b -> p (a b)"), in_=alq_d), writes=alq.b)
        S.dma("pool", lambda h: h.dma_start(out=alq[64:66].rearrange("p a b -> p (a b)"), in_=alq_d), writes=alq.b)
        S.dma("sp", lambda h: h.dma_start(out=alk[:].rearrange("p a b -> p (a b)"), in_=alk_d), writes=alk.b)
        evc = [0]

        def indexer(c):
            units = []
            for il in range(4):
                i = 4 * c + il
                ncols = 128 * (i + 1)
                for kc in range((ncols + 511) // 512):
                    for g in range(8):
                        units.append((il, kc, g))

            def dots(u):
                il, kc, g = units[u]
                i = 4 * c + il
                n = min(512, 128 * (i + 1) - 512 * kc)
                tok0 = 128 * i + 16 * g
                Db = PSB[u % 4]
                for hf in range(2):
                    rs = slice(64 * hf, 64 * hf + 64)
                    S.op("pe", lambda h, rs=rs: h.matmul(Db[rs, 0:n], lhsT=IQT[rs, tok0:tok0 + 16, :].rearrange("p t a -> p (t a)"), rhs=IKT[rs, 512 * kc:512 * kc + n], start=True, stop=True),
                         reads=IQT.b + IKT.b, writes=Db.b, inc=(hf == 1), pe_acc=True)
                if evc[0] % 4 != 3:
                    S.op("act", lambda h: h.activation(out=R[:, u % 4, 0:n], in_=Db[:, 0:n], func=AF.Relu), reads=Db.b, writes=[R.b[u % 4]])
                else:
                    S.op("dve", lambda h: h.tensor_scalar(out=R[:, u % 4, 0:n], in0=Db[:, 0:n], scalar1=0.0, scalar2=None, op0=ALU.max), reads=Db.b, writes=[R.b[u % 4]])
                evc[0] += 1

            def headsum(u):
                il, kc, g = units[u]
                i = 4 * c + il
                ncols = 128 * (i + 1)
                n = min(512, ncols - 512 * kc)
                ISb = PSB[4 + kc % 2]
                wb = i % 2
                if kc == 0 and g == 0:
                    S.op("pool", lambda h: h.tensor_tensor(out=Wblk[:, wb], in0=esel[:], in1=wcolT[:, 8 * i:8 * i + 8].unsqueeze(2).to_broadcast([128, 8, 128]), op=ALU.mult), reads=esel.b + wcolT.b, writes=[Wblk.b[wb]])
                S.op("pe", lambda h: h.matmul(ISb[:, 0:n], lhsT=Wblk[:, wb, g, :], rhs=R[:, u % 4, 0:n], start=(g == 0), stop=(g == 7)),
                     reads=[Wblk.b[wb], R.b[u % 4]], writes=ISb.b, inc=(g == 7), pe_acc=True)
                if g == 7:
                    S.op("act", lambda h: h.activation(out=IS[:, il, 512 * kc:512 * kc + n], in_=ISb[:, 0:n], func=AF.Copy), reads=ISb.b, writes=[IS.b[il]])
                    if 512 * kc + n == ncols:
                        S.op("dve", lambda h: h.tensor_reduce(out=bs[:, 20 + il:21 + il], in_=IS[:, il, 0:ncols], axis=AX.X, op=ALU.max, apply_absolute_value=True), reads=[IS.b[il]], writes=bs.b)
                        S.op("pool", lambda h: h.affine_select(out=IS[:, il, 128 * i:128 * i + 128], in_=IS[:, il, 128 * i:128 * i + 128], pattern=[[-1, 128]], compare_op=ALU.is_ge, fill=-1e30, base=0, channel_multiplier=1),
                             reads=[IS.b[il]], writes=[IS.b[il]])

            nu = len(units)
            dots(0)
            if nu > 1:
                dots(1)
            for u in range(nu):
                if u + 2 < nu:
                    dots(u + 2)
                headsum(u)

        nm2 = sb(es, "nm2", [128, 4], F32)
        cs2 = sb(es, "cs2", [128, 4], F32)

        def bisect_gen(c, share_act=False):
            S.op("dve", lambda h: h.tensor_scalar(out=bs[:, 0:4], in0=bs[:, 20:24], scalar1=-1.001, scalar2=-1e-3, op0=ALU.mult, op1=ALU.add), reads=bs.b, writes=bs.b)
            S.op("dve", lambda h: h.tensor_scalar(out=bs[:, 4:8], in0=bs[:, 20:24], scalar1=2.002, scalar2=2e-3, op0=ALU.mult, op1=ALU.add), reads=bs.b, writes=bs.b)
            for jb in range(1, NBIS + 1):
                stp = 2.0 ** (-jb)
                S.op("dve", lambda h, stp=stp: h.scalar_tensor_tensor(out=bs[:, 8:12], in0=bs[:, 4:8], scalar=stp, in1=bs[:, 0:4], op0=ALU.mult, op1=ALU.add), reads=bs.b, writes=bs.b)
                act_tiles = (1, 2) if share_act else ()
                if share_act:
                    S.op("dve", lambda h: h.tensor_scalar(out=nm2[:], in0=bs[:, 8:12], scalar1=-1.0, scalar2=None, op0=ALU.mult), reads=bs.b, writes=nm2.b)
                    for il in act_tiles:
                        ncols = 128 * (4 * c + il + 1)
                        S.op("act", lambda h, il=il, ncols=ncols: h.activation(out=NM[:, 0:ncols], in_=IS[:, il, 0:ncols], func=AF.Sign, bias=nm2[:, il:il + 1], scale=1.0, accum_out=cs2[:, il:il + 1]),
                             reads=[IS.b[il]] + nm2.b, writes=NM.b + cs2.b)
                for il in range(4):
                    if il in act_tiles:
                        continue
                    ncols = 128 * (4 * c + il + 1)
                    S.op("dve", lambda h, il=il, ncols=ncols: h.tensor_scalar(out=cjunk[:, 0:ncols], in0=IS[:, il, 0:ncols], scalar1=bs[:, 8 + il:9 + il], scalar2=0.0, op0=ALU.is_ge, op1=ALU.add, accum_out=bs[:, 12 + il:13 + il]),
                         reads=[IS.b[il]] + bs.b, writes=cjunk.b + bs.b)
                for il in act_tiles:
                    ncols = 128 * (4 * c + il + 1)
                    S.op("dve", lambda h, il=il, ncols=ncols: h.tensor_scalar(out=bs[:, 12 + il:13 + il], in0=cs2[:, il:il + 1], scalar1=0.5, scalar2=0.5 * ncols, op0=ALU.mult, op1=ALU.add), reads=cs2.b + bs.b, writes=bs.b)
                S.op("dve", lambda h, stp=stp: h.tensor_scalar(out=bs[:, 16:20], in0=bs[:, 12:16], scalar1=255.5, scalar2=stp, op0=ALU.is_ge, op1=ALU.mult), reads=bs.b, writes=bs.b)
                S.op("dve", lambda h: h.tensor_tensor(out=bs[:, 16:20], in0=bs[:, 16:20], in1=bs[:, 4:8], op=ALU.mult), reads=bs.b, writes=bs.b)
                S.op("dve", lambda h: h.tensor_tensor(out=bs[:, 0:4], in0=bs[:, 0:4], in1=bs[:, 16:20], op=ALU.add), reads=bs.b, writes=bs.b)
                yield

        def mask_epilogue(c):
            tb = 0
            nb_ = c % 2
            for il in range(4):
                i = 4 * c + il
                ncols = 128 * (i + 1)
                S.op("dve", lambda h, il=il, ncols=ncols: h.tensor_scalar(out=NM[:, 0:ncols], in0=IS[:, il, 0:ncols], scalar1=bs[:, il:il + 1], scalar2=NEG, op0=ALU.is_lt, op1=ALU.mult), reads=[IS.b[il]] + bs.b, writes=NM.b)
                for j0 in range(0, i + 1, 8):
                    nb = min(8, i + 1 - j0)
                    bi = 6 + tb % 2
                    tb += 1
                    for jj in range(nb):
                        S.op("pe", lambda h, bi=bi, jj=jj, j0=j0: h.transpose(psv_bf(bi)[:, jj * 128:(jj + 1) * 128], NM[:, (j0 + jj) * 128:(j0 + jj + 1) * 128], identb[:]),
                             reads=NM.b + identb.b, writes=PSB[bi].b, inc=(jj == nb - 1), pe_acc=True)
                    S.op("act", lambda h, bi=bi, j0=j0, nb=nb, il=il: h.activation(out=NMT2[:, nb_, j0:j0 + nb, il * 128:(il + 1) * 128], in_=psv_bf(bi)[:, 0:nb * 128].rearrange("p (a b) -> p a b", b=128), func=AF.Copy),
                         reads=PSB[bi].b, writes=[NMT2.b[nb_]])

        order = [3, 2, 1, 0]
        indexer(order[0])
        for _ in bisect_gen(order[0], share_act=True):
            pass
        mask_epilogue(order[0])
        for oi, c in enumerate(order):
            gen = None
            cn = order[oi + 1] if oi + 1 < 4 else None
            if cn is not None:
                indexer(cn)
                gen = bisect_gen(cn)
            nsteps = 4 * (4 * c + 4)
            every = max(1, nsteps // (NBIS + 1))

            def tick(k, n, gen=gen, every=every):
                if gen is not None and k % every == 0:
                    next(gen, None)
            nb_ = c % 2
            attention_chunk(
                sc, c, KTa, lambda hh: hh // 4, QTa,
                aug_lhs=lambda hh: (sc["ones67"][64 * (hh % 2):64 * (hh % 2) + 2, :], sc["ones67"].b),
                aug_rhs=lambda hh, col0: (alq[64 * (hh % 2):64 * (hh % 2) + 2, hh, col0:512], alq.b),
                mask_rhs=lambda j, col0, ncols, nb_=nb_: (NMT2[:, nb_, j, col0:512], [NMT2.b[nb_]]),
                bias_ap=lambda hh, j, c=c: (alk[:, hh, 4 * c - j + 3:4 * c - j + 4], alk.b),
                Vt=Va, v_idx=lambda hh: hh // 4, tick=tick)
            if gen is not None:
                for _ in gen:
                    pass
                mask_epilogue(cn)
            out_norm(sc, c, 0)
        if dbg:
            for nm, T_, shp, dt_ in [("IS", IS, [128, 4 * L], F32), ("bs", bs, [128, 32], F32), ("osb", sc["o_sb"], [128, 4 * 512], F32)]:
                dd = dbg_out(nm, shp, dt_)
                src_ap = T_[:] if len(T_.t.shape) == 2 else T_[:].rearrange("p a b -> p (a b)")
                S.dma("sp", lambda h, dd=dd, src_ap=src_ap: h.dma_start(out=dd, in_=src_ap), reads=T_.b, writes=[OUTB])
            d4 = dbg_out("oT", [128, 8 * L], BF16)
            S.dma("sp", lambda h: h.dma_start(out=d4, in_=HT[:].rearrange("p k t -> p (k t)")), reads=HT.b, writes=[OUTB])
        if phase_end(3):
            return nc, dbg_d
    es_dsa.close()

    with ExitStack() as es:
        X1 = sb(es, "X1", [128, NT, D], F32, nbuf=NT)
        x_v = x_d.rearrange("(i p) d -> i p d", p=128)
        with ExitStack() as es2:
            wo = sb(es2, "wo", [128, 8, D], BF16, nbuf=8)
            xt2 = sb(es2, "xt2", [128, 2, D], F32, nbuf=2)
            wout_v = wout_d.rearrange("(c p) d -> p c d", p=128)
            for fc in range(8):
                S.dma("pool", lambda h, fc=fc: h.dma_start(out=wo[:, fc, :], in_=wout_v[:, fc, :]), writes=[wo.b[fc]])
            for fc in range(8):
                S.op("pool", lambda h, fc=fc: h.tensor_tensor(out=wo[:, fc, :], in0=wo[:, fc, :], in1=gate_m[:], op=ALU.mult), reads=[wo.b[fc]] + gate_m.b, writes=[wo.b[fc]])
            for i in range(NT):
                bi = i % 2
                S.dma("sp", lambda h, i=i, bi=bi: h.dma_start(out=xt2[:, bi, :], in_=x_v[i]), writes=[xt2.b[bi]])
                for half in range(2):
                    bank = PSB[(2 * i + half) % 4]
                    for fc in range(8):
                        S.op("pe", lambda h, bank=bank, fc=fc, i=i, half=half: h.matmul(bank[:, :], lhsT=HT[:, fc, i * 128:(i + 1) * 128], rhs=wo[:, fc, half * 512:(half + 1) * 512], start=(fc == 0), stop=(fc == 7)),
                             reads=[HT.b[fc], wo.b[fc]], writes=bank.b, inc=(fc == 7), pe_acc=True)
                    S.op("dve", lambda h, bank=bank, i=i, half=half, bi=bi: h.tensor_tensor(out=X1[:, i, half * 512:(half + 1) * 512], in0=bank[:, :], in1=xt2[:, bi, half * 512:(half + 1) * 512], op=ALU.add),
                         reads=bank.b + [xt2.b[bi]], writes=[X1.b[i]])
            if dbg:
                d5 = dbg_out("X1", [128, NT * D], F32)
                S.dma("sp", lambda h: h.dma_start(out=d5, in_=X1[:].rearrange("p a b -> p (a b)")), reads=X1.b, writes=[OUTB])
            if phase_end(4):
                return nc, dbg_d
        with ExitStack() as es2:
            norm_to_HT(es2, lambda i: (X1[:, i, :], [X1.b[i]]), 16, 24, "2")
            if phase_end(5):
                return nc, dbg_d
        with ExitStack() as es2:
            combT = sb(es2, "combT", [32, L], BF16)
            sel = sb(es2, "sel", [32, NE * 128], BF16)
            es3 = ExitStack()
            wr = sb(es3, "wr", [128, 8, 36], BF16)
            brt = sb(es3, "brt", [128, 36], F32)
            lg = sb(es3, "lg", [128, NT, 36], F32)
            rt = sb(es3, "rt", [128, NT, 64], F32)
            comb = sb(es3, "comb", [128, NT, 32], BF16)
            S.dma("pool", lambda h: h.dma_start(out=wr[:], in_=wrt_d.rearrange("(p k) n -> p k n", k=8)), writes=wr.b)
            S.dma("sp", lambda h: h.dma_start(out=brt[:], in_=brt_d.partition_broadcast(128)), writes=brt.b)
            for i in range(NT):
                bank = PSB[i % 4]
                for kk in range(8):
                    S.op("pe", lambda h, bank=bank, kk=kk, i=i: h.matmul(bank[:, 0:36], lhsT=HT[:, kk, i * 128:(i + 1) * 128], rhs=wr[:, kk, :], start=(kk == 0), stop=(kk == 7)),
                         reads=[HT.b[kk]] + wr.b, writes=bank.b, inc=(kk == 7), pe_acc=True)
                S.op("dve", lambda h, bank=bank, i=i: h.tensor_tensor(out=lg[:, i, :], in0=bank[:, 0:36], in1=brt[:], op=ALU.add), reads=bank.b + brt.b, writes=lg.b)
            RB = rt.b + lg.b

            def dv(fn):
                S.op("dve", fn, reads=RB, writes=RB)

            def bc(ap, n):
                return ap.unsqueeze(2).to_broadcast([128, NT, n])
            gl = lg[:, :, 0:4]
            gmax, gsum, pg, m1, m2, w1, w2 = (rt[:, :, k] for k in range(7))
            ohg, gsh, el, tmp8, oh1, oh2, el2 = rt[:, :, 8:12], rt[:, :, 12:16], rt[:, :, 16:24], rt[:, :, 24:32], rt[:, :, 32:40], rt[:, :, 40:48], rt[:, :, 48:56]
            c8 = rt[:, :, 56:64]
            dv(lambda h: h.tensor_reduce(out=gmax, in_=gl, axis=AX.X, op=ALU.max))
            dv(lambda h: h.tensor_tensor(out=ohg, in0=gl, in1=bc(gmax, 4), op=ALU.is_ge))
            dv(lambda h: h.tensor_tensor(out=gsh, in0=gl, in1=bc(gmax, 4), op=ALU.subtract))
            S.op("act", lambda h: h.activation(out=gsh, in_=gsh, func=AF.Exp), reads=RB, writes=RB)
            dv(lambda h: h.tensor_reduce(out=gsum, in_=gsh, axis=AX.X, op=ALU.add))
            dv(lambda h: h.reciprocal(pg, gsum))
            for g in range(4):
                src_e = lg[:, :, 4 + 8 * g:12 + 8 * g]
                if g == 0:
                    dv(lambda h, src_e=src_e, g=g: h.tensor_tensor(out=el, in0=src_e, in1=bc(ohg[:, :, g], 8), op=ALU.mult))
                else:
                    dv(lambda h, src_e=src_e, g=g: h.tensor_tensor(out=tmp8, in0=src_e, in1=bc(ohg[:, :, g], 8), op=ALU.mult))
                    dv(lambda h: h.tensor_tensor(out=el, in0=el, in1=tmp8, op=ALU.add))
            dv(lambda h: h.tensor_reduce(out=m1, in_=el, axis=AX.X, op=ALU.max))
            dv(lambda h: h.tensor_tensor(out=oh1, in0=el, in1=bc(m1, 8), op=ALU.is_ge))
            dv(lambda h: h.scalar_tensor_tensor(out=el2, in0=oh1, scalar=-1e30, in1=el, op0=ALU.mult, op1=ALU.add))
            dv(lambda h: h.tensor_reduce(out=m2, in_=el2, axis=AX.X, op=ALU.max))
            dv(lambda h: h.tensor_tensor(out=oh2, in0=el2, in1=bc(m2, 8), op=ALU.is_ge))
            dv(lambda h: h.tensor_tensor(out=w2, in0=m2, in1=m1, op=ALU.subtract))
            S.op("act", lambda h: h.activation(out=w2, in_=w2, func=AF.Exp), reads=RB, writes=RB)
            dv(lambda h: h.tensor_scalar(out=w1, in0=w2, scalar1=1.0, scalar2=None, op0=ALU.add))
            dv(lambda h: h.reciprocal(w1, w1))
            dv(lambda h: h.tensor_tensor(out=w2, in0=w2, in1=w1, op=ALU.mult))
            dv(lambda h: h.tensor_tensor(out=w1, in0=w1, in1=pg, op=ALU.mult))
            dv(lambda h: h.tensor_tensor(out=w2, in0=w2, in1=pg, op=ALU.mult))
            dv(lambda h: h.tensor_tensor(out=c8, in0=oh1, in1=bc(w1, 8), op=ALU.mult))
            dv(lambda h: h.tensor_tensor(out=tmp8, in0=oh2, in1=bc(w2, 8), op=ALU.mult))
            dv(lambda h: h.tensor_tensor(out=c8, in0=c8, in1=tmp8, op=ALU.add))
            for g in range(4):
                S.op("dve", lambda h, g=g: h.tensor_tensor(out=comb[:, :, 8 * g:8 * g + 8], in0=c8, in1=bc(ohg[:, :, g], 8), op=ALU.mult), reads=RB, writes=comb.b)
            for i in range(NT):
                bi = 4 + (i // 8)
                S.op("pe", lambda h, bi=bi, i=i: h.transpose(psv_bf(bi)[0:32, (i % 8) * 128:(i % 8 + 1) * 128], comb[:, i, :], identb[:]), reads=comb.b + identb.b, writes=PSB[bi].b, inc=(i % 8 == 7), pe_acc=True)
            for hb in range(2):
                S.op("dve", lambda h, hb=hb: h.tensor_copy(combT[:, hb * 1024:(hb + 1) * 1024], psv_bf(4 + hb)[0:32, :]), reads=PSB[4 + hb].b, writes=combT.b)
            S.op("dve", lambda h: h.tensor_copy(sel[:].rearrange("k (e p) -> k e p", p=128), identb[0:32, 0:32].unsqueeze(2).to_broadcast([32, NE, 128])), reads=identb.b, writes=sel.b)
            S.emit()
            es3.close()
            wgu = sb(es2, "wgu", [128, 3, 2, 8, DFF], BF16, nbuf=3)
            wdn = sb(es2, "wdn", [128, 4, 2, D], BF16, nbuf=4)
            cbb = sb(es2, "cbb", [128, 2, L], BF16, nbuf=2)
            aT = sb(es2, "aT", [128, 4, 2, L], BF16, nbuf=4)
            sl = sb(es2, "sl", [128, 2, 512], BF16, nbuf=2)
            t1 = sb(es2, "t1", [128, 2, 512], BF16, nbuf=2)
            q = 0
            def load_gu(e):
                b = e % 3
                S.dma("pool", lambda h: h.dma_start(out=wgu[:, b, 0], in_=wg_d[e].rearrange("(p k) f -> p k f", k=8)), writes=[wgu.b[b]])
                S.dma("pool", lambda h: h.dma_start(out=wgu[:, b, 1], in_=wu_d[e].rearrange("(p k) f -> p k f", k=8)), writes=[wgu.b[b]])
            load_gu(0)
            load_gu(1)
            for rnd in range(8):
                for er in range(4):
                    e = 4 * rnd + er
                    b = e % 2
                    wb3 = e % 3
                    if e + 2 < NE:
                        load_gu(e + 2)
                    S.dma("pool", lambda h, e=e, er=er: h.dma_start(out=wdn[:, er], in_=wd_d[e].rearrange("(c p) d -> p c d", p=128)), writes=[wdn.b[er]])
                    for tcn in range(4):
                        cbank = PSB[4 + tcn]
                        S.op("pe", lambda h, cbank=cbank, e=e, tcn=tcn: h.matmul(cbank[:, :], lhsT=sel[:, e * 128:(e + 1) * 128], rhs=combT[:, tcn * 512:(tcn + 1) * 512], start=True, stop=True),
                             reads=sel.b + combT.b, writes=cbank.b)
                        if tcn % 2 == 0:
                            S.op("act", lambda h, cbank=cbank, b=b, tcn=tcn: h.activation(out=cbb[:, b, tcn * 512:(tcn + 1) * 512], in_=cbank[:, :], func=AF.Copy), reads=cbank.b, writes=[cbb.b[b]])
                        else:
                            S.op("dve", lambda h, cbank=cbank, b=b, tcn=tcn: h.tensor_copy(cbb[:, b, tcn * 512:(tcn + 1) * 512], cbank[:, :]), reads=cbank.b, writes=[cbb.b[b]])
                    for tcn in range(4):
                        for fc in range(2):
                            Gb = PSB[(2 * q) % 4]
                            Ub = PSB[(2 * q + 1) % 4]
                            qb = q % 2
                            q += 1
                            for gu, bank in ((0, Gb), (1, Ub)):
                                for kk in range(8):
                                    S.op("pe", lambda h, bank=bank, gu=gu, kk=kk, wb3=wb3, fc=fc, tcn=tcn: h.matmul(bank[:, :], lhsT=wgu[:, wb3, gu, kk, fc * 128:(fc + 1) * 128], rhs=HT[:, kk, tcn * 512:(tcn + 1) * 512], start=(kk == 0), stop=(kk == 7)),
                                         reads=[wgu.b[wb3], HT.b[kk]], writes=bank.b, inc=(kk == 7), pe_acc=True)
                            S.op("act", lambda h, Gb=Gb, qb=qb: h.activation(out=sl[:, qb, :], in_=Gb[:, :], func=AF.Silu), reads=Gb.b, writes=[sl.b[qb]])
                            S.op("dve", lambda h, Ub=Ub, qb=qb: h.tensor_tensor(out=t1[:, qb, :], in0=Ub[:, :], in1=sl[:, qb, :], op=ALU.mult), reads=Ub.b + [sl.b[qb]], writes=[t1.b[qb]])
                            S.op("pool", lambda h, qb=qb, er=er, fc=fc, tcn=tcn, b=b: h.tensor_tensor(out=aT[:, er, fc, tcn * 512:(tcn + 1) * 512], in0=t1[:, qb, :], in1=cbb[:, b, tcn * 512:(tcn + 1) * 512], op=ALU.mult),
                                 reads=[t1.b[qb], cbb.b[b]], writes=[aT.b[er]])
                    S.op("pool", lambda h, er=er: h.tensor_tensor(out=wdn[:, er], in0=wdn[:, er], in1=gate_f[:].unsqueeze(1).to_broadcast([128, 2, D]), op=ALU.mult), reads=[wdn.b[er]] + gate_f.b, writes=[wdn.b[er]])
                for i in range(NT):
                    for half in range(2):
                        bank = PSB[4 + (2 * i + half) % 4]
                        for er in range(4):
                            for fc in range(2):
                                S.op("pe", lambda h, bank=bank, er=er, fc=fc, i=i, half=half: h.matmul(bank[:, :], lhsT=aT[:, er, fc, i * 128:(i + 1) * 128], rhs=wdn[:, er, fc, half * 512:(half + 1) * 512], start=(er == 0 and fc == 0), stop=(er == 3 and fc == 1)),
                                     reads=[aT.b[er], wdn.b[er]], writes=bank.b, inc=(er == 3 and fc == 1), pe_acc=True)
                        S.op("dve", lambda h, bank=bank, i=i, half=half: h.tensor_tensor(out=X1[:, i, half * 512:(half + 1) * 512], in0=bank[:, :], in1=X1[:, i, half * 512:(half + 1) * 512], op=ALU.add),
                             reads=bank.b + [X1.b[i]], writes=[X1.b[i]])
            if phase_end(6):
                return nc, dbg_d
        with ExitStack() as es2:
            gfin = sb(es2, "gfin", [128, D], F32)
            yt = sb(es2, "yt", [128, 2, D], F32, nbuf=2)
            fj = sb(es2, "fj", [128, D], BF16)
            fs = sb(es2, "fs", [128, 2 * NT], F32)
            y_v = y_d.rearrange("(i p) d -> i p d", p=128)
            S.dma("sp", lambda h: h.dma_start(out=gfin[:], in_=rows_d[2:3, :].partition_broadcast(128)), writes=gfin.b)
            for g in range(4):
                for il in range(4):
                    i = 4 * g + il
                    S.op("act", lambda h, i=i: h.activation(out=fj[:], in_=X1[:, i, :], func=AF.Square, accum_out=fs[:, i:i + 1]), reads=[X1.b[i]], writes=fj.b + fs.b)
                S.op("act", lambda h, g=g: h.activation(out=fs[:, NT + 4 * g:NT + 4 * g + 4], in_=fs[:, 4 * g:4 * g + 4], func=AF.Sqrt, bias=EPS, scale=1.0 / D), reads=fs.b, writes=fs.b)
                S.op("dve", lambda h, g=g: h.reciprocal(fs[:, NT + 4 * g:NT + 4 * g + 4], fs[:, NT + 4 * g:NT + 4 * g + 4]), reads=fs.b, writes=fs.b)
                for il in range(4):
                    i = 4 * g + il
                    bi = i % 2
                    S.op("dve", lambda h, i=i, bi=bi: h.scalar_tensor_tensor(out=yt[:, bi, :], in0=X1[:, i, :], scalar=fs[:, NT + i:NT + i + 1], in1=gfin[:], op0=ALU.mult, op1=ALU.mult), reads=[X1.b[i]] + fs.b + gfin.b, writes=[yt.b[bi]])
                    S.dma("sp", lambda h, i=i, bi=bi: h.dma_start(out=y_v[i], in_=yt[:, bi, :]), reads=[yt.b[bi]], writes=[OUTB])
            S.wait_all("sp", [OUTB])
            S.emit()
    es_all.close()
    S.close()
    return nc, dbg_d


def _consts():
    identf = np.eye(128, dtype=np.float32)
    tri = np.zeros((128, 512), np.float32)
    s = np.arange(128)[:, None]
    t = np.arange(128)[None, :]
    tri[:, 0:128] = np.where(s > t, NEG, 0.0)
    p = np.arange(128)
    esel = np.zeros((128, 8, 128), np.float32)
    for g in range(8):
        esel[p, g, 16 * g + (p % 64) // 4] = 1.0
    slopes = np.exp2(-8.0 * np.arange(1, 9, dtype=np.float64) / 8).astype(np.float32)
    alk = np.zeros((128, 8, 16), np.float32)
    for di in range(16):
        dj = di - 3
        alk[:, :, di] = slopes[None, :] * (p[:, None] - 128.0 * dj)
    tl = np.arange(512)
    alq = np.zeros((2, 8, 512), np.float32)
    alq[0] = -8.0 * slopes[:, None] * (256.0 * (tl // 256))[None, :]
    alq[1] = -8.0 * slopes[:, None] * (tl % 256)[None, :]
    return dict(identf=identf, tri=tri, esel=esel.reshape(128, 1024), alibi_k=alk.reshape(128, 128), alibi_q=alq.reshape(2, 4096))


def _pk(v):
    return np.ascontiguousarray(np.asarray(v, np.float32).reshape(128, 8).T)


def _host_prep(inp):
    f = lambda a: np.ascontiguousarray(np.asarray(a, dtype=np.float32))
    x = f(inp["x"]); c = f(inp["c"])
    w_ada = f(inp["w_ada"][0]); b_ada = f(inp["b_ada"][0])
    w_in = f(inp["w_in"][0])
    cols = lambda a, b: w_in[:, a:b]
    aq, ak, av, iq, ik, iw, bq, bk, bv, bf = (cols(0, 512), cols(512, 640), cols(640, 768), cols(768, 1280), cols(1280, 1344),
                                              cols(1344, 1352), cols(1352, 1864), cols(1864, 2376), cols(2376, 2888), cols(2888, 2896))
    w_fm = np.concatenate([aq, ak[:, 0:64], ak[:, 0:64], ak[:, 64:128], ak[:, 64:128], iq, ik, ik, bq, bk, bf], axis=1)
    assert w_fm.shape[1] == 2440
    w_fm_pad = np.zeros((1024, 4 * 640), np.float32)
    w_fm_pad[:, :2440] = w_fm
    w_fm = np.ascontiguousarray(w_fm_pad.reshape(128, 8, 4, 640).transpose(2, 0, 1, 3).reshape(4 * 128, 8 * 640))
    w_tm = np.concatenate([bv, av, iw[:, [0, 2, 4, 6, 1, 3, 5, 7]]], axis=1)
    w_rt = np.concatenate([f(inp["w_group"][0])] + [f(inp["w_router"][0][g]) for g in range(4)], axis=1)
    brt = np.concatenate([f(inp["b_group"][0]), f(inp["b_router"][0]).reshape(-1)])[None, :]
    g_out = np.concatenate([f(inp["g_out_a"][0]), f(inp["g_out_b"][0])])
    rows = np.stack([b_ada[2048:3072], b_ada[5120:6144], f(inp["g_final"])])
    shared = dict(rows=np.ascontiguousarray(rows), brt=np.ascontiguousarray(brt), w_ada=w_ada, w_fm=np.ascontiguousarray(w_fm),
                  w_tm=np.ascontiguousarray(w_tm), nbf=f(inp["b_forget"][0]).reshape(8, 1), w_out=f(inp["w_out"][0]),
                  w_rt=np.ascontiguousarray(w_rt), w_gate=f(inp["w_gate"][0]), w_up=f(inp["w_up"][0]), w_down=f(inp["w_down"][0]))
    shared.update(_consts())
    sm_common = [_pk(b_ada[0:1024]), _pk(b_ada[1024:2048]), _pk(b_ada[3072:4096]), _pk(b_ada[4096:5120]),
                 _pk(inp["g_mix"][0]), _pk(inp["g_ffn"][0]), g_out.reshape(8, 128)]
    in_maps = []
    for b in range(8):
        m = dict(shared)
        m["x"] = x[b]
        m["smalls"] = np.ascontiguousarray(np.concatenate([_pk(c[b])] + sm_common, axis=0))
        in_maps.append(m)
    return in_maps


_CACHE = {}


def kernel(**inputs):
    in_maps = _host_prep(inputs)
    if "nc" not in _CACHE:
        _CACHE["nc"] = build_program(dbg=False)[0]
    res = run_bass_kernel_spmd(_CACHE["nc"], in_maps, core_ids=list(range(8)))
    return np.stack([np.asarray(r["y"], dtype=np.float32) for r in res.results], axis=0)
```

```python
from contextlib import ExitStack
import numpy as np
import concourse.bass as bass
import concourse.mybir as mybir
from concourse.bass_utils import run_bass_kernel_spmd

F32 = mybir.dt.float32
BF16 = mybir.dt.bfloat16
AF = mybir.ActivationFunctionType
ALU = mybir.AluOpType
AX = mybir.AxisListType

D = 1024
L = 2048
NT = 16
NE = 32
DFF = 256
EPS = 1e-6
NEG = -30000.0
NBIS = 16


class Buf:
    __slots__ = ("name", "lw", "rd")

    def __init__(self, name=""):
        self.name = name
        self.lw = None
        self.rd = []


class Sched:
    ENGS = ("pe", "act", "dve", "pool", "sp")

    def __init__(self, nc, n_dma_sems=32):
        self.nc = nc
        self.prog = {e: [] for e in self.ENGS}
        self.cnt = {e: 0 for e in self.ENGS}
        self.seen = {e: {} for e in self.ENGS}
        self.pend_rd = {e: [] for e in self.ENGS}
        self.pend_wr = {e: [] for e in self.ENGS}
        self.sems = {}
        self.n_dma = n_dma_sems
        self.dma_val = [0] * n_dma_sems
        self.dma_rr = 0
        self.dma_rr2 = [0, 0]
        self._ctx = []

    def open(self):
        nc = self.nc
        for e in self.ENGS:
            cm = nc.semaphore("s_" + e)
            self.sems[e] = cm.__enter__()
            self._ctx.append(cm)
        for i in range(self.n_dma):
            cm = nc.semaphore("s_dma%d" % i)
            self.sems[("dma", i)] = cm.__enter__()
            self._ctx.append(cm)

    def close(self):
        for cm in reversed(self._ctx):
            cm.__exit__(None, None, None)

    def _wait(self, eng, tok):
        if tok is None:
            return
        key, val = tok
        if self.seen[eng].get(key, 0) >= val:
            return
        self.seen[eng][key] = val
        sem = self.sems[key]
        self.prog[eng].append(lambda h, sem=sem, val=val: h.wait_ge(sem, val))

    def _deps(self, eng, reads, writes, pe_acc=False):
        for b in reads:
            self._wait(eng, b.lw)
        for b in writes:
            if not (pe_acc and b.lw is not None and b.lw[0] == "pe"):
                self._wait(eng, b.lw)
            for t in b.rd:
                self._wait(eng, t)

    def op(self, eng, fn, reads=(), writes=(), inc=True, pe_acc=False):
        reads = list(reads)
        writes = list(writes)
        self._deps(eng, reads, writes, pe_acc=pe_acc)
        if not inc:
            self.pend_rd[eng].extend(reads)
            self.pend_wr[eng].extend(writes)
            self.prog[eng].append(lambda h, fn=fn: fn(h))
            return
        self.cnt[eng] += 1
        tok = (eng, self.cnt[eng])
        sem = self.sems[eng]
        self.prog[eng].append(lambda h, fn=fn, sem=sem: fn(h).then_inc(sem, 1))
        for b in reads + self.pend_rd[eng]:
            b.rd.append(tok)
        for b in writes + self.pend_wr[eng]:
            b.lw = tok
            b.rd = []
        self.pend_rd[eng] = []
        self.pend_wr[eng] = []

    def dma(self, eng, fn, reads=(), writes=()):
        reads = list(reads)
        writes = list(writes)
        self._deps(eng, reads, writes)
        half = self.n_dma // 2
        qi = 0 if eng == "sp" else 1
        i = qi * half + self.dma_rr2[qi]
        self.dma_rr2[qi] = (self.dma_rr2[qi] + 1) % half
        key = ("dma", i)
        if self.dma_val[i] > 0:
            self._wait(eng, (key, self.dma_val[i]))
        self.dma_val[i] += 16
        tok = (key, self.dma_val[i])
        sem = self.sems[key]
        self.prog[eng].append(lambda h, fn=fn, sem=sem: fn(h).then_inc(sem, 16))
        for b in reads:
            b.rd.append(tok)
        for b in writes:
            b.lw = tok
            b.rd = []
        return tok

    def wait_all(self, eng, bufs):
        for b in bufs:
            self._wait(eng, b.lw)
            for t in b.rd:
                self._wait(eng, t)

    def emit(self):
        nc = self.nc
        for i in range(self.n_dma):
            if self.dma_val[i] > 0:
                self._wait("sp", (("dma", i), self.dma_val[i]))
        prog = self.prog
        with nc.Block() as block:
            @block.tensor
            def _(h):
                for f in prog["pe"]:
                    f(h)

            @block.scalar
            def _(h):
                for f in prog["act"]:
                    f(h)

            @block.vector
            def _(h):
                for f in prog["dve"]:
                    f(h)

            @block.gpsimd
            def _(h):
                for f in prog["pool"]:
                    f(h)

            @block.sync
            def _(h):
                for f in prog["sp"]:
                    f(h)
        self.prog = {e: [] for e in self.ENGS}


class Tn:
    def __init__(self, t, nbuf=1, name=""):
        self.t = t
        self.b = [Buf("%s%d" % (name, i)) for i in range(nbuf)]

    def __getitem__(self, k):
        return self.t[k]


def build_program(dbg=False, stop_after=99):
    nc = bass.Bass("TRN2", target_bir_lowering=False)
    S = Sched(nc)

    def din(name, shape, dt=F32):
        return nc.dram_tensor(name, list(shape), dt, kind="ExternalInput").ap()

    x_d = din("x", [L, D])
    smalls_d = din("smalls", [64, 128])
    rows_d = din("rows", [3, D])
    brt_d = din("brt", [1, 36])
    wada_d = din("w_ada", [D, 6 * D])
    wfm_d = din("w_fm", [4 * 128, 8 * 640])
    wtm_d = din("w_tm", [D, 648])
    nbf_d = din("nbf", [8, 1])
    wout_d = din("w_out", [D, D])
    wrt_d = din("w_rt", [D, 36])
    wg_d = din("w_gate", [NE, D, DFF])
    wu_d = din("w_up", [NE, D, DFF])
    wd_d = din("w_down", [NE, DFF, D])
    identf_d = din("identf", [128, 128])
    tri_d = din("tri", [128, 512])
    esel_d = din("esel", [128, 1024])
    alk_d = din("alibi_k", [128, 128])
    alq_d = din("alibi_q", [2, 8 * 512])
    y_d = nc.dram_tensor("y", [L, D], F32, kind="ExternalOutput").ap()
    iwe_d = nc.dram_tensor("iw_e", [128, 64], F32, kind="Internal").ap()
    iwo_d = nc.dram_tensor("iw_o", [128, 64], F32, kind="Internal").ap()
    combT_d = nc.dram_tensor("combT", [NE, L], BF16, kind="Internal").ap()
    dbg_d = {}

    def dbg_out(name, shape, dt=F32):
        dbg_d[name] = nc.dram_tensor("dbg_" + name, list(shape), dt, kind="ExternalOutput").ap()
        return dbg_d[name]

    S.open()
    es_all = ExitStack()

    def sb(es, name, shape, dt=F32, nbuf=1):
        t = es.enter_context(nc.sbuf_tensor("sb_" + name, list(shape), dt))
        return Tn(t, nbuf, name)

    def ps(es, name, shape, dt=F32, nbuf=1):
        t = es.enter_context(nc.psum_tensor("ps_" + name, list(shape), dt))
        return Tn(t, nbuf, name)

    OUTB = Buf("out")

    def phase_end(n):
        if stop_after == n:
            S.wait_all("sp", [OUTB])
            S.emit()
            return True
        S.emit()
        return False

    P = es_all
    HT = sb(P, "HT", [128, 8, L], BF16, nbuf=8)
    identb = sb(P, "identb", [128, 128], BF16)
    identf = sb(P, "identf", [128, 128], F32)
    smT = sb(P, "smT", [128, 64], F32)
    modp = sb(P, "modp", [128, 64], F32)
    gate_m = sb(P, "gate_m", [128, D], F32)
    gate_f = sb(P, "gate_f", [128, D], F32)
    scb = sb(P, "scb", [128, 8], BF16)
    screp = sb(P, "screp", [128, 8, 128], BF16)
    PSB = [ps(P, "psb%d" % i, [128, 512], F32) for i in range(8)]

    def psv_bf(i):
        return PSB[i].t[:].bitcast(BF16)

    es_dsa = ExitStack()
    QTa = sb(es_dsa, "QTa", [128, 4, L], BF16, nbuf=4)
    KTa = sb(es_dsa, "KTa", [128, 2, L], BF16, nbuf=2)
    IQT = sb(es_dsa, "IQT", [128, L, 4], BF16, nbuf=1)
    IKT = sb(es_dsa, "IKT", [128, L], BF16)
    Va = sb(es_dsa, "Va", [128, NT, 2, 65], BF16)
    wcolT = sb(es_dsa, "wcolT", [128, 128], F32)

    S.dma("sp", lambda h: h.dma_start(out=identf[:], in_=identf_d), writes=identf.b)
    S.dma("pool", lambda h: h.dma_start(out=identb[:], in_=identf_d), writes=identb.b)
    S.op("pool", lambda h: h.memset(Va[:, :, :, 64:65], 1.0), writes=Va.b)

    def norm_to_HT(es, src_tile_fn, a0, b0, tag):
        xn = sb(es, "xn" + tag, [128, 4, D], BF16, nbuf=4)
        junk = sb(es, "junk" + tag, [128, D], BF16)
        ss = sb(es, "ss" + tag, [128, NT], F32)
        rstd = sb(es, "rstd" + tag, [128, NT], F32)
        for g in range(4):
            srcs = [src_tile_fn(g * 4 + il) for il in range(4)]
            for il in range(4):
                i = g * 4 + il
                src, sbufs = srcs[il]
                S.op("act", lambda h, src=src, i=i: h.activation(out=junk[:], in_=src, func=AF.Square, accum_out=ss[:, i:i + 1]), reads=sbufs, writes=junk.b + ss.b)
            S.op("act", lambda h, g=g: h.activation(out=rstd[:, 4 * g:4 * g + 4], in_=ss[:, 4 * g:4 * g + 4], func=AF.Sqrt, bias=EPS, scale=1.0 / D), reads=ss.b, writes=rstd.b)
            S.op("dve", lambda h, g=g: h.reciprocal(rstd[:, 4 * g:4 * g + 4], rstd[:, 4 * g:4 * g + 4]), reads=rstd.b, writes=rstd.b)
            for il in range(4):
                i = g * 4 + il
                src, sbufs = srcs[il]
                S.op("dve", lambda h, src=src, i=i, il=il: h.tensor_scalar(out=xn[:, il, :], in0=src, scalar1=rstd[:, i:i + 1], scalar2=None, op0=ALU.mult), reads=sbufs + rstd.b, writes=[xn.b[il]])
            for kk in range(8):
                bank = PSB[kk // 2]
                off = (kk % 2) * 512
                for il in range(4):
                    S.op("pe", lambda h, bank=bank, off=off, il=il, kk=kk: h.transpose(psv_bf(PSB.index(bank))[:, off + il * 128: off + (il + 1) * 128], xn[:, il, kk::8], identb[:]),
                         reads=[xn.b[il]] + identb.b, writes=bank.b, inc=(il == 3), pe_acc=True)
                dst = HT[:, kk, g * 512:(g + 1) * 512]
                srcp = psv_bf(kk // 2)[:, off:off + 512]
                if kk % 2 == 0:
                    S.op("act", lambda h, dst=dst, srcp=srcp, kk=kk: h.activation(out=dst, in_=srcp, func=AF.Identity, scale=modp[:, a0 + kk:a0 + kk + 1], bias=modp[:, b0 + kk:b0 + kk + 1]),
                         reads=bank.b + modp.b, writes=[HT.b[kk]])
                else:
                    S.op("dve", lambda h, dst=dst, srcp=srcp, kk=kk: h.tensor_scalar(out=dst, in0=srcp, scalar1=modp[:, a0 + kk:a0 + kk + 1], scalar2=modp[:, b0 + kk:b0 + kk + 1], op0=ALU.mult, op1=ALU.add),
                         reads=bank.b + modp.b, writes=[HT.b[kk]])
        return rstd

    wada_v = wada_d.rearrange("(p k) n -> p k n", k=8)

    def mod_load(wa, bi, piece):
        S.dma("pool", lambda h: h.dma_start(out=wa[:, bi, :, :], in_=wada_v[:, :, piece * D:(piece + 1) * D]), writes=[wa.b[bi]])

    def mod_vec_piece(wa, bi, sl, bank):
        for kk in range(8):
            for k2 in range(8):
                S.op("pe", lambda h, kk=kk, k2=k2: h.matmul(bank[:, sl * 8 + kk:sl * 8 + kk + 1], lhsT=wa[:, bi, k2, kk::8], rhs=scb[:, k2:k2 + 1], start=(k2 == 0), stop=(k2 == 7)),
                     reads=[wa.b[bi]] + scb.b, writes=bank.b, inc=(k2 == 7 and kk == 7), pe_acc=True)

    def mod_gate_piece(wa, bi, gt, gi, brow, banks):
        for half in range(2):
            bank = banks[half]
            for k2 in range(8):
                S.op("pe", lambda h, bank=bank, k2=k2, half=half: h.matmul(bank[:, :], lhsT=screp[:, k2, :], rhs=wa[:, bi, k2, half * 512:(half + 1) * 512], start=(k2 == 0), stop=(k2 == 7)),
                     reads=[wa.b[bi]] + screp.b, writes=bank.b, inc=(k2 == 7), pe_acc=True)
            S.op("dve", lambda h, bank=bank, half=half: h.tensor_tensor(out=gt[:, half * 512:(half + 1) * 512], in0=bank[:, :], in1=brow[:, gi, half * 512:(half + 1) * 512], op=ALU.add),
                 reads=bank.b + [brow.b[gi]], writes=gt.b)

    with ExitStack() as es:
        sm_in = sb(es, "sm_in", [64, 128], F32)
        wa = sb(es, "wa", [128, 2, 8, D], BF16, nbuf=2)
        sc32 = sb(es, "sc32", [128, 8], F32)
        S.dma("sp", lambda h: h.dma_start(out=sm_in[:], in_=smalls_d), writes=sm_in.b)
        mod_load(wa, 0, 0)
        mod_load(wa, 1, 1)
        S.op("pe", lambda h: h.transpose(PSB[0][:, 0:64], sm_in[:], identf[0:64, 0:64]), reads=sm_in.b + identf.b, writes=PSB[0].b)
        S.op("dve", lambda h: h.tensor_copy(smT[:], PSB[0][:, 0:64]), reads=PSB[0].b, writes=smT.b)
        S.op("act", lambda h: h.activation(out=sc32[:], in_=smT[:, 0:8], func=AF.Silu), reads=smT.b, writes=sc32.b)
        S.op("dve", lambda h: h.tensor_copy(scb[:], sc32[:]), reads=sc32.b, writes=scb.b)
        S.op("dve", lambda h: h.tensor_copy(screp[:], sc32[:].unsqueeze(2).to_broadcast([128, 8, 128])), reads=sc32.b, writes=screp.b)
        mod_vec_piece(wa, 0, 0, PSB[1])
        mod_vec_piece(wa, 1, 1, PSB[1])
        S.op("dve", lambda h: h.tensor_tensor(out=modp[:, 32:48], in0=PSB[1][:, 0:16], in1=smT[:, 8:24], op=ALU.add), reads=PSB[1].b + smT.b, writes=modp.b)
        S.op("dve", lambda h: h.scalar_tensor_tensor(out=modp[:, 0:8], in0=modp[:, 40:48], scalar=1.0, in1=smT[:, 40:48], op0=ALU.add, op1=ALU.mult), reads=modp.b + smT.b, writes=modp.b)
        S.op("dve", lambda h: h.tensor_copy(modp[:, 8:16], modp[:, 32:40]), reads=modp.b, writes=modp.b)
        xt = sb(es, "xt", [128, 4, D], F32, nbuf=4)
        x_v = x_d.rearrange("(i p) d -> i p d", p=128)

        def src1(i):
            bi = i % 4
            S.dma("sp", lambda h, i=i, bi=bi: h.dma_start(out=xt[:, bi, :], in_=x_v[i]), writes=[xt.b[bi]])
            return xt[:, bi, :], [xt.b[bi]]

        norm_to_HT(es, src1, 0, 8, "1")
        if dbg:
            d3 = dbg_out("hT", [128, 8 * L], BF16)
            S.dma("sp", lambda h: h.dma_start(out=d3, in_=HT[:].rearrange("p k t -> p (k t)")), reads=HT.b, writes=[OUTB])
        S.emit()

    def deferred_mod(es):
        wa2 = sb(es, "wa2", [128, 1, 8, D], BF16, nbuf=1)
        brow = sb(es, "brow", [128, 1, D], F32, nbuf=1)
        S.dma("sp", lambda h: h.dma_start(out=brow[:, 0, :], in_=rows_d[0:1, :].partition_broadcast(128)), writes=[brow.b[0]])
        mod_load(wa2, 0, 3)

        def st_a():
            mod_vec_piece(wa2, 0, 0, PSB[5])
            S.op("dve", lambda h: h.tensor_tensor(out=modp[:, 48:56], in0=PSB[5][:, 0:8], in1=smT[:, 24:32], op=ALU.add), reads=PSB[5].b + smT.b, writes=modp.b)
            mod_load(wa2, 0, 4)

        def st_b():
            mod_vec_piece(wa2, 0, 1, PSB[5])
            S.op("dve", lambda h: h.tensor_tensor(out=modp[:, 56:64], in0=PSB[5][:, 8:16], in1=smT[:, 32:40], op=ALU.add), reads=PSB[5].b + smT.b, writes=modp.b)
            S.op("dve", lambda h: h.scalar_tensor_tensor(out=modp[:, 16:24], in0=modp[:, 56:64], scalar=1.0, in1=smT[:, 48:56], op0=ALU.add, op1=ALU.mult), reads=modp.b + smT.b, writes=modp.b)
            S.op("dve", lambda h: h.tensor_copy(modp[:, 24:32], modp[:, 48:56]), reads=modp.b, writes=modp.b)
            mod_load(wa2, 0, 2)

        def st_c():
            mod_gate_piece(wa2, 0, gate_m, 0, brow, (PSB[4], PSB[5]))
            S.dma("sp", lambda h: h.dma_start(out=brow[:, 0, :], in_=rows_d[1:2, :].partition_broadcast(128)), writes=[brow.b[0]])
            mod_load(wa2, 0, 5)

        def st_d():
            mod_gate_piece(wa2, 0, gate_f, 0, brow, (PSB[4], PSB[5]))
        return st_a, st_b, st_c, st_d

    es_fox = ExitStack()
    QTb = sb(es_fox, "QTb", [128, 4, L], BF16, nbuf=4)
    KTb = sb(es_fox, "KTb", [128, 4, L], BF16, nbuf=4)
    Vb = sb(es_fox, "Vb", [128, NT, 8, 65], BF16)
    cum3 = sb(es_fox, "cum3", [128, 4, L], BF16)
    S.op("pool", lambda h: h.memset(cum3[:], 0.0), writes=cum3.b)
    csT = sb(es_fox, "csT", [128, NT, 8], F32)
    S.op("pool", lambda h: h.memset(Vb[:, :, :, 64:65], 1.0), writes=Vb.b)

    with ExitStack() as es:
        wf = sb(es, "wf", [128, 2, 8, 640], BF16, nbuf=2)
        wt = sb(es, "wt", [128, 8, 648], BF16, nbuf=8)
        iw_tm = sb(es, "iw_tm", [128, NT, 8], F32)
        e1 = sb(es, "e1", [8, L], F32)
        nbf = sb(es, "nbf", [8, 1], F32)
        bfg = sb(es, "bfg", [8, 1], F32)
        wfm_v = wfm_d.rearrange("(q p) (k c) -> q p k c", p=128, c=640)
        wtm_v = wtm_d.rearrange("(p k) n -> p k n", k=8)
        for kk in range(8):
            S.dma("pool", lambda h, kk=kk: h.dma_start(out=wt[:, kk, :], in_=wtm_v[:, kk, :]), writes=[wt.b[kk]])
        S.dma("sp", lambda h: h.dma_start(out=bfg[:], in_=nbf_d), writes=bfg.b)
        S.op("dve", lambda h: h.tensor_scalar(out=nbf[:], in0=bfg[:], scalar1=-1.0, scalar2=None, op0=ALU.mult), reads=bfg.b, writes=nbf.b)
        dests = []
        for p_ in range(4):
            dests.append((QTa, p_))
        dests += [(KTa, 0), (KTa, 1)]
        for p_ in range(4):
            dests.append((IQT, p_))
        dests.append((IKT, None))
        for p_ in range(4):
            dests.append((QTb, p_))
        for p_ in range(4):
            dests.append((KTb, p_))
        pieces = [(0, 5), (5, 10), (10, 15), (15, 19)]
        cnt_ = {'ev': 0, 'pb': 0}
        def fm_part():
            ev = 0
            pb = 0
            for pi, (c0, c1) in enumerate(pieces):
                bi = pi % 2
                ncol = (c1 - c0) * 128
                S.dma("pool", lambda h, bi=bi, pi=pi: h.dma_start(out=wf[:, bi, :, :], in_=wfm_v[pi]), writes=[wf.b[bi]])
                for ch in range(c0, c1):
                    T_, idx = dests[ch]
                    for ng in range(4):
                        bank = PSB[4 + (pb % 4)]
                        pb += 1
                        for kk in range(8):
                            S.op("pe", lambda h, bank=bank, bi=bi, kk=kk, ch=ch, c0=c0, ng=ng: h.matmul(bank[:, :], lhsT=wf[:, bi, kk, (ch - c0) * 128:(ch - c0 + 1) * 128], rhs=HT[:, kk, ng * 512:(ng + 1) * 512], start=(kk == 0), stop=(kk == 7)),
                                 reads=[wf.b[bi], HT.b[kk]], writes=bank.b, inc=(kk == 7), pe_acc=True)
                        if idx is None:
                            dst = T_[:, ng * 512:(ng + 1) * 512]
                            wb_ = T_.b
                        elif T_ is IQT:
                            dst = T_[:, ng * 512:(ng + 1) * 512, idx]
                            wb_ = T_.b
                        else:
                            dst = T_[:, idx, ng * 512:(ng + 1) * 512]
                            wb_ = [T_.b[idx]]
                        if ev % 2 == 0:
                            S.op("act", lambda h, dst=dst, bank=bank: h.activation(out=dst, in_=bank[:, :], func=AF.Copy), reads=bank.b, writes=wb_)
                        else:
                            S.op("dve", lambda h, dst=dst, bank=bank: h.tensor_copy(dst, bank[:, :]), reads=bank.b, writes=wb_)
                        ev += 1
        wbf = sb(es, "wbf", [128, 8, 8], BF16)
        S.dma("pool", lambda h: h.dma_start(out=wbf[:], in_=wfm_v[3][:, :, 512:520]), writes=wbf.b)

        def bf_part():
            for ng in range(4):
                bank = PSB[4 + ng]
                for kk in range(8):
                    S.op("pe", lambda h, bank=bank, kk=kk, ng=ng: h.matmul(bank[0:8, :], lhsT=wbf[:, kk, :], rhs=HT[:, kk, ng * 512:(ng + 1) * 512], start=(kk == 0), stop=(kk == 7)),
                         reads=wbf.b + [HT.b[kk]], writes=bank.b, inc=(kk == 7), pe_acc=True)
                S.op("act", lambda h, bank=bank, ng=ng: h.activation(out=e1[:, ng * 512:(ng + 1) * 512], in_=bank[0:8, :], func=AF.Exp, scale=-1.0, bias=nbf[:, 0:1]), reads=bank.b + nbf.b, writes=e1.b)
        def tm_part():
            for i in range(NT):
                bA = PSB[(2 * i) % 4]
                bB = PSB[(2 * i + 1) % 4]
                for kk in range(8):
                    S.op("pe", lambda h, bA=bA, kk=kk, i=i: h.matmul(bA[:, :], lhsT=HT[:, kk, i * 128:(i + 1) * 128], rhs=wt[:, kk, 0:512], start=(kk == 0), stop=(kk == 7)),
                         reads=[HT.b[kk], wt.b[kk]], writes=bA.b, inc=(kk == 7), pe_acc=True)
                for kk in range(8):
                    S.op("pe", lambda h, bB=bB, kk=kk, i=i: h.matmul(bB[:, 0:136], lhsT=HT[:, kk, i * 128:(i + 1) * 128], rhs=wt[:, kk, 512:648], start=(kk == 0), stop=(kk == 7)),
                         reads=[HT.b[kk], wt.b[kk]], writes=bB.b, inc=(kk == 7), pe_acc=True)
                S.op("act", lambda h, bA=bA, i=i: h.activation(out=Vb[:, i, :, 0:64], in_=bA[:, :].rearrange("p (h d) -> p h d", h=8), func=AF.Copy), reads=bA.b, writes=Vb.b)
                S.op("dve", lambda h, bB=bB, i=i: h.tensor_copy(Va[:, i, :, 0:64], bB[:, 0:128].rearrange("p (h d) -> p h d", h=2)), reads=bB.b, writes=Va.b)
                S.op("dve", lambda h, bB=bB, i=i: h.tensor_copy(iw_tm[:, i, :], bB[:, 128:136]), reads=bB.b, writes=iw_tm.b)
        tm_part()
        bf_part()
        S.op("act", lambda h: h.activation(out=e1[:], in_=e1[:], func=AF.Ln, bias=1.0, scale=1.0), reads=e1.b, writes=e1.b)
        cs = sb(es, "cs", [8, L], F32)
        S.op("dve", lambda h: h.tensor_tensor_scan(out=cs[:], data0=e1[:], data1=e1[:], initial=0.0, op0=ALU.add, op1=ALU.max), reads=e1.b, writes=cs.b)
        for j in range(NT):
            S.op("pe", lambda h, j=j: h.transpose(PSB[4][:, j * 8:(j + 1) * 8], cs[:, j * 128:(j + 1) * 128], identf[0:8, 0:8]), reads=cs.b + identf.b, writes=PSB[4].b, inc=(j == NT - 1), pe_acc=True)
        S.op("dve", lambda h: h.tensor_copy(csT[:].rearrange("p j h -> p (j h)"), PSB[4][:, 0:128]), reads=PSB[4].b, writes=csT.b)
        hi3 = Tn(e1.t, 2, "hi3")
        hi3v = e1[:].bitcast(BF16).rearrange("p (a t) -> p a t", a=2)
        S.op("dve", lambda h: h.tensor_scalar(out=cs[:], in0=cs[:], scalar1=-8.0, scalar2=None, op0=ALU.mult), reads=cs.b, writes=cs.b)
        for r in range(3):
            S.op("dve", lambda h, r=r: h.tensor_copy(hi3v[:, r % 2, :], cs[:]), reads=cs.b, writes=[hi3.b[r % 2]] + (e1.b if r < 2 else []))
            if r < 2:
                S.op("dve", lambda h, r=r: h.tensor_tensor(out=cs[:], in0=cs[:], in1=hi3v[:, r % 2, :], op=ALU.subtract), reads=cs.b + [hi3.b[r % 2]], writes=cs.b)
            for hh in range(8):
                pp = 64 * (hh % 2) + r
                S.dma("sp", lambda h, hh=hh, pp=pp, r=r: h.dma_start(out=cum3[pp:pp + 1, hh // 2, :], in_=hi3v[hh:hh + 1, r % 2, :]), reads=[hi3.b[r % 2]], writes=cum3.b)
        iwe_flat = iwe_d.rearrange("g c -> (g c)").rearrange("(i t b) -> t i b", i=16, t=128, b=4)
        iwo_flat = iwo_d.rearrange("g c -> (g c)").rearrange("(i t b) -> t i b", i=16, t=128, b=4)
        SCR = Buf("scr")
        S.dma("sp", lambda h: h.dma_start(out=iwe_flat, in_=iw_tm[:, :, 0:4]), reads=iw_tm.b, writes=[SCR])
        S.dma("sp", lambda h: h.dma_start(out=iwo_flat, in_=iw_tm[:, :, 4:8]), reads=iw_tm.b, writes=[SCR])
        wc_in = sb(es, "wc_in", [128, 128], F32)
        S.dma("sp", lambda h: h.dma_start(out=wc_in[:, 0:64], in_=iwe_d), reads=[SCR], writes=wc_in.b)
        S.dma("sp", lambda h: h.dma_start(out=wc_in[:, 64:128], in_=iwo_d), reads=[SCR], writes=wc_in.b)
        fm_part()
        S.op("pe", lambda h: h.transpose(PSB[5][:, 0:128], wc_in[:], identf[:]), reads=wc_in.b + identf.b, writes=PSB[5].b)
        S.op("dve", lambda h: h.tensor_copy(wcolT[:], PSB[5][:, 0:128]), reads=PSB[5].b, writes=wcolT.b)
        if dbg:
            for nm, T_, shp, dt_ in [("QTa", QTa, [128, 4 * L], BF16), ("KTa", KTa, [128, 2 * L], BF16), ("IKT", IKT, [128, L], BF16),
                                     ("QTb", QTb, [128, 4 * L], BF16), ("KTb", KTb, [128, 4 * L], BF16), ("Vb", Vb, [128, NT * 8 * 65], BF16), ("Va", Va, [128, NT * 2 * 65], BF16),
                                     ("csT", csT, [128, NT * 8], F32), ("cum3", cum3, [128, 4 * L], BF16), ("wcolT", wcolT, [128, 128], F32)]:
                dd = dbg_out(nm, shp, dt_)
                nd = len(T_.t.shape)
                if nd == 2:
                    src_ap = T_[:]
                elif nd == 3:
                    src_ap = T_[:].rearrange("p a b -> p (a b)")
                else:
                    src_ap = T_[:].rearrange("p a b c -> p (a b c)")
                S.dma("sp", lambda h, dd=dd, src_ap=src_ap: h.dma_start(out=dd, in_=src_ap), reads=T_.b, writes=[OUTB])
        if phase_end(1):
            return nc, dbg_d

    def attn_scratch(es, tag):
        sc = {}
        sc["ones67"] = sb(es, "ones67" + tag, [128, 128], BF16)
        sc["PT"] = sb(es, "PT" + tag, [128, 4, 512], BF16, nbuf=4)
        sc["o_sb"] = sb(es, "o_sb" + tag, [128, 4, 512], F32)
        sc["ob"] = sb(es, "ob" + tag, [128, 4, 512], BF16)
        sc["rec"] = sb(es, "rec" + tag, [128, 8, 4], F32)
        sc["oss"] = sb(es, "oss" + tag, [128, 8], F32)
        sc["ojunk"] = sb(es, "ojunk" + tag, [128, 512], BF16)
        S.op("pool", lambda h: h.memset(sc["ones67"][:], 1.0), writes=sc["ones67"].b)
        return sc

    def attention_chunk(sc, c, KT, kt_idx, QT, aug_lhs, aug_rhs, mask_rhs, bias_ap, Vt, v_idx, tick=None):
        PT, o_sb, rec = sc["PT"], sc["o_sb"], sc["rec"]
        T0 = 512 * c
        nj = 4 * c + 4
        steps = [(pr, j) for pr in range(4) for j in range(nj)]

        def s_stage(k):
            pr, j = steps[k]
            col0 = max(0, j - 4 * c) * 128
            ncols = 512 - col0
            heads = (2 * pr, 2 * pr + 1)
            Sbs = (PSB[(2 * k) % 4], PSB[(2 * k + 1) % 4])
            for hh, Sb in zip(heads, Sbs):
                rows = slice(64 * (hh % 2), 64 * (hh % 2) + 64)
                S.op("pe", lambda h, hh=hh, Sb=Sb, rows=rows: h.matmul(Sb[:, 0:ncols], lhsT=KT[rows, kt_idx(hh), j * 128:(j + 1) * 128], rhs=QT[rows, pr, T0 + col0:T0 + 512], start=True, stop=False),
                     reads=[KT.b[kt_idx(hh)], QT.b[pr]], writes=Sb.b, inc=False, pe_acc=True)
            mr, mbufs = mask_rhs(j, col0, ncols)
            if mr is not None:
                for hh, Sb in zip(heads, Sbs):
                    S.op("pe", lambda h, Sb=Sb: h.matmul(Sb[:, 0:ncols], lhsT=identb[:], rhs=mr, start=False, stop=False),
                         reads=identb.b + mbufs, writes=Sb.b, inc=False, pe_acc=True)
            for hh, Sb in zip(heads, Sbs):
                al, albufs = aug_lhs(hh)
                ar, arbufs = aug_rhs(hh, col0)
                S.op("pe", lambda h, Sb=Sb, al=al, ar=ar: h.matmul(Sb[:, 0:ncols], lhsT=al, rhs=ar, start=False, stop=True),
                     reads=albufs + arbufs, writes=Sb.b, inc=True, pe_acc=True)
            for q_, (hh, Sb) in enumerate(zip(heads, Sbs)):
                pt = (2 * k + q_) % 4
                bap, bbufs = bias_ap(hh, j)
                S.op("act", lambda h, Sb=Sb, pt=pt, bap=bap: h.activation(out=PT[:, pt, 0:ncols], in_=Sb[:, 0:ncols], func=AF.Exp, scale=0.125, bias=bap),
                     reads=Sb.b + bbufs, writes=[PT.b[pt]])

        def pv_stage(k):
            pr, j = steps[k]
            col0 = max(0, j - 4 * c) * 128
            il0 = max(0, j - 4 * c)
            for q_ in range(2):
                hh = 2 * pr + q_
                pt = (2 * k + q_) % 4
                Ob = PSB[(6 if pr % 2 == 0 else 4) + q_]
                for il in range(il0, 4):
                    i = 4 * c + il
                    first = (j == 0 and il == il0)
                    S.op("pe", lambda h, il=il, i=i, first=first, Ob=Ob, pt=pt, hh=hh: h.matmul(Ob[:, il * 65:(il + 1) * 65], lhsT=PT[:, pt, il * 128 - col0:il * 128 - col0 + 128], rhs=Vt[:, j, v_idx(hh), :], start=first, stop=(j == i), skip_group_check=True),
                         reads=[PT.b[pt]] + Vt.b, writes=Ob.b, inc=(il == 3), pe_acc=True)
                if j == nj - 1:
                    Ov = Ob[:, 0:260].rearrange("p (a b) -> p a b", b=65)
                    S.op("dve", lambda h, Ov=Ov, hh=hh: h.reciprocal(rec[:, hh, :], Ov[:, :, 64]), reads=Ob.b, writes=rec.b)
                    S.op("dve", lambda h, Ov=Ov, hh=hh: h.tensor_tensor(out=o_sb[:, :, hh * 64:(hh + 1) * 64], in0=Ov[:, :, 0:64], in1=rec[:, hh, :].unsqueeze(2).to_broadcast([128, 4, 64]), op=ALU.mult),
                         reads=Ob.b + rec.b, writes=o_sb.b)

        n = len(steps)
        s_stage(0)
        for k in range(n):
            if k + 1 < n:
                s_stage(k + 1)
            pv_stage(k)
            if tick is not None:
                tick(k, n)

    def out_norm(sc, c, base):
        o_sb, ob, oss, ojunk = sc["o_sb"], sc["ob"], sc["oss"], sc["ojunk"]
        T0 = 512 * c
        for il in range(4):
            S.op("act", lambda h, il=il: h.activation(out=ojunk[:], in_=o_sb[:, il, :], func=AF.Square, accum_out=oss[:, il:il + 1]), reads=o_sb.b, writes=ojunk.b + oss.b)
        S.op("act", lambda h: h.activation(out=oss[:, 4:8], in_=oss[:, 0:4], func=AF.Sqrt, bias=EPS, scale=1.0 / 512), reads=oss.b, writes=oss.b)
        S.op("dve", lambda h: h.reciprocal(oss[:, 4:8], oss[:, 4:8]), reads=oss.b, writes=oss.b)
        S.op("dve", lambda h: h.tensor_tensor(out=ob[:], in0=o_sb[:], in1=oss[:, 4:8].unsqueeze(2).to_broadcast([128, 4, 512]), op=ALU.mult), reads=o_sb.b + oss.b, writes=ob.b)
        for fc in range(4):
            bank = PSB[4 + fc % 2]
            bi = 4 + fc % 2
            for il in range(4):
                S.op("pe", lambda h, bi=bi, il=il, fc=fc: h.transpose(psv_bf(bi)[:, il * 128:(il + 1) * 128], ob[:, il, fc * 128:(fc + 1) * 128], identb[:]),
                     reads=ob.b + identb.b, writes=bank.b, inc=(il == 3), pe_acc=True)
            S.op("act", lambda h, bi=bi, fc=fc: h.activation(out=HT[:, base + fc, T0:T0 + 512], in_=psv_bf(bi)[:, 0:512], func=AF.Identity, scale=smT[:, 56 + base + fc:57 + base + fc]),
                 reads=bank.b + smT.b, writes=[HT.b[base + fc]])

    with ExitStack() as es:
        sc = attn_scratch(es, "f")
        triw = sb(es, "triw", [128, 512], BF16)
        S.dma("pool", lambda h: h.dma_start(out=triw[:], in_=tri_d), writes=triw.b)
        mod_stages = deferred_mod(es)
        for c in range(4):
            if c >= 1:
                mod_stages[c - 1]()
            attention_chunk(
                sc, c, KTb, lambda hh: hh // 2, QTb,
                aug_lhs=lambda hh: (sc["ones67"][64 * (hh % 2):64 * (hh % 2) + 64, :], sc["ones67"].b),
                aug_rhs=lambda hh, col0, c=c: (cum3[64 * (hh % 2):64 * (hh % 2) + 64, hh // 2, 512 * c + col0:512 * c + 512], cum3.b),
                mask_rhs=lambda j, col0, ncols, c=c: ((triw[:, 0:ncols], triw.b) if j >= 4 * c else (None, [])),
                bias_ap=lambda hh, j: (csT[:, j, hh:hh + 1], csT.b),
                Vt=Vb, v_idx=lambda hh: hh)
            out_norm(sc, c, 4)
        mod_stages[3]()
        if dbg:
            d4b = dbg_out("oTb", [128, 8 * L], BF16)
            S.dma("sp", lambda h: h.dma_start(out=d4b, in_=HT[:].rearrange("p k t -> p (k t)")), reads=HT.b, writes=[OUTB])
        if phase_end(2):
            return nc, dbg_d
    es_fox.close()

    with ExitStack() as es:
        sc = attn_scratch(es, "a")
        IS = sb(es, "IS", [128, 4, L], F32, nbuf=4)
        NM = sb(es, "NM", [128, L], BF16)
        NMT2 = sb(es, "NMT", [128, 2, NT, 512], BF16, nbuf=2)
        R = sb(es, "R", [128, 4, 512], BF16, nbuf=4)
        Wblk = sb(es, "Wblk", [128, 2, 8, 128], BF16, nbuf=2)
        esel = sb(es, "esel", [128, 8, 128], BF16)
        alk = sb(es, "alk", [128, 8, 16], F32)
        alq = sb(es, "alq", [128, 4, 512], BF16)
        S.op("pool", lambda h: h.memset(alq[:], 0.0), writes=alq.b)
        bs = sb(es, "bs", [128, 32], F32)
        cjunk = sb(es, "cjunk", [128, L], BF16)
        S.dma("pool", lambda h: h.dma_start(out=esel[:].rearrange("p a b -> p (a b)"), in_=esel_d), writes=esel.b)
        alq_v = alq_d.rearrange("r (h t) -> r h t", t=512)
        S.dma("pool", lambda h: h.dma_start(out=alq[0:2, :, :], in_=alq_v[:, 0::2, :]), writes=alq.b)
        S.dma("pool", lambda h: h.dma_start(out=alq[64:66, :, :], in_=alq_v[:, 1::2, :]), writes=alq.b)
        S.dma("sp", lambda h: h.dma_start(out=alk[:].rearrange("p a b -> p (a b)"), in_=alk_d), writes=alk.b)
        evc = [0]

        def indexer(c):
            units = []
            for il in range(4):
                i = 4 * c + il
                ncols = 128 * (i + 1)
                for kc in range((ncols + 511) // 512):
                    for g in range(8):
                        units.append((il, kc, g))

            def dots(u):
                il, kc, g = units[u]
                i = 4 * c + il
                n = min(512, 128 * (i + 1) - 512 * kc)
                tok0 = 128 * i + 16 * g
                Db = PSB[u % 4]
                for hf in range(2):
                    rs = slice(64 * hf, 64 * hf + 64)
                    S.op("pe", lambda h, rs=rs: h.matmul(Db[rs, 0:n], lhsT=IQT[rs, tok0:tok0 + 16, :].rearrange("p t a -> p (t a)"), rhs=IKT[rs, 512 * kc:512 * kc + n], start=True, stop=True),
                         reads=IQT.b + IKT.b, writes=Db.b, inc=(hf == 1), pe_acc=True)
                if evc[0] % 4 != 3:
                    S.op("act", lambda h: h.activation(out=R[:, u % 4, 0:n], in_=Db[:, 0:n], func=AF.Relu), reads=Db.b, writes=[R.b[u % 4]])
                else:
                    S.op("dve", lambda h: h.tensor_scalar(out=R[:, u % 4, 0:n], in0=Db[:, 0:n], scalar1=0.0, scalar2=None, op0=ALU.max), reads=Db.b, writes=[R.b[u % 4]])
                evc[0] += 1

            def headsum(u):
                il, kc, g = units[u]
                i = 4 * c + il
                ncols = 128 * (i + 1)
                n = min(512, ncols - 512 * kc)
                ISb = PSB[4 + kc % 2]
                wb = i % 2
                if kc == 0 and g == 0:
                    S.op("pool", lambda h: h.tensor_tensor(out=Wblk[:, wb], in0=esel[:], in1=wcolT[:, 8 * i:8 * i + 8].unsqueeze(2).to_broadcast([128, 8, 128]), op=ALU.mult), reads=esel.b + wcolT.b, writes=[Wblk.b[wb]])
                S.op("pe", lambda h: h.matmul(ISb[:, 0:n], lhsT=Wblk[:, wb, g, :], rhs=R[:, u % 4, 0:n], start=(g == 0), stop=(g == 7)),
                     reads=[Wblk.b[wb], R.b[u % 4]], writes=ISb.b, inc=(g == 7), pe_acc=True)
                if g == 7:
                    S.op("act", lambda h: h.activation(out=IS[:, il, 512 * kc:512 * kc + n], in_=ISb[:, 0:n], func=AF.Copy), reads=ISb.b, writes=[IS.b[il]])
                    if 512 * kc + n == ncols:
                        S.op("dve", lambda h: h.tensor_reduce(out=bs[:, 20 + il:21 + il], in_=IS[:, il, 0:ncols], axis=AX.X, op=ALU.max, apply_absolute_value=True), reads=[IS.b[il]], writes=bs.b)
                        S.op("pool", lambda h: h.affine_select(out=IS[:, il, 128 * i:128 * i + 128], in_=IS[:, il, 128 * i:128 * i + 128], pattern=[[-1, 128]], compare_op=ALU.is_ge, fill=-1e30, base=0, channel_multiplier=1),
                             reads=[IS.b[il]], writes=[IS.b[il]])

            nu = len(units)
            dots(0)
            if nu > 1:
                dots(1)
            for u in range(nu):
                if u + 2 < nu:
                    dots(u + 2)
                headsum(u)

        nm2 = sb(es, "nm2", [128, 4], F32)
        cs2 = sb(es, "cs2", [128, 4], F32)

        def bisect_gen(c, share_act=False):
            S.op("dve", lambda h: h.tensor_scalar(out=bs[:, 0:4], in0=bs[:, 20:24], scalar1=-1.001, scalar2=-1e-3, op0=ALU.mult, op1=ALU.add), reads=bs.b, writes=bs.b)
            S.op("dve", lambda h: h.tensor_scalar(out=bs[:, 4:8], in0=bs[:, 20:24], scalar1=2.002, scalar2=2e-3, op0=ALU.mult, op1=ALU.add), reads=bs.b, writes=bs.b)
            for jb in range(1, NBIS + 1):
                stp = 2.0 ** (-jb)
                S.op("dve", lambda h, stp=stp: h.scalar_tensor_tensor(out=bs[:, 8:12], in0=bs[:, 4:8], scalar=stp, in1=bs[:, 0:4], op0=ALU.mult, op1=ALU.add), reads=bs.b, writes=bs.b)
                act_tiles = (1, 2) if share_act else ()
                if share_act:
                    S.op("dve", lambda h: h.tensor_scalar(out=nm2[:], in0=bs[:, 8:12], scalar1=-1.0, scalar2=None, op0=ALU.mult), reads=bs.b, writes=nm2.b)
                    for il in act_tiles:
                        ncols = 128 * (4 * c + il + 1)
                        S.op("act", lambda h, il=il, ncols=ncols: h.activation(out=NM[:, 0:ncols], in_=IS[:, il, 0:ncols], func=AF.Sign, bias=nm2[:, il:il + 1], scale=1.0, accum_out=cs2[:, il:il + 1]),
                             reads=[IS.b[il]] + nm2.b, writes=NM.b + cs2.b)
                for il in range(4):
                    if il in act_tiles:
                        continue
                    ncols = 128 * (4 * c + il + 1)
                    S.op("dve", lambda h, il=il, ncols=ncols: h.tensor_scalar(out=cjunk[:, 0:ncols], in0=IS[:, il, 0:ncols], scalar1=bs[:, 8 + il:9 + il], scalar2=0.0, op0=ALU.is_ge, op1=ALU.add, accum_out=bs[:, 12 + il:13 + il]),
                         reads=[IS.b[il]] + bs.b, writes=cjunk.b + bs.b)
                for il in act_tiles:
                    ncols = 128 * (4 * c + il + 1)
                    S.op("dve", lambda h, il=il, ncols=ncols: h.tensor_scalar(out=bs[:, 12 + il:13 + il], in0=cs2[:, il:il + 1], scalar1=0.5, scalar2=0.5 * ncols, op0=ALU.mult, op1=ALU.add), reads=cs2.b + bs.b, writes=bs.b)
                S.op("dve", lambda h, stp=stp: h.tensor_scalar(out=bs[:, 16:20], in0=bs[:, 12:16], scalar1=255.5, scalar2=stp, op0=ALU.is_ge, op1=ALU.mult), reads=bs.b, writes=bs.b)
                S.op("dve", lambda h: h.tensor_tensor(out=bs[:, 16:20], in0=bs[:, 16:20], in1=bs[:, 4:8], op=ALU.mult), reads=bs.b, writes=bs.b)
                S.op("dve", lambda h: h.tensor_tensor(out=bs[:, 0:4], in0=bs[:, 0:4], in1=bs[:, 16:20], op=ALU.add), reads=bs.b, writes=bs.b)
                yield

        def mask_epilogue(c):
            tb = 0
            nb_ = c % 2
            for il in range(4):
                i = 4 * c + il
                ncols = 128 * (i + 1)
                S.op("dve", lambda h, il=il, ncols=ncols: h.tensor_scalar(out=NM[:, 0:ncols], in0=IS[:, il, 0:ncols], scalar1=bs[:, il:il + 1], scalar2=NEG, op0=ALU.is_lt, op1=ALU.mult), reads=[IS.b[il]] + bs.b, writes=NM.b)
                for j0 in range(0, i + 1, 8):
                    nb = min(8, i + 1 - j0)
                    bi = 6 + tb % 2
                    tb += 1
                    for jj in range(nb):
                        S.op("pe", lambda h, bi=bi, jj=jj, j0=j0: h.transpose(psv_bf(bi)[:, jj * 128:(jj + 1) * 128], NM[:, (j0 + jj) * 128:(j0 + jj + 1) * 128], identb[:]),
                             reads=NM.b + identb.b, writes=PSB[bi].b, inc=(jj == nb - 1), pe_acc=True)
                    S.op("act", lambda h, bi=bi, j0=j0, nb=nb, il=il: h.activation(out=NMT2[:, nb_, j0:j0 + nb, il * 128:(il + 1) * 128], in_=psv_bf(bi)[:, 0:nb * 128].rearrange("p (a b) -> p a b", b=128), func=AF.Copy),
                         reads=PSB[bi].b, writes=[NMT2.b[nb_]])

        order = [3, 2, 1, 0]
        indexer(order[0])
        for _ in bisect_gen(order[0], share_act=True):
            pass
        mask_epilogue(order[0])
        for oi, c in enumerate(order):
            gen = None
            cn = order[oi + 1] if oi + 1 < 4 else None
            if cn is not None:
                indexer(cn)
                gen = bisect_gen(cn)
            nsteps = 4 * (4 * c + 4)
            every = max(1, nsteps // (NBIS + 1))

            def tick(k, n, gen=gen, every=every):
                if gen is not None and k % every == 0:
                    next(gen, None)
            nb_ = c % 2
            attention_chunk(
                sc, c, KTa, lambda hh: hh // 4, QTa,
                aug_lhs=lambda hh: (sc["ones67"][64 * (hh % 2):64 * (hh % 2) + 64, :], sc["ones67"].b),
                aug_rhs=lambda hh, col0: (alq[64 * (hh % 2):64 * (hh % 2) + 64, hh // 2, col0:512], alq.b),
                mask_rhs=lambda j, col0, ncols, nb_=nb_: (NMT2[:, nb_, j, col0:512], [NMT2.b[nb_]]),
                bias_ap=lambda hh, j, c=c: (alk[:, hh, 4 * c - j + 3:4 * c - j + 4], alk.b),
                Vt=Va, v_idx=lambda hh: hh // 4, tick=tick)
            if gen is not None:
                for _ in gen:
                    pass
                mask_epilogue(cn)
            out_norm(sc, c, 0)
        if dbg:
            for nm, T_, shp, dt_ in [("IS", IS, [128, 4 * L], F32), ("bs", bs, [128, 32], F32), ("osb", sc["o_sb"], [128, 4 * 512], F32)]:
                dd = dbg_out(nm, shp, dt_)
                src_ap = T_[:] if len(T_.t.shape) == 2 else T_[:].rearrange("p a b -> p (a b)")
                S.dma("sp", lambda h, dd=dd, src_ap=src_ap: h.dma_start(out=dd, in_=src_ap), reads=T_.b, writes=[OUTB])
            d4 = dbg_out("oT", [128, 8 * L], BF16)
            S.dma("sp", lambda h: h.dma_start(out=d4, in_=HT[:].rearrange("p k t -> p (k t)")), reads=HT.b, writes=[OUTB])
        if phase_end(3):
            return nc, dbg_d
    es_dsa.close()

    with ExitStack() as es:
        X1 = sb(es, "X1", [128, NT, D], F32, nbuf=NT)
        x_v = x_d.rearrange("(i p) d -> i p d", p=128)
        with ExitStack() as es2:
            wo = sb(es2, "wo", [128, 8, D], BF16, nbuf=8)
            xt2 = sb(es2, "xt2", [128, 2, D], F32, nbuf=2)
            wout_v = wout_d.rearrange("(c p) d -> p c d", p=128)
            for fc in range(8):
                S.dma("pool", lambda h, fc=fc: h.dma_start(out=wo[:, fc, :], in_=wout_v[:, fc, :]), writes=[wo.b[fc]])
            for fc in range(8):
                S.op("pool", lambda h, fc=fc: h.tensor_tensor(out=wo[:, fc, :], in0=wo[:, fc, :], in1=gate_m[:], op=ALU.mult), reads=[wo.b[fc]] + gate_m.b, writes=[wo.b[fc]])
            for i in range(NT):
                bi = i % 2
                S.dma("sp", lambda h, i=i, bi=bi: h.dma_start(out=xt2[:, bi, :], in_=x_v[i]), writes=[xt2.b[bi]])
                for half in range(2):
                    bank = PSB[(2 * i + half) % 4]
                    for fc in range(8):
                        S.op("pe", lambda h, bank=bank, fc=fc, i=i, half=half: h.matmul(bank[:, :], lhsT=HT[:, fc, i * 128:(i + 1) * 128], rhs=wo[:, fc, half * 512:(half + 1) * 512], start=(fc == 0), stop=(fc == 7)),
                             reads=[HT.b[fc], wo.b[fc]], writes=bank.b, inc=(fc == 7), pe_acc=True)
                    S.op("dve", lambda h, bank=bank, i=i, half=half, bi=bi: h.tensor_tensor(out=X1[:, i, half * 512:(half + 1) * 512], in0=bank[:, :], in1=xt2[:, bi, half * 512:(half + 1) * 512], op=ALU.add),
                         reads=bank.b + [xt2.b[bi]], writes=[X1.b[i]])
            if dbg:
                d5 = dbg_out("X1", [128, NT * D], F32)
                S.dma("sp", lambda h: h.dma_start(out=d5, in_=X1[:].rearrange("p a b -> p (a b)")), reads=X1.b, writes=[OUTB])
            if phase_end(4):
                return nc, dbg_d
        with ExitStack() as es2:
            norm_to_HT(es2, lambda i: (X1[:, i, :], [X1.b[i]]), 16, 24, "2")
            if phase_end(5):
                return nc, dbg_d
        with ExitStack() as es2:
            combT = sb(es2, "combT", [32, L], BF16)
            sel = sb(es2, "sel", [32, NE * 128], BF16)
            es3 = ExitStack()
            wr = sb(es3, "wr", [128, 8, 36], BF16)
            brt = sb(es3, "brt", [128, 36], F32)
            lg = sb(es3, "lg", [128, NT, 36], F32)
            rt = sb(es3, "rt", [128, NT, 64], F32)
            comb = sb(es3, "comb", [128, NT, 32], BF16)
            S.dma("pool", lambda h: h.dma_start(out=wr[:], in_=wrt_d.rearrange("(p k) n -> p k n", k=8)), writes=wr.b)
            S.dma("sp", lambda h: h.dma_start(out=brt[:], in_=brt_d.partition_broadcast(128)), writes=brt.b)
            for i in range(NT):
                bank = PSB[i % 4]
                for kk in range(8):
                    S.op("pe", lambda h, bank=bank, kk=kk, i=i: h.matmul(bank[:, 0:36], lhsT=HT[:, kk, i * 128:(i + 1) * 128], rhs=wr[:, kk, :], start=(kk == 0), stop=(kk == 7)),
                         reads=[HT.b[kk]] + wr.b, writes=bank.b, inc=(kk == 7), pe_acc=True)
                S.op("dve", lambda h, bank=bank, i=i: h.tensor_tensor(out=lg[:, i, :], in0=bank[:, 0:36], in1=brt[:], op=ALU.add), reads=bank.b + brt.b, writes=lg.b)
            RB = rt.b + lg.b

            def dv(fn):
                S.op("dve", fn, reads=RB, writes=RB)

            def bc(ap, n):
                return ap.unsqueeze(2).to_broadcast([128, NT, n])
            gl = lg[:, :, 0:4]
            gmax, gsum, pg, m1, m2, w1, w2 = (rt[:, :, k] for k in range(7))
            ohg, gsh, el, tmp8, oh1, oh2, el2 = rt[:, :, 8:12], rt[:, :, 12:16], rt[:, :, 16:24], rt[:, :, 24:32], rt[:, :, 32:40], rt[:, :, 40:48], rt[:, :, 48:56]
            c8 = rt[:, :, 56:64]
            dv(lambda h: h.tensor_reduce(out=gmax, in_=gl, axis=AX.X, op=ALU.max))
            dv(lambda h: h.tensor_tensor(out=ohg, in0=gl, in1=bc(gmax, 4), op=ALU.is_ge))
            dv(lambda h: h.tensor_tensor(out=gsh, in0=gl, in1=bc(gmax, 4), op=ALU.subtract))
            S.op("act", lambda h: h.activation(out=gsh, in_=gsh, func=AF.Exp), reads=RB, writes=RB)
            dv(lambda h: h.tensor_reduce(out=gsum, in_=gsh, axis=AX.X, op=ALU.add))
            dv(lambda h: h.reciprocal(pg, gsum))
            for g in range(4):
                src_e = lg[:, :, 4 + 8 * g:12 + 8 * g]
                if g == 0:
                    dv(lambda h, src_e=src_e, g=g: h.tensor_tensor(out=el, in0=src_e, in1=bc(ohg[:, :, g], 8), op=ALU.mult))
                else:
                    dv(lambda h, src_e=src_e, g=g: h.tensor_tensor(out=tmp8, in0=src_e, in1=bc(ohg[:, :, g], 8), op=ALU.mult))
                    dv(lambda h: h.tensor_tensor(out=el, in0=el, in1=tmp8, op=ALU.add))
            dv(lambda h: h.tensor_reduce(out=m1, in_=el, axis=AX.X, op=ALU.max))
            dv(lambda h: h.tensor_tensor(out=oh1, in0=el, in1=bc(m1, 8), op=ALU.is_ge))
            dv(lambda h: h.scalar_tensor_tensor(out=el2, in0=oh1, scalar=-1e30, in1=el, op0=ALU.mult, op1=ALU.add))
            dv(lambda h: h.tensor_reduce(out=m2, in_=el2, axis=AX.X, op=ALU.max))
            dv(lambda h: h.tensor_tensor(out=oh2, in0=el2, in1=bc(m2, 8), op=ALU.is_ge))
            dv(lambda h: h.tensor_tensor(out=w2, in0=m2, in1=m1, op=ALU.subtract))
            S.op("act", lambda h: h.activation(out=w2, in_=w2, func=AF.Exp), reads=RB, writes=RB)
            dv(lambda h: h.tensor_scalar(out=w1, in0=w2, scalar1=1.0, scalar2=None, op0=ALU.add))
            dv(lambda h: h.reciprocal(w1, w1))
            dv(lambda h: h.tensor_tensor(out=w2, in0=w2, in1=w1, op=ALU.mult))
            dv(lambda h: h.tensor_tensor(out=w1, in0=w1, in1=pg, op=ALU.mult))
            dv(lambda h: h.tensor_tensor(out=w2, in0=w2, in1=pg, op=ALU.mult))
            dv(lambda h: h.tensor_tensor(out=c8, in0=oh1, in1=bc(w1, 8), op=ALU.mult))
            dv(lambda h: h.tensor_tensor(out=tmp8, in0=oh2, in1=bc(w2, 8), op=ALU.mult))
            dv(lambda h: h.tensor_tensor(out=c8, in0=c8, in1=tmp8, op=ALU.add))
            for g in range(4):
                S.op("dve", lambda h, g=g: h.tensor_tensor(out=comb[:, :, 8 * g:8 * g + 8], in0=c8, in1=bc(ohg[:, :, g], 8), op=ALU.mult), reads=RB, writes=comb.b)
            for i in range(NT):
                bi = 4 + (i // 8)
                S.op("pe", lambda h, bi=bi, i=i: h.transpose(psv_bf(bi)[0:32, (i % 8) * 128:(i % 8 + 1) * 128], comb[:, i, :], identb[:]), reads=comb.b + identb.b, writes=PSB[bi].b, inc=(i % 8 == 7), pe_acc=True)
            for hb in range(2):
                S.op("dve", lambda h, hb=hb: h.tensor_copy(combT[:, hb * 1024:(hb + 1) * 1024], psv_bf(4 + hb)[0:32, :]), reads=PSB[4 + hb].b, writes=combT.b)
            S.op("dve", lambda h: h.tensor_copy(sel[:].rearrange("k (e p) -> k e p", p=128), identb[0:32, 0:32].unsqueeze(2).to_broadcast([32, NE, 128])), reads=identb.b, writes=sel.b)
            S.emit()
            es3.close()
            wgu = sb(es2, "wgu", [128, 3, 2, 8, DFF], BF16, nbuf=3)
            wdn = sb(es2, "wdn", [128, 4, 2, D], BF16, nbuf=4)
            cbb = sb(es2, "cbb", [128, 2, L], BF16, nbuf=2)
            aT = sb(es2, "aT", [128, 4, 2, L], BF16, nbuf=4)
            sl = sb(es2, "sl", [128, 2, 512], BF16, nbuf=2)
            t1 = sb(es2, "t1", [128, 2, 512], BF16, nbuf=2)
            q = 0
            def load_gu(e):
                b = e % 3
                S.dma("pool", lambda h: h.dma_start(out=wgu[:, b, 0], in_=wg_d[e].rearrange("(p k) f -> p k f", k=8)), writes=[wgu.b[b]])
                S.dma("pool", lambda h: h.dma_start(out=wgu[:, b, 1], in_=wu_d[e].rearrange("(p k) f -> p k f", k=8)), writes=[wgu.b[b]])
            load_gu(0)
            load_gu(1)
            for rnd in range(8):
                for er in range(4):
                    e = 4 * rnd + er
                    b = e % 2
                    wb3 = e % 3
                    if e + 2 < NE:
                        load_gu(e + 2)
                    S.dma("pool", lambda h, e=e, er=er: h.dma_start(out=wdn[:, er], in_=wd_d[e].rearrange("(c p) d -> p c d", p=128)), writes=[wdn.b[er]])
                    for tcn in range(4):
                        cbank = PSB[4 + tcn]
                        S.op("pe", lambda h, cbank=cbank, e=e, tcn=tcn: h.matmul(cbank[:, :], lhsT=sel[:, e * 128:(e + 1) * 128], rhs=combT[:, tcn * 512:(tcn + 1) * 512], start=True, stop=True),
                             reads=sel.b + combT.b, writes=cbank.b)
                        if tcn % 2 == 0:
                            S.op("act", lambda h, cbank=cbank, b=b, tcn=tcn: h.activation(out=cbb[:, b, tcn * 512:(tcn + 1) * 512], in_=cbank[:, :], func=AF.Copy), reads=cbank.b, writes=[cbb.b[b]])
                        else:
                            S.op("dve", lambda h, cbank=cbank, b=b, tcn=tcn: h.tensor_copy(cbb[:, b, tcn * 512:(tcn + 1) * 512], cbank[:, :]), reads=cbank.b, writes=[cbb.b[b]])
                    for tcn in range(4):
                        for fc in range(2):
                            Gb = PSB[(2 * q) % 4]
                            Ub = PSB[(2 * q + 1) % 4]
                            qb = q % 2
                            q += 1
                            for gu, bank in ((0, Gb), (1, Ub)):
                                for kk in range(8):
                                    S.op("pe", lambda h, bank=bank, gu=gu, kk=kk, wb3=wb3, fc=fc, tcn=tcn: h.matmul(bank[:, :], lhsT=wgu[:, wb3, gu, kk, fc * 128:(fc + 1) * 128], rhs=HT[:, kk, tcn * 512:(tcn + 1) * 512], start=(kk == 0), stop=(kk == 7)),
                                         reads=[wgu.b[wb3], HT.b[kk]], writes=bank.b, inc=(kk == 7), pe_acc=True)
                            S.op("act", lambda h, Gb=Gb, qb=qb: h.activation(out=sl[:, qb, :], in_=Gb[:, :], func=AF.Silu), reads=Gb.b, writes=[sl.b[qb]])
                            S.op("dve", lambda h, Ub=Ub, qb=qb: h.tensor_tensor(out=t1[:, qb, :], in0=Ub[:, :], in1=sl[:, qb, :], op=ALU.mult), reads=Ub.b + [sl.b[qb]], writes=[t1.b[qb]])
                            S.op("pool", lambda h, qb=qb, er=er, fc=fc, tcn=tcn, b=b: h.tensor_tensor(out=aT[:, er, fc, tcn * 512:(tcn + 1) * 512], in0=t1[:, qb, :], in1=cbb[:, b, tcn * 512:(tcn + 1) * 512], op=ALU.mult),
                                 reads=[t1.b[qb], cbb.b[b]], writes=[aT.b[er]])
                    S.op("pool", lambda h, er=er: h.tensor_tensor(out=wdn[:, er], in0=wdn[:, er], in1=gate_f[:].unsqueeze(1).to_broadcast([128, 2, D]), op=ALU.mult), reads=[wdn.b[er]] + gate_f.b, writes=[wdn.b[er]])
                for i in range(NT):
                    for half in range(2):
                        bank = PSB[4 + (2 * i + half) % 4]
                        for er in range(4):
                            for fc in range(2):
                                S.op("pe", lambda h, bank=bank, er=er, fc=fc, i=i, half=half: h.matmul(bank[:, :], lhsT=aT[:, er, fc, i * 128:(i + 1) * 128], rhs=wdn[:, er, fc, half * 512:(half + 1) * 512], start=(er == 0 and fc == 0), stop=(er == 3 and fc == 1)),
                                     reads=[aT.b[er], wdn.b[er]], writes=bank.b, inc=(er == 3 and fc == 1), pe_acc=True)
                        S.op("dve", lambda h, bank=bank, i=i, half=half: h.tensor_tensor(out=X1[:, i, half * 512:(half + 1) * 512], in0=bank[:, :], in1=X1[:, i, half * 512:(half + 1) * 512], op=ALU.add),
                             reads=bank.b + [X1.b[i]], writes=[X1.b[i]])
            if phase_end(6):
                return nc, dbg_d
        with ExitStack() as es2:
            gfin = sb(es2, "gfin", [128, D], F32)
            yt = sb(es2, "yt", [128, 2, D], F32, nbuf=2)
            fj = sb(es2, "fj", [128, D], BF16)
            fs = sb(es2, "fs", [128, 2 * NT], F32)
            y_v = y_d.rearrange("(i p) d -> i p d", p=128)
            S.dma("sp", lambda h: h.dma_start(out=gfin[:], in_=rows_d[2:3, :].partition_broadcast(128)), writes=gfin.b)
            for g in range(4):
                for il in range(4):
                    i = 4 * g + il
                    S.op("act", lambda h, i=i: h.activation(out=fj[:], in_=X1[:, i, :], func=AF.Square, accum_out=fs[:, i:i + 1]), reads=[X1.b[i]], writes=fj.b + fs.b)
                S.op("act", lambda h, g=g: h.activation(out=fs[:, NT + 4 * g:NT + 4 * g + 4], in_=fs[:, 4 * g:4 * g + 4], func=AF.Sqrt, bias=EPS, scale=1.0 / D), reads=fs.b, writes=fs.b)
                S.op("dve", lambda h, g=g: h.reciprocal(fs[:, NT + 4 * g:NT + 4 * g + 4], fs[:, NT + 4 * g:NT + 4 * g + 4]), reads=fs.b, writes=fs.b)
                for il in range(4):
                    i = 4 * g + il
                    bi = i % 2
                    S.op("dve", lambda h, i=i, bi=bi: h.scalar_tensor_tensor(out=yt[:, bi, :], in0=X1[:, i, :], scalar=fs[:, NT + i:NT + i + 1], in1=gfin[:], op0=ALU.mult, op1=ALU.mult), reads=[X1.b[i]] + fs.b + gfin.b, writes=[yt.b[bi]])
                    S.dma("sp", lambda h, i=i, bi=bi: h.dma_start(out=y_v[i], in_=yt[:, bi, :]), reads=[yt.b[bi]], writes=[OUTB])
            S.wait_all("sp", [OUTB])
            S.emit()
    es_all.close()
    S.close()
    return nc, dbg_d


def _consts():
    identf = np.eye(128, dtype=np.float32)
    tri = np.zeros((128, 512), np.float32)
    s = np.arange(128)[:, None]
    t = np.arange(128)[None, :]
    tri[:, 0:128] = np.where(s > t, NEG, 0.0)
    p = np.arange(128)
    esel = np.zeros((128, 8, 128), np.float32)
    for g in range(8):
        esel[p, g, 16 * g + (p % 64) // 4] = 1.0
    slopes = np.exp2(-8.0 * np.arange(1, 9, dtype=np.float64) / 8).astype(np.float32)
    alk = np.zeros((128, 8, 16), np.float32)
    for di in range(16):
        dj = di - 3
        alk[:, :, di] = slopes[None, :] * (p[:, None] - 128.0 * dj)
    tl = np.arange(512)
    alq = np.zeros((2, 8, 512), np.float32)
    alq[0] = -8.0 * slopes[:, None] * (256.0 * (tl // 256))[None, :]
    alq[1] = -8.0 * slopes[:, None] * (tl % 256)[None, :]
    return dict(identf=identf, tri=tri, esel=esel.reshape(128, 1024), alibi_k=alk.reshape(128, 128), alibi_q=alq.reshape(2, 4096))


def _pk(v):
    return np.ascontiguousarray(np.asarray(v, np.float32).reshape(128, 8).T)


def _host_prep(inp):
    f = lambda a: np.ascontiguousarray(np.asarray(a, dtype=np.float32))
    x = f(inp["x"]); c = f(inp["c"])
    w_ada = f(inp["w_ada"][0]); b_ada = f(inp["b_ada"][0])
    w_in = f(inp["w_in"][0])
    cols = lambda a, b: w_in[:, a:b]
    aq, ak, av, iq, ik, iw, bq, bk, bv, bf = (cols(0, 512), cols(512, 640), cols(640, 768), cols(768, 1280), cols(1280, 1344),
                                              cols(1344, 1352), cols(1352, 1864), cols(1864, 2376), cols(2376, 2888), cols(2888, 2896))
    w_fm = np.concatenate([aq, ak[:, 0:64], ak[:, 0:64], ak[:, 64:128], ak[:, 64:128], iq, ik, ik, bq, bk, bf], axis=1)
    assert w_fm.shape[1] == 2440
    w_fm_pad = np.zeros((1024, 4 * 640), np.float32)
    w_fm_pad[:, :2440] = w_fm
    w_fm = np.ascontiguousarray(w_fm_pad.reshape(128, 8, 4, 640).transpose(2, 0, 1, 3).reshape(4 * 128, 8 * 640))
    w_tm = np.concatenate([bv, av, iw[:, [0, 2, 4, 6, 1, 3, 5, 7]]], axis=1)
    w_rt = np.concatenate([f(inp["w_group"][0])] + [f(inp["w_router"][0][g]) for g in range(4)], axis=1)
    brt = np.concatenate([f(inp["b_group"][0]), f(inp["b_router"][0]).reshape(-1)])[None, :]
    g_out = np.concatenate([f(inp["g_out_a"][0]), f(inp["g_out_b"][0])])
    rows = np.stack([b_ada[2048:3072], b_ada[5120:6144], f(inp["g_final"])])
    shared = dict(rows=np.ascontiguousarray(rows), brt=np.ascontiguousarray(brt), w_ada=w_ada, w_fm=np.ascontiguousarray(w_fm),
                  w_tm=np.ascontiguousarray(w_tm), nbf=f(inp["b_forget"][0]).reshape(8, 1), w_out=f(inp["w_out"][0]),
                  w_rt=np.ascontiguousarray(w_rt), w_gate=f(inp["w_gate"][0]), w_up=f(inp["w_up"][0]), w_down=f(inp["w_down"][0]))
    shared.update(_consts())
    sm_common = [_pk(b_ada[0:1024]), _pk(b_ada[1024:2048]), _pk(b_ada[3072:4096]), _pk(b_ada[4096:5120]),
                 _pk(inp["g_mix"][0]), _pk(inp["g_ffn"][0]), g_out.reshape(8, 128)]
    in_maps = []
    for b in range(8):
        m = dict(shared)
        m["x"] = x[b]
        m["smalls"] = np.ascontiguousarray(np.concatenate([_pk(c[b])] + sm_common, axis=0))
        in_maps.append(m)
    return in_maps


_CACHE = {}


def kernel(**inputs):
    in_maps = _host_prep(inputs)
    if "nc" not in _CACHE:
        _CACHE["nc"] = build_program(dbg=False)[0]
    res = run_bass_kernel_spmd(_CACHE["nc"], in_maps, core_ids=list(range(8)))
    return np.stack([np.asarray(r["y"], dtype=np.float32) for r in res.results], axis=0)
```
